# Optimizing a Trainium2 kernel written in Bass

```python
import math
import jax, jax.numpy as jnp
from jax import lax
import numpy as np

D_MODEL = 1024
BATCH = 4
SEQ = 8192
DEPTH = 2

CHUNK = 64
N_MEM = 256
BRANCH_W = 256
N_BRANCH = 4
D_A = BRANCH_W
CONV_W = 3
D_B = BRANCH_W
SG_BLOCK = 128
SG_GROUPS = 4
RW_HEADS = 4
RW_HEAD_DIM = 64
D_C = RW_HEADS * RW_HEAD_DIM
RW_DECAY_LORA = 64
RW_AAA_LORA = 64
RW_GATE_LORA = 128
RW_IN = 3 * D_C + RW_DECAY_LORA + RW_AAA_LORA + RW_GATE_LORA
RW_LN_EPS = 64e-5
D_S = BRANCH_W
S5_CH = 16
S5_GROUPS = D_S // S5_CH
S5_STATE = 64
OFF_B = 3 * D_A
OFF_C = OFF_B + 2 * D_B
OFF_D = OFF_C + RW_IN
OFF_G = OFF_D + D_S
N_IN = OFF_G + N_BRANCH * D_MODEL
XA_HEADS = 4
XA_HEAD_DIM = D_MODEL // XA_HEADS
N_EXPERTS = 32
TOP_K = 4
D_FF = D_MODEL
SWIGLU_LIMIT = 7.0
SWIGLU_ALPHA = 1.702
MOE_BLOCK = 512
LN_EPS = 1e-5
DN_ALPHA = (2 * DEPTH) ** 0.25
DN_BETA = (8 * DEPTH) ** -0.25

kernel_name = "hybrid_chunk_causal_gated_branch_moe_encoder"


def layer_norm(x, g, b, eps=LN_EPS):
    xf = x.astype(jnp.float32)
    mu = xf.mean(-1, keepdims=True)
    var = jnp.square(xf - mu).mean(-1, keepdims=True)
    y = (xf - mu) * lax.rsqrt(var + eps)
    return (y * g.astype(jnp.float32) + b.astype(jnp.float32)).astype(x.dtype)


def shift_time(z, n=1):
    return jnp.pad(z, ((0, 0), (n, 0), (0, 0)))[:, :z.shape[1]]


def short_conv_mixer(z, conv_w):
    b_gate, c_gate, hin = jnp.split(z, 3, axis=-1)
    ch = c_gate * hin
    y = conv_w[CONV_W - 1] * ch
    for i in range(CONV_W - 1):
        y = y + conv_w[i] * shift_time(ch, CONV_W - 1 - i)
    return b_gate * y


def spatial_gating_mixer(z, norm_g, norm_b, w_s, b_s):
    u, v = jnp.split(z, 2, axis=-1)
    v = layer_norm(v, norm_g, norm_b)
    B_, S_, _ = v.shape
    nb = S_ // SG_BLOCK
    v = v.reshape(B_, nb, SG_BLOCK, SG_GROUPS, D_B // SG_GROUPS)
    pos = jnp.arange(SG_BLOCK)
    mask = (pos[None, :] // CHUNK) <= (pos[:, None] // CHUNK)
    w = jnp.where(mask[None], w_s, 0)
    sv = jnp.einsum('gij,bnjgc->bnigc', w, v) + b_s.T[None, None, :, :, None]
    return u * sv.reshape(B_, S_, D_B)


def rwkv7_mixer(z, mu, w0, w_up, a0, a_up, g_up, k_k, k_a, r_k, ln_g, ln_b):
    B_, S_, _ = z.shape
    f32 = jnp.float32
    z = z + (shift_time(z) - z) * mu
    r, k, v, xw, xa, xg = jnp.split(
        z, [D_C, 2 * D_C, 3 * D_C, 3 * D_C + RW_DECAY_LORA,
            3 * D_C + RW_DECAY_LORA + RW_AAA_LORA], axis=-1)
    w = -jax.nn.softplus(-(w0 + jnp.tanh(xw) @ w_up)) - 0.5
    decay = jnp.exp(-jnp.exp(w.astype(f32)))
    a = jax.nn.sigmoid(a0 + xa @ a_up)
    g = jax.nn.sigmoid(xg) @ g_up
    kk = k * k_k
    k = k * (1.0 + (a - 1.0) * k_a)

    def heads(t):
        return t.astype(f32).reshape(B_, S_, RW_HEADS, RW_HEAD_DIM)

    r, k, v, kk, a, decay = (heads(t) for t in (r, k, v, kk, a, decay))
    kk = kk / jnp.maximum(jnp.linalg.norm(kk, axis=-1, keepdims=True), 1e-12)

    def step(state, inp):
        r_t, w_t, k_t, v_t, kk_t, a_t = inp
        sa = jnp.einsum('bhvk,bhk->bhv', state, -kk_t)
        state = (state * w_t[:, :, None, :]
                 + sa[..., None] * (kk_t * a_t)[:, :, None, :]
                 + v_t[..., None] * k_t[:, :, None, :])
        y_t = jnp.einsum('bhvk,bhk->bhv', state, r_t)
        return state, y_t

    xs = tuple(jnp.moveaxis(t, 1, 0) for t in (r, decay, k, v, kk, a))
    state0 = jnp.zeros((B_, RW_HEADS, RW_HEAD_DIM, RW_HEAD_DIM), f32)
    _, y = lax.scan(step, state0, xs)
    y = jnp.moveaxis(y, 0, 1)
    m = y.mean(-1, keepdims=True)
    var = jnp.square(y - m).mean(-1, keepdims=True)
    yn = ((y - m) * lax.rsqrt(var + RW_LN_EPS)).reshape(B_, S_, D_C)
    yn = yn * ln_g.astype(f32) + ln_b.astype(f32)
    bonus = (jnp.sum(r * k * r_k.astype(f32), -1, keepdims=True) * v).reshape(B_, S_, D_C)
    return ((yn + bonus) * g.astype(f32)).astype(z.dtype)


def _complex_affine_combine(e1, e2):
    a1r, a1i, b1r, b1i = e1
    a2r, a2i, b2r, b2i = e2
    return (a2r * a1r - a2i * a1i,
            a2r * a1i + a2i * a1r,
            a2r * b1r - a2i * b1i + b2r,
            a2r * b1i + a2i * b1r + b2i)


def s5_mixer(u, a_re, a_im, b_re, b_im, c_re, c_im, d, log_dt, glu_w, glu_b):
    B_, S_, _ = u.shape
    f32 = jnp.float32
    a_re, a_im, b_re, b_im, c_re, c_im = (p.astype(f32) for p in (a_re, a_im, b_re, b_im, c_re, c_im))
    uf = u.astype(f32)
    dt = jnp.exp(log_dt.astype(f32))[:, None]
    mag = jnp.exp(a_re * dt)
    abar_re = mag * jnp.cos(a_im * dt)
    abar_im = mag * jnp.sin(a_im * dt)
    den = a_re * a_re + a_im * a_im
    num_re = abar_re - 1.0
    coef_re = (num_re * a_re + abar_im * a_im) / den
    coef_im = (abar_im * a_re - num_re * a_im) / den
    bbar_re = coef_re[..., None] * b_re - coef_im[..., None] * b_im
    bbar_im = coef_re[..., None] * b_im + coef_im[..., None] * b_re
    ug = uf.reshape(B_, S_, S5_GROUPS, S5_CH)
    bu_re = jnp.einsum('gpc,bsgc->bsgp', bbar_re, ug)
    bu_im = jnp.einsum('gpc,bsgc->bsgp', bbar_im, ug)
    shp = (1, S_, S5_GROUPS, S5_STATE)
    elems = (jnp.broadcast_to(abar_re, shp), jnp.broadcast_to(abar_im, shp), bu_re, bu_im)
    _, _, x_re, x_im = lax.associative_scan(_complex_affine_combine, elems, axis=1)
    y = jnp.einsum('gcp,bsgp->bsgc', c_re, x_re) - jnp.einsum('gcp,bsgp->bsgc', c_im, x_im)
    y = y.reshape(B_, S_, D_S) + d.astype(f32) * uf
    y = jax.nn.gelu(y)
    y = y * jax.nn.sigmoid(y @ glu_w.astype(f32) + glu_b.astype(f32))
    return y.astype(u.dtype)


def memory_cross_attention(h, mem, wq, wk, wv, wo):
    B_, S_, _ = h.shape
    M_ = mem.shape[1]
    q = (h @ wq).reshape(B_, S_, XA_HEADS, XA_HEAD_DIM)
    k = (mem @ wk).reshape(B_, M_, XA_HEADS, XA_HEAD_DIM)
    v = (mem @ wv).reshape(B_, M_, XA_HEADS, XA_HEAD_DIM)
    s = jnp.einsum('bshd,bmhd->bhsm', q.astype(jnp.float32), k.astype(jnp.float32))
    p = jax.nn.softmax(s * (XA_HEAD_DIM ** -0.5), axis=-1).astype(v.dtype)
    o = jnp.einsum('bhsm,bmhd->bshd', p, v).reshape(B_, S_, D_MODEL)
    return o @ wo


def moe_ffn(h, router_w, router_b, w1, b1, w2, b2):
    B_, S_, D_ = h.shape
    t = h.reshape(B_ * S_, D_)
    n_tok = t.shape[0]
    n_asg = n_tok * TOP_K
    logits = (t @ router_w + router_b).astype(jnp.float32)
    top_val, top_idx = lax.top_k(logits, TOP_K)
    top_w = jax.nn.softmax(top_val, axis=-1)
    flat_e = top_idx.reshape(-1)
    order = jnp.argsort(flat_e)
    sorted_e = flat_e[order]
    counts = jnp.bincount(flat_e, length=N_EXPERTS)
    padded = (counts + MOE_BLOCK - 1) // MOE_BLOCK * MOE_BLOCK
    pad_end = jnp.cumsum(padded)
    pad_start = pad_end - padded
    grp_start = jnp.cumsum(counts) - counts
    dest = pad_start[sorted_e] + jnp.arange(n_asg) - grp_start[sorted_e]
    n_blocks = -(-n_asg // MOE_BLOCK) + N_EXPERTS
    n_slots = n_blocks * MOE_BLOCK
    slot_tok = jnp.full((n_slots,), n_tok, jnp.int32).at[dest].set((order // TOP_K).astype(jnp.int32))
    slot_w = jnp.zeros((n_slots,), jnp.float32).at[dest].set(top_w.reshape(-1)[order])
    blk_e = jnp.minimum(jnp.searchsorted(pad_end, jnp.arange(n_blocks) * MOE_BLOCK, side='right'),
                        N_EXPERTS - 1)
    t_pad = jnp.concatenate([t, jnp.zeros((1, D_), t.dtype)], axis=0)
    xb = t_pad[slot_tok].reshape(n_blocks, MOE_BLOCK, D_)

    def expert_block(args):
        xblk, e = args
        hid = xblk @ w1[e] + b1[e]
        gate = jnp.minimum(hid[:, 0::2], SWIGLU_LIMIT)
        up = jnp.clip(hid[:, 1::2], -SWIGLU_LIMIT, SWIGLU_LIMIT)
        act = (up + 1.0) * gate * jax.nn.sigmoid(SWIGLU_ALPHA * gate)
        return act @ w2[e] + b2[e]

    yb = lax.map(expert_block, (xb, blk_e))
    y = (yb.reshape(n_slots, D_).astype(jnp.float32) * slot_w[:, None]).astype(t.dtype)
    out = jnp.zeros((n_tok + 1, D_), t.dtype).at[slot_tok].add(y)[:n_tok]
    return out.reshape(B_, S_, D_)


def setup_inputs(seed: int = 0) -> dict:
    key = jax.random.key(seed)
    keys = jax.random.split(key, 64)
    counter = [0]

    def nk():
        k = keys[counter[0]]
        counter[0] += 1
        return k

    def nrm(shape, scale):
        return jax.random.normal(nk(), shape, jnp.float32) * scale

    def unif(shape, lo, hi):
        return jax.random.uniform(nk(), shape, jnp.float32, lo, hi)

    L, D = DEPTH, D_MODEL
    inp = {}
    inp["x"] = nrm((BATCH, SEQ, D), 1.0)
    inp["mem"] = nrm((BATCH, N_MEM, D), 1.0)
    inp["ln_in_g"] = 1.0 + nrm((D,), 0.02)
    inp["ln_in_b"] = nrm((D,), 0.02)
    inp["w_in"] = nrm((L, D, N_IN), D ** -0.5)
    inp["conv_w"] = nrm((L, CONV_W, D_A), CONV_W ** -0.5)
    inp["sg_norm_g"] = 1.0 + nrm((L, D_B), 0.02)
    inp["sg_norm_b"] = nrm((L, D_B), 0.02)
    inp["sg_w"] = nrm((L, SG_GROUPS, SG_BLOCK, SG_BLOCK), SG_BLOCK ** -0.5)
    inp["sg_b"] = 1.0 + nrm((L, SG_GROUPS, SG_BLOCK), 0.02)
    inp["rw_mu"] = unif((L, RW_IN), 0.0, 1.0)
    inp["rw_w0"] = unif((L, D_C), -6.5, -1.0)
    inp["rw_w_up"] = nrm((L, RW_DECAY_LORA, D_C), 0.1)
    inp["rw_a0"] = nrm((L, D_C), 0.1)
    inp["rw_a_up"] = nrm((L, RW_AAA_LORA, D_C), 0.1)
    inp["rw_g_up"] = nrm((L, RW_GATE_LORA, D_C), RW_GATE_LORA ** -0.5)
    inp["rw_k_k"] = 0.85 + nrm((L, D_C), 0.02)
    inp["rw_k_a"] = 1.0 + nrm((L, D_C), 0.02)
    inp["rw_r_k"] = nrm((L, RW_HEADS, RW_HEAD_DIM), 0.1)
    inp["rw_ln_g"] = 1.0 + nrm((L, D_C), 0.02)
    inp["rw_ln_b"] = nrm((L, D_C), 0.02)
    inp["s5_a_re"] = -0.5 + nrm((L, S5_GROUPS, S5_STATE), 0.01)
    inp["s5_a_im"] = (jnp.pi * jnp.arange(S5_STATE, dtype=jnp.float32))[None, None, :] + nrm((L, S5_GROUPS, S5_STATE), 0.01)
    inp["s5_b_re"] = nrm((L, S5_GROUPS, S5_STATE, S5_CH), (2 * S5_CH) ** -0.5)
    inp["s5_b_im"] = nrm((L, S5_GROUPS, S5_STATE, S5_CH), (2 * S5_CH) ** -0.5)
    inp["s5_c_re"] = nrm((L, S5_GROUPS, S5_CH, S5_STATE), (2 * S5_STATE) ** -0.5)
    inp["s5_c_im"] = nrm((L, S5_GROUPS, S5_CH, S5_STATE), (2 * S5_STATE) ** -0.5)
    inp["s5_d"] = nrm((L, D_S), 1.0)
    inp["s5_log_dt"] = unif((L, S5_GROUPS), math.log(1e-3), math.log(1e-1))
    inp["s5_glu_w"] = nrm((L, D_S, D_S), D_S ** -0.5)
    inp["s5_glu_b"] = nrm((L, D_S), 0.02)
    inp["br_proj"] = nrm((L, N_BRANCH, BRANCH_W, D), BRANCH_W ** -0.5)
    inp["gate_b"] = nrm((L, N_BRANCH, D), 0.02)
    inp["w_out"] = nrm((L, D, D), D ** -0.5 * DN_BETA)
    inp["ln1_g"] = 1.0 + nrm((L, D), 0.02)
    inp["ln1_b"] = nrm((L, D), 0.02)
    inp["xa_wq"] = nrm((L, D, D), D ** -0.5)
    inp["xa_wk"] = nrm((L, D, D), D ** -0.5)
    inp["xa_wv"] = nrm((L, D, D), D ** -0.5)
    inp["xa_wo"] = nrm((L, D, D), D ** -0.5 * DN_BETA)
    inp["ln2_g"] = 1.0 + nrm((L, D), 0.02)
    inp["ln2_b"] = nrm((L, D), 0.02)
    inp["router_w"] = nrm((L, D, N_EXPERTS), D ** -0.5)
    inp["router_b"] = nrm((L, N_EXPERTS), 0.01)
    inp["ex_w1"] = nrm((L, N_EXPERTS, D, 2 * D_FF), D ** -0.5)
    inp["ex_b1"] = nrm((L, N_EXPERTS, 2 * D_FF), 0.01)
    inp["ex_w2"] = nrm((L, N_EXPERTS, D_FF, D), D_FF ** -0.5 * DN_BETA)
    inp["ex_b2"] = nrm((L, N_EXPERTS, D), 0.01)
    inp["ln3_g"] = 1.0 + nrm((L, D), 0.02)
    inp["ln3_b"] = nrm((L, D), 0.02)
    return inp


def reference(x, mem, ln_in_g, ln_in_b, w_in, conv_w, sg_norm_g, sg_norm_b, sg_w, sg_b,
              rw_mu, rw_w0, rw_w_up, rw_a0, rw_a_up, rw_g_up, rw_k_k, rw_k_a, rw_r_k,
              rw_ln_g, rw_ln_b, s5_a_re, s5_a_im, s5_b_re, s5_b_im, s5_c_re, s5_c_im,
              s5_d, s5_log_dt, s5_glu_w, s5_glu_b, br_proj, gate_b, w_out, ln1_g, ln1_b,
              xa_wq, xa_wk, xa_wv, xa_wo, ln2_g, ln2_b, router_w, router_b,
              ex_w1, ex_b1, ex_w2, ex_b2, ln3_g, ln3_b):
    h = layer_norm(x, ln_in_g, ln_in_b)
    for l in range(DEPTH):
        z = h @ w_in[l]
        za, zb, zc, zd, zg = jnp.split(z, [OFF_B, OFF_C, OFF_D, OFF_G], axis=-1)
        o_a = short_conv_mixer(za, conv_w[l])
        o_b = spatial_gating_mixer(zb, sg_norm_g[l], sg_norm_b[l], sg_w[l], sg_b[l])
        o_c = rwkv7_mixer(zc, rw_mu[l], rw_w0[l], rw_w_up[l], rw_a0[l], rw_a_up[l],
                          rw_g_up[l], rw_k_k[l], rw_k_a[l], rw_r_k[l], rw_ln_g[l], rw_ln_b[l])
        o_d = s5_mixer(zd, s5_a_re[l], s5_a_im[l], s5_b_re[l], s5_b_im[l], s5_c_re[l],
                       s5_c_im[l], s5_d[l], s5_log_dt[l], s5_glu_w[l], s5_glu_b[l])
        branches = (o_a, o_b, o_c, o_d)
        gate_pre = jnp.split(zg, N_BRANCH, axis=-1)
        merged = None
        for i in range(N_BRANCH):
            term = jax.nn.sigmoid(gate_pre[i] + gate_b[l, i]) * (branches[i] @ br_proj[l, i])
            merged = term if merged is None else merged + term
        h = layer_norm(DN_ALPHA * h + merged @ w_out[l], ln1_g[l], ln1_b[l])
        h = layer_norm(DN_ALPHA * h + memory_cross_attention(h, mem, xa_wq[l], xa_wk[l],
                                                             xa_wv[l], xa_wo[l]),
                       ln2_g[l], ln2_b[l])
        h = layer_norm(DN_ALPHA * h + moe_ffn(h, router_w[l], router_b[l], ex_w1[l], ex_b1[l],
                                              ex_w2[l], ex_b2[l]),
                       ln3_g[l], ln3_b[l])
    return h
```

```python
import math
from contextlib import ExitStack
import numpy as np
import concourse.bass as bass
import concourse.mybir as mybir
from concourse.bass_utils import run_bass_kernel_spmd

F32 = mybir.dt.float32
BF16 = mybir.dt.bfloat16
I32 = mybir.dt.int32
AF = mybir.ActivationFunctionType
ALU = mybir.AluOpType
AX = mybir.AxisListType

ENGS = ("pe", "dve", "act", "pool", "sp")
STORE_Q = "act"


class Tl:
    __slots__ = ("name", "t", "w", "r", "dkey")

    def __init__(self, name, t=None):
        self.name = name
        self.t = t
        self.w = None
        self.r = {}
        self.dkey = None

    def __getitem__(self, idx):
        return self.t[idx]


class _Proxy:
    def __init__(self):
        self.call = None

    def __getattr__(self, name):
        def rec(*a, **k):
            assert self.call is None
            self.call = (name, a, k)
        return rec


class RegConst:
    cache = {}

    def __init__(self, v):
        self.v = v

    def get(self, e):
        key = (id(e), self.v)
        if key not in RegConst.cache:
            RegConst.cache[key] = e.to_reg(self.v)
        return RegConst.cache[key]


def _record(fn):
    p = _Proxy()
    fn(p)
    name, a, k = p.call

    def run(e):
        k2 = {kk: (vv.get(e) if isinstance(vv, RegConst) else vv) for kk, vv in k.items()}
        return getattr(e, name)(*a, **k2)
    return run


class Sched:
    def __init__(self, nc, es):
        self.nc = nc
        self.es = es
        self.q = {e: [] for e in ENGS}
        self.cnt = {e: 0 for e in ENGS}
        self.seen = {e: {} for e in ENGS}
        self.sem = {}
        for e in ENGS:
            self.sem[e] = es.enter_context(nc.semaphore("c_" + e))
        self.dtot = {}
        self.ndsem = 0
        self.free_dsems = []

    def sb(self, name, shape, dt, st=None):
        self.uid = getattr(self, "uid", 0) + 1
        name = "t%d_%s" % (self.uid, name)
        t = (st or self.es).enter_context(self.nc.sbuf_tensor(name, list(shape), dt))
        return Tl(name, t)

    def ps(self, name, shape, dt=F32, st=None):
        name = "pp_" + name
        t = (st or self.es).enter_context(self.nc.psum_tensor(name, list(shape), dt))
        return Tl(name, t)

    def res(self, name):
        return Tl(name, None)

    def _dsem(self, tl):
        if tl.dkey is None:
            if self.free_dsems:
                tl.dkey = self.free_dsems.pop()
            else:
                k = "d%d" % self.ndsem
                self.ndsem += 1
                self.sem[k] = self.es.enter_context(self.nc.semaphore(k))
                self.dtot[k] = 0
                tl.dkey = k
        return tl.dkey

    def release(self, tl):
        if tl.dkey is not None:
            self.free_dsems.append(tl.dkey)
            tl.dkey = None

    def _waits(self, eng, reads, writes):
        waits = {}
        seen = self.seen[eng]

        def need(ev):
            if ev is None:
                return
            k, v = ev
            if k in self.dtot:
                v = self.dtot[k]
            elif k == eng and eng in ("pe", "sp"):
                return
            if seen.get(k, 0) < v and waits.get(k, 0) < v:
                waits[k] = v

        for t in reads:
            need(t.w)
        for t in writes:
            need(t.w)
            for k, v in t.r.items():
                need((k, v))
        for k, v in waits.items():
            seen[k] = v
        return list(waits.items())

    def _mark(self, ev, reads, writes):
        k, v = ev
        for t in reads:
            if t.r.get(k, 0) < v:
                t.r[k] = v
        for t in writes:
            t.w = ev
            t.r = {}

    def op(self, eng, fn, reads=(), writes=()):
        fn = _record(fn)
        waits = self._waits(eng, reads, writes)
        self.cnt[eng] += 1
        ev = (eng, self.cnt[eng])
        self._mark(ev, reads, writes)
        self.q[eng].append((waits, fn, (eng, 1)))

    def dma(self, q, out, in_, reads=(), writes=(), sem_tile=None, **kw):
        if q == "sp" and not writes:
            q = STORE_Q
        waits = self._waits(q, reads, writes)
        k = self._dsem(sem_tile)
        self.dtot[k] += 16
        ev = (k, self.dtot[k])
        self._mark(ev, reads, writes)
        self.q[q].append((waits, lambda e, out=out, in_=in_, kw=kw: e.dma_start(out=out, in_=in_, **kw), (k, 16)))

    def dma_fn(self, q, fn, reads=(), writes=(), sem_tile=None):
        waits = self._waits(q, reads, writes)
        k = self._dsem(sem_tile)
        self.dtot[k] += 16
        ev = (k, self.dtot[k])
        self._mark(ev, reads, writes)
        self.q[q].append((waits, _record(fn), (k, 16)))

    def barrier(self):
        waits = []
        seen = self.seen["sp"]
        for e in ENGS:
            if e != "sp" and seen.get(e, 0) < self.cnt[e]:
                waits.append((e, self.cnt[e]))
                seen[e] = self.cnt[e]
        for k, v in self.dtot.items():
            if seen.get(k, 0) < v:
                waits.append((k, v))
                seen[k] = v
        self.cnt["sp"] += 1
        ev = ("sp", self.cnt["sp"])
        self.q["sp"].append((waits, lambda e: e.nop(), ("sp", 1)))
        for e in ENGS:
            if e != "sp":
                self.q[e].append(([ev], None, None))
                self.seen[e]["sp"] = ev[1]
                for k, v in self.dtot.items():
                    self.seen[e][k] = v
                for e2 in ENGS:
                    self.seen[e][e2] = max(self.seen[e].get(e2, 0), self.cnt[e2])

    def emit(self):
        nc = self.nc
        self.barrier()
        with nc.Block() as block:
            def run(engname):
                def body(eng):
                    for waits, fn, inc in self.q[engname]:
                        for k, v in waits:
                            eng.wait_ge(self.sem[k], v)
                        if fn is not None:
                            ins = fn(eng)
                            ins.then_inc(self.sem[inc[0]], inc[1])
                return body
            block.tensor(run("pe"))
            block.vector(run("dve"))
            block.scalar(run("act"))
            block.gpsimd(run("pool"))
            block.sync(run("sp"))


D = 1024
NMEM = 256
OFF_A, OFF_B, OFF_C, OFF_D, OFF_G = 0, 768, 1280, 2304, 2560
N_IN = 6656
NE = 32
LN_EPS = 1e-5
DEPTH = 2
DN_ALPHA = (2 * DEPTH) ** 0.25

PARAMS = [
    ("ln_in_g", (D,)), ("ln_in_b", (D,)), ("w_in", (2, D, N_IN)), ("conv_w", (2, 3, 256)),
    ("sg_norm_g", (2, 256)), ("sg_norm_b", (2, 256)), ("sg_w", (2, 4, 128, 128)), ("sg_b", (2, 4, 128)),
    ("rw_mu", (2, 1024)), ("rw_w0", (2, 256)), ("rw_w_up", (2, 64, 256)), ("rw_a0", (2, 256)),
    ("rw_a_up", (2, 64, 256)), ("rw_g_up", (2, 128, 256)), ("rw_k_k", (2, 256)), ("rw_k_a", (2, 256)),
    ("rw_r_k", (2, 4, 64)), ("rw_ln_g", (2, 256)), ("rw_ln_b", (2, 256)),
    ("s5_a_re", (2, 16, 64)), ("s5_a_im", (2, 16, 64)), ("s5_b_re", (2, 16, 64, 16)), ("s5_b_im", (2, 16, 64, 16)),
    ("s5_c_re", (2, 16, 16, 64)), ("s5_c_im", (2, 16, 16, 64)), ("s5_d", (2, 256)), ("s5_log_dt", (2, 16)),
    ("s5_glu_w", (2, 256, 256)), ("s5_glu_b", (2, 256)), ("br_proj", (2, 4, 256, D)), ("gate_b", (2, 4, D)),
    ("w_out", (2, D, D)), ("ln1_g", (2, D)), ("ln1_b", (2, D)), ("xa_wq", (2, D, D)), ("xa_wk", (2, D, D)),
    ("xa_wv", (2, D, D)), ("xa_wo", (2, D, D)), ("ln2_g", (2, D)), ("ln2_b", (2, D)),
    ("router_w", (2, D, NE)), ("router_b", (2, NE)), ("ex_w1", (2, NE, D, 2 * D)), ("ex_b1", (2, NE, 2 * D)),
    ("ex_w2", (2, NE, D, D)), ("ex_b2", (2, NE, D)), ("ln3_g", (2, D)), ("ln3_b", (2, D)),
]


def col(ap1d):
    return ap1d.rearrange("(p o) -> p o", o=1)


class Ctx:
    pass


def build(T, nlayers=2, dbg=False, phases=None):
    assert T % 512 == 0
    NT = T // 512
    nc = bass.Bass("TRN2", target_bir_lowering=False)
    RegConst.cache = {}
    P = {}
    x_in = nc.dram_tensor("x", [T, D], F32, kind="ExternalInput").ap()
    mem_in = nc.dram_tensor("mem", [NMEM, D], F32, kind="ExternalInput").ap()
    for name, shp in PARAMS:
        P[name] = nc.dram_tensor(name, list(shp), F32, kind="ExternalInput").ap()
    out = nc.dram_tensor("out", [T, D], F32, kind="ExternalOutput").ap()
    skind = "ExternalOutput" if dbg else "Internal"

    def scr(name, shape, dt):
        return nc.dram_tensor(name, list(shape), dt, kind=skind).ap()

    h_tok = scr("h_tok", [T, D], F32)
    hT = scr("hT", [D, T], BF16)
    zT = scr("zT", [OFF_G, T], F32)
    zbv = scr("zbv", [T, 256], F32)
    gT = scr("gT", [4 * D, T], BF16)
    oT = scr("oT", [4 * 256, T], BF16)
    gate_d = scr("gate_d", [T, NE], F32)
    MOE_BLK = 512
    MOE_NB = (T * 4) // MOE_BLK + NE
    xs_d = scr("xs_d", [MOE_NB * MOE_BLK, D], F32)
    ys_d = scr("ys_d", [MOE_NB * MOE_BLK, D], F32)

    with ExitStack() as es:
        S = Sched(nc, es)
        C = Ctx()
        C.nc, C.S, C.P, C.T, C.NT = nc, S, P, T, NT
        C.ident = S.sb("ident", [128, 128], F32)
        S.op("pool", lambda e: e.memset(C.ident[:], 0.0), writes=[C.ident])
        S.op("pool", lambda e: e.affine_select(out=C.ident[:], in_=C.ident[:], compare_op=ALU.not_equal, fill=1.0,
                                               base=0, pattern=[[-1, 128]], channel_multiplier=1),
             reads=[C.ident], writes=[C.ident])
        C.ones_b = S.sb("ones_b", [128, 128], BF16)
        S.op("pool", lambda e: e.memset(C.ones_b[:], 1.0), writes=[C.ones_b])
        C.ones_f = S.sb("ones_f", [128, 128], F32)
        S.op("pool", lambda e: e.memset(C.ones_f[:], 1.0), writes=[C.ones_f])
        C.rweps = S.sb("rweps", [128, 1], F32)
        S.op("pool", lambda e: e.memset(C.rweps[:], 64e-5), writes=[C.rweps])
        C.psl = [S.ps("ps%d" % i, [128, 512], F32) for i in range(8)]
        C.psi = 0

        def psum():
            t = C.psl[C.psi % 8]
            C.psi += 1
            return t
        C.psum = psum
        C.lnst = [S.sb("lnst%d" % i, [128, 2, 6], F32) for i in range(2)]
        C.lnmv = [S.sb("lnmv%d" % i, [128, 2], F32) for i in range(2)]
        C.lnrs = [S.sb("lnrs%d" % i, [128, 1], F32) for i in range(2)]
        C.lni = 0
        C.evi = 0
        C.h_tok, C.hT, C.zT, C.zbv, C.gT, C.oT, C.gate_d = h_tok, hT, zT, zbv, gT, oT, gate_d
        C.x_in, C.mem_in, C.out = x_in, mem_in, out
        C.xs_d, C.ys_d, C.MOE_NB = xs_d, ys_d, MOE_NB

        ph = phases
        for l in range(nlayers):
            last = (l == nlayers - 1)
            if l == 0:
                phase_ln_in(C)
                S.barrier()
            if ph is None or "inproj" in ph:
                phase_inproj(C, l)
                S.barrier()
            if ph is None or "conv" in ph:
                phase_conv(C, l)
                S.barrier()
            if ph is None or "sgu" in ph:
                phase_sgu(C, l)
                S.barrier()
            if ph is None or "rwkv" in ph:
                phase_rwkv(C, l)
                S.barrier()
            if ph is None or "s5" in ph:
                phase_s5(C, l)
                S.barrier()
            if ph is None or "merge" in ph:
                phase_merge(C, l)
                S.barrier()
            if ph is None or "attn" in ph:
                phase_attn(C, l)
                S.barrier()
            if ph is None or "moe" in ph:
                phase_moe_sparse(C, l, C.out if last else None)
                S.barrier()
        S.emit()
    return nc


def evac_eng(C):
    C.evi += 1
    return "act" if C.evi % 2 else "dve"


def copy_op(eng_name, out, in_):
    if eng_name == "act":
        return lambda e: e.copy(out=out, in_=in_)
    return lambda e: e.tensor_copy(out=out, in_=in_)


def load_bc(C, st, name, ap1d, n, q="sp"):
    S = C.S
    t = S.sb(name, [128, n], F32, st)
    S.dma(q, t[:], ap1d.partition_broadcast(128), writes=[t], sem_tile=t)
    return t


def ln_tile(C, src, dst, g_bc, b_bc, eps=LN_EPS):
    S = C.S
    i = C.lni % 2
    C.lni += 1
    st, mv, rs = C.lnst[i], C.lnmv[i], C.lnrs[i]
    sa, da = src[0], dst[0]
    srct, dstt = src[1], dst[1]
    S.op("dve", lambda e: e.bn_stats(out=st[:, 0, :], in_=sa[:, 0:512]), reads=[srct], writes=[st])
    S.op("dve", lambda e: e.bn_stats(out=st[:, 1, :], in_=sa[:, 512:1024]), reads=[srct, st], writes=[st])
    S.op("dve", lambda e: e.bn_aggr(out=mv[:], in_=st[:].rearrange("p a b -> p (a b)")), reads=[st], writes=[mv])
    S.op("act", lambda e: e.activation(out=rs[:], in_=mv[:, 1:2], func=AF.Sqrt, bias=C.eps_col[:, 0:1], scale=1.0),
         reads=[mv, C.eps_col], writes=[rs])
    S.op("dve", lambda e: e.reciprocal(out=rs[:], in_=rs[:]), reads=[rs], writes=[rs])
    S.op("dve", lambda e: e.tensor_scalar(out=da, in0=sa, scalar1=mv[:, 0:1], scalar2=rs[:, 0:1],
                                          op0=ALU.subtract, op1=ALU.mult), reads=[srct, mv, rs], writes=[dstt])
    S.op("pool", lambda e: e.tensor_tensor(out=da, in0=da, in1=g_bc[:], op=ALU.mult), reads=[dstt, g_bc], writes=[dstt])
    S.op("pool", lambda e: e.tensor_tensor(out=da, in0=da, in1=b_bc[:], op=ALU.add), reads=[dstt, b_bc], writes=[dstt])


def transpose_to(C, src_ap, src_t, dst_t, dst_fn, nblk=8):
    S = C.S
    for g in range(nblk // 4):
        ps = C.psum()
        for j in range(4):
            kc = g * 4 + j
            S.op("pe", lambda e, kc=kc, j=j, ps=ps: e.transpose(out=ps[:, j * 128:(j + 1) * 128],
                                                                 in_=src_ap[:, kc * 128:(kc + 1) * 128],
                                                                 identity=C.ident[:]),
                 reads=[src_t, C.ident], writes=[ps])
        en = evac_eng(C)
        S.op(en, copy_op(en, dst_fn(g), ps[:, :].rearrange("p (a b) -> p a b", a=4)), reads=[ps], writes=[dst_t])


def store_h(C, st, hts, hTs, tile_i, sub, out_ap=None):
    S = C.S
    tok0 = tile_i * 512 + sub * 128
    S.dma("sp", C.h_tok[tok0:tok0 + 128, :], hts[:], reads=[hts], sem_tile=hts)
    if out_ap is not None:
        S.dma("sp", out_ap[tok0:tok0 + 128, :], hts[:], reads=[hts], sem_tile=hts)
    transpose_to(C, hts[:], hts, hTs, lambda g: hTs[:, g * 4:(g + 1) * 4, sub * 128:(sub + 1) * 128])
    if sub == 3:
        S.dma("sp", C.hT.rearrange("(kc p) t -> p kc t", p=128)[:, :, tile_i * 512:(tile_i + 1) * 512], hTs[:],
              reads=[hTs], sem_tile=hTs)


def load_w_bf16(C, t, w_ap, q="pool"):
    C.S.dma(q, t[:], w_ap.rearrange("(kc p) n -> p kc n", p=128), writes=[t], sem_tile=t)


def phase_ln_in(C):
    S, T, NT = C.S, C.T, C.NT
    with ExitStack() as st:
        C.eps_col = S.sb("eps_col", [128, 1], F32)
        g_bc = load_bc(C, st, "lnin_g", C.P["ln_in_g"], D)
        b_bc = load_bc(C, st, "lnin_b", C.P["ln_in_b"], D)
        xt = [S.sb("lnin_x%d" % i, [128, D], F32, st) for i in range(2)]
        hTs = [S.sb("lnin_hT%d" % i, [128, 8, 512], BF16, st) for i in range(2)]
        S.op("pool", lambda e: e.memset(C.eps_col[:], LN_EPS), writes=[C.eps_col])
        n = 0
        for i in range(NT):
            for sub in range(4):
                x = xt[n % 2]
                n += 1
                tok0 = i * 512 + sub * 128
                S.dma("sp", x[:], C.x_in[tok0:tok0 + 128, :], writes=[x], sem_tile=x)
                ln_tile(C, (x[:], x), (x[:], x), g_bc, b_bc)
                store_h(C, st, x, hTs[i % 2], i, sub)
        S.barrier()
        for t in [g_bc, b_bc] + xt + hTs:
            S.release(t)


def load_cols(C, st, name, ap_rows, n, q="sp", m=128):
    S = C.S
    tmp = S.sb(name + "_r", [n, m], F32, st)
    res = S.sb(name, [m, n], F32, st)
    S.dma(q, tmp[:], ap_rows, writes=[tmp], sem_tile=tmp)
    ps = C.psum()
    S.op("pe", lambda e: e.transpose(out=ps[0:m, 0:n], in_=tmp[:, :], identity=C.ident[0:n, 0:n]),
         reads=[tmp, C.ident], writes=[ps])
    S.op("dve", lambda e: e.tensor_copy(out=res[:], in_=ps[0:m, 0:n]), reads=[ps], writes=[res])
    C.S.release_later = getattr(C.S, "release_later", [])
    return res


def phase_inproj(C, l):
    S, T, NT, P = C.S, C.T, C.NT, C.P
    w_in = P["w_in"][l]
    with ExitStack() as st:
        wbuf = [S.sb("ip_w%d" % i, [128, 8, 1024], BF16, st) for i in range(2)]
        wb2 = S.sb("ip_wb", [128, 8, 1024], BF16, st)
        hTh = [S.sb("ip_h%d" % i, [128, 8, 513], BF16, st) for i in range(2)]
        zst = [S.sb("ip_z%d" % i, [128, 4, 512], F32, st) for i in range(2)]
        gst = [S.sb("ip_g%d" % i, [128, 4, 512], BF16, st) for i in range(2)]
        gb = load_cols(C, st, "ip_gb", P["gate_b"][l].rearrange("i (j p) -> (i j) p", p=128), 32)
        groups = [("F", 0, 1024, 0), ("V", 1024, 256, 0), ("C", OFF_C, 1024, OFF_C), ("F", OFF_D, 256, OFF_D)]
        for i in range(4):
            groups.append(("G", OFF_G + i * 1024, 1024, i * 1024))
        nld = 0
        nst = 0
        for gi, (kind, c0, ncol, r0) in enumerate(groups):
            w = wbuf[gi % 2]
            if kind != "C":
                S.dma("pool", w[:, :, 0:ncol], w_in[:, c0:c0 + ncol].rearrange("(kc p) n -> p kc n", p=128),
                      writes=[w], sem_tile=w)
            else:
                with ExitStack() as st2:
                    wraw = S.sb("ip_wraw", [128, 8, 1024], F32, st2)
                    mu = load_bc(C, st2, "ip_mu", P["rw_mu"][l], 1024)
                    tmp = [S.sb("ip_tmp%d" % i, [128, 1024], F32, st2) for i in range(2)]
                    S.dma("sp", wraw[:], w_in[:, c0:c0 + ncol].rearrange("(kc p) n -> p kc n", p=128),
                          writes=[wraw], sem_tile=wraw)
                    for kc in range(8):
                        tk = tmp[kc % 2]
                        S.op("dve", lambda e, kc=kc, tk=tk: e.tensor_tensor(out=tk[:], in0=wraw[:, kc, :], in1=mu[:], op=ALU.mult),
                             reads=[wraw, mu], writes=[tk])
                        S.op("act", lambda e, kc=kc, tk=tk: e.copy(out=wb2[:, kc, :], in_=tk[:]), reads=[tk], writes=[wb2])
                        S.op("pool", lambda e, kc=kc, tk=tk: e.tensor_tensor(out=w[:, kc, :], in0=wraw[:, kc, :], in1=tk[:], op=ALU.subtract),
                             reads=[wraw, tk], writes=[w])
                    S.barrier()
                    for t_ in [wraw, mu] + tmp:
                        S.release(t_)
            for i in range(NT):
                hh = hTh[nld % 2]
                prev = hTh[(nld + 1) % 2]
                nld += 1
                t0 = i * 512
                S.dma("sp", hh[:, :, 1:513], C.hT.rearrange("(kc p) t -> p kc t", p=128)[:, :, t0:t0 + 512],
                      writes=[hh], sem_tile=hh)
                if kind == "C":
                    if i == 0:
                        S.op("pool", lambda e, hh=hh: e.memset(hh[:, :, 0:1], 0.0), writes=[hh])
                    else:
                        S.op("pool", lambda e, hh=hh, prev=prev: e.tensor_copy(out=hh[:, :, 0:1], in_=prev[:, :, 512:513]),
                             reads=[prev], writes=[hh])
                if kind == "V":
                    zs = zst[nst % 2]
                    nst += 1
                    for sub in range(4):
                        ps = C.psum()
                        for kc in range(8):
                            S.op("pe", lambda e, kc=kc, sub=sub, ps=ps, hh=hh: e.matmul(
                                ps[:, 0:256], lhsT=hh[:, kc, 1 + sub * 128:1 + (sub + 1) * 128], rhs=w[:, kc, 0:256],
                                start=(kc == 0), stop=(kc == 7)), reads=[hh, w], writes=[ps])
                        en = evac_eng(C)
                        S.op(en, copy_op(en, zs[:, sub, 0:256], ps[:, 0:256]), reads=[ps], writes=[zs])
                    S.dma("sp", C.zbv[t0:t0 + 512, :].rearrange("(s p) c -> p s c", p=128), zs[:, :, 0:256],
                          reads=[zs], sem_tile=zs)
                    continue
                nct = ncol // 128
                for cg in range(0, nct, 4):
                    ncg = min(4, nct - cg)
                    if kind == "G":
                        zs = gst[nst % 2]
                    else:
                        zs = zst[nst % 2]
                    nst += 1
                    for j in range(ncg):
                        ct = cg + j
                        ps = C.psum()
                        if kind == "C":
                            for kc in range(8):
                                S.op("pe", lambda e, kc=kc, ct=ct, ps=ps, hh=hh: e.matmul(
                                    ps[:, :], lhsT=w[:, kc, ct * 128:(ct + 1) * 128], rhs=hh[:, kc, 1:513],
                                    start=(kc == 0), stop=False), reads=[hh, w], writes=[ps])
                            for kc in range(8):
                                S.op("pe", lambda e, kc=kc, ct=ct, ps=ps, hh=hh: e.matmul(
                                    ps[:, :], lhsT=wb2[:, kc, ct * 128:(ct + 1) * 128], rhs=hh[:, kc, 0:512],
                                    start=False, stop=(kc == 7)), reads=[hh, wb2], writes=[ps])
                        else:
                            for kc in range(8):
                                S.op("pe", lambda e, kc=kc, ct=ct, ps=ps, hh=hh: e.matmul(
                                    ps[:, :], lhsT=w[:, kc, ct * 128:(ct + 1) * 128], rhs=hh[:, kc, 1:513],
                                    start=(kc == 0), stop=(kc == 7)), reads=[hh, w], writes=[ps])
                        if kind == "G":
                            gcol = (r0 // 128) + ct
                            S.op("act", lambda e, j=j, ps=ps, zs=zs, gcol=gcol: e.activation(
                                out=zs[:, j, :], in_=ps[:, :], func=AF.Sigmoid, bias=gb[:, gcol:gcol + 1], scale=1.0),
                                reads=[ps, gb], writes=[zs])
                        else:
                            en = evac_eng(C)
                            S.op(en, copy_op(en, zs[:, j, :], ps[:, :]), reads=[ps], writes=[zs])
                    dst = C.gT if kind == "G" else C.zT
                    rr = r0 + cg * 128
                    S.dma("sp", dst[rr:rr + ncg * 128, t0:t0 + 512].rearrange("(j p) t -> p j t", p=128), zs[:, 0:ncg, :],
                          reads=[zs], sem_tile=zs)
        S.barrier()
        for t_ in wbuf + [wb2] + hTh + zst + gst:
            S.release(t_)


def phase_conv(C, l):
    S, T, NT, P = C.S, C.T, C.NT, C.P
    with ExitStack() as st:
        cw = load_cols(C, st, "cv_w", P["conv_w"][l].rearrange("k (f p) -> (k f) p", p=128), 6)
        za = [S.sb("cv_za%d" % i, [128, 6, 512], F32, st) for i in range(2)]
        ch = [S.sb("cv_ch%d" % i, [128, 2, 514], F32, st) for i in range(2)]
        yt = [S.sb("cv_y%d" % i, [128, 512], F32, st) for i in range(2)]
        ost = [S.sb("cv_o%d" % i, [128, 2, 512], BF16, st) for i in range(2)]
        ny = 0
        for i in range(NT):
            t0 = i * 512
            z = za[i % 2]
            c = ch[i % 2]
            cp = ch[(i + 1) % 2]
            o = ost[i % 2]
            S.dma("sp", z[:], C.zT[0:768, t0:t0 + 512].rearrange("(j p) t -> p j t", p=128), writes=[z], sem_tile=z)
            if i == 0:
                S.op("pool", lambda e, c=c: e.memset(c[:, :, 0:2], 0.0), writes=[c])
            else:
                S.op("pool", lambda e, c=c, cp=cp: e.tensor_copy(out=c[:, :, 0:2], in_=cp[:, :, 512:514]), reads=[cp], writes=[c])
            S.op("pool", lambda e, c=c, z=z: e.tensor_tensor(out=c[:, :, 2:514], in0=z[:, 2:4, :], in1=z[:, 4:6, :], op=ALU.mult),
                 reads=[z, c], writes=[c])
            for f in range(2):
                y = yt[ny % 2]
                ny += 1
                S.op("dve", lambda e, f=f, y=y, c=c: e.tensor_scalar(out=y[:], in0=c[:, f, 2:514], scalar1=cw[:, 4 + f:5 + f],
                                                                      scalar2=None, op0=ALU.mult), reads=[c, cw], writes=[y])
                S.op("dve", lambda e, f=f, y=y, c=c: e.scalar_tensor_tensor(out=y[:], in0=c[:, f, 1:513], scalar=cw[:, 2 + f:3 + f],
                                                                             in1=y[:], op0=ALU.mult, op1=ALU.add), reads=[c, cw, y], writes=[y])
                S.op("dve", lambda e, f=f, y=y, c=c: e.scalar_tensor_tensor(out=y[:], in0=c[:, f, 0:512], scalar=cw[:, f:f + 1],
                                                                             in1=y[:], op0=ALU.mult, op1=ALU.add), reads=[c, cw, y], writes=[y])
                S.op("pool", lambda e, f=f, y=y, z=z, o=o: e.tensor_tensor(out=o[:, f, :], in0=y[:], in1=z[:, f, :], op=ALU.mult),
                     reads=[y, z], writes=[o])
            S.dma("sp", C.oT[0:256, t0:t0 + 512].rearrange("(f p) t -> p f t", p=128), o[:], reads=[o], sem_tile=o)
        S.barrier()
        for t_ in za + ch + yt + ost:
            S.release(t_)


def phase_sgu(C, l):
    S, T, NT, P = C.S, C.T, C.NT, C.P
    with ExitStack() as st:
        g_bc = load_bc(C, st, "sg_g", P["sg_norm_g"][l], 256)
        b_bc = load_bc(C, st, "sg_b", P["sg_norm_b"][l], 256)
        sb_bc = load_bc(C, st, "sg_sb", P["sg_b"][l].rearrange("g i -> (g i)"), 512)
        wraw = S.sb("sg_wraw", [128, 4, 128], F32, st)
        wsT = S.sb("sg_wsT", [128, 4, 128], F32, st)
        S.dma("sp", wraw[:], P["sg_w"][l].rearrange("g i j -> i g j"), writes=[wraw], sem_tile=wraw)
        ps = C.psum()
        for g in range(4):
            S.op("pe", lambda e, g=g: e.transpose(out=ps[:, g * 128:(g + 1) * 128], in_=wraw[:, g, :], identity=C.ident[:]),
                 reads=[wraw, C.ident], writes=[ps])
        S.op("dve", lambda e: e.tensor_copy(out=wsT[:], in_=ps[:, :].rearrange("p (g i) -> p g i", g=4)), reads=[ps], writes=[wsT])
        S.op("dve", lambda e: e.memset(wsT[64:128, :, 0:64], 0.0), reads=[wsT], writes=[wsT])
        vt = [S.sb("sg_v%d" % i, [128, 4, 256], F32, st) for i in range(2)]
        ut = [S.sb("sg_u%d" % i, [64, 4, 512], F32, st) for i in range(2)]
        ot = [S.sb("sg_o%d" % i, [64, 4, 512], BF16, st) for i in range(2)]
        svt = [S.sb("sg_sv%d" % i, [64, 512], F32, st) for i in range(2)]
        stt = [S.sb("sg_st%d" % i, [128, 6], F32, st) for i in range(2)]
        mvt = [S.sb("sg_mv%d" % i, [128, 2], F32, st) for i in range(2)]
        rst = [S.sb("sg_rs%d" % i, [128, 1], F32, st) for i in range(2)]
        n = 0
        for i in range(NT):
            t0 = i * 512
            v, u, o = vt[i % 2], ut[i % 2], ot[i % 2]
            S.dma("sp", v[:], C.zbv[t0:t0 + 512, :].rearrange("(s p) c -> p s c", p=128), writes=[v], sem_tile=v)
            S.dma("sp", u[:], C.zT[768:1024, t0:t0 + 512].rearrange("(g c) t -> c g t", c=64), writes=[u], sem_tile=u)
            for s in range(4):
                sx, mv, rs, sv = stt[n % 2], mvt[n % 2], rst[n % 2], svt[n % 2]
                n += 1
                S.op("dve", lambda e, s=s, sx=sx, v=v: e.bn_stats(out=sx[:], in_=v[:, s, :]), reads=[v], writes=[sx])
                S.op("dve", lambda e, sx=sx, mv=mv: e.bn_aggr(out=mv[:], in_=sx[:]), reads=[sx], writes=[mv])
                S.op("act", lambda e, mv=mv, rs=rs: e.activation(out=rs[:], in_=mv[:, 1:2], func=AF.Sqrt, bias=C.eps_col[:, 0:1], scale=1.0),
                     reads=[mv, C.eps_col], writes=[rs])
                S.op("dve", lambda e, rs=rs: e.reciprocal(out=rs[:], in_=rs[:]), reads=[rs], writes=[rs])
                S.op("dve", lambda e, s=s, v=v, mv=mv, rs=rs: e.tensor_scalar(out=v[:, s, :], in0=v[:, s, :], scalar1=mv[:, 0:1],
                                                                                scalar2=rs[:, 0:1], op0=ALU.subtract, op1=ALU.mult),
                     reads=[v, mv, rs], writes=[v])
                S.op("pool", lambda e, s=s, v=v: e.tensor_tensor(out=v[:, s, :], in0=v[:, s, :], in1=g_bc[:], op=ALU.mult),
                     reads=[v, g_bc], writes=[v])
                S.op("pool", lambda e, s=s, v=v: e.tensor_tensor(out=v[:, s, :], in0=v[:, s, :], in1=b_bc[:], op=ALU.add),
                     reads=[v, b_bc], writes=[v])
                ps = C.psum()
                for g in range(4):
                    S.op("pe", lambda e, g=g, s=s, v=v, ps=ps: e.matmul(ps[0:64, g * 128:(g + 1) * 128], lhsT=v[:, s, g * 64:(g + 1) * 64],
                                                                         rhs=wsT[:, g, :], start=True, stop=True),
                         reads=[v, wsT], writes=[ps])
                S.op("dve", lambda e, ps=ps, sv=sv: e.tensor_tensor(out=sv[:], in0=ps[0:64, :], in1=sb_bc[0:64, :], op=ALU.add),
                     reads=[ps, sb_bc], writes=[sv])
                S.op("pool", lambda e, s=s, sv=sv, u=u, o=o: e.tensor_tensor(out=o[:, :, s * 128:(s + 1) * 128],
                                                                              in0=sv[:, :].rearrange("c (g i) -> c g i", g=4),
                                                                              in1=u[:, :, s * 128:(s + 1) * 128], op=ALU.mult),
                     reads=[sv, u], writes=[o])
            S.dma("sp", C.oT[256:512, t0:t0 + 512].rearrange("(g c) t -> c g t", c=64), o[:], reads=[o], sem_tile=o)
        S.barrier()
        for t_ in [g_bc, b_bc, sb_bc, wraw] + vt + ut + ot:
            S.release(t_)


def phase_zero_branch(C, r0):
    S, T, NT = C.S, C.T, C.NT
    with ExitStack() as st:
        z = S.sb("zb_z", [128, 2, 512], BF16, st)
        S.op("pool", lambda e: e.memset(z[:], 0.0), writes=[z])
        for i in range(NT):
            S.dma("sp", C.oT[r0:r0 + 256, i * 512:(i + 1) * 512].rearrange("(f p) t -> p f t", p=128), z[:], reads=[z], sem_tile=z)
        S.barrier()
        S.release(z)


def proj_res_ln(C, st, inT, W, g_bc, b_bc, tile_i, hres, hTs, out_ap=None):
    S = C.S
    for sub in range(4):
        hr = hres[C.hri % 2]
        C.hri += 1
        tok0 = tile_i * 512 + sub * 128
        S.dma("sp", hr[:], C.h_tok[tok0:tok0 + 128, :], writes=[hr], sem_tile=hr)
        for half in range(2):
            ps = C.psum()
            for kc in range(8):
                S.op("pe", lambda e, kc=kc, ps=ps, half=half, sub=sub: e.matmul(
                    ps[:, :], lhsT=inT[:, kc, sub * 128:(sub + 1) * 128], rhs=W[:, kc, half * 512:(half + 1) * 512],
                    start=(kc == 0), stop=(kc == 7)), reads=[inT, W], writes=[ps])
            S.op("dve", lambda e, ps=ps, half=half, hr=hr: e.scalar_tensor_tensor(
                out=hr[:, half * 512:(half + 1) * 512], in0=hr[:, half * 512:(half + 1) * 512], scalar=DN_ALPHA, in1=ps[:, :],
                op0=ALU.mult, op1=ALU.add), reads=[hr, ps], writes=[hr])
        ln_tile(C, (hr[:], hr), (hr[:], hr), g_bc, b_bc)
        store_h(C, st, hr, hTs, tile_i, sub, out_ap)


def phase_merge(C, l):
    S, T, NT, P = C.S, C.T, C.NT, C.P
    with ExitStack() as st:
        brp = S.sb("mg_brp", [128, 8, 1024], BF16, st)
        S.dma("pool", brp[:], P["br_proj"][l].rearrange("i (kc p) n -> p (i kc) n", p=128), writes=[brp], sem_tile=brp)
        wout = S.sb("mg_wout", [128, 8, 1024], BF16, st)
        load_w_bf16(C, wout, P["w_out"][l])
        g_bc = load_bc(C, st, "mg_g", P["ln1_g"][l], D)
        b_bc = load_bc(C, st, "mg_b", P["ln1_b"][l], D)
        oTt = [S.sb("mg_o%d" % i, [128, 8, 512], BF16, st) for i in range(2)]
        gTt = [S.sb("mg_g%d" % i, [128, 4, 512], BF16, st) for i in range(2)]
        mT = [S.sb("mg_m%d" % i, [128, 8, 512], BF16, st) for i in range(2)]
        tm = [S.sb("mg_t%d" % i, [128, 4, 512], F32, st) for i in range(2)]
        hres = [S.sb("mg_hr%d" % i, [128, D], F32, st) for i in range(2)]
        hTs = [S.sb("mg_hT%d" % i, [128, 8, 512], BF16, st) for i in range(2)]
        C.hri = 0
        ng = 0
        for i in range(NT):
            t0 = i * 512
            o = oTt[i % 2]
            m = mT[i % 2]
            S.dma("sp", o[:], C.oT[:, t0:t0 + 512].rearrange("(j p) t -> p j t", p=128), writes=[o], sem_tile=o)
            for ct in range(8):
                g = gTt[ng % 2]
                t4 = tm[ng % 2]
                ng += 1
                S.dma("sp", g[:], C.gT.rearrange("(i ct p) t -> p i ct t", p=128, ct=8)[:, :, ct, t0:t0 + 512], writes=[g], sem_tile=g)
                for b in range(4):
                    ps = C.psum()
                    for kc in range(2):
                        S.op("pe", lambda e, b=b, kc=kc, ct=ct, ps=ps, o=o: e.matmul(
                            ps[:, :], lhsT=brp[:, b * 2 + kc, ct * 128:(ct + 1) * 128], rhs=o[:, b * 2 + kc, :],
                            start=(kc == 0), stop=(kc == 1)), reads=[brp, o], writes=[ps])
                    S.op("dve", lambda e, b=b, ps=ps, g=g, t4=t4: e.tensor_tensor(out=t4[:, b, :], in0=ps[:, :], in1=g[:, b, :], op=ALU.mult),
                         reads=[ps, g], writes=[t4])
                S.op("pool", lambda e, t4=t4: e.tensor_tensor(out=t4[:, 0:2, :], in0=t4[:, 0:2, :], in1=t4[:, 2:4, :], op=ALU.add),
                     reads=[t4], writes=[t4])
                S.op("pool", lambda e, t4=t4, m=m, ct=ct: e.tensor_tensor(out=m[:, ct, :], in0=t4[:, 0, :], in1=t4[:, 1, :], op=ALU.add),
                     reads=[t4], writes=[m])
            proj_res_ln(C, st, m, wout, g_bc, b_bc, i, hres, hTs[i % 2])
        S.barrier()
        for t_ in [brp, wout, g_bc, b_bc] + oTt + gTt + mT + hres + hTs:
            S.release(t_)


def phase_attn(C, l):
    S, T, NT, P = C.S, C.T, C.NT, C.P
    with ExitStack() as st:
        wq = S.sb("at_wq", [128, 8, 1024], BF16, st)
        wo = S.sb("at_wo", [128, 8, 1024], BF16, st)
        load_w_bf16(C, wq, P["xa_wq"][l])
        load_w_bf16(C, wo, P["xa_wo"][l])
        g_bc = load_bc(C, st, "at_g", P["ln2_g"][l], D)
        b_bc = load_bc(C, st, "at_b", P["ln2_b"][l], D)
        kT = S.sb("at_kT", [128, 8, 256], BF16, st)
        vv = S.sb("at_v", [128, 2, 1024], BF16, st)
        with ExitStack() as st2:
            wk = S.sb("at_wk", [128, 8, 1024], BF16, st2)
            wv = S.sb("at_wv", [128, 8, 1024], BF16, st2)
            load_w_bf16(C, wk, P["xa_wk"][l])
            load_w_bf16(C, wv, P["xa_wv"][l])
            mt = S.sb("at_mem", [128, 2, 1024], F32, st2)
            memT = S.sb("at_memT", [128, 8, 256], BF16, st2)
            S.dma("sp", mt[:], C.mem_in.rearrange("(s p) c -> p s c", p=128), writes=[mt], sem_tile=mt)
            for s in range(2):
                transpose_to(C, mt[:, s, :], mt, memT, lambda g, s=s: memT[:, g * 4:(g + 1) * 4, s * 128:(s + 1) * 128])
            for ct in range(8):
                ps = C.psum()
                for kc in range(8):
                    S.op("pe", lambda e, kc=kc, ct=ct, ps=ps: e.matmul(ps[:, 0:256], lhsT=wk[:, kc, ct * 128:(ct + 1) * 128],
                                                                        rhs=memT[:, kc, :], start=(kc == 0), stop=(kc == 7)),
                         reads=[wk, memT], writes=[ps])
                en = evac_eng(C)
                S.op(en, copy_op(en, kT[:, ct, :], ps[:, 0:256]), reads=[ps], writes=[kT])
            for s in range(2):
                for half in range(2):
                    ps = C.psum()
                    for kc in range(8):
                        S.op("pe", lambda e, kc=kc, s=s, half=half, ps=ps: e.matmul(
                            ps[:, :], lhsT=memT[:, kc, s * 128:(s + 1) * 128], rhs=wv[:, kc, half * 512:(half + 1) * 512],
                            start=(kc == 0), stop=(kc == 7)), reads=[wv, memT], writes=[ps])
                    en = evac_eng(C)
                    S.op(en, copy_op(en, vv[:, s, half * 512:(half + 1) * 512], ps[:, :]), reads=[ps], writes=[vv])
            S.barrier()
            for t_ in [wk, wv, mt]:
                S.release(t_)
        hTt = [S.sb("at_h%d" % i, [128, 8, 512], BF16, st) for i in range(2)]
        qT = [S.sb("at_q%d" % i, [128, 8, 512], BF16, st) for i in range(2)]
        aT = [S.sb("at_a%d" % i, [128, 8, 512], BF16, st) for i in range(2)]
        pt = [S.sb("at_p%d" % i, [128, 256], F32, st) for i in range(4)]
        pT = [S.sb("at_pT%d" % i, [128, 2, 128], BF16, st) for i in range(4)]
        mx = [S.sb("at_mx%d" % i, [128, 1], F32, st) for i in range(4)]
        sm = [S.sb("at_sm%d" % i, [128, 1], F32, st) for i in range(4)]
        hres = [S.sb("at_hr%d" % i, [128, D], F32, st) for i in range(2)]
        hTs = [S.sb("at_hT%d" % i, [128, 8, 512], BF16, st) for i in range(2)]
        C.hri = 0
        n = 0
        for i in range(NT):
            t0 = i * 512
            hh, q, a = hTt[i % 2], qT[i % 2], aT[i % 2]
            S.dma("sp", hh[:], C.hT.rearrange("(kc p) t -> p kc t", p=128)[:, :, t0:t0 + 512], writes=[hh], sem_tile=hh)
            for ct in range(8):
                ps = C.psum()
                for kc in range(8):
                    S.op("pe", lambda e, kc=kc, ct=ct, ps=ps, hh=hh: e.matmul(ps[:, :], lhsT=wq[:, kc, ct * 128:(ct + 1) * 128],
                                                                               rhs=hh[:, kc, :], start=(kc == 0), stop=(kc == 7)),
                         reads=[wq, hh], writes=[ps])
                S.op("act", lambda e, ct=ct, ps=ps, q=q: e.activation(out=q[:, ct, :], in_=ps[:, :], func=AF.Copy, scale=1.0 / 16.0),
                     reads=[ps], writes=[q])
            for sub in range(4):
                pss = []
                for hd in range(4):
                    ps = C.psum()
                    pss.append(ps)
                    for j in range(2):
                        S.op("pe", lambda e: e.matmul(ps[:, 0:256], lhsT=q[:, 2 * hd + j, sub * 128:(sub + 1) * 128], rhs=kT[:, 2 * hd + j, :],
                                                      start=(j == 0), stop=(j == 1)), reads=[q, kT], writes=[ps])
                for hd in range(4):
                    ps, p_, mx_, sm_ = pss[hd], pt[hd], mx[hd], sm[hd]
                    S.op("dve", lambda e: e.reduce_max(out=mx_[:], in_=ps[:, 0:256], axis=AX.X, negate=True), reads=[ps], writes=[mx_])
                    S.op("act", lambda e: e.activation(out=p_[:], in_=ps[:, 0:256], func=AF.Exp, bias=mx_[:, 0:1], scale=1.0, accum_out=sm_[:]),
                         reads=[ps, mx_], writes=[p_, sm_])
                    S.op("dve", lambda e: e.reciprocal(out=sm_[:], in_=sm_[:]), reads=[sm_], writes=[sm_])
                    S.op("dve", lambda e: e.tensor_scalar(out=p_[:], in0=p_[:], scalar1=sm_[:, 0:1], scalar2=None, op0=ALU.mult), reads=[p_, sm_], writes=[p_])
                for hd in range(4):
                    p_, pT_ = pt[hd], pT[hd]
                    ps2 = C.psum()
                    for j in range(2):
                        S.op("pe", lambda e: e.transpose(out=ps2[:, j * 128:(j + 1) * 128], in_=p_[:, j * 128:(j + 1) * 128], identity=C.ident[:]),
                             reads=[p_, C.ident], writes=[ps2])
                    S.op("act", lambda e: e.copy(out=pT_[:], in_=ps2[:, 0:256].rearrange("p (a b) -> p a b", a=2)), reads=[ps2], writes=[pT_])
                for hd in range(4):
                    pT_ = pT[hd]
                    ps3 = C.psum()
                    for j in range(2):
                        ct = 2 * hd + j
                        for mt_ in range(2):
                            S.op("pe", lambda e: e.matmul(ps3[:, j * 128:(j + 1) * 128], lhsT=vv[:, mt_, ct * 128:(ct + 1) * 128], rhs=pT_[:, mt_, :],
                                                          start=(mt_ == 0), stop=(mt_ == 1)), reads=[vv, pT_], writes=[ps3])
                    S.op("dve", lambda e: e.tensor_copy(out=a[:, 2 * hd:2 * hd + 2, sub * 128:(sub + 1) * 128],
                                                        in_=ps3[:, 0:256].rearrange("p (a b) -> p a b", a=2)), reads=[ps3], writes=[a])
            proj_res_ln(C, st, a, wo, g_bc, b_bc, i, hres, hTs[i % 2])
        S.barrier()
        for t_ in [wq, wo, g_bc, b_bc] + hTt + hres + hTs:
            S.release(t_)


def phase_router(C, l):
    S, T, NT, P = C.S, C.T, C.NT, C.P
    with ExitStack() as st:
        rw = S.sb("rt_w", [128, 8, NE], F32, st)
        S.dma("sp", rw[:], P["router_w"][l].rearrange("(kc p) n -> p kc n", p=128), writes=[rw], sem_tile=rw)
        rb = load_bc(C, st, "rt_b", P["router_b"][l], NE)
        ht = [S.sb("rt_h%d" % i, [128, D], F32, st) for i in range(2)]
        hTf = [S.sb("rt_hT%d" % i, [128, 8, 128], F32, st) for i in range(2)]
        lg = [S.sb("rt_lg%d" % i, [128, NE], F32, st) for i in range(2)]
        t8 = [S.sb("rt_t8%d" % i, [128, 8], F32, st) for i in range(2)]
        ex = [S.sb("rt_ex%d" % i, [128, NE], F32, st) for i in range(2)]
        mk = [S.sb("rt_mk%d" % i, [128, NE], F32, st) for i in range(2)]
        sm = [S.sb("rt_sm%d" % i, [128, 1], F32, st) for i in range(2)]
        for n in range(T // 128):
            h, hf, lg_, t8_, ex_, mk_, sm_ = ht[n % 2], hTf[n % 2], lg[n % 2], t8[n % 2], ex[n % 2], mk[n % 2], sm[n % 2]
            S.dma("sp", h[:], C.h_tok[n * 128:(n + 1) * 128, :], writes=[h], sem_tile=h)
            transpose_to(C, h[:], h, hf, lambda g, hf=hf: hf[:, g * 4:(g + 1) * 4, :])
            ps = C.psum()
            for kc in range(8):
                S.op("pe", lambda e, kc=kc, ps=ps, hf=hf: e.matmul(ps[:, 0:NE], lhsT=hf[:, kc, :], rhs=rw[:, kc, :],
                                                                    start=(kc == 0), stop=(kc == 7)), reads=[hf, rw], writes=[ps])
            S.op("dve", lambda e, ps=ps, lg_=lg_: e.tensor_tensor(out=lg_[:], in0=ps[:, 0:NE], in1=rb[:], op=ALU.add),
                 reads=[ps, rb], writes=[lg_])
            S.op("dve", lambda e, lg_=lg_, t8_=t8_: e.max(out=t8_[:], in_=lg_[:]), reads=[lg_], writes=[t8_])
            S.op("dve", lambda e, lg_=lg_, t8_=t8_, mk_=mk_: e.tensor_scalar(out=mk_[:], in0=lg_[:], scalar1=t8_[:, 3:4], scalar2=None, op0=ALU.is_ge),
                 reads=[lg_, t8_], writes=[mk_])
            S.op("dve", lambda e, lg_=lg_, t8_=t8_, ex_=ex_: e.tensor_scalar(out=ex_[:], in0=lg_[:], scalar1=t8_[:, 0:1], scalar2=None, op0=ALU.subtract),
                 reads=[lg_, t8_], writes=[ex_])
            S.op("act", lambda e, ex_=ex_: e.activation(out=ex_[:], in_=ex_[:], func=AF.Exp), reads=[ex_], writes=[ex_])
            S.op("dve", lambda e, ex_=ex_, mk_=mk_: e.tensor_tensor(out=ex_[:], in0=ex_[:], in1=mk_[:], op=ALU.mult), reads=[ex_, mk_], writes=[ex_])
            S.op("dve", lambda e, ex_=ex_, sm_=sm_: e.reduce_sum(out=sm_[:], in_=ex_[:], axis=AX.X), reads=[ex_], writes=[sm_])
            S.op("dve", lambda e, sm_=sm_: e.reciprocal(out=sm_[:], in_=sm_[:]), reads=[sm_], writes=[sm_])
            S.op("dve", lambda e, ex_=ex_, sm_=sm_: e.tensor_scalar(out=ex_[:], in0=ex_[:], scalar1=sm_[:, 0:1], scalar2=None, op0=ALU.mult),
                 reads=[ex_, sm_], writes=[ex_])
            S.dma("sp", C.gate_d[n * 128:(n + 1) * 128, :], ex_[:], reads=[ex_], sem_tile=ex_)
        S.barrier()
        for t_ in [rw, rb] + ht + ex:
            S.release(t_)


def phase_moe(C, l, out_ap):
    S, T, NT, P = C.S, C.T, C.NT, C.P
    ST = 1024 if T >= 1024 else 512
    nsub = ST // 128
    ntt = ST // 512
    with ExitStack() as st:
        g_bc = load_bc(C, st, "mo_g", P["ln3_g"][l], D)
        b_bc = load_bc(C, st, "mo_b", P["ln3_b"][l], D)
        w1t = [S.sb("mo_w1%d" % i, [128, 8, 2048], BF16, st) for i in range(2)]
        w2 = [S.sb("mo_w2%d" % i, [128, 8, 1024], BF16, st) for i in range(2)]
        b1r = [S.sb("mo_b1r%d" % i, [1, 2048], BF16, st) for i in range(2)]
        ones_row = S.sb("mo_ones", [1, 512], BF16, st)
        S.op("pool", lambda e: e.memset(ones_row[:], 1.0), writes=[ones_row])
        b2 = [S.sb("mo_b2%d" % i, [1, 1024], BF16, st) for i in range(2)]
        acc = S.sb("mo_acc", [128, nsub, 1024], F32, st)
        hTt = S.sb("mo_hT", [128, 8, ST], BF16, st)
        gt = S.sb("mo_gate", [128, nsub, NE], F32, st)
        actT = [S.sb("mo_act%d" % i, [128, 8, 512], BF16, st) for i in range(2)]
        gq = [S.sb("mo_gq%d" % i, [128, 512], F32, st) for i in range(2)]
        sg = [S.sb("mo_sg%d" % i, [128, 512], F32, st) for i in range(2)]
        uq = [S.sb("mo_uq%d" % i, [128, 512], F32, st) for i in range(2)]
        hTs = [S.sb("mo_hTs%d" % i, [128, 8, 512], BF16, st) for i in range(1)] * 2
        w1 = P["ex_w1"][l]
        nw = 0
        na = 0
        nq = 0
        for sti in range(T // ST):
            tok0 = sti * ST
            S.dma("sp", acc[:], C.h_tok[tok0:tok0 + ST, :].rearrange("(s p) c -> p s c", p=128), writes=[acc], sem_tile=acc)
            S.dma("sp", hTt[:], C.hT.rearrange("(kc p) t -> p kc t", p=128)[:, :, tok0:tok0 + ST], writes=[hTt], sem_tile=hTt)
            S.dma("sp", gt[:], C.gate_d[tok0:tok0 + ST, :].rearrange("(s p) c -> p s c", p=128), writes=[gt], sem_tile=gt)
            S.op("pool", lambda e: e.tensor_scalar(out=acc[:], in0=acc[:], scalar1=DN_ALPHA, scalar2=None, op0=ALU.mult),
                 reads=[acc], writes=[acc])
            for ex in range(NE):
                w1_, w2_, b1r_, b2_ = w1t[nw % 2], w2[nw % 2], b1r[nw % 2], b2[nw % 2]
                nw += 1
                S.dma("pool", w1_[:], w1[ex].rearrange("(kc p) n -> p kc n", p=128), writes=[w1_], sem_tile=w1_)
                S.dma("pool", w2_[:], P["ex_w2"][l, ex].rearrange("(kc p) n -> p kc n", p=128), writes=[w2_], sem_tile=w2_)
                S.dma("pool", b2_[:], P["ex_b2"][l, ex:ex + 1, :], writes=[b2_], sem_tile=b2_)
                S.dma("pool", b1r_[:], P["ex_b1"][l, ex:ex + 1, :], writes=[b1r_], sem_tile=b1r_)
                for tt in range(ntt):
                    a = actT[na % 2]
                    na += 1
                    for ft in range(8):
                        g_, s_, u_ = gq[nq % 2], sg[nq % 2], uq[nq % 2]
                        nq += 1
                        psg = C.psum()
                        S.op("pe", lambda e, ft=ft, psg=psg, b1r_=b1r_: e.matmul(
                            psg[:, :], lhsT=b1r_[0:1, ft * 256:(ft + 1) * 256:2], rhs=ones_row[0:1, :], start=True, stop=False),
                            reads=[b1r_, ones_row], writes=[psg])
                        for kc in range(8):
                            S.op("pe", lambda e, kc=kc, ft=ft, psg=psg, w1_=w1_, tt=tt: e.matmul(
                                psg[:, :], lhsT=w1_[:, kc, ft * 256:(ft + 1) * 256:2], rhs=hTt[:, kc, tt * 512:(tt + 1) * 512],
                                start=False, stop=(kc == 7)), reads=[w1_, hTt], writes=[psg])
                        psu = C.psum()
                        S.op("pe", lambda e, ft=ft, psu=psu, b1r_=b1r_: e.matmul(
                            psu[:, :], lhsT=b1r_[0:1, ft * 256 + 1:(ft + 1) * 256:2], rhs=ones_row[0:1, :], start=True, stop=False),
                            reads=[b1r_, ones_row], writes=[psu])
                        for kc in range(8):
                            S.op("pe", lambda e, kc=kc, ft=ft, psu=psu, w1_=w1_, tt=tt: e.matmul(
                                psu[:, :], lhsT=w1_[:, kc, ft * 256 + 1:(ft + 1) * 256:2], rhs=hTt[:, kc, tt * 512:(tt + 1) * 512],
                                start=False, stop=(kc == 7)), reads=[w1_, hTt], writes=[psu])
                        S.op("dve", lambda e, psg=psg, g_=g_: e.tensor_scalar(
                            out=g_[:], in0=psg[:, :], scalar1=7.0, scalar2=None, op0=ALU.min), reads=[psg], writes=[g_])
                        S.op("act", lambda e, g_=g_, s_=s_: e.activation(out=s_[:], in_=g_[:], func=AF.Sigmoid, scale=1.702),
                             reads=[g_], writes=[s_])
                        S.op("dve", lambda e, psu=psu, u_=u_: e.tensor_scalar(
                            out=u_[:], in0=psu[:, :], scalar1=7.0, scalar2=-7.0, op0=ALU.min, op1=ALU.max), reads=[psu], writes=[u_])
                        S.op("pool", lambda e, g_=g_, s_=s_: e.tensor_tensor(out=s_[:], in0=g_[:], in1=s_[:], op=ALU.mult),
                             reads=[g_, s_], writes=[s_])
                        S.op("dve", lambda e, ft=ft, s_=s_, u_=u_, a=a: e.scalar_tensor_tensor(out=a[:, ft, :], in0=u_[:], scalar=1.0, in1=s_[:],
                                                                                         op0=ALU.add, op1=ALU.mult), reads=[s_, u_], writes=[a])
                    for sub in range(4):
                        s_idx = tt * 4 + sub
                        for half in range(2):
                            ps = C.psum()
                            S.op("pe", lambda e, ps=ps, half=half, b2_=b2_: e.matmul(
                                ps[:, :], lhsT=C.ones_b[0:1, :], rhs=b2_[0:1, half * 512:(half + 1) * 512], start=True, stop=False),
                                reads=[C.ones_b, b2_], writes=[ps])
                            for ft in range(8):
                                S.op("pe", lambda e, ft=ft, ps=ps, half=half, sub=sub, a=a, w2_=w2_: e.matmul(
                                    ps[:, :], lhsT=a[:, ft, sub * 128:(sub + 1) * 128], rhs=w2_[:, ft, half * 512:(half + 1) * 512],
                                    start=False, stop=(ft == 7)), reads=[a, w2_], writes=[ps])
                            S.op("dve", lambda e, ps=ps, half=half, s_idx=s_idx, ex=ex: e.scalar_tensor_tensor(
                                out=acc[:, s_idx, half * 512:(half + 1) * 512], in0=ps[:, :], scalar=gt[:, s_idx, ex:ex + 1],
                                in1=acc[:, s_idx, half * 512:(half + 1) * 512], op0=ALU.mult, op1=ALU.add),
                                reads=[ps, gt, acc], writes=[acc])
            for tt in range(ntt):
                tile_i = sti * ntt + tt
                for sub in range(4):
                    s_idx = tt * 4 + sub
                    ln_tile(C, (acc[:, s_idx, :], acc), (acc[:, s_idx, :], acc), g_bc, b_bc)
                    S_store_sub(C, acc, s_idx, hTs[tile_i % 2], tile_i, sub, out_ap)
        S.barrier()
        for t_ in [g_bc, b_bc, acc, hTt, gt] + w1t + w2 + b1r + b2 + hTs:
            S.release(t_)


def S_store_sub(C, acc, s_idx, hTs, tile_i, sub, out_ap):
    S = C.S
    tok0 = tile_i * 512 + sub * 128
    S.dma("sp", C.h_tok[tok0:tok0 + 128, :], acc[:, s_idx, :], reads=[acc], sem_tile=acc)
    if out_ap is not None:
        S.dma("sp", out_ap[tok0:tok0 + 128, :], acc[:, s_idx, :], reads=[acc], sem_tile=acc)
    transpose_to(C, acc[:, s_idx, :], acc, hTs, lambda g: hTs[:, g * 4:(g + 1) * 4, sub * 128:(sub + 1) * 128])
    if sub == 3:
        S.dma("sp", C.hT.rearrange("(kc p) t -> p kc t", p=128)[:, :, tile_i * 512:(tile_i + 1) * 512], hTs[:],
              reads=[hTs], sem_tile=hTs)


RW_EPS = 64e-5
RW_FP32R = False
RW_C = math.exp(-0.5)


def phase_rwkv(C, l):
    S, T, P = C.S, C.T, C.P
    MT = 256
    NCH = MT // 32
    with ExitStack() as st:
        def cols64(nm, key):
            return load_cols(C, st, nm, P[key][l].rearrange("(h n) -> h n", n=64), 4, m=64)
        w0c, a0c, kkc, kac, lgc, lbc = (cols64("rw_" + k, "rw_" + k) for k in ("w0", "a0", "k_k", "k_a", "ln_g", "ln_b"))
        rkc = load_cols(C, st, "rw_rk", P["rw_r_k"][l], 4, m=64)
        wup = S.sb("rw_wup", [64, 256], F32, st)
        aup = S.sb("rw_aup", [64, 256], F32, st)
        gup = S.sb("rw_gup", [128, 256], F32, st)
        S.dma("sp", wup[:], P["rw_w_up"][l], writes=[wup], sem_tile=wup)
        S.dma("sp", aup[:], P["rw_a_up"][l], writes=[aup], sem_tile=aup)
        S.dma("sp", gup[:], P["rw_g_up"][l], writes=[gup], sem_tile=gup)
        bd = S.sb("rw_bd", [128, 128], F32, st)
        S.op("pool", lambda e: e.memset(bd[:], 0.0), writes=[bd])
        for h in range(4):
            S.op("pool", lambda e, h=h: e.memset(bd[32 * h:32 * h + 32, 32 * h:32 * h + 32], 1.0), reads=[bd], writes=[bd])
        mA = S.sb("rw_mA", [128, 4, 128], F32, st)
        mB = S.sb("rw_mB", [128, 2, 128], F32, st)
        for j in range(4):
            cmp = ALU.is_gt if j < 2 else ALU.is_ge
            S.op("pool", lambda e, j=j, cmp=cmp: e.affine_select(out=mA[:, j, :], in_=bd[:], compare_op=cmp, fill=0.0, base=0,
                                                                 pattern=[[1, 128]], channel_multiplier=-1), reads=[bd], writes=[mA])
        for j in range(2):
            S.op("pool", lambda e, j=j: e.affine_select(out=mB[:, j, :], in_=bd[:], compare_op=ALU.is_gt, fill=0.0, base=0,
                                                        pattern=[[-1, 128]], channel_multiplier=1), reads=[bd], writes=[mB])
        cmask = S.sb("rw_cmask", [64, 4 * MT], F32, st)
        S.op("pool", lambda e: e.memset(cmask[:], 1.0), writes=[cmask])
        S.op("pool", lambda e: e.memset(cmask[:, :].rearrange("p (c l) -> p c l", l=32)[:, :, 0:1], 0.0), reads=[cmask], writes=[cmask])
        o64 = S.sb("rw_o64", [64, 64], F32, st)
        S.op("pool", lambda e: e.memset(o64[:], 1.0), writes=[o64])
        o64m = S.sb("rw_o64m", [64, 64], F32, st)
        S.op("pool", lambda e: e.memset(o64m[:], 1.0 / 64.0), writes=[o64m])
        Ebd = S.sb("rw_Ebd", [128, 256], F32, st)
        S.op("pool", lambda e: e.memset(Ebd[:], 0.0), writes=[Ebd])
        S0 = [S.sb("rw_S%d" % i, [64, 256], F32, st) for i in range(2)]
        S.op("pool", lambda e: e.memset(S0[0][:], 0.0), writes=[S0[0]])
        nS = 0

        def kh(nm):
            return S.sb(nm, [64, 4, MT], F32, st)
        r_t, k_t, v_t, a_t, g_t, ka_t, b_t, lw_t, G_t, eG_t, bon_t, Y_t, x1, x2 = (
            kh("rw_" + n) for n in ("r", "k", "v", "a", "g", "ka", "b", "lw", "G", "eG", "bon", "Y", "x1", "x2"))
        rC, kC, bC, kaC, vC = (S.sb("rw_c" + n, [64, NCH, 128], F32, st) for n in ("r", "k", "b", "ka", "v"))
        xw_t = S.sb("rw_xw", [64, 2, MT], F32, st)
        xg_t = S.sb("rw_xg", [128, MT], F32, st)
        o_t = S.sb("rw_o", [64, 4, MT], BF16, st)
        GRP = 4
        NSET = 8
        RR = (lambda ap: ap.bitcast(mybir.dt.float32r)) if RW_FP32R else (lambda ap: ap)
        TT_ = [S.sb("rw_TT%d" % i, [128, 4, 64], F32, st) for i in range(NSET)]
        AA = [S.sb("rw_AA%d" % i, [128, 4, 128], F32, st) for i in range(NSET)]
        AB = [S.sb("rw_AB%d" % i, [128, 2, 128], F32, st) for i in range(NSET)]
        Vbds = [S.sb("rw_Vbd%d" % i, [128, 256], F32, st) for i in range(NSET)]
        for vb in Vbds:
            S.op("pool", lambda e: e.memset(vb[:], 0.0), writes=[vb])
        PP = [None, None]
        PPx = [S.sb("rw_PP%d" % i, [128, 2, 128], F32, st) for i in range(2 * NSET)]
        MM = [S.sb("rw_MM%d" % i, [128, 2, 128], F32, st) for i in range(NSET)]
        Kh = [S.sb("rw_Kh%d" % i, [64, 128], F32, st) for i in range(NSET)]
        Gh = [S.sb("rw_Gh%d" % i, [128, 128], F32, st) for i in range(NSET)]
        E2 = [S.sb("rw_E2%d" % i, [128, 64], F32, st) for i in range(NSET)]
        ET = [S.sb("rw_ET%d" % i, [128, 64], F32, st) for i in range(NSET)]
        tmpS = S.sb("rw_tmpS", [64, 256], F32, st)
        nchunk = 0
        zc = C.zT

        def perhead(fn):
            for h in range(4):
                fn(h)

        def fl(t):
            return t[:, :, :].rearrange("p h t -> p (h t)")

        for mi in range(T // MT):
            t0 = mi * MT
            for j, tl in enumerate((r_t, k_t, v_t)):
                S.dma("sp", tl[:], zc[OFF_C + j * 256:OFF_C + (j + 1) * 256, t0:t0 + MT].rearrange("(h n) t -> n h t", n=64),
                      writes=[tl], sem_tile=tl)
            S.dma("sp", xw_t[:], zc[OFF_C + 768:OFF_C + 896, t0:t0 + MT].rearrange("(a n) t -> n a t", n=64), writes=[xw_t], sem_tile=xw_t)
            S.dma("sp", xg_t[:], zc[OFF_C + 896:OFF_C + 1024, t0:t0 + MT], writes=[xg_t], sem_tile=xg_t)
            S.op("act", lambda e: e.activation(out=xw_t[:, 0, :], in_=xw_t[:, 0, :], func=AF.Tanh), reads=[xw_t], writes=[xw_t])
            S.op("act", lambda e: e.activation(out=xg_t[:], in_=xg_t[:], func=AF.Sigmoid), reads=[xg_t], writes=[xg_t])
            for h in range(4):
                ps = C.psum()
                S.op("pe", lambda e, h=h, ps=ps: e.matmul(ps[0:64, 0:MT], lhsT=wup[:, h * 64:(h + 1) * 64], rhs=xw_t[:, 0, :], start=True, stop=True),
                     reads=[wup, xw_t], writes=[ps])
                S.op("act", lambda e, h=h, ps=ps: e.activation(out=lw_t[:, h, :], in_=ps[0:64, 0:MT], func=AF.Sigmoid, bias=w0c[:, h:h + 1], scale=1.0),
                     reads=[ps, w0c], writes=[lw_t])
                ps = C.psum()
                S.op("pe", lambda e, h=h, ps=ps: e.matmul(ps[0:64, 0:MT], lhsT=aup[:, h * 64:(h + 1) * 64], rhs=xw_t[:, 1, :], start=True, stop=True),
                     reads=[aup, xw_t], writes=[ps])
                S.op("act", lambda e, h=h, ps=ps: e.activation(out=a_t[:, h, :], in_=ps[0:64, 0:MT], func=AF.Sigmoid, bias=a0c[:, h:h + 1], scale=1.0),
                     reads=[ps, a0c], writes=[a_t])
                ps = C.psum()
                S.op("pe", lambda e, h=h, ps=ps: e.matmul(ps[0:64, 0:MT], lhsT=gup[:, h * 64:(h + 1) * 64], rhs=xg_t[:, :], start=True, stop=True),
                     reads=[gup, xg_t], writes=[ps])
                S.op("dve", lambda e, h=h, ps=ps: e.tensor_copy(out=g_t[:, h, :], in_=ps[0:64, 0:MT]), reads=[ps], writes=[g_t])
            S.op("pool", lambda e: e.tensor_scalar(out=fl(lw_t), in0=fl(lw_t), scalar1=-RW_C, scalar2=None, op0=ALU.mult), reads=[lw_t], writes=[lw_t])
            for h in range(4):
                S.op("dve", lambda e, h=h: e.tensor_scalar(out=ka_t[:, h, :], in0=k_t[:, h, :], scalar1=kkc[:, h:h + 1], scalar2=None, op0=ALU.mult),
                     reads=[k_t, kkc], writes=[ka_t])
            S.op("pool", lambda e: e.tensor_tensor(out=fl(x1), in0=fl(ka_t), in1=fl(ka_t), op=ALU.mult), reads=[ka_t], writes=[x1])
            for h in range(4):
                ps = C.psum()
                S.op("pe", lambda e, h=h, ps=ps: e.matmul(ps[0:64, 0:MT], lhsT=o64[:, :], rhs=x1[:, h, :], start=True, stop=True), reads=[o64, x1], writes=[ps])
                S.op("act", lambda e, h=h, ps=ps: e.activation(out=x2[:, h, :], in_=ps[0:64, 0:MT], func=AF.Sqrt), reads=[ps], writes=[x2])
            S.op("dve", lambda e: e.tensor_scalar(out=fl(x2), in0=fl(x2), scalar1=1e-12, scalar2=None, op0=ALU.max), reads=[x2], writes=[x2])
            S.op("dve", lambda e: e.reciprocal(out=fl(x2), in_=fl(x2)), reads=[x2], writes=[x2])
            S.op("pool", lambda e: e.tensor_tensor(out=fl(ka_t), in0=fl(ka_t), in1=fl(x2), op=ALU.mult), reads=[ka_t, x2], writes=[ka_t])
            for h in range(4):
                S.op("dve", lambda e, h=h: e.tensor_scalar(out=x1[:, h, :], in0=a_t[:, h, :], scalar1=-1.0, scalar2=kac[:, h:h + 1], op0=ALU.add, op1=ALU.mult),
                     reads=[a_t, kac], writes=[x1])
            S.op("dve", lambda e: e.scalar_tensor_tensor(out=fl(k_t), in0=fl(x1), scalar=1.0, in1=fl(k_t), op0=ALU.add, op1=ALU.mult),
                 reads=[x1, k_t], writes=[k_t])
            S.op("pool", lambda e: e.tensor_tensor(out=fl(b_t), in0=fl(ka_t), in1=fl(a_t), op=ALU.mult), reads=[ka_t, a_t], writes=[b_t])
            S.op("pool", lambda e: e.tensor_tensor(out=fl(x1), in0=fl(r_t), in1=fl(k_t), op=ALU.mult), reads=[r_t, k_t], writes=[x1])
            for h in range(4):
                S.op("dve", lambda e, h=h: e.tensor_scalar(out=x1[:, h, :], in0=x1[:, h, :], scalar1=rkc[:, h:h + 1], scalar2=None, op0=ALU.mult),
                     reads=[x1, rkc], writes=[x1])
            for h in range(4):
                ps = C.psum()
                S.op("pe", lambda e, h=h, ps=ps: e.matmul(ps[0:64, 0:MT], lhsT=o64[:, :], rhs=x1[:, h, :], start=True, stop=True), reads=[o64, x1], writes=[ps])
                S.op("dve", lambda e, h=h, ps=ps: e.tensor_tensor(out=bon_t[:, h, :], in0=ps[0:64, 0:MT], in1=v_t[:, h, :], op=ALU.mult),
                     reads=[ps, v_t], writes=[bon_t])
            S.op("dve", lambda e: e.tensor_tensor_scan(out=fl(G_t), data0=cmask[:, :], data1=fl(lw_t), initial=0.0, op0=ALU.mult, op1=ALU.add),
                 reads=[cmask, lw_t], writes=[G_t])
            S.op("act", lambda e: e.activation(out=fl(eG_t), in_=fl(G_t), func=AF.Exp), reads=[G_t], writes=[eG_t])
            S.op("pool", lambda e: e.tensor_tensor(out=fl(r_t), in0=fl(r_t), in1=fl(eG_t), op=ALU.mult), reads=[r_t, eG_t], writes=[r_t])
            S.op("pool", lambda e: e.tensor_tensor(out=fl(x1), in0=fl(G_t), in1=fl(lw_t), op=ALU.subtract), reads=[G_t, lw_t], writes=[x1])
            S.op("act", lambda e: e.activation(out=fl(x1), in_=fl(x1), func=AF.Exp), reads=[x1], writes=[x1])
            S.op("pool", lambda e: e.tensor_tensor(out=fl(ka_t), in0=fl(ka_t), in1=fl(x1), op=ALU.mult), reads=[ka_t, x1], writes=[ka_t])
            S.op("act", lambda e: e.activation(out=fl(x2), in_=fl(G_t), func=AF.Exp, scale=-1.0), reads=[G_t], writes=[x2])
            S.op("pool", lambda e: e.tensor_tensor(out=fl(b_t), in0=fl(b_t), in1=fl(x2), op=ALU.mult), reads=[b_t, x2], writes=[b_t])
            S.op("pool", lambda e: e.tensor_tensor(out=fl(k_t), in0=fl(k_t), in1=fl(x2), op=ALU.mult), reads=[k_t, x2], writes=[k_t])
            for j, (src, dst) in enumerate(((r_t, rC), (k_t, kC), (b_t, bC), (ka_t, kaC), (v_t, vC))):
                en = "act" if j % 2 else "pool"
                S.op(en, copy_op("act" if en == "act" else "dve", dst[:, :, :].rearrange("p c (h t) -> p h c t", h=4),
                                 src[:, :, :].rearrange("p h (c t) -> p h c t", t=32)), reads=[src], writes=[dst])
            def stage_a(c, si):
                TT, A_, B_, M_, Kh_, Gh_, E2_, Vb_ = TT_[si], AA[si], AB[si], MM[si], Kh[si], Gh[si], E2[si], Vbds[si]
                PPs = (PPx[2 * si], PPx[2 * si + 1])
                rc, kc_, bc, kac_, vc = rC[:, c, :], kC[:, c, :], bC[:, c, :], kaC[:, c, :], vC[:, c, :]
                ps = C.psum()
                for j, (src, srct) in enumerate(((bc, bC), (kc_, kC), (kac_, kaC), (vc, vC))):
                    S.op("pe", lambda e: e.transpose(out=ps[:, j * 64:(j + 1) * 64], in_=src, identity=C.ident[0:64, 0:64]),
                         reads=[srct, C.ident], writes=[ps])
                S.op("act", lambda e: e.copy(out=TT[:], in_=ps[:, 0:256].rearrange("p (a b) -> p a b", a=4)), reads=[ps], writes=[TT])
                for h in range(4):
                    S.op("pool", lambda e: e.tensor_copy(out=Vb_[32 * h:32 * h + 32, 64 * h:64 * h + 64], in_=TT[32 * h:32 * h + 32, 3, :]),
                         reads=[TT], writes=[Vb_])
                yield
                psA = C.psum()
                for j, (lt, ltt, rt, rtt) in enumerate(((bc, bC, kac_, kaC), (kc_, kC, kac_, kaC), (bc, bC, rc, rC), (kc_, kC, rc, rC))):
                    S.op("pe", lambda e: e.matmul(psA[:, j * 128:(j + 1) * 128], lhsT=RR(lt), rhs=RR(rt), start=True, stop=True),
                         reads=[ltt, rtt], writes=[psA])
                S.op("dve", lambda e: e.tensor_tensor(out=A_[:], in0=psA[:, :].rearrange("p (a b) -> p a b", a=4), in1=mA[:], op=ALU.mult),
                     reads=[psA, mA], writes=[A_])
                psB = C.psum()
                for j, (lt, ltt, rt, rtt) in enumerate(((kac_, kaC, bc, bC), (kac_, kaC, kc_, kC))):
                    S.op("pe", lambda e: e.matmul(psB[:, j * 128:(j + 1) * 128], lhsT=RR(lt), rhs=RR(rt), start=True, stop=True),
                         reads=[ltt, rtt], writes=[psB])
                S.op("dve", lambda e: e.tensor_tensor(out=B_[:], in0=psB[:, 0:256].rearrange("p (a b) -> p a b", a=2), in1=mB[:], op=ALU.mult),
                     reads=[psB, mB], writes=[B_])
                S.op("pool", lambda e: e.tensor_tensor(out=M_[:, 0, :], in0=C.ident[:], in1=A_[:, 0, :], op=ALU.subtract), reads=[A_, C.ident], writes=[M_])
                S.op("pool", lambda e: e.tensor_tensor(out=M_[:, 1, :], in0=C.ident[:], in1=B_[:, 0, :], op=ALU.subtract), reads=[B_, C.ident, M_], writes=[M_])
                yield
                cur = (A_[:, 0, :], B_[:, 0, :], A_, B_)
                for it in range(4):
                    p_ap, pt_ap, p_t1, p_t2 = cur
                    psQ = C.psum()
                    S.op("pe", lambda e: e.matmul(psQ[:, 0:128], lhsT=RR(pt_ap), rhs=RR(p_ap), start=True, stop=True), reads=[p_t1, p_t2], writes=[psQ])
                    S.op("pe", lambda e: e.matmul(psQ[:, 128:256], lhsT=RR(p_ap), rhs=RR(pt_ap), start=True, stop=True), reads=[p_t1, p_t2], writes=[psQ])
                    Pn = PPs[it % 2]
                    S.op("act", lambda e: e.copy(out=Pn[:], in_=psQ[:, 0:256].rearrange("p (a b) -> p a b", a=2)), reads=[psQ], writes=[Pn])
                    yield
                    psU = C.psum()
                    S.op("pe", lambda e: e.matmul(psU[:, 0:128], lhsT=RR(M_[:, 1, :]), rhs=RR(Pn[:, 0, :]), start=True, stop=True), reads=[M_, Pn], writes=[psU])
                    if it < 3:
                        S.op("pe", lambda e: e.matmul(psU[:, 128:256], lhsT=RR(Pn[:, 0, :]), rhs=RR(M_[:, 1, :]), start=True, stop=True), reads=[M_, Pn], writes=[psU])
                        S.op("dve", lambda e: e.tensor_tensor(out=M_[:], in0=psU[:, 0:256].rearrange("p (a b) -> p a b", a=2), in1=M_[:], op=ALU.add),
                             reads=[psU, M_], writes=[M_])
                    else:
                        S.op("dve", lambda e: e.tensor_tensor(out=M_[:, 0, :], in0=psU[:, 0:128], in1=M_[:, 0, :], op=ALU.add), reads=[psU, M_], writes=[M_])
                    cur = (Pn[:, 0, :], Pn[:, 1, :], Pn, Pn)
                    yield
                psK = C.psum()
                S.op("pe", lambda e: e.matmul(psK[0:64, 0:128], lhsT=TT[:, 2, :], rhs=M_[:, 0, :], start=True, stop=True), reads=[TT, M_], writes=[psK])
                S.op("act", lambda e: e.copy(out=Kh_[:], in_=psK[0:64, 0:128]), reads=[psK], writes=[Kh_])
                psG = C.psum()
                S.op("pe", lambda e: e.matmul(psG[:, 0:128], lhsT=RR(B_[:, 1, :]), rhs=RR(M_[:, 0, :]), start=True, stop=True), reads=[B_, M_], writes=[psG])
                S.op("act", lambda e: e.copy(out=Gh_[:], in_=psG[:, 0:128]), reads=[psG], writes=[Gh_])
                yield
                psE2 = C.psum()
                S.op("pe", lambda e: e.matmul(psE2[:, 0:64], lhsT=Gh_[:, :], rhs=TT[:, 3, :], start=True, stop=True), reads=[Gh_, TT], writes=[psE2])
                S.op("act", lambda e: e.copy(out=E2_[:], in_=psE2[:, 0:64]), reads=[psE2], writes=[E2_])
                yield

            for c in range(NCH):
                if c % GRP == 0:
                    gens = [stage_a(c + q, (nchunk + q) % NSET) for q in range(GRP)]
                    alive = list(gens)
                    while alive:
                        for g_ in list(alive):
                            try:
                                next(g_)
                            except StopIteration:
                                alive.remove(g_)
                cs = slice(32 * c, 32 * c + 32)
                si = nchunk % NSET
                nchunk += 1
                TT, A_, B_, M_, Kh_, Gh_, E2_, ET_, Vbd = TT_[si], AA[si], AB[si], MM[si], Kh[si], Gh[si], E2[si], ET[si], Vbds[si]
                Sc = S0[nS % 2]
                Sn = S0[(nS + 1) % 2]
                nS += 1
                psE = C.psum()
                S.op("pe", lambda e, psE=psE, Kh_=Kh_, Sc=Sc: e.matmul(psE[:, 0:256], lhsT=Kh_[:, :], rhs=Sc[:, :], start=True, stop=True),
                     reads=[Kh_, Sc], writes=[psE])
                for h in range(4):
                    S.op("dve", lambda e, h=h, psE=psE, ET_=ET_, E2_=E2_: e.scalar_tensor_tensor(
                        out=ET_[32 * h:32 * h + 32, :], in0=psE[32 * h:32 * h + 32, 64 * h:64 * h + 64], scalar=-1.0,
                        in1=E2_[32 * h:32 * h + 32, :], op0=ALU.mult, op1=ALU.subtract), reads=[psE, E2_, ET_], writes=[ET_])
                for h in range(4):
                    S.op("act", lambda e, h=h, ET_=ET_: e.copy(out=Ebd[32 * h:32 * h + 32, 64 * h:64 * h + 64], in_=ET_[32 * h:32 * h + 32, :]),
                         reads=[ET_], writes=[Ebd])
                psY = C.psum()
                S.op("pe", lambda e, psY=psY, TT=TT, A_=A_: e.matmul(psY[0:64, 0:128], lhsT=TT[:, 3, :], rhs=A_[:, 3, :], start=True, stop=False),
                     reads=[TT, A_], writes=[psY])
                S.op("pe", lambda e, psY=psY, ET_=ET_, A_=A_: e.matmul(psY[0:64, 0:128], lhsT=ET_[:, :], rhs=A_[:, 2, :], start=False, stop=False),
                     reads=[ET_, A_], writes=[psY])
                for h in range(4):
                    S.op("pe", lambda e, h=h, psY=psY, Sc=Sc, cs=cs: e.matmul(psY[0:64, 32 * h:32 * h + 32], lhsT=Sc[:, 64 * h:64 * h + 64], rhs=rC[:, c, 32 * h:32 * h + 32],
                                                                         start=False, stop=(h == 3)), reads=[Sc, rC], writes=[psY])
                S.op("act", lambda e, psY=psY, cs=cs: e.copy(out=Y_t[:, :, cs], in_=psY[0:64, 0:128].rearrange("p (h t) -> p h t", h=4)), reads=[psY], writes=[Y_t])
                psS = C.psum()
                S.op("pe", lambda e, psS=psS, TT=TT: e.matmul(psS[0:64, 0:256], lhsT=TT[:, 0, :], rhs=Ebd[:, :], start=True, stop=False),
                     reads=[TT, Ebd], writes=[psS])
                S.op("pe", lambda e, psS=psS, TT=TT: e.matmul(psS[0:64, 0:256], lhsT=TT[:, 1, :], rhs=Vbd[:, :], start=False, stop=True),
                     reads=[TT, Vbd], writes=[psS])
                S.op("dve", lambda e, psS=psS, Sc=Sc: e.tensor_tensor(out=tmpS[:], in0=psS[0:64, 0:256], in1=Sc[:], op=ALU.add), reads=[psS, Sc], writes=[tmpS])
                for h in range(4):
                    S.op("dve", lambda e, h=h, Sn=Sn, c=c: e.tensor_scalar(out=Sn[:, 64 * h:64 * h + 64], in0=tmpS[:, 64 * h:64 * h + 64],
                                                                        scalar1=eG_t[:, h, 32 * c + 31:32 * c + 32], scalar2=None, op0=ALU.mult),
                         reads=[tmpS, eG_t, Sn], writes=[Sn])
            for h in range(4):
                ps = C.psum()
                S.op("pe", lambda e, h=h, ps=ps: e.matmul(ps[0:64, 0:MT], lhsT=o64m[:, :], rhs=Y_t[:, h, :], start=True, stop=True), reads=[o64m, Y_t], writes=[ps])
                S.op("dve", lambda e, h=h, ps=ps: e.tensor_tensor(out=x1[:, h, :], in0=Y_t[:, h, :], in1=ps[0:64, 0:MT], op=ALU.subtract),
                     reads=[ps, Y_t], writes=[x1])
            S.op("pool", lambda e: e.tensor_tensor(out=fl(x2), in0=fl(x1), in1=fl(x1), op=ALU.mult), reads=[x1], writes=[x2])
            for h in range(4):
                ps = C.psum()
                S.op("pe", lambda e, h=h, ps=ps: e.matmul(ps[0:64, 0:MT], lhsT=o64m[:, :], rhs=x2[:, h, :], start=True, stop=True), reads=[o64m, x2], writes=[ps])
                S.op("act", lambda e, h=h, ps=ps: e.activation(out=G_t[:, h, :], in_=ps[0:64, 0:MT], func=AF.Sqrt, bias=C.rweps[0:64, 0:1], scale=1.0),
                     reads=[ps, C.rweps], writes=[G_t])
            S.op("dve", lambda e: e.reciprocal(out=fl(G_t), in_=fl(G_t)), reads=[G_t], writes=[G_t])
            S.op("pool", lambda e: e.tensor_tensor(out=fl(x1), in0=fl(x1), in1=fl(G_t), op=ALU.mult), reads=[x1, G_t], writes=[x1])
            for h in range(4):
                S.op("dve", lambda e, h=h: e.tensor_scalar(out=x1[:, h, :], in0=x1[:, h, :], scalar1=lgc[:, h:h + 1], scalar2=lbc[:, h:h + 1],
                                                           op0=ALU.mult, op1=ALU.add), reads=[x1, lgc, lbc], writes=[x1])
            S.op("pool", lambda e: e.tensor_tensor(out=fl(x1), in0=fl(x1), in1=fl(bon_t), op=ALU.add), reads=[x1, bon_t], writes=[x1])
            S.op("pool", lambda e: e.tensor_tensor(out=fl(o_t), in0=fl(x1), in1=fl(g_t), op=ALU.mult), reads=[x1, g_t], writes=[o_t])
            S.dma("sp", C.oT[512:768, t0:t0 + MT].rearrange("(h n) t -> n h t", n=64), o_t[:], reads=[o_t], sem_tile=o_t)
        S.barrier()
        for t_ in [wup, aup, gup, r_t, k_t, v_t, xw_t, xg_t, o_t]:
            S.release(t_)


TWO_PI = 2.0 * math.pi


def phase_s5(C, l):
    S, T, NT, P = C.S, C.T, C.NT, C.P
    with ExitStack() as st:
        def small(nm, n=8, dt=F32):
            return S.sb("s5_" + nm, [128, n], dt, st)
        are = load_cols(C, st, "s5_are", P["s5_a_re"][l].rearrange("(k a) p -> k (a p)", a=2), 8)
        aim = load_cols(C, st, "s5_aim", P["s5_a_im"][l].rearrange("(k a) p -> k (a p)", a=2), 8)
        ldt = load_bc(C, st, "s5_ldt", P["s5_log_dt"][l], 16)
        dcol = load_cols(C, st, "s5_d", P["s5_d"][l].rearrange("(a p) -> a p", p=128), 2)
        gbcol = load_cols(C, st, "s5_gb", P["s5_glu_b"][l].rearrange("(a p) -> a p", p=128), 2)
        gluw = S.sb("s5_gluw", [128, 2, 256], BF16, st)
        S.dma("pool", gluw[:], P["s5_glu_w"][l].rearrange("(a p) n -> p a n", p=128), writes=[gluw], sem_tile=gluw)
        negpi = small("negpi", 1)
        S.op("pool", lambda e: e.memset(negpi[:], 0.0), writes=[negpi])
        dt_, lre, th, mag, thr, sc, cc, abr, abi, den, cre, cim, ncre, t1, t2, Rre, Rim = (
            small(n) for n in ("dt", "lre", "th", "mag", "thr", "sc", "cc", "abr", "abi", "den", "cre", "cim", "ncre", "t1", "t2", "Rre", "Rim"))
        qi = S.sb("s5_qi", [128, 512], I32, st)
        qf = S.sb("s5_qf", [128, 512], F32, st)
        tb = S.sb("s5_tb", [128, 512], F32, st)
        jf = S.sb("s5_jf", [128, 512], F32, st)
        ang = S.sb("s5_ang", [128, 512], F32, st)

        def rr_sin(dst_ap, dst_t, src_ap, src_t, n, shift=0.0):
            a_, q_, f_, t_ = ang[:, 0:n], qi[:, 0:n], qf[:, 0:n], tb[:, 0:n]
            S.op("dve", lambda e: e.tensor_scalar(out=a_, in0=src_ap, scalar1=shift, scalar2=None, op0=ALU.add), reads=[src_t], writes=[ang])
            S.op("dve", lambda e: e.tensor_scalar(out=q_, in0=a_, scalar1=1.0 / TWO_PI, scalar2=None, op0=ALU.mult), reads=[ang], writes=[qi])
            S.op("dve", lambda e: e.tensor_copy(out=f_, in_=q_), reads=[qi], writes=[qf])
            S.op("dve", lambda e: e.scalar_tensor_tensor(out=a_, in0=f_, scalar=-TWO_PI, in1=a_, op0=ALU.mult, op1=ALU.add), reads=[qf, ang], writes=[ang])
            S.op("dve", lambda e: e.tensor_scalar(out=t_, in0=a_, scalar1=math.pi, scalar2=None, op0=ALU.is_gt), reads=[ang], writes=[tb])
            S.op("dve", lambda e: e.scalar_tensor_tensor(out=a_, in0=t_, scalar=-TWO_PI, in1=a_, op0=ALU.mult, op1=ALU.add), reads=[tb, ang], writes=[ang])
            S.op("dve", lambda e: e.tensor_scalar(out=t_, in0=a_, scalar1=-math.pi, scalar2=None, op0=ALU.is_lt), reads=[ang], writes=[tb])
            S.op("dve", lambda e: e.scalar_tensor_tensor(out=a_, in0=t_, scalar=TWO_PI, in1=a_, op0=ALU.mult, op1=ALU.add), reads=[tb, ang], writes=[ang])
            S.op("dve", lambda e: e.tensor_scalar(out=a_, in0=a_, scalar1=3.1415925, scalar2=-3.1415925, op0=ALU.min, op1=ALU.max), reads=[ang], writes=[ang])
            S.op("act", lambda e: e.activation(out=dst_ap, in_=a_, func=AF.Sin, bias=negpi[:, 0:1], scale=1.0), reads=[ang, negpi], writes=[dst_t])

        S.op("dve", lambda e: e.tensor_copy(out=dt_[0:64, :], in_=ldt[0:64, 0:16:2]), reads=[ldt], writes=[dt_])
        S.op("dve", lambda e: e.tensor_copy(out=dt_[64:128, :], in_=ldt[64:128, 1:16:2]), reads=[ldt, dt_], writes=[dt_])
        S.op("act", lambda e: e.activation(out=dt_[:], in_=dt_[:], func=AF.Exp), reads=[dt_], writes=[dt_])
        S.op("dve", lambda e: e.tensor_tensor(out=lre[:], in0=are[:], in1=dt_[:], op=ALU.mult), reads=[are, dt_], writes=[lre])
        S.op("dve", lambda e: e.tensor_tensor(out=th[:], in0=aim[:], in1=dt_[:], op=ALU.mult), reads=[aim, dt_], writes=[th])
        S.op("act", lambda e: e.activation(out=mag[:], in_=lre[:], func=AF.Exp), reads=[lre], writes=[mag])
        rr_sin(sc[:], sc, th[:], th, 8)
        rr_sin(cc[:], cc, th[:], th, 8, shift=math.pi / 2)
        rr_sin(t1[:], t1, th[:], th, 8)
        S.op("dve", lambda e: e.tensor_copy(out=thr[:], in_=ang[:, 0:8]), reads=[ang], writes=[thr])
        S.op("dve", lambda e: e.tensor_tensor(out=abr[:], in0=mag[:], in1=cc[:], op=ALU.mult), reads=[mag, cc], writes=[abr])
        S.op("dve", lambda e: e.tensor_tensor(out=abi[:], in0=mag[:], in1=sc[:], op=ALU.mult), reads=[mag, sc], writes=[abi])
        S.op("dve", lambda e: e.tensor_tensor(out=den[:], in0=are[:], in1=are[:], op=ALU.mult), reads=[are], writes=[den])
        S.op("dve", lambda e: e.tensor_tensor(out=t1[:], in0=aim[:], in1=aim[:], op=ALU.mult), reads=[aim], writes=[t1])
        S.op("dve", lambda e: e.tensor_tensor(out=den[:], in0=den[:], in1=t1[:], op=ALU.add), reads=[den, t1], writes=[den])
        S.op("dve", lambda e: e.reciprocal(out=den[:], in_=den[:]), reads=[den], writes=[den])
        S.op("dve", lambda e: e.tensor_scalar(out=t1[:], in0=abr[:], scalar1=-1.0, scalar2=None, op0=ALU.add), reads=[abr], writes=[t1])
        S.op("dve", lambda e: e.tensor_tensor(out=cre[:], in0=t1[:], in1=are[:], op=ALU.mult), reads=[t1, are], writes=[cre])
        S.op("dve", lambda e: e.tensor_tensor(out=t2[:], in0=abi[:], in1=aim[:], op=ALU.mult), reads=[abi, aim], writes=[t2])
        S.op("dve", lambda e: e.tensor_tensor(out=cre[:], in0=cre[:], in1=t2[:], op=ALU.add), reads=[cre, t2], writes=[cre])
        S.op("dve", lambda e: e.tensor_tensor(out=cre[:], in0=cre[:], in1=den[:], op=ALU.mult), reads=[cre, den], writes=[cre])
        S.op("dve", lambda e: e.tensor_tensor(out=cim[:], in0=abi[:], in1=are[:], op=ALU.mult), reads=[abi, are], writes=[cim])
        S.op("dve", lambda e: e.tensor_tensor(out=t2[:], in0=t1[:], in1=aim[:], op=ALU.mult), reads=[t1, aim], writes=[t2])
        S.op("dve", lambda e: e.tensor_tensor(out=cim[:], in0=cim[:], in1=t2[:], op=ALU.subtract), reads=[cim, t2], writes=[cim])
        S.op("dve", lambda e: e.tensor_tensor(out=cim[:], in0=cim[:], in1=den[:], op=ALU.mult), reads=[cim, den], writes=[cim])
        S.op("dve", lambda e: e.tensor_scalar(out=ncre[:], in0=cre[:], scalar1=-1.0, scalar2=None, op0=ALU.mult), reads=[cre], writes=[ncre])
        S.op("dve", lambda e: e.tensor_scalar(out=t2[:], in0=thr[:], scalar1=512.0, scalar2=None, op0=ALU.mult), reads=[thr], writes=[t2])
        rr_sin(Rim[:], Rim, t2[:], t2, 8)
        rr_sin(Rre[:], Rre, t2[:], t2, 8, shift=math.pi / 2)
        S.op("pool", lambda e: e.iota(qi[:], pattern=[[1, 512]], base=0, channel_multiplier=0), writes=[qi])
        S.op("dve", lambda e: e.tensor_copy(out=jf[:], in_=qi[:]), reads=[qi], writes=[jf])
        cosT = S.sb("s5_cosT", [128, 8, 512], F32, st)
        sinT = S.sb("s5_sinT", [128, 8, 512], F32, st)
        TiR = S.sb("s5_TiR", [128, 8, 512], F32, st)
        TiI = S.sb("s5_TiI", [128, 8, 512], F32, st)
        magT = S.sb("s5_magT", [128, 8, 512], F32, st)
        a2 = S.sb("s5_a2", [128, 512], F32, st)
        for k in range(8):
            S.op("pool", lambda e, k=k: e.tensor_scalar(out=a2[:], in0=jf[:], scalar1=thr[:, k:k + 1], scalar2=None, op0=ALU.mult), reads=[jf, thr], writes=[a2])
            rr_sin(sinT[:, k, :], sinT, a2[:], a2, 512)
            rr_sin(cosT[:, k, :], cosT, a2[:], a2, 512, shift=math.pi / 2)
            S.op("pool", lambda e, k=k: e.tensor_scalar(out=magT[:, k, :], in0=jf[:], scalar1=0.0, scalar2=mag[:, k:k + 1], op0=ALU.mult, op1=ALU.add),
                 reads=[jf, mag], writes=[magT])
            S.op("dve", lambda e, k=k: e.tensor_scalar(out=TiR[:, k, :], in0=cosT[:, k, :], scalar1=cre[:, k:k + 1], scalar2=None, op0=ALU.mult), reads=[cosT, cre], writes=[TiR])
            S.op("dve", lambda e, k=k: e.scalar_tensor_tensor(out=TiR[:, k, :], in0=sinT[:, k, :], scalar=cim[:, k:k + 1], in1=TiR[:, k, :], op0=ALU.mult, op1=ALU.add),
                 reads=[sinT, cim, TiR], writes=[TiR])
            S.op("dve", lambda e, k=k: e.tensor_scalar(out=TiI[:, k, :], in0=cosT[:, k, :], scalar1=cim[:, k:k + 1], scalar2=None, op0=ALU.mult), reads=[cosT, cim], writes=[TiI])
            S.op("dve", lambda e, k=k: e.scalar_tensor_tensor(out=TiI[:, k, :], in0=sinT[:, k, :], scalar=ncre[:, k:k + 1], in1=TiI[:, k, :], op0=ALU.mult, op1=ALU.add),
                 reads=[sinT, ncre, TiI], writes=[TiI])
        BT = [S.sb("s5_BT%d" % i, [16, 16, 64], BF16, st) for i in range(2)]
        CT = [S.sb("s5_CT%d" % i, [128, 8, 128], BF16, st) for i in range(2)]
        with ExitStack() as st2:
            braw = S.sb("s5_braw", [64, 16, 16], F32, st2)
            craw = S.sb("s5_craw", [16, 16, 64], F32, st2)
            for ri, (bk, ck) in enumerate((("s5_b_re", "s5_c_re"), ("s5_b_im", "s5_c_im"))):
                S.dma("sp", braw[:], P[bk][l].rearrange("g p c -> p g c"), writes=[braw], sem_tile=braw)
                for half in range(2):
                    ps = C.psum()
                    for g8 in range(8):
                        g = half * 8 + g8
                        S.op("pe", lambda e, g=g, g8=g8, ps=ps: e.transpose(out=ps[0:16, g8 * 64:(g8 + 1) * 64], in_=braw[:, g, :], identity=C.ident[0:64, 0:64]),
                             reads=[braw, C.ident], writes=[ps])
                    S.op("act", lambda e, half=half, ps=ps, ri=ri: e.copy(out=BT[ri][:, half * 8:(half + 1) * 8, :], in_=ps[0:16, :].rearrange("p (g q) -> p g q", g=8)),
                         reads=[ps], writes=[BT[ri]])
                S.op("pool", lambda e, ri=ri: e.memset(CT[ri][:], 0.0), writes=[CT[ri]])
                S.dma("sp", craw[:], P[ck][l].rearrange("g c p -> c g p"), writes=[craw], sem_tile=craw)
                for k in range(8):
                    ps = C.psum()
                    S.op("pe", lambda e, k=k, ps=ps: e.transpose(out=ps[:, 0:16], in_=craw[:, 2 * k:2 * k + 2, :].rearrange("c a p -> c (a p)"), identity=C.ident[0:16, 0:16]),
                         reads=[craw, C.ident], writes=[ps])
                    c0 = (k % 4) * 32
                    sgn = 1.0 if ri == 0 else -1.0
                    S.op("act", lambda e, k=k, ps=ps, ri=ri, c0=c0, sgn=sgn: e.activation(out=CT[ri][0:64, k, c0:c0 + 16], in_=ps[0:64, 0:16], func=AF.Copy, scale=sgn),
                         reads=[ps], writes=[CT[ri]])
                    S.op("act", lambda e, k=k, ps=ps, ri=ri, c0=c0, sgn=sgn: e.activation(out=CT[ri][64:128, k, c0 + 16:c0 + 32], in_=ps[64:128, 0:16], func=AF.Copy, scale=sgn),
                         reads=[ps], writes=[CT[ri]])
            S.barrier()
            S.release(braw)
            S.release(craw)
        u16b = [S.sb("s5_u16b%d" % i, [16, 16, 512], BF16, st) for i in range(1)]
        ufm = [S.sb("s5_ufm%d" % i, [128, 2, 512], F32, st) for i in range(2)]
        pr = [S.sb("s5_pr%d" % i, [128, 4, 512], F32, st) for i in range(1)] * 2
        win = [S.sb("s5_win%d" % i, [128, 2, 512], F32, st) for i in range(2)]
        ww = [S.sb("s5_w%d" % i, [128, 2, 512], F32, st) for i in range(2)]
        xr = [S.sb("s5_xr%d" % i, [128, 4, 512], F32, st) for i in range(1)] * 2
        xx = [S.sb("s5_x%d" % i, [128, 2, 512], BF16, st) for i in range(8)] * 2
        wl = S.sb("s5_wl", [128, 2, 8], F32, st)
        cy = [S.sb("s5_cy%d" % i, [128, 2], F32, st) for i in range(2)]
        ct1 = [S.sb("s5_ct%d" % i, [128, 2], F32, st) for i in range(2)]
        yv = [S.sb("s5_yv%d" % i, [128, 512], F32, st) for i in range(2)]
        yg = [S.sb("s5_yg%d" % i, [128, 2, 512], BF16, st) for i in range(2)]
        sgt = [S.sb("s5_sg%d" % i, [128, 512], BF16, st) for i in range(2)]
        ost = [S.sb("s5_o%d" % i, [128, 2, 512], BF16, st) for i in range(2)]
        n = 0
        for i in range(NT):
            t0 = i * 512
            ub_, uf_ = u16b[0], ufm[i % 2]
            S.dma("pool", ub_[:], C.zT[OFF_D:OFF_D + 256, t0:t0 + 512].rearrange("(g c) t -> c g t", c=16), writes=[ub_], sem_tile=ub_)
            S.dma("sp", uf_[:], C.zT[OFF_D:OFF_D + 256, t0:t0 + 512].rearrange("(a p) t -> p a t", p=128), writes=[uf_], sem_tile=uf_)
            for k in range(8):
                pr_, win_, w_, xr_, cy_, ct_ = pr[n % 2], win[n % 2], ww[n % 2], xr[n % 2], cy[n % 2], ct1[n % 2]
                x_ = xx[(i % 2) * 8 + k]
                n += 1
                psr = C.psum()
                psi = C.psum()
                for gl in range(2):
                    g = 2 * k + gl
                    S.op("pe", lambda e, g=g, gl=gl, psr=psr, ub_=ub_: e.matmul(psr[64 * gl:64 * gl + 64, :], lhsT=BT[0][:, g, :], rhs=ub_[:, g, :], start=True, stop=True),
                         reads=[BT[0], ub_], writes=[psr])
                    S.op("pe", lambda e, g=g, gl=gl, psi=psi, ub_=ub_: e.matmul(psi[64 * gl:64 * gl + 64, :], lhsT=BT[1][:, g, :], rhs=ub_[:, g, :], start=True, stop=True),
                         reads=[BT[1], ub_], writes=[psi])
                S.op("dve", lambda e, k=k, psr=psr, pr_=pr_: e.tensor_tensor(out=pr_[:, 0, :], in0=psr[:, :], in1=TiR[:, k, :], op=ALU.mult), reads=[psr, TiR], writes=[pr_])
                S.op("dve", lambda e, k=k, psi=psi, pr_=pr_: e.tensor_tensor(out=pr_[:, 1, :], in0=psi[:, :], in1=TiI[:, k, :], op=ALU.mult), reads=[psi, TiI, pr_], writes=[pr_])
                S.op("dve", lambda e, k=k, psi=psi, pr_=pr_: e.tensor_tensor(out=pr_[:, 2, :], in0=psi[:, :], in1=TiR[:, k, :], op=ALU.mult), reads=[psi, TiR, pr_], writes=[pr_])
                S.op("dve", lambda e, k=k, psr=psr, pr_=pr_: e.tensor_tensor(out=pr_[:, 3, :], in0=psr[:, :], in1=TiI[:, k, :], op=ALU.mult), reads=[psr, TiI, pr_], writes=[pr_])
                S.op("pool", lambda e, pr_=pr_, win_=win_: e.tensor_tensor(out=win_[:, 0, :], in0=pr_[:, 0, :], in1=pr_[:, 1, :], op=ALU.subtract), reads=[pr_], writes=[win_])
                S.op("pool", lambda e, pr_=pr_, win_=win_: e.tensor_tensor(out=win_[:, 1, :], in0=pr_[:, 2, :], in1=pr_[:, 3, :], op=ALU.add), reads=[pr_, win_], writes=[win_])
                if i == 0:
                    S.op("dve", lambda e, cy_=cy_: e.memset(cy_[:], 0.0), writes=[cy_])
                else:
                    S.op("dve", lambda e, k=k, ct_=ct_: e.tensor_scalar(out=ct_[:, 0:1], in0=wl[:, 1, k:k + 1], scalar1=Rim[:, k:k + 1], scalar2=None, op0=ALU.mult),
                         reads=[wl, Rim], writes=[ct_])
                    S.op("dve", lambda e, k=k, ct_=ct_: e.tensor_scalar(out=ct_[:, 1:2], in0=wl[:, 0, k:k + 1], scalar1=Rim[:, k:k + 1], scalar2=None, op0=ALU.mult),
                         reads=[wl, Rim, ct_], writes=[ct_])
                    S.op("dve", lambda e, k=k, ct_=ct_, cy_=cy_: e.scalar_tensor_tensor(out=cy_[:, 0:1], in0=wl[:, 0, k:k + 1], scalar=Rre[:, k:k + 1], in1=ct_[:, 0:1],
                                                                                     op0=ALU.mult, op1=ALU.subtract), reads=[wl, Rre, ct_], writes=[cy_])
                    S.op("dve", lambda e, k=k, ct_=ct_, cy_=cy_: e.scalar_tensor_tensor(out=cy_[:, 1:2], in0=wl[:, 1, k:k + 1], scalar=Rre[:, k:k + 1], in1=ct_[:, 1:2],
                                                                                     op0=ALU.mult, op1=ALU.add), reads=[wl, Rre, ct_, cy_], writes=[cy_])
                for c2 in range(2):
                    S.op("dve", lambda e, k=k, c2=c2, w_=w_, win_=win_, cy_=cy_: e.tensor_tensor_scan(
                        out=w_[:, c2, :], data0=magT[:, k, :], data1=win_[:, c2, :], initial=cy_[:, c2:c2 + 1], op0=ALU.mult, op1=ALU.add),
                        reads=[magT, win_, cy_, w_], writes=[w_])
                S.op("dve", lambda e, k=k, w_=w_: e.tensor_copy(out=wl[:, :, k:k + 1], in_=w_[:, :, 511:512]), reads=[w_, wl], writes=[wl])
                S.op("pool", lambda e, k=k, w_=w_, xr_=xr_: e.tensor_tensor(out=xr_[:, 0, :], in0=w_[:, 0, :], in1=cosT[:, k, :], op=ALU.mult), reads=[w_, cosT], writes=[xr_])
                S.op("pool", lambda e, k=k, w_=w_, xr_=xr_: e.tensor_tensor(out=xr_[:, 1, :], in0=w_[:, 1, :], in1=sinT[:, k, :], op=ALU.mult), reads=[w_, sinT, xr_], writes=[xr_])
                S.op("dve", lambda e, k=k, w_=w_, xr_=xr_: e.tensor_tensor(out=xr_[:, 2, :], in0=w_[:, 0, :], in1=sinT[:, k, :], op=ALU.mult), reads=[w_, sinT, xr_], writes=[xr_])
                S.op("dve", lambda e, k=k, w_=w_, xr_=xr_: e.tensor_tensor(out=xr_[:, 3, :], in0=w_[:, 1, :], in1=cosT[:, k, :], op=ALU.mult), reads=[w_, cosT, xr_], writes=[xr_])
                S.op("pool", lambda e, xr_=xr_, x_=x_: e.tensor_tensor(out=x_[:, 0, :], in0=xr_[:, 0, :], in1=xr_[:, 1, :], op=ALU.subtract), reads=[xr_], writes=[x_])
                S.op("pool", lambda e, xr_=xr_, x_=x_: e.tensor_tensor(out=x_[:, 1, :], in0=xr_[:, 2, :], in1=xr_[:, 3, :], op=ALU.add), reads=[xr_, x_], writes=[x_])
            yg_ = yg[i % 2]
            o_ = ost[i % 2]
            for ct in range(2):
                yv_ = yv[ct]
                psy = C.psum()
                for kk in range(4):
                    k = ct * 4 + kk
                    x_ = xx[(i % 2) * 8 + k]
                    S.op("pe", lambda e, k=k, kk=kk, psy=psy, x_=x_: e.matmul(psy[:, :], lhsT=CT[0][:, k, :], rhs=x_[:, 0, :], start=(kk == 0), stop=False),
                         reads=[CT[0], x_], writes=[psy])
                    S.op("pe", lambda e, k=k, kk=kk, psy=psy, x_=x_: e.matmul(psy[:, :], lhsT=CT[1][:, k, :], rhs=x_[:, 1, :], start=False, stop=(kk == 3)),
                         reads=[CT[1], x_], writes=[psy])
                S.op("dve", lambda e, ct=ct, psy=psy, yv_=yv_, uf_=uf_: e.scalar_tensor_tensor(out=yv_[:], in0=uf_[:, ct, :], scalar=dcol[:, ct:ct + 1], in1=psy[:, :],
                                                                                      op0=ALU.mult, op1=ALU.add), reads=[uf_, dcol, psy], writes=[yv_])
                S.op("act", lambda e, ct=ct, yv_=yv_, yg_=yg_: e.activation(out=yg_[:, ct, :], in_=yv_[:], func=AF.Gelu), reads=[yv_], writes=[yg_])
            for co in range(2):
                sg_ = sgt[co]
                psz = C.psum()
                for ct in range(2):
                    S.op("pe", lambda e, ct=ct, co=co, psz=psz, yg_=yg_: e.matmul(psz[:, :], lhsT=gluw[:, ct, co * 128:(co + 1) * 128], rhs=yg_[:, ct, :],
                                                                          start=(ct == 0), stop=(ct == 1)), reads=[gluw, yg_], writes=[psz])
                S.op("act", lambda e, co=co, psz=psz, sg_=sg_: e.activation(out=sg_[:], in_=psz[:, :], func=AF.Sigmoid, bias=gbcol[:, co:co + 1], scale=1.0),
                     reads=[psz, gbcol], writes=[sg_])
                S.op("pool", lambda e, co=co, sg_=sg_, yg_=yg_, o_=o_: e.tensor_tensor(out=o_[:, co, :], in0=yg_[:, co, :], in1=sg_[:], op=ALU.mult),
                     reads=[sg_, yg_], writes=[o_])
            S.dma("sp", C.oT[768:1024, t0:t0 + 512].rearrange("(a p) t -> p a t", p=128), o_[:], reads=[o_], sem_tile=o_)
        S.barrier()
        for t_ in [gluw, ldt] + u16b + ufm + ost:
            S.release(t_)


_NC_CACHE = {}


def kernel(**inputs):
    x = np.ascontiguousarray(np.asarray(inputs["x"], dtype=np.float32))
    mem = np.ascontiguousarray(np.asarray(inputs["mem"], dtype=np.float32))
    B, T, _ = x.shape
    if T not in _NC_CACHE:
        _NC_CACHE[T] = build(T, nlayers=DEPTH, dbg=False)
    nc = _NC_CACHE[T]
    params = {name: np.ascontiguousarray(np.asarray(inputs[name], dtype=np.float32)) for name, _ in PARAMS}
    n_cores = 8
    in_maps = []
    for c in range(n_cores):
        b = c % B
        m = {"x": x[b], "mem": mem[b]}
        m.update(params)
        in_maps.append(m)
    res = run_bass_kernel_spmd(nc, in_maps, core_ids=list(range(n_cores)))
    out = np.stack([np.asarray(res.results[b]["out"], dtype=np.float32) for b in range(B)], axis=0)
    return out


def phase_moe_sparse(C, l, out_ap):
    S, T, NT, P = C.S, C.T, C.NT, C.P
    NS = T // 128
    BLK = 512
    NB = C.MOE_NB
    w1flat = P["ex_w1"].rearrange("l e k n -> (l e k) n")
    w2flat = P["ex_w2"].rearrange("l e k n -> (l e k) n")
    U32 = mybir.dt.uint32
    with ExitStack() as st:
        g_bc = load_bc(C, st, "ms_g", P["ln3_g"][l], D)
        b_bc = load_bc(C, st, "ms_b", P["ln3_b"][l], D)
        e_all = S.sb("ms_e", [128, NS, 4], F32, st)
        r_all = S.sb("ms_r", [128, NS, 4], F32, st)
        w_all = S.sb("ms_w", [128, NS, 4], F32, st)
        d_all = S.sb("ms_d", [128, NS, 4], F32, st)
        d_int = S.sb("ms_di", [128, NS, 4], I32, st)
        blk = S.sb("ms_blk", [128, NB], F32, st)
        blk2 = S.sb("ms_blk2", [128, NB], F32, st)
        skp = S.sb("ms_skp", [128, NB], F32, st)
        oh_all = S.sb("ms_oh", [128, NB], F32, st)
        widx_f = S.sb("ms_wf", [128, NB, 8], F32, st)
        widx_i = S.sb("ms_wi", [128, NB, 8], I32, st)
        base = [S.sb("ms_base%d" % i, [128, NE], F32, st) for i in range(2)]
        iota_e = S.sb("ms_iotae", [128, NE], F32, st)
        iota_p = S.sb("ms_iotap", [128, 1], F32, st)
        iw = S.sb("ms_iw", [128, 8], F32, st)
        itmp = S.sb("ms_itmp", [128, NE], I32, st)
        Ls = S.sb("ms_Ls", [128, 128], F32, st)
        ones32 = S.sb("ms_ones32", [128, NE], F32, st)
        padf = S.sb("ms_padf", [128, NE], F32, st)
        pend = S.sb("ms_pend", [128, NE], F32, st)
        pstart = S.sb("ms_pstart", [128, NE], F32, st)
        b1all = S.sb("ms_b1", [32, 2048], BF16, st)
        b2all = S.sb("ms_b2", [32, 1024], BF16, st)
        S.dma("pool", b1all[:], P["ex_b1"][l], writes=[b1all], sem_tile=b1all)
        S.dma("pool", b2all[:], P["ex_b2"][l], writes=[b2all], sem_tile=b2all)
        S.op("pool", lambda e: e.iota(itmp[:], pattern=[[1, NE]], base=0, channel_multiplier=0), writes=[itmp])
        S.op("dve", lambda e: e.tensor_copy(out=iota_e[:], in_=itmp[:]), reads=[itmp], writes=[iota_e])
        S.op("pool", lambda e: e.iota(itmp[:, 0:1], pattern=[[1, 1]], base=0, channel_multiplier=1), reads=[iota_e], writes=[itmp])
        S.op("dve", lambda e: e.tensor_copy(out=iota_p[:], in_=itmp[:, 0:1]), reads=[itmp], writes=[iota_p])
        S.op("pool", lambda e: e.iota(itmp[:, 0:8], pattern=[[128, 8]], base=0, channel_multiplier=1), reads=[iota_p], writes=[itmp])
        S.op("dve", lambda e: e.tensor_copy(out=iw[:], in_=itmp[:, 0:8]), reads=[itmp], writes=[iw])
        S.op("pool", lambda e: e.memset(ones32[:], 1.0), writes=[ones32])
        S.op("pool", lambda e: e.memset(base[0][:], 0.0), writes=[base[0]])
        S.op("pool", lambda e: e.affine_select(out=Ls[:], in_=C.ones_f[:], compare_op=ALU.is_gt, fill=0.0, base=0,
                                               pattern=[[1, 128]], channel_multiplier=-1), reads=[C.ones_f], writes=[Ls])
        with ExitStack() as st2:
            rw = S.sb("rt_w", [128, 8, NE], F32, st2)
            S.dma("sp", rw[:], P["router_w"][l].rearrange("(kc p) n -> p kc n", p=128), writes=[rw], sem_tile=rw)
            rb = load_bc(C, st2, "rt_b", P["router_b"][l], NE)
            ht = [S.sb("rt_h%d" % i, [128, D], F32, st2) for i in range(2)]
            hTf = [S.sb("rt_hT%d" % i, [128, 8, 128], F32, st2) for i in range(2)]
            lg = [S.sb("rt_lg%d" % i, [128, NE], F32, st2) for i in range(2)]
            t8 = [S.sb("rt_t8%d" % i, [128, 8], F32, st2) for i in range(2)]
            mk = [S.sb("rt_mk%d" % i, [128, NE], F32, st2) for i in range(2)]
            rg = [S.sb("rt_rg%d" % i, [128, NE], F32, st2) for i in range(2)]
            ew = [S.sb("rt_ew%d" % i, [128, 4], F32, st2) for i in range(2)]
            sm = [S.sb("rt_sm%d" % i, [128, 2], F32, st2) for i in range(2)]
            tq = [S.sb("rt_tq%d" % i, [128, NE], F32, st2) for i in range(4)]
            ntq = 0
            for n in range(NS):
                h, hf, lg_, t8_, mk_, rg_, ew_, sm_ = ht[n % 2], hTf[n % 2], lg[n % 2], t8[n % 2], mk[n % 2], rg[n % 2], ew[n % 2], sm[n % 2]
                bc_, bn_ = base[n % 2], base[(n + 1) % 2]
                S.dma("sp", h[:], C.h_tok[n * 128:(n + 1) * 128, :], writes=[h], sem_tile=h)
                transpose_to(C, h[:], h, hf, lambda g, hf=hf: hf[:, g * 4:(g + 1) * 4, :])
                ps = C.psum()
                for kc in range(8):
                    S.op("pe", lambda e, kc=kc, ps=ps, hf=hf: e.matmul(ps[:, 0:NE], lhsT=hf[:, kc, :], rhs=rw[:, kc, :],
                                                                        start=(kc == 0), stop=(kc == 7)), reads=[hf, rw], writes=[ps])
                S.op("dve", lambda e: e.tensor_tensor(out=lg_[:], in0=ps[:, 0:NE], in1=rb[:], op=ALU.add), reads=[ps, rb], writes=[lg_])
                S.op("dve", lambda e: e.max(out=t8_[:], in_=lg_[:]), reads=[lg_], writes=[t8_])
                S.op("dve", lambda e: e.tensor_scalar(out=mk_[:], in0=lg_[:], scalar1=t8_[:, 3:4], scalar2=None, op0=ALU.is_ge), reads=[lg_, t8_], writes=[mk_])
                S.op("dve", lambda e: e.tensor_scalar(out=sm_[:, 0:1], in0=t8_[:, 0:1], scalar1=-1.0, scalar2=None, op0=ALU.mult), reads=[t8_], writes=[sm_])
                S.op("act", lambda e: e.activation(out=ew_[:], in_=t8_[:, 0:4], func=AF.Exp, bias=sm_[:, 0:1], scale=1.0), reads=[t8_, sm_], writes=[ew_])
                S.op("dve", lambda e: e.reduce_sum(out=sm_[:, 1:2], in_=ew_[:], axis=AX.X), reads=[ew_, sm_], writes=[sm_])
                S.op("dve", lambda e: e.reciprocal(out=sm_[:, 1:2], in_=sm_[:, 1:2]), reads=[sm_], writes=[sm_])
                S.op("dve", lambda e: e.tensor_scalar(out=w_all[:, n, :], in0=ew_[:], scalar1=sm_[:, 1:2], scalar2=None, op0=ALU.mult), reads=[ew_, sm_], writes=[w_all])
                ps2 = C.psum()
                S.op("pe", lambda e: e.matmul(ps2[:, 0:NE], lhsT=Ls[:, :], rhs=mk_[:, :], start=True, stop=True), reads=[Ls, mk_], writes=[ps2])
                S.op("pe", lambda e: e.matmul(ps2[:, NE:2 * NE], lhsT=C.ones_f[:, :], rhs=mk_[:, :], start=True, stop=True), reads=[C.ones_f, mk_], writes=[ps2])
                S.op("dve", lambda e: e.tensor_tensor(out=rg_[:], in0=ps2[:, 0:NE], in1=bc_[:], op=ALU.add), reads=[ps2, bc_], writes=[rg_])
                S.op("dve", lambda e: e.tensor_tensor(out=bn_[:], in0=ps2[:, NE:2 * NE], in1=bc_[:], op=ALU.add), reads=[ps2, bc_], writes=[bn_])
                for j in range(4):
                    ta, tb_ = tq[ntq % 4], tq[(ntq + 1) % 4]
                    ntq += 2
                    S.op("dve", lambda e: e.scalar_tensor_tensor(out=ta[:], in0=lg_[:], scalar=t8_[:, j:j + 1], in1=iota_e[:], op0=ALU.is_equal, op1=ALU.mult),
                         reads=[lg_, t8_, iota_e], writes=[ta])
                    S.op("dve", lambda e: e.reduce_sum(out=e_all[:, n, j:j + 1], in_=ta[:], axis=AX.X), reads=[ta], writes=[e_all])
                    S.op("dve", lambda e: e.scalar_tensor_tensor(out=tb_[:], in0=lg_[:], scalar=t8_[:, j:j + 1], in1=rg_[:], op0=ALU.is_equal, op1=ALU.mult),
                         reads=[lg_, t8_, rg_], writes=[tb_])
                    S.op("dve", lambda e: e.reduce_sum(out=r_all[:, n, j:j + 1], in_=tb_[:], axis=AX.X), reads=[tb_], writes=[r_all])
            cnt = base[NS % 2]
            S.op("dve", lambda e: e.tensor_scalar(out=padf[:], in0=cnt[:], scalar1=float(BLK - 1), scalar2=None, op0=ALU.add), reads=[cnt], writes=[padf])
            S.op("dve", lambda e: e.tensor_copy(out=itmp[:], in_=padf[:]), reads=[padf], writes=[itmp])
            S.op("dve", lambda e: e.tensor_scalar(out=itmp[:], in0=itmp[:], scalar1=9, scalar2=9, op0=ALU.arith_shift_right, op1=ALU.logical_shift_left),
                 reads=[itmp], writes=[itmp])
            S.op("dve", lambda e: e.tensor_copy(out=padf[:], in_=itmp[:]), reads=[itmp], writes=[padf])
            S.op("dve", lambda e: e.tensor_tensor_scan(out=pend[:], data0=ones32[:], data1=padf[:], initial=0.0, op0=ALU.mult, op1=ALU.add),
                 reads=[ones32, padf], writes=[pend])
            S.op("dve", lambda e: e.tensor_tensor(out=pstart[:], in0=pend[:], in1=padf[:], op=ALU.subtract), reads=[pend, padf], writes=[pstart])
            for n in range(NS):
                for j in range(4):
                    ta = tq[ntq % 4]
                    ntq += 1
                    S.op("dve", lambda e: e.scalar_tensor_tensor(out=ta[:], in0=iota_e[:], scalar=e_all[:, n, j:j + 1], in1=pstart[:], op0=ALU.is_equal, op1=ALU.mult),
                         reads=[iota_e, e_all, pstart], writes=[ta])
                    S.op("dve", lambda e: e.reduce_sum(out=d_all[:, n, j:j + 1], in_=ta[:], axis=AX.X), reads=[ta], writes=[d_all])
            fl3 = lambda t: t[:, :, :].rearrange("p a b -> p (a b)")
            S.op("dve", lambda e: e.tensor_tensor(out=fl3(d_all), in0=fl3(d_all), in1=fl3(r_all), op=ALU.add), reads=[d_all, r_all], writes=[d_all])
            S.op("dve", lambda e: e.tensor_copy(out=fl3(d_int), in_=fl3(d_all)), reads=[d_all], writes=[d_int])
            for b in range(NB):
                ta = tq[ntq % 4]
                ntq += 1
                S.op("dve", lambda e: e.tensor_scalar(out=ta[:], in0=pend[:], scalar1=float(b * BLK), scalar2=None, op0=ALU.is_le), reads=[pend], writes=[ta])
                S.op("dve", lambda e: e.reduce_sum(out=blk[:, b:b + 1], in_=ta[:], axis=AX.X), reads=[ta], writes=[blk])
            S.op("dve", lambda e: e.tensor_scalar(out=blk[:], in0=blk[:], scalar1=float(NE - 1), scalar2=None, op0=ALU.min), reads=[blk], writes=[blk])
            S.op("dve", lambda e: e.tensor_scalar(out=oh_all[:], in0=blk[:], scalar1=iota_p[:, 0:1], scalar2=None, op0=ALU.is_equal), reads=[blk, iota_p], writes=[oh_all])
            S.op("dve", lambda e: e.tensor_scalar(out=blk2[:], in0=blk[:], scalar1=1024.0, scalar2=float(l * NE * 1024), op0=ALU.mult, op1=ALU.add),
                 reads=[blk], writes=[blk2])
            S.op("dve", lambda e: e.memset(skp[:], 0.0), writes=[skp])
            S.op("dve", lambda e: e.tensor_tensor(out=skp[:, 2:NB], in0=blk[:, 2:NB], in1=blk[:, 0:NB - 2], op=ALU.is_equal), reads=[blk, skp], writes=[skp])
            S.op("dve", lambda e: e.scalar_tensor_tensor(out=blk2[:], in0=skp[:], scalar=float(1 << 22), in1=blk2[:], op0=ALU.mult, op1=ALU.add),
                 reads=[skp, blk2], writes=[blk2])
            for b in range(NB):
                S.op("dve", lambda e: e.tensor_scalar(out=widx_f[:, b, :], in0=iw[:], scalar1=blk2[:, b:b + 1], scalar2=None, op0=ALU.add),
                     reads=[iw, blk2], writes=[widx_f])
            S.op("dve", lambda e: e.tensor_copy(out=fl3(widx_i), in_=fl3(widx_f)), reads=[widx_f], writes=[widx_i])
            for n in range(NS):
                h = ht[n % 2]
                S.dma("sp", h[:], C.h_tok[n * 128:(n + 1) * 128, :], writes=[h], sem_tile=h)
                for j in range(4):
                    S.dma_fn("pool", lambda e: e.indirect_dma_start(
                        out=C.xs_d, out_offset=bass.IndirectOffsetOnAxis(d_int[:, n, j:j + 1].bitcast(U32), 0), in_=h[:], in_offset=None),
                        reads=[h, d_int], sem_tile=h)
            S.barrier()
            for t_ in [rw, rb] + ht:
                S.release(t_)
        with ExitStack() as st2:
            w1t = [S.sb("mo_w1%d" % i, [128, 8, 2048], BF16, st2) for i in range(2)]
            w2t = [S.sb("mo_w2%d" % i, [128, 8, 1024], BF16, st2) for i in range(2)]
            xsb = S.sb("mo_xs", [128, 4, 1024], F32, st2)
            ysb = S.sb("mo_ys", [128, 4, 1024], F32, st2)
            xT = [S.sb("mo_xT%d" % i, [128, 8, 512], BF16, st2) for i in range(2)]
            actT = [S.sb("mo_act%d" % i, [128, 8, 512], BF16, st2) for i in range(2)]
            ohb = [S.sb("mo_ohb%d" % i, [32, 512], BF16, st2) for i in range(2)]
            ones_r = S.sb("mo_onesr", [32, 512], F32, st2)
            S.op("pool", lambda e: e.memset(ones_r[:], 1.0), writes=[ones_r])
            gq = [S.sb("mo_gq%d" % i, [128, 512], F32, st2) for i in range(2)]
            sg = [S.sb("mo_sg%d" % i, [128, 512], F32, st2) for i in range(2)]
            uq = [S.sb("mo_uq%d" % i, [128, 512], F32, st2) for i in range(2)]
            nq = 0
            for b in range(NB):
                w1_, w2_, x_, a, oh_ = w1t[b % 2], w2t[b % 2], xT[b % 2], actT[b % 2], ohb[b % 2]
                for kc in range(8):
                    S.dma_fn("pool", lambda e: e.indirect_dma_start(
                        out=w1_[:, kc, :], out_offset=None, in_=w1flat, in_offset=bass.IndirectOffsetOnAxis(widx_i[:, b, kc:kc + 1].bitcast(U32), 0),
                        bounds_check=RegConst(2 * NE * 1024 - 1), oob_is_err=False),
                        reads=[widx_i], writes=[w1_], sem_tile=w1_)
                for kc in range(8):
                    S.dma_fn("pool", lambda e: e.indirect_dma_start(
                        out=w2_[:, kc, :], out_offset=None, in_=w2flat, in_offset=bass.IndirectOffsetOnAxis(widx_i[:, b, kc:kc + 1].bitcast(U32), 0),
                        bounds_check=RegConst(2 * NE * 1024 - 1), oob_is_err=False),
                        reads=[widx_i], writes=[w2_], sem_tile=w2_)
                S.dma("sp", xsb[:], C.xs_d[b * BLK:(b + 1) * BLK, :].rearrange("(s p) c -> p s c", p=128), writes=[xsb], sem_tile=xsb)
                for s4 in range(4):
                    transpose_to(C, xsb[:, s4, :], xsb, x_, lambda g, s4=s4, x_=x_: x_[:, g * 4:(g + 1) * 4, s4 * 128:(s4 + 1) * 128])
                S.op("dve", lambda e: e.tensor_scalar(out=oh_[:], in0=ones_r[:], scalar1=oh_all[0:32, b:b + 1], scalar2=None, op0=ALU.mult),
                     reads=[ones_r, oh_all], writes=[oh_])
                for ft in range(8):
                    g_, s_, u_ = gq[nq % 2], sg[nq % 2], uq[nq % 2]
                    nq += 1
                    psg = C.psum()
                    S.op("pe", lambda e: e.matmul(psg[:, :], lhsT=b1all[0:32, ft * 256:(ft + 1) * 256:2], rhs=oh_[:, :], start=True, stop=False),
                         reads=[b1all, oh_], writes=[psg])
                    for kc in range(8):
                        S.op("pe", lambda e: e.matmul(psg[:, :], lhsT=w1_[:, kc, ft * 256:(ft + 1) * 256:2], rhs=x_[:, kc, :], start=False, stop=(kc == 7)),
                             reads=[w1_, x_], writes=[psg])
                    psu = C.psum()
                    S.op("pe", lambda e: e.matmul(psu[:, :], lhsT=b1all[0:32, ft * 256 + 1:(ft + 1) * 256:2], rhs=oh_[:, :], start=True, stop=False),
                         reads=[b1all, oh_], writes=[psu])
                    for kc in range(8):
                        S.op("pe", lambda e: e.matmul(psu[:, :], lhsT=w1_[:, kc, ft * 256 + 1:(ft + 1) * 256:2], rhs=x_[:, kc, :], start=False, stop=(kc == 7)),
                             reads=[w1_, x_], writes=[psu])
                    S.op("dve", lambda e: e.tensor_scalar(out=g_[:], in0=psg[:, :], scalar1=7.0, scalar2=None, op0=ALU.min), reads=[psg], writes=[g_])
                    S.op("act", lambda e: e.activation(out=s_[:], in_=g_[:], func=AF.Sigmoid, scale=1.702), reads=[g_], writes=[s_])
                    S.op("dve", lambda e: e.tensor_scalar(out=u_[:], in0=psu[:, :], scalar1=7.0, scalar2=-7.0, op0=ALU.min, op1=ALU.max), reads=[psu], writes=[u_])
                    S.op("dve", lambda e: e.tensor_tensor(out=s_[:], in0=g_[:], in1=s_[:], op=ALU.mult), reads=[g_, s_], writes=[s_])
                    S.op("dve", lambda e: e.scalar_tensor_tensor(out=a[:, ft, :], in0=u_[:], scalar=1.0, in1=s_[:], op0=ALU.add, op1=ALU.mult),
                         reads=[s_, u_], writes=[a])
                for s4 in range(4):
                    for half in range(2):
                        ps = C.psum()
                        S.op("pe", lambda e: e.matmul(ps[:, :], lhsT=oh_[:, 0:128], rhs=b2all[0:32, half * 512:(half + 1) * 512], start=True, stop=False),
                             reads=[oh_, b2all], writes=[ps])
                        for ft in range(8):
                            S.op("pe", lambda e: e.matmul(ps[:, :], lhsT=a[:, ft, s4 * 128:(s4 + 1) * 128], rhs=w2_[:, ft, half * 512:(half + 1) * 512],
                                                          start=False, stop=(ft == 7)), reads=[a, w2_], writes=[ps])
                        en = evac_eng(C)
                        S.op(en, copy_op(en, ysb[:, s4, half * 512:(half + 1) * 512], ps[:, :]), reads=[ps], writes=[ysb])
                S.dma("sp", C.ys_d[b * BLK:(b + 1) * BLK, :].rearrange("(s p) c -> p s c", p=128), ysb[:], reads=[ysb], sem_tile=ysb)
            S.barrier()
            for t_ in w1t + w2t + [xsb, ysb]:
                S.release(t_)
        with ExitStack() as st2:
            acc = [S.sb("mc_acc%d" % i, [128, D], F32, st2) for i in range(2)]
            gj = [S.sb("mc_g%d" % i, [128, D], F32, st2) for i in range(4)]
            hTs = [S.sb("mc_hT%d" % i, [128, 8, 512], BF16, st2) for i in range(2)]
            ng = 0
            for n in range(NS):
                a_ = acc[n % 2]
                S.dma("sp", a_[:], C.h_tok[n * 128:(n + 1) * 128, :], writes=[a_], sem_tile=a_)
                S.op("act", lambda e: e.activation(out=a_[:], in_=a_[:], func=AF.Copy, scale=DN_ALPHA), reads=[a_], writes=[a_])
                for j in range(4):
                    g_ = gj[ng % 4]
                    ng += 1
                    S.dma_fn("pool", lambda e: e.indirect_dma_start(
                        out=g_[:], out_offset=None, in_=C.ys_d, in_offset=bass.IndirectOffsetOnAxis(d_int[:, n, j:j + 1].bitcast(U32), 0)),
                        reads=[d_int], writes=[g_], sem_tile=g_)
                    S.op("dve", lambda e: e.scalar_tensor_tensor(out=a_[:], in0=g_[:], scalar=w_all[:, n, j:j + 1], in1=a_[:], op0=ALU.mult, op1=ALU.add),
                         reads=[g_, w_all, a_], writes=[a_])
                ln_tile(C, (a_[:], a_), (a_[:], a_), g_bc, b_bc)
                store_h(C, st2, a_, hTs[(n // 4) % 2], n // 4, n % 4, out_ap)
            S.barrier()
            for t_ in acc + gj + hTs:
                S.release(t_)
        for t_ in [g_bc, b_bc, b1all, b2all]:
            S.release(t_)
```

```python
import math
from contextlib import ExitStack
import numpy as np
import concourse.bass as bass
import concourse.mybir as mybir
from concourse.bass_utils import run_bass_kernel_spmd

F32 = mybir.dt.float32
BF16 = mybir.dt.bfloat16
I32 = mybir.dt.int32
AF = mybir.ActivationFunctionType
ALU = mybir.AluOpType
AX = mybir.AxisListType

ENGS = ("pe", "dve", "act", "pool", "sp")
STORE_Q = "act"


class Tl:
    __slots__ = ("name", "t", "w", "r", "dkey")

    def __init__(self, name, t=None):
        self.name = name
        self.t = t
        self.w = None
        self.r = {}
        self.dkey = None

    def __getitem__(self, idx):
        return self.t[idx]


class _Proxy:
    def __init__(self):
        self.call = None

    def __getattr__(self, name):
        def rec(*a, **k):
            assert self.call is None
            self.call = (name, a, k)
        return rec


class RegConst:
    cache = {}

    def __init__(self, v):
        self.v = v

    def get(self, e):
        key = (id(e), self.v)
        if key not in RegConst.cache:
            RegConst.cache[key] = e.to_reg(self.v)
        return RegConst.cache[key]


def _record(fn):
    p = _Proxy()
    fn(p)
    name, a, k = p.call

    def run(e):
        k2 = {kk: (vv.get(e) if isinstance(vv, RegConst) else vv) for kk, vv in k.items()}
        return getattr(e, name)(*a, **k2)
    return run


class Sched:
    def __init__(self, nc, es):
        self.nc = nc
        self.es = es
        self.q = {e: [] for e in ENGS}
        self.cnt = {e: 0 for e in ENGS}
        self.seen = {e: {} for e in ENGS}
        self.sem = {}
        for e in ENGS:
            self.sem[e] = es.enter_context(nc.semaphore("c_" + e))
        self.dtot = {}
        self.ndsem = 0
        self.free_dsems = []

    def sb(self, name, shape, dt, st=None):
        self.uid = getattr(self, "uid", 0) + 1
        name = "t%d_%s" % (self.uid, name)
        t = (st or self.es).enter_context(self.nc.sbuf_tensor(name, list(shape), dt))
        return Tl(name, t)

    def ps(self, name, shape, dt=F32, st=None):
        name = "pp_" + name
        t = (st or self.es).enter_context(self.nc.psum_tensor(name, list(shape), dt))
        return Tl(name, t)

    def res(self, name):
        return Tl(name, None)

    def _dsem(self, tl):
        if tl.dkey is None:
            if self.free_dsems:
                tl.dkey = self.free_dsems.pop()
            else:
                k = "d%d" % self.ndsem
                self.ndsem += 1
                self.sem[k] = self.es.enter_context(self.nc.semaphore(k))
                self.dtot[k] = 0
                tl.dkey = k
        return tl.dkey

    def release(self, tl):
        if tl.dkey is not None:
            self.free_dsems.append(tl.dkey)
            tl.dkey = None

    def _waits(self, eng, reads, writes):
        waits = {}
        seen = self.seen[eng]

        def need(ev):
            if ev is None:
                return
            k, v = ev
            if k in self.dtot:
                v = self.dtot[k]
            elif k == eng and eng in ("pe", "sp"):
                return
            if seen.get(k, 0) < v and waits.get(k, 0) < v:
                waits[k] = v

        for t in reads:
            need(t.w)
        for t in writes:
            need(t.w)
            for k, v in t.r.items():
                need((k, v))
        for k, v in waits.items():
            seen[k] = v
        return list(waits.items())

    def _mark(self, ev, reads, writes):
        k, v = ev
        for t in reads:
            if t.r.get(k, 0) < v:
                t.r[k] = v
        for t in writes:
            t.w = ev
            t.r = {}

    def op(self, eng, fn, reads=(), writes=()):
        fn = _record(fn)
        waits = self._waits(eng, reads, writes)
        self.cnt[eng] += 1
        ev = (eng, self.cnt[eng])
        self._mark(ev, reads, writes)
        self.q[eng].append((waits, fn, (eng, 1)))

    def dma(self, q, out, in_, reads=(), writes=(), sem_tile=None, **kw):
        if q == "sp" and not writes:
            q = STORE_Q
        waits = self._waits(q, reads, writes)
        k = self._dsem(sem_tile)
        self.dtot[k] += 16
        ev = (k, self.dtot[k])
        self._mark(ev, reads, writes)
        self.q[q].append((waits, lambda e, out=out, in_=in_, kw=kw: e.dma_start(out=out, in_=in_, **kw), (k, 16)))

    def dma_fn(self, q, fn, reads=(), writes=(), sem_tile=None):
        waits = self._waits(q, reads, writes)
        k = self._dsem(sem_tile)
        self.dtot[k] += 16
        ev = (k, self.dtot[k])
        self._mark(ev, reads, writes)
        self.q[q].append((waits, _record(fn), (k, 16)))

    def barrier(self):
        waits = []
        seen = self.seen["sp"]
        for e in ENGS:
            if e != "sp" and seen.get(e, 0) < self.cnt[e]:
                waits.append((e, self.cnt[e]))
                seen[e] = self.cnt[e]
        for k, v in self.dtot.items():
            if seen.get(k, 0) < v:
                waits.append((k, v))
                seen[k] = v
        self.cnt["sp"] += 1
        ev = ("sp", self.cnt["sp"])
        self.q["sp"].append((waits, lambda e: e.nop(), ("sp", 1)))
        for e in ENGS:
            if e != "sp":
                self.q[e].append(([ev], None, None))
                self.seen[e]["sp"] = ev[1]
                for k, v in self.dtot.items():
                    self.seen[e][k] = v
                for e2 in ENGS:
                    self.seen[e][e2] = max(self.seen[e].get(e2, 0), self.cnt[e2])

    def emit(self):
        nc = self.nc
        self.barrier()
        with nc.Block() as block:
            def run(engname):
                def body(eng):
                    for waits, fn, inc in self.q[engname]:
                        for k, v in waits:
                            eng.wait_ge(self.sem[k], v)
                        if fn is not None:
                            ins = fn(eng)
                            ins.then_inc(self.sem[inc[0]], inc[1])
                return body
            block.tensor(run("pe"))
            block.vector(run("dve"))
            block.scalar(run("act"))
            block.gpsimd(run("pool"))
            block.sync(run("sp"))


D = 1024
NMEM = 256
OFF_A, OFF_B, OFF_C, OFF_D, OFF_G = 0, 768, 1280, 2304, 2560
N_IN = 6656
NE = 32
LN_EPS = 1e-5
DEPTH = 2
DN_ALPHA = (2 * DEPTH) ** 0.25

PARAMS = [
    ("ln_in_g", (D,)), ("ln_in_b", (D,)), ("w_in", (2, D, N_IN)), ("conv_w", (2, 3, 256)),
    ("sg_norm_g", (2, 256)), ("sg_norm_b", (2, 256)), ("sg_w", (2, 4, 128, 128)), ("sg_b", (2, 4, 128)),
    ("rw_mu", (2, 1024)), ("rw_w0", (2, 256)), ("rw_w_up", (2, 64, 256)), ("rw_a0", (2, 256)),
    ("rw_a_up", (2, 64, 256)), ("rw_g_up", (2, 128, 256)), ("rw_k_k", (2, 256)), ("rw_k_a", (2, 256)),
    ("rw_r_k", (2, 4, 64)), ("rw_ln_g", (2, 256)), ("rw_ln_b", (2, 256)),
    ("s5_a_re", (2, 16, 64)), ("s5_a_im", (2, 16, 64)), ("s5_b_re", (2, 16, 64, 16)), ("s5_b_im", (2, 16, 64, 16)),
    ("s5_c_re", (2, 16, 16, 64)), ("s5_c_im", (2, 16, 16, 64)), ("s5_d", (2, 256)), ("s5_log_dt", (2, 16)),
    ("s5_glu_w", (2, 256, 256)), ("s5_glu_b", (2, 256)), ("br_proj", (2, 4, 256, D)), ("gate_b", (2, 4, D)),
    ("w_out", (2, D, D)), ("ln1_g", (2, D)), ("ln1_b", (2, D)), ("xa_wq", (2, D, D)), ("xa_wk", (2, D, D)),
    ("xa_wv", (2, D, D)), ("xa_wo", (2, D, D)), ("ln2_g", (2, D)), ("ln2_b", (2, D)),
    ("router_w", (2, D, NE)), ("router_b", (2, NE)), ("ex_w1", (2, NE, D, 2 * D)), ("ex_b1", (2, NE, 2 * D)),
    ("ex_w2", (2, NE, D, D)), ("ex_b2", (2, NE, D)), ("ln3_g", (2, D)), ("ln3_b", (2, D)),
]


def col(ap1d):
    return ap1d.rearrange("(p o) -> p o", o=1)


class Ctx:
    pass


def build(T, nlayers=2, dbg=False, phases=None):
    assert T % 512 == 0
    NT = T // 512
    nc = bass.Bass("TRN2", target_bir_lowering=False)
    RegConst.cache = {}
    P = {}
    x_in = nc.dram_tensor("x", [T, D], F32, kind="ExternalInput").ap()
    mem_in = nc.dram_tensor("mem", [NMEM, D], F32, kind="ExternalInput").ap()
    for name, shp in PARAMS:
        P[name] = nc.dram_tensor(name, list(shp), F32, kind="ExternalInput").ap()
    out = nc.dram_tensor("out", [T, D], F32, kind="ExternalOutput").ap()
    skind = "ExternalOutput" if dbg else "Internal"

    def scr(name, shape, dt):
        return nc.dram_tensor(name, list(shape), dt, kind=skind).ap()

    h_tok = scr("h_tok", [T, D], F32)
    hT = scr("hT", [D, T], BF16)
    zT = scr("zT", [OFF_G, T], F32)
    zbv = scr("zbv", [T, 256], F32)
    gT = scr("gT", [4 * D, T], BF16)
    oT = scr("oT", [4 * 256, T], BF16)
    gate_d = scr("gate_d", [T, NE], F32)
    MOE_BLK = 512
    MOE_NB = (T * 4) // MOE_BLK + NE
    xs_d = scr("xs_d", [MOE_NB * MOE_BLK, D], F32)
    ys_d = scr("ys_d", [MOE_NB * MOE_BLK, D], F32)

    with ExitStack() as es:
        S = Sched(nc, es)
        C = Ctx()
        C.nc, C.S, C.P, C.T, C.NT = nc, S, P, T, NT
        C.ident = S.sb("ident", [128, 128], F32)
        S.op("pool", lambda e: e.memset(C.ident[:], 0.0), writes=[C.ident])
        S.op("pool", lambda e: e.affine_select(out=C.ident[:], in_=C.ident[:], compare_op=ALU.not_equal, fill=1.0,
                                               base=0, pattern=[[-1, 128]], channel_multiplier=1),
             reads=[C.ident], writes=[C.ident])
        C.ones_b = S.sb("ones_b", [128, 128], BF16)
        S.op("pool", lambda e: e.memset(C.ones_b[:], 1.0), writes=[C.ones_b])
        C.ones_f = S.sb("ones_f", [128, 128], F32)
        S.op("pool", lambda e: e.memset(C.ones_f[:], 1.0), writes=[C.ones_f])
        C.rweps = S.sb("rweps", [128, 1], F32)
        S.op("pool", lambda e: e.memset(C.rweps[:], 64e-5), writes=[C.rweps])
        C.psl = [S.ps("ps%d" % i, [128, 512], F32) for i in range(8)]
        C.psi = 0

        def psum():
            t = C.psl[C.psi % 8]
            C.psi += 1
            return t
        C.psum = psum
        C.lnst = [S.sb("lnst%d" % i, [128, 2, 6], F32) for i in range(2)]
        C.lnmv = [S.sb("lnmv%d" % i, [128, 2], F32) for i in range(2)]
        C.lnrs = [S.sb("lnrs%d" % i, [128, 1], F32) for i in range(2)]
        C.lni = 0
        C.evi = 0
        C.h_tok, C.hT, C.zT, C.zbv, C.gT, C.oT, C.gate_d = h_tok, hT, zT, zbv, gT, oT, gate_d
        C.x_in, C.mem_in, C.out = x_in, mem_in, out
        C.xs_d, C.ys_d, C.MOE_NB = xs_d, ys_d, MOE_NB

        ph = phases
        for l in range(nlayers):
            last = (l == nlayers - 1)
            if l == 0:
                phase_ln_in(C)
                S.barrier()
            if ph is None or "inproj" in ph:
                phase_inproj(C, l)
                S.barrier()
            if ph is None or "conv" in ph:
                phase_conv(C, l)
                S.barrier()
            if ph is None or "sgu" in ph:
                phase_sgu(C, l)
                S.barrier()
            if ph is None or "rwkv" in ph:
                phase_rwkv(C, l)
                S.barrier()
            if ph is None or "s5" in ph:
                phase_s5(C, l)
                S.barrier()
            if ph is None or "merge" in ph:
                phase_merge(C, l)
                S.barrier()
            if ph is None or "attn" in ph:
                phase_attn(C, l)
                S.barrier()
            if ph is None or "moe" in ph:
                phase_moe_sparse(C, l, C.out if last else None)
                S.barrier()
        S.emit()
    return nc


def evac_eng(C):
    C.evi += 1
    return "act" if C.evi % 2 else "dve"


def copy_op(eng_name, out, in_):
    if eng_name == "act":
        return lambda e: e.copy(out=out, in_=in_)
    return lambda e: e.tensor_copy(out=out, in_=in_)


def load_bc(C, st, name, ap1d, n, q="sp"):
    S = C.S
    t = S.sb(name, [128, n], F32, st)
    S.dma(q, t[:], ap1d.partition_broadcast(128), writes=[t], sem_tile=t)
    return t


def ln_tile(C, src, dst, g_bc, b_bc, eps=LN_EPS):
    S = C.S
    i = C.lni % 2
    C.lni += 1
    st, mv, rs = C.lnst[i], C.lnmv[i], C.lnrs[i]
    sa, da = src[0], dst[0]
    srct, dstt = src[1], dst[1]
    S.op("dve", lambda e: e.bn_stats(out=st[:, 0, :], in_=sa[:, 0:512]), reads=[srct], writes=[st])
    S.op("dve", lambda e: e.bn_stats(out=st[:, 1, :], in_=sa[:, 512:1024]), reads=[srct, st], writes=[st])
    S.op("dve", lambda e: e.bn_aggr(out=mv[:], in_=st[:].rearrange("p a b -> p (a b)")), reads=[st], writes=[mv])
    S.op("act", lambda e: e.activation(out=rs[:], in_=mv[:, 1:2], func=AF.Sqrt, bias=C.eps_col[:, 0:1], scale=1.0),
         reads=[mv, C.eps_col], writes=[rs])
    S.op("dve", lambda e: e.reciprocal(out=rs[:], in_=rs[:]), reads=[rs], writes=[rs])
    S.op("dve", lambda e: e.tensor_scalar(out=da, in0=sa, scalar1=mv[:, 0:1], scalar2=rs[:, 0:1],
                                          op0=ALU.subtract, op1=ALU.mult), reads=[srct, mv, rs], writes=[dstt])
    S.op("pool", lambda e: e.tensor_tensor(out=da, in0=da, in1=g_bc[:], op=ALU.mult), reads=[dstt, g_bc], writes=[dstt])
    S.op("pool", lambda e: e.tensor_tensor(out=da, in0=da, in1=b_bc[:], op=ALU.add), reads=[dstt, b_bc], writes=[dstt])


def transpose_to(C, src_ap, src_t, dst_t, dst_fn, nblk=8):
    S = C.S
    for g in range(nblk // 4):
        ps = C.psum()
        for j in range(4):
            kc = g * 4 + j
            S.op("pe", lambda e, kc=kc, j=j, ps=ps: e.transpose(out=ps[:, j * 128:(j + 1) * 128],
                                                                 in_=src_ap[:, kc * 128:(kc + 1) * 128],
                                                                 identity=C.ident[:]),
                 reads=[src_t, C.ident], writes=[ps])
        en = evac_eng(C)
        S.op(en, copy_op(en, dst_fn(g), ps[:, :].rearrange("p (a b) -> p a b", a=4)), reads=[ps], writes=[dst_t])


def store_h(C, st, hts, hTs, tile_i, sub, out_ap=None):
    S = C.S
    tok0 = tile_i * 512 + sub * 128
    S.dma("sp", C.h_tok[tok0:tok0 + 128, :], hts[:], reads=[hts], sem_tile=hts)
    if out_ap is not None:
        S.dma("sp", out_ap[tok0:tok0 + 128, :], hts[:], reads=[hts], sem_tile=hts)
    transpose_to(C, hts[:], hts, hTs, lambda g: hTs[:, g * 4:(g + 1) * 4, sub * 128:(sub + 1) * 128])
    if sub == 3:
        S.dma("sp", C.hT.rearrange("(kc p) t -> p kc t", p=128)[:, :, tile_i * 512:(tile_i + 1) * 512], hTs[:],
              reads=[hTs], sem_tile=hTs)


def load_w_bf16(C, t, w_ap, q="pool"):
    C.S.dma(q, t[:], w_ap.rearrange("(kc p) n -> p kc n", p=128), writes=[t], sem_tile=t)


def phase_ln_in(C):
    S, T, NT = C.S, C.T, C.NT
    with ExitStack() as st:
        C.eps_col = S.sb("eps_col", [128, 1], F32)
        g_bc = load_bc(C, st, "lnin_g", C.P["ln_in_g"], D)
        b_bc = load_bc(C, st, "lnin_b", C.P["ln_in_b"], D)
        xt = [S.sb("lnin_x%d" % i, [128, D], F32, st) for i in range(2)]
        hTs = [S.sb("lnin_hT%d" % i, [128, 8, 512], BF16, st) for i in range(2)]
        S.op("pool", lambda e: e.memset(C.eps_col[:], LN_EPS), writes=[C.eps_col])
        n = 0
        for i in range(NT):
            for sub in range(4):
                x = xt[n % 2]
                n += 1
                tok0 = i * 512 + sub * 128
                S.dma("sp", x[:], C.x_in[tok0:tok0 + 128, :], writes=[x], sem_tile=x)
                ln_tile(C, (x[:], x), (x[:], x), g_bc, b_bc)
                store_h(C, st, x, hTs[i % 2], i, sub)
        S.barrier()
        for t in [g_bc, b_bc] + xt + hTs:
            S.release(t)


def load_cols(C, st, name, ap_rows, n, q="sp", m=128):
    S = C.S
    tmp = S.sb(name + "_r", [n, m], F32, st)
    res = S.sb(name, [m, n], F32, st)
    S.dma(q, tmp[:], ap_rows, writes=[tmp], sem_tile=tmp)
    ps = C.psum()
    S.op("pe", lambda e: e.transpose(out=ps[0:m, 0:n], in_=tmp[:, :], identity=C.ident[0:n, 0:n]),
         reads=[tmp, C.ident], writes=[ps])
    S.op("dve", lambda e: e.tensor_copy(out=res[:], in_=ps[0:m, 0:n]), reads=[ps], writes=[res])
    C.S.release_later = getattr(C.S, "release_later", [])
    return res


def phase_inproj(C, l):
    S, T, NT, P = C.S, C.T, C.NT, C.P
    w_in = P["w_in"][l]
    with ExitStack() as st:
        wbuf = [S.sb("ip_w%d" % i, [128, 8, 1024], BF16, st) for i in range(2)]
        wb2 = S.sb("ip_wb", [128, 8, 1024], BF16, st)
        hTh = [S.sb("ip_h%d" % i, [128, 8, 513], BF16, st) for i in range(2)]
        zst = [S.sb("ip_z%d" % i, [128, 4, 512], F32, st) for i in range(2)]
        gst = [S.sb("ip_g%d" % i, [128, 4, 512], BF16, st) for i in range(2)]
        gb = load_cols(C, st, "ip_gb", P["gate_b"][l].rearrange("i (j p) -> (i j) p", p=128), 32)
        groups = [("F", 0, 1024, 0), ("V", 1024, 256, 0), ("C", OFF_C, 1024, OFF_C), ("F", OFF_D, 256, OFF_D)]
        for i in range(4):
            groups.append(("G", OFF_G + i * 1024, 1024, i * 1024))
        nld = 0
        nst = 0
        for gi, (kind, c0, ncol, r0) in enumerate(groups):
            w = wbuf[gi % 2]
            if kind != "C":
                S.dma("pool", w[:, :, 0:ncol], w_in[:, c0:c0 + ncol].rearrange("(kc p) n -> p kc n", p=128),
                      writes=[w], sem_tile=w)
            else:
                with ExitStack() as st2:
                    wraw = S.sb("ip_wraw", [128, 8, 1024], F32, st2)
                    mu = load_bc(C, st2, "ip_mu", P["rw_mu"][l], 1024)
                    tmp = [S.sb("ip_tmp%d" % i, [128, 1024], F32, st2) for i in range(2)]
                    S.dma("sp", wraw[:], w_in[:, c0:c0 + ncol].rearrange("(kc p) n -> p kc n", p=128),
                          writes=[wraw], sem_tile=wraw)
                    for kc in range(8):
                        tk = tmp[kc % 2]
                        S.op("dve", lambda e, kc=kc, tk=tk: e.tensor_tensor(out=tk[:], in0=wraw[:, kc, :], in1=mu[:], op=ALU.mult),
                             reads=[wraw, mu], writes=[tk])
                        S.op("act", lambda e, kc=kc, tk=tk: e.copy(out=wb2[:, kc, :], in_=tk[:]), reads=[tk], writes=[wb2])
                        S.op("pool", lambda e, kc=kc, tk=tk: e.tensor_tensor(out=w[:, kc, :], in0=wraw[:, kc, :], in1=tk[:], op=ALU.subtract),
                             reads=[wraw, tk], writes=[w])
                    S.barrier()
                    for t_ in [wraw, mu] + tmp:
                        S.release(t_)
            for i in range(NT):
                hh = hTh[nld % 2]
                prev = hTh[(nld + 1) % 2]
                nld += 1
                t0 = i * 512
                S.dma("sp", hh[:, :, 1:513], C.hT.rearrange("(kc p) t -> p kc t", p=128)[:, :, t0:t0 + 512],
                      writes=[hh], sem_tile=hh)
                if kind == "C":
                    if i == 0:
                        S.op("pool", lambda e, hh=hh: e.memset(hh[:, :, 0:1], 0.0), writes=[hh])
                    else:
                        S.op("pool", lambda e, hh=hh, prev=prev: e.tensor_copy(out=hh[:, :, 0:1], in_=prev[:, :, 512:513]),
                             reads=[prev], writes=[hh])
                if kind == "V":
                    zs = zst[nst % 2]
                    nst += 1
                    for sub in range(4):
                        ps = C.psum()
                        for kc in range(8):
                            S.op("pe", lambda e, kc=kc, sub=sub, ps=ps, hh=hh: e.matmul(
                                ps[:, 0:256], lhsT=hh[:, kc, 1 + sub * 128:1 + (sub + 1) * 128], rhs=w[:, kc, 0:256],
                                start=(kc == 0), stop=(kc == 7)), reads=[hh, w], writes=[ps])
                        en = evac_eng(C)
                        S.op(en, copy_op(en, zs[:, sub, 0:256], ps[:, 0:256]), reads=[ps], writes=[zs])
                    S.dma("sp", C.zbv[t0:t0 + 512, :].rearrange("(s p) c -> p s c", p=128), zs[:, :, 0:256],
                          reads=[zs], sem_tile=zs)
                    continue
                nct = ncol // 128
                for cg in range(0, nct, 4):
                    ncg = min(4, nct - cg)
                    if kind == "G":
                        zs = gst[nst % 2]
                    else:
                        zs = zst[nst % 2]
                    nst += 1
                    for j in range(ncg):
                        ct = cg + j
                        ps = C.psum()
                        if kind == "C":
                            for kc in range(8):
                                S.op("pe", lambda e, kc=kc, ct=ct, ps=ps, hh=hh: e.matmul(
                                    ps[:, :], lhsT=w[:, kc, ct * 128:(ct + 1) * 128], rhs=hh[:, kc, 1:513],
                                    start=(kc == 0), stop=False), reads=[hh, w], writes=[ps])
                            for kc in range(8):
                                S.op("pe", lambda e, kc=kc, ct=ct, ps=ps, hh=hh: e.matmul(
                                    ps[:, :], lhsT=wb2[:, kc, ct * 128:(ct + 1) * 128], rhs=hh[:, kc, 0:512],
                                    start=False, stop=(kc == 7)), reads=[hh, wb2], writes=[ps])
                        else:
                            for kc in range(8):
                                S.op("pe", lambda e, kc=kc, ct=ct, ps=ps, hh=hh: e.matmul(
                                    ps[:, :], lhsT=w[:, kc, ct * 128:(ct + 1) * 128], rhs=hh[:, kc, 1:513],
                                    start=(kc == 0), stop=(kc == 7)), reads=[hh, w], writes=[ps])
                        if kind == "G":
                            gcol = (r0 // 128) + ct
                            S.op("act", lambda e, j=j, ps=ps, zs=zs, gcol=gcol: e.activation(
                                out=zs[:, j, :], in_=ps[:, :], func=AF.Sigmoid, bias=gb[:, gcol:gcol + 1], scale=1.0),
                                reads=[ps, gb], writes=[zs])
                        else:
                            en = evac_eng(C)
                            S.op(en, copy_op(en, zs[:, j, :], ps[:, :]), reads=[ps], writes=[zs])
                    dst = C.gT if kind == "G" else C.zT
                    rr = r0 + cg * 128
                    S.dma("sp", dst[rr:rr + ncg * 128, t0:t0 + 512].rearrange("(j p) t -> p j t", p=128), zs[:, 0:ncg, :],
                          reads=[zs], sem_tile=zs)
        S.barrier()
        for t_ in wbuf + [wb2] + hTh + zst + gst:
            S.release(t_)


def phase_conv(C, l):
    S, T, NT, P = C.S, C.T, C.NT, C.P
    with ExitStack() as st:
        cw = load_cols(C, st, "cv_w", P["conv_w"][l].rearrange("k (f p) -> (k f) p", p=128), 6)
        za = [S.sb("cv_za%d" % i, [128, 6, 512], F32, st) for i in range(2)]
        ch = [S.sb("cv_ch%d" % i, [128, 2, 514], F32, st) for i in range(2)]
        yt = [S.sb("cv_y%d" % i, [128, 512], F32, st) for i in range(2)]
        ost = [S.sb("cv_o%d" % i, [128, 2, 512], BF16, st) for i in range(2)]
        ny = 0
        for i in range(NT):
            t0 = i * 512
            z = za[i % 2]
            c = ch[i % 2]
            cp = ch[(i + 1) % 2]
            o = ost[i % 2]
            S.dma("sp", z[:], C.zT[0:768, t0:t0 + 512].rearrange("(j p) t -> p j t", p=128), writes=[z], sem_tile=z)
            if i == 0:
                S.op("pool", lambda e, c=c: e.memset(c[:, :, 0:2], 0.0), writes=[c])
            else:
                S.op("pool", lambda e, c=c, cp=cp: e.tensor_copy(out=c[:, :, 0:2], in_=cp[:, :, 512:514]), reads=[cp], writes=[c])
            S.op("pool", lambda e, c=c, z=z: e.tensor_tensor(out=c[:, :, 2:514], in0=z[:, 2:4, :], in1=z[:, 4:6, :], op=ALU.mult),
                 reads=[z, c], writes=[c])
            for f in range(2):
                y = yt[ny % 2]
                ny += 1
                S.op("dve", lambda e, f=f, y=y, c=c: e.tensor_scalar(out=y[:], in0=c[:, f, 2:514], scalar1=cw[:, 4 + f:5 + f],
                                                                      scalar2=None, op0=ALU.mult), reads=[c, cw], writes=[y])
                S.op("dve", lambda e, f=f, y=y, c=c: e.scalar_tensor_tensor(out=y[:], in0=c[:, f, 1:513], scalar=cw[:, 2 + f:3 + f],
                                                                             in1=y[:], op0=ALU.mult, op1=ALU.add), reads=[c, cw, y], writes=[y])
                S.op("dve", lambda e, f=f, y=y, c=c: e.scalar_tensor_tensor(out=y[:], in0=c[:, f, 0:512], scalar=cw[:, f:f + 1],
                                                                             in1=y[:], op0=ALU.mult, op1=ALU.add), reads=[c, cw, y], writes=[y])
                S.op("pool", lambda e, f=f, y=y, z=z, o=o: e.tensor_tensor(out=o[:, f, :], in0=y[:], in1=z[:, f, :], op=ALU.mult),
                     reads=[y, z], writes=[o])
            S.dma("sp", C.oT[0:256, t0:t0 + 512].rearrange("(f p) t -> p f t", p=128), o[:], reads=[o], sem_tile=o)
        S.barrier()
        for t_ in za + ch + yt + ost:
            S.release(t_)


def phase_sgu(C, l):
    S, T, NT, P = C.S, C.T, C.NT, C.P
    with ExitStack() as st:
        g_bc = load_bc(C, st, "sg_g", P["sg_norm_g"][l], 256)
        b_bc = load_bc(C, st, "sg_b", P["sg_norm_b"][l], 256)
        sb_bc = load_bc(C, st, "sg_sb", P["sg_b"][l].rearrange("g i -> (g i)"), 512)
        wraw = S.sb("sg_wraw", [128, 4, 128], F32, st)
        wsT = S.sb("sg_wsT", [128, 4, 128], F32, st)
        S.dma("sp", wraw[:], P["sg_w"][l].rearrange("g i j -> i g j"), writes=[wraw], sem_tile=wraw)
        ps = C.psum()
        for g in range(4):
            S.op("pe", lambda e, g=g: e.transpose(out=ps[:, g * 128:(g + 1) * 128], in_=wraw[:, g, :], identity=C.ident[:]),
                 reads=[wraw, C.ident], writes=[ps])
        S.op("dve", lambda e: e.tensor_copy(out=wsT[:], in_=ps[:, :].rearrange("p (g i) -> p g i", g=4)), reads=[ps], writes=[wsT])
        S.op("dve", lambda e: e.memset(wsT[64:128, :, 0:64], 0.0), reads=[wsT], writes=[wsT])
        vt = [S.sb("sg_v%d" % i, [128, 4, 256], F32, st) for i in range(2)]
        ut = [S.sb("sg_u%d" % i, [64, 4, 512], F32, st) for i in range(2)]
        ot = [S.sb("sg_o%d" % i, [64, 4, 512], BF16, st) for i in range(2)]
        svt = [S.sb("sg_sv%d" % i, [64, 512], F32, st) for i in range(2)]
        stt = [S.sb("sg_st%d" % i, [128, 6], F32, st) for i in range(2)]
        mvt = [S.sb("sg_mv%d" % i, [128, 2], F32, st) for i in range(2)]
        rst = [S.sb("sg_rs%d" % i, [128, 1], F32, st) for i in range(2)]
        n = 0
        for i in range(NT):
            t0 = i * 512
            v, u, o = vt[i % 2], ut[i % 2], ot[i % 2]
            S.dma("sp", v[:], C.zbv[t0:t0 + 512, :].rearrange("(s p) c -> p s c", p=128), writes=[v], sem_tile=v)
            S.dma("sp", u[:], C.zT[768:1024, t0:t0 + 512].rearrange("(g c) t -> c g t", c=64), writes=[u], sem_tile=u)
            for s in range(4):
                sx, mv, rs, sv = stt[n % 2], mvt[n % 2], rst[n % 2], svt[n % 2]
                n += 1
                S.op("dve", lambda e, s=s, sx=sx, v=v: e.bn_stats(out=sx[:], in_=v[:, s, :]), reads=[v], writes=[sx])
                S.op("dve", lambda e, sx=sx, mv=mv: e.bn_aggr(out=mv[:], in_=sx[:]), reads=[sx], writes=[mv])
                S.op("act", lambda e, mv=mv, rs=rs: e.activation(out=rs[:], in_=mv[:, 1:2], func=AF.Sqrt, bias=C.eps_col[:, 0:1], scale=1.0),
                     reads=[mv, C.eps_col], writes=[rs])
                S.op("dve", lambda e, rs=rs: e.reciprocal(out=rs[:], in_=rs[:]), reads=[rs], writes=[rs])
                S.op("dve", lambda e, s=s, v=v, mv=mv, rs=rs: e.tensor_scalar(out=v[:, s, :], in0=v[:, s, :], scalar1=mv[:, 0:1],
                                                                                scalar2=rs[:, 0:1], op0=ALU.subtract, op1=ALU.mult),
                     reads=[v, mv, rs], writes=[v])
                S.op("pool", lambda e, s=s, v=v: e.tensor_tensor(out=v[:, s, :], in0=v[:, s, :], in1=g_bc[:], op=ALU.mult),
                     reads=[v, g_bc], writes=[v])
                S.op("pool", lambda e, s=s, v=v: e.tensor_tensor(out=v[:, s, :], in0=v[:, s, :], in1=b_bc[:], op=ALU.add),
                     reads=[v, b_bc], writes=[v])
                ps = C.psum()
                for g in range(4):
                    S.op("pe", lambda e, g=g, s=s, v=v, ps=ps: e.matmul(ps[0:64, g * 128:(g + 1) * 128], lhsT=v[:, s, g * 64:(g + 1) * 64],
                                                                         rhs=wsT[:, g, :], start=True, stop=True),
                         reads=[v, wsT], writes=[ps])
                S.op("dve", lambda e, ps=ps, sv=sv: e.tensor_tensor(out=sv[:], in0=ps[0:64, :], in1=sb_bc[0:64, :], op=ALU.add),
                     reads=[ps, sb_bc], writes=[sv])
                S.op("pool", lambda e, s=s, sv=sv, u=u, o=o: e.tensor_tensor(out=o[:, :, s * 128:(s + 1) * 128],
                                                                              in0=sv[:, :].rearrange("c (g i) -> c g i", g=4),
                                                                              in1=u[:, :, s * 128:(s + 1) * 128], op=ALU.mult),
                     reads=[sv, u], writes=[o])
            S.dma("sp", C.oT[256:512, t0:t0 + 512].rearrange("(g c) t -> c g t", c=64), o[:], reads=[o], sem_tile=o)
        S.barrier()
        for t_ in [g_bc, b_bc, sb_bc, wraw] + vt + ut + ot:
            S.release(t_)


def phase_zero_branch(C, r0):
    S, T, NT = C.S, C.T, C.NT
    with ExitStack() as st:
        z = S.sb("zb_z", [128, 2, 512], BF16, st)
        S.op("pool", lambda e: e.memset(z[:], 0.0), writes=[z])
        for i in range(NT):
            S.dma("sp", C.oT[r0:r0 + 256, i * 512:(i + 1) * 512].rearrange("(f p) t -> p f t", p=128), z[:], reads=[z], sem_tile=z)
        S.barrier()
        S.release(z)


def proj_res_ln(C, st, inT, W, g_bc, b_bc, tile_i, hres, hTs, out_ap=None):
    S = C.S
    for sub in range(4):
        hr = hres[C.hri % 2]
        C.hri += 1
        tok0 = tile_i * 512 + sub * 128
        S.dma("sp", hr[:], C.h_tok[tok0:tok0 + 128, :], writes=[hr], sem_tile=hr)
        for half in range(2):
            ps = C.psum()
            for kc in range(8):
                S.op("pe", lambda e, kc=kc, ps=ps, half=half, sub=sub: e.matmul(
                    ps[:, :], lhsT=inT[:, kc, sub * 128:(sub + 1) * 128], rhs=W[:, kc, half * 512:(half + 1) * 512],
                    start=(kc == 0), stop=(kc == 7)), reads=[inT, W], writes=[ps])
            S.op("dve", lambda e, ps=ps, half=half, hr=hr: e.scalar_tensor_tensor(
                out=hr[:, half * 512:(half + 1) * 512], in0=hr[:, half * 512:(half + 1) * 512], scalar=DN_ALPHA, in1=ps[:, :],
                op0=ALU.mult, op1=ALU.add), reads=[hr, ps], writes=[hr])
        ln_tile(C, (hr[:], hr), (hr[:], hr), g_bc, b_bc)
        store_h(C, st, hr, hTs, tile_i, sub, out_ap)


def phase_merge(C, l):
    S, T, NT, P = C.S, C.T, C.NT, C.P
    with ExitStack() as st:
        brp = S.sb("mg_brp", [128, 8, 1024], BF16, st)
        S.dma("pool", brp[:], P["br_proj"][l].rearrange("i (kc p) n -> p (i kc) n", p=128), writes=[brp], sem_tile=brp)
        wout = S.sb("mg_wout", [128, 8, 1024], BF16, st)
        load_w_bf16(C, wout, P["w_out"][l])
        g_bc = load_bc(C, st, "mg_g", P["ln1_g"][l], D)
        b_bc = load_bc(C, st, "mg_b", P["ln1_b"][l], D)
        oTt = [S.sb("mg_o%d" % i, [128, 8, 512], BF16, st) for i in range(2)]
        gTt = [S.sb("mg_g%d" % i, [128, 4, 512], BF16, st) for i in range(2)]
        mT = [S.sb("mg_m%d" % i, [128, 8, 512], BF16, st) for i in range(2)]
        tm = [S.sb("mg_t%d" % i, [128, 4, 512], F32, st) for i in range(2)]
        hres = [S.sb("mg_hr%d" % i, [128, D], F32, st) for i in range(2)]
        hTs = [S.sb("mg_hT%d" % i, [128, 8, 512], BF16, st) for i in range(2)]
        C.hri = 0
        ng = 0
        for i in range(NT):
            t0 = i * 512
            o = oTt[i % 2]
            m = mT[i % 2]
            S.dma("sp", o[:], C.oT[:, t0:t0 + 512].rearrange("(j p) t -> p j t", p=128), writes=[o], sem_tile=o)
            for ct in range(8):
                g = gTt[ng % 2]
                t4 = tm[ng % 2]
                ng += 1
                S.dma("sp", g[:], C.gT.rearrange("(i ct p) t -> p i ct t", p=128, ct=8)[:, :, ct, t0:t0 + 512], writes=[g], sem_tile=g)
                for b in range(4):
                    ps = C.psum()
                    for kc in range(2):
                        S.op("pe", lambda e, b=b, kc=kc, ct=ct, ps=ps, o=o: e.matmul(
                            ps[:, :], lhsT=brp[:, b * 2 + kc, ct * 128:(ct + 1) * 128], rhs=o[:, b * 2 + kc, :],
                            start=(kc == 0), stop=(kc == 1)), reads=[brp, o], writes=[ps])
                    S.op("dve", lambda e, b=b, ps=ps, g=g, t4=t4: e.tensor_tensor(out=t4[:, b, :], in0=ps[:, :], in1=g[:, b, :], op=ALU.mult),
                         reads=[ps, g], writes=[t4])
                S.op("pool", lambda e, t4=t4: e.tensor_tensor(out=t4[:, 0:2, :], in0=t4[:, 0:2, :], in1=t4[:, 2:4, :], op=ALU.add),
                     reads=[t4], writes=[t4])
                S.op("pool", lambda e, t4=t4, m=m, ct=ct: e.tensor_tensor(out=m[:, ct, :], in0=t4[:, 0, :], in1=t4[:, 1, :], op=ALU.add),
                     reads=[t4], writes=[m])
            proj_res_ln(C, st, m, wout, g_bc, b_bc, i, hres, hTs[i % 2])
        S.barrier()
        for t_ in [brp, wout, g_bc, b_bc] + oTt + gTt + mT + hres + hTs:
            S.release(t_)


def phase_attn(C, l):
    S, T, NT, P = C.S, C.T, C.NT, C.P
    with ExitStack() as st:
        wq = S.sb("at_wq", [128, 8, 1024], BF16, st)
        wo = S.sb("at_wo", [128, 8, 1024], BF16, st)
        load_w_bf16(C, wq, P["xa_wq"][l])
        load_w_bf16(C, wo, P["xa_wo"][l])
        g_bc = load_bc(C, st, "at_g", P["ln2_g"][l], D)
        b_bc = load_bc(C, st, "at_b", P["ln2_b"][l], D)
        kT = S.sb("at_kT", [128, 8, 256], BF16, st)
        vv = S.sb("at_v", [128, 2, 1024], BF16, st)
        with ExitStack() as st2:
            wk = S.sb("at_wk", [128, 8, 1024], BF16, st2)
            wv = S.sb("at_wv", [128, 8, 1024], BF16, st2)
            load_w_bf16(C, wk, P["xa_wk"][l])
            load_w_bf16(C, wv, P["xa_wv"][l])
            mt = S.sb("at_mem", [128, 2, 1024], F32, st2)
            memT = S.sb("at_memT", [128, 8, 256], BF16, st2)
            S.dma("sp", mt[:], C.mem_in.rearrange("(s p) c -> p s c", p=128), writes=[mt], sem_tile=mt)
            for s in range(2):
                transpose_to(C, mt[:, s, :], mt, memT, lambda g, s=s: memT[:, g * 4:(g + 1) * 4, s * 128:(s + 1) * 128])
            for ct in range(8):
                ps = C.psum()
                for kc in range(8):
                    S.op("pe", lambda e, kc=kc, ct=ct, ps=ps: e.matmul(ps[:, 0:256], lhsT=wk[:, kc, ct * 128:(ct + 1) * 128],
                                                                        rhs=memT[:, kc, :], start=(kc == 0), stop=(kc == 7)),
                         reads=[wk, memT], writes=[ps])
                en = evac_eng(C)
                S.op(en, copy_op(en, kT[:, ct, :], ps[:, 0:256]), reads=[ps], writes=[kT])
            for s in range(2):
                for half in range(2):
                    ps = C.psum()
                    for kc in range(8):
                        S.op("pe", lambda e, kc=kc, s=s, half=half, ps=ps: e.matmul(
                            ps[:, :], lhsT=memT[:, kc, s * 128:(s + 1) * 128], rhs=wv[:, kc, half * 512:(half + 1) * 512],
                            start=(kc == 0), stop=(kc == 7)), reads=[wv, memT], writes=[ps])
                    en = evac_eng(C)
                    S.op(en, copy_op(en, vv[:, s, half * 512:(half + 1) * 512], ps[:, :]), reads=[ps], writes=[vv])
            S.barrier()
            for t_ in [wk, wv, mt]:
                S.release(t_)
        hTt = [S.sb("at_h%d" % i, [128, 8, 512], BF16, st) for i in range(2)]
        qT = [S.sb("at_q%d" % i, [128, 8, 512], BF16, st) for i in range(2)]
        aT = [S.sb("at_a%d" % i, [128, 8, 512], BF16, st) for i in range(2)]
        pt = [S.sb("at_p%d" % i, [128, 256], F32, st) for i in range(4)]
        pT = [S.sb("at_pT%d" % i, [128, 2, 128], BF16, st) for i in range(4)]
        mx = [S.sb("at_mx%d" % i, [128, 1], F32, st) for i in range(4)]
        sm = [S.sb("at_sm%d" % i, [128, 1], F32, st) for i in range(4)]
        hres = [S.sb("at_hr%d" % i, [128, D], F32, st) for i in range(2)]
        hTs = [S.sb("at_hT%d" % i, [128, 8, 512], BF16, st) for i in range(2)]
        C.hri = 0
        n = 0
        for i in range(NT):
            t0 = i * 512
            hh, q, a = hTt[i % 2], qT[i % 2], aT[i % 2]
            S.dma("sp", hh[:], C.hT.rearrange("(kc p) t -> p kc t", p=128)[:, :, t0:t0 + 512], writes=[hh], sem_tile=hh)
            for ct in range(8):
                ps = C.psum()
                for kc in range(8):
                    S.op("pe", lambda e, kc=kc, ct=ct, ps=ps, hh=hh: e.matmul(ps[:, :], lhsT=wq[:, kc, ct * 128:(ct + 1) * 128],
                                                                               rhs=hh[:, kc, :], start=(kc == 0), stop=(kc == 7)),
                         reads=[wq, hh], writes=[ps])
                S.op("act", lambda e, ct=ct, ps=ps, q=q: e.activation(out=q[:, ct, :], in_=ps[:, :], func=AF.Copy, scale=1.0 / 16.0),
                     reads=[ps], writes=[q])
            for sub in range(4):
                pss = []
                for hd in range(4):
                    ps = C.psum()
                    pss.append(ps)
                    for j in range(2):
                        S.op("pe", lambda e: e.matmul(ps[:, 0:256], lhsT=q[:, 2 * hd + j, sub * 128:(sub + 1) * 128], rhs=kT[:, 2 * hd + j, :],
                                                      start=(j == 0), stop=(j == 1)), reads=[q, kT], writes=[ps])
                for hd in range(4):
                    ps, p_, mx_, sm_ = pss[hd], pt[hd], mx[hd], sm[hd]
                    S.op("dve", lambda e: e.reduce_max(out=mx_[:], in_=ps[:, 0:256], axis=AX.X, negate=True), reads=[ps], writes=[mx_])
                    S.op("act", lambda e: e.activation(out=p_[:], in_=ps[:, 0:256], func=AF.Exp, bias=mx_[:, 0:1], scale=1.0, accum_out=sm_[:]),
                         reads=[ps, mx_], writes=[p_, sm_])
                    S.op("dve", lambda e: e.reciprocal(out=sm_[:], in_=sm_[:]), reads=[sm_], writes=[sm_])
                    S.op("dve", lambda e: e.tensor_scalar(out=p_[:], in0=p_[:], scalar1=sm_[:, 0:1], scalar2=None, op0=ALU.mult), reads=[p_, sm_], writes=[p_])
                for hd in range(4):
                    p_, pT_ = pt[hd], pT[hd]
                    ps2 = C.psum()
                    for j in range(2):
                        S.op("pe", lambda e: e.transpose(out=ps2[:, j * 128:(j + 1) * 128], in_=p_[:, j * 128:(j + 1) * 128], identity=C.ident[:]),
                             reads=[p_, C.ident], writes=[ps2])
                    S.op("act", lambda e: e.copy(out=pT_[:], in_=ps2[:, 0:256].rearrange("p (a b) -> p a b", a=2)), reads=[ps2], writes=[pT_])
                for hd in range(4):
                    pT_ = pT[hd]
                    ps3 = C.psum()
                    for j in range(2):
                        ct = 2 * hd + j
                        for mt_ in range(2):
                            S.op("pe", lambda e: e.matmul(ps3[:, j * 128:(j + 1) * 128], lhsT=vv[:, mt_, ct * 128:(ct + 1) * 128], rhs=pT_[:, mt_, :],
                                                          start=(mt_ == 0), stop=(mt_ == 1)), reads=[vv, pT_], writes=[ps3])
                    S.op("dve", lambda e: e.tensor_copy(out=a[:, 2 * hd:2 * hd + 2, sub * 128:(sub + 1) * 128],
                                                        in_=ps3[:, 0:256].rearrange("p (a b) -> p a b", a=2)), reads=[ps3], writes=[a])
            proj_res_ln(C, st, a, wo, g_bc, b_bc, i, hres, hTs[i % 2])
        S.barrier()
        for t_ in [wq, wo, g_bc, b_bc] + hTt + hres + hTs:
            S.release(t_)


def phase_router(C, l):
    S, T, NT, P = C.S, C.T, C.NT, C.P
    with ExitStack() as st:
        rw = S.sb("rt_w", [128, 8, NE], F32, st)
        S.dma("sp", rw[:], P["router_w"][l].rearrange("(kc p) n -> p kc n", p=128), writes=[rw], sem_tile=rw)
        rb = load_bc(C, st, "rt_b", P["router_b"][l], NE)
        ht = [S.sb("rt_h%d" % i, [128, D], F32, st) for i in range(2)]
        hTf = [S.sb("rt_hT%d" % i, [128, 8, 128], F32, st) for i in range(2)]
        lg = [S.sb("rt_lg%d" % i, [128, NE], F32, st) for i in range(2)]
        t8 = [S.sb("rt_t8%d" % i, [128, 8], F32, st) for i in range(2)]
        ex = [S.sb("rt_ex%d" % i, [128, NE], F32, st) for i in range(2)]
        mk = [S.sb("rt_mk%d" % i, [128, NE], F32, st) for i in range(2)]
        sm = [S.sb("rt_sm%d" % i, [128, 1], F32, st) for i in range(2)]
        for n in range(T // 128):
            h, hf, lg_, t8_, ex_, mk_, sm_ = ht[n % 2], hTf[n % 2], lg[n % 2], t8[n % 2], ex[n % 2], mk[n % 2], sm[n % 2]
            S.dma("sp", h[:], C.h_tok[n * 128:(n + 1) * 128, :], writes=[h], sem_tile=h)
            transpose_to(C, h[:], h, hf, lambda g, hf=hf: hf[:, g * 4:(g + 1) * 4, :])
            ps = C.psum()
            for kc in range(8):
                S.op("pe", lambda e, kc=kc, ps=ps, hf=hf: e.matmul(ps[:, 0:NE], lhsT=hf[:, kc, :], rhs=rw[:, kc, :],
                                                                    start=(kc == 0), stop=(kc == 7)), reads=[hf, rw], writes=[ps])
            S.op("dve", lambda e, ps=ps, lg_=lg_: e.tensor_tensor(out=lg_[:], in0=ps[:, 0:NE], in1=rb[:], op=ALU.add),
                 reads=[ps, rb], writes=[lg_])
            S.op("dve", lambda e, lg_=lg_, t8_=t8_: e.max(out=t8_[:], in_=lg_[:]), reads=[lg_], writes=[t8_])
            S.op("dve", lambda e, lg_=lg_, t8_=t8_, mk_=mk_: e.tensor_scalar(out=mk_[:], in0=lg_[:], scalar1=t8_[:, 3:4], scalar2=None, op0=ALU.is_ge),
                 reads=[lg_, t8_], writes=[mk_])
            S.op("dve", lambda e, lg_=lg_, t8_=t8_, ex_=ex_: e.tensor_scalar(out=ex_[:], in0=lg_[:], scalar1=t8_[:, 0:1], scalar2=None, op0=ALU.subtract),
                 reads=[lg_, t8_], writes=[ex_])
            S.op("act", lambda e, ex_=ex_: e.activation(out=ex_[:], in_=ex_[:], func=AF.Exp), reads=[ex_], writes=[ex_])
            S.op("dve", lambda e, ex_=ex_, mk_=mk_: e.tensor_tensor(out=ex_[:], in0=ex_[:], in1=mk_[:], op=ALU.mult), reads=[ex_, mk_], writes=[ex_])
            S.op("dve", lambda e, ex_=ex_, sm_=sm_: e.reduce_sum(out=sm_[:], in_=ex_[:], axis=AX.X), reads=[ex_], writes=[sm_])
            S.op("dve", lambda e, sm_=sm_: e.reciprocal(out=sm_[:], in_=sm_[:]), reads=[sm_], writes=[sm_])
            S.op("dve", lambda e, ex_=ex_, sm_=sm_: e.tensor_scalar(out=ex_[:], in0=ex_[:], scalar1=sm_[:, 0:1], scalar2=None, op0=ALU.mult),
                 reads=[ex_, sm_], writes=[ex_])
            S.dma("sp", C.gate_d[n * 128:(n + 1) * 128, :], ex_[:], reads=[ex_], sem_tile=ex_)
        S.barrier()
        for t_ in [rw, rb] + ht + ex:
            S.release(t_)


def phase_moe(C, l, out_ap):
    S, T, NT, P = C.S, C.T, C.NT, C.P
    ST = 1024 if T >= 1024 else 512
    nsub = ST // 128
    ntt = ST // 512
    with ExitStack() as st:
        g_bc = load_bc(C, st, "mo_g", P["ln3_g"][l], D)
        b_bc = load_bc(C, st, "mo_b", P["ln3_b"][l], D)
        w1t = [S.sb("mo_w1%d" % i, [128, 8, 2048], BF16, st) for i in range(2)]
        w2 = [S.sb("mo_w2%d" % i, [128, 8, 1024], BF16, st) for i in range(2)]
        b1r = [S.sb("mo_b1r%d" % i, [1, 2048], BF16, st) for i in range(2)]
        ones_row = S.sb("mo_ones", [1, 512], BF16, st)
        S.op("pool", lambda e: e.memset(ones_row[:], 1.0), writes=[ones_row])
        b2 = [S.sb("mo_b2%d" % i, [1, 1024], BF16, st) for i in range(2)]
        acc = S.sb("mo_acc", [128, nsub, 1024], F32, st)
        hTt = S.sb("mo_hT", [128, 8, ST], BF16, st)
        gt = S.sb("mo_gate", [128, nsub, NE], F32, st)
        actT = [S.sb("mo_act%d" % i, [128, 8, 512], BF16, st) for i in range(2)]
        gq = [S.sb("mo_gq%d" % i, [128, 512], F32, st) for i in range(2)]
        sg = [S.sb("mo_sg%d" % i, [128, 512], F32, st) for i in range(2)]
        uq = [S.sb("mo_uq%d" % i, [128, 512], F32, st) for i in range(2)]
        hTs = [S.sb("mo_hTs%d" % i, [128, 8, 512], BF16, st) for i in range(1)] * 2
        w1 = P["ex_w1"][l]
        nw = 0
        na = 0
        nq = 0
        for sti in range(T // ST):
            tok0 = sti * ST
            S.dma("sp", acc[:], C.h_tok[tok0:tok0 + ST, :].rearrange("(s p) c -> p s c", p=128), writes=[acc], sem_tile=acc)
            S.dma("sp", hTt[:], C.hT.rearrange("(kc p) t -> p kc t", p=128)[:, :, tok0:tok0 + ST], writes=[hTt], sem_tile=hTt)
            S.dma("sp", gt[:], C.gate_d[tok0:tok0 + ST, :].rearrange("(s p) c -> p s c", p=128), writes=[gt], sem_tile=gt)
            S.op("pool", lambda e: e.tensor_scalar(out=acc[:], in0=acc[:], scalar1=DN_ALPHA, scalar2=None, op0=ALU.mult),
                 reads=[acc], writes=[acc])
            for ex in range(NE):
                w1_, w2_, b1r_, b2_ = w1t[nw % 2], w2[nw % 2], b1r[nw % 2], b2[nw % 2]
                nw += 1
                S.dma("pool", w1_[:], w1[ex].rearrange("(kc p) n -> p kc n", p=128), writes=[w1_], sem_tile=w1_)
                S.dma("pool", w2_[:], P["ex_w2"][l, ex].rearrange("(kc p) n -> p kc n", p=128), writes=[w2_], sem_tile=w2_)
                S.dma("pool", b2_[:], P["ex_b2"][l, ex:ex + 1, :], writes=[b2_], sem_tile=b2_)
                S.dma("pool", b1r_[:], P["ex_b1"][l, ex:ex + 1, :], writes=[b1r_], sem_tile=b1r_)
                for tt in range(ntt):
                    a = actT[na % 2]
                    na += 1
                    for ft in range(8):
                        g_, s_, u_ = gq[nq % 2], sg[nq % 2], uq[nq % 2]
                        nq += 1
                        psg = C.psum()
                        S.op("pe", lambda e, ft=ft, psg=psg, b1r_=b1r_: e.matmul(
                            psg[:, :], lhsT=b1r_[0:1, ft * 256:(ft + 1) * 256:2], rhs=ones_row[0:1, :], start=True, stop=False),
                            reads=[b1r_, ones_row], writes=[psg])
                        for kc in range(8):
                            S.op("pe", lambda e, kc=kc, ft=ft, psg=psg, w1_=w1_, tt=tt: e.matmul(
                                psg[:, :], lhsT=w1_[:, kc, ft * 256:(ft + 1) * 256:2], rhs=hTt[:, kc, tt * 512:(tt + 1) * 512],
                                start=False, stop=(kc == 7)), reads=[w1_, hTt], writes=[psg])
                        psu = C.psum()
                        S.op("pe", lambda e, ft=ft, psu=psu, b1r_=b1r_: e.matmul(
                            psu[:, :], lhsT=b1r_[0:1, ft * 256 + 1:(ft + 1) * 256:2], rhs=ones_row[0:1, :], start=True, stop=False),
                            reads=[b1r_, ones_row], writes=[psu])
                        for kc in range(8):
                            S.op("pe", lambda e, kc=kc, ft=ft, psu=psu, w1_=w1_, tt=tt: e.matmul(
                                psu[:, :], lhsT=w1_[:, kc, ft * 256 + 1:(ft + 1) * 256:2], rhs=hTt[:, kc, tt * 512:(tt + 1) * 512],
                                start=False, stop=(kc == 7)), reads=[w1_, hTt], writes=[psu])
                        S.op("dve", lambda e, psg=psg, g_=g_: e.tensor_scalar(
                            out=g_[:], in0=psg[:, :], scalar1=7.0, scalar2=None, op0=ALU.min), reads=[psg], writes=[g_])
                        S.op("act", lambda e, g_=g_, s_=s_: e.activation(out=s_[:], in_=g_[:], func=AF.Sigmoid, scale=1.702),
                             reads=[g_], writes=[s_])
                        S.op("dve", lambda e, psu=psu, u_=u_: e.tensor_scalar(
                            out=u_[:], in0=psu[:, :], scalar1=7.0, scalar2=-7.0, op0=ALU.min, op1=ALU.max), reads=[psu], writes=[u_])
                        S.op("pool", lambda e, g_=g_, s_=s_: e.tensor_tensor(out=s_[:], in0=g_[:], in1=s_[:], op=ALU.mult),
                             reads=[g_, s_], writes=[s_])
                        S.op("dve", lambda e, ft=ft, s_=s_, u_=u_, a=a: e.scalar_tensor_tensor(out=a[:, ft, :], in0=u_[:], scalar=1.0, in1=s_[:],
                                                                                         op0=ALU.add, op1=ALU.mult), reads=[s_, u_], writes=[a])
                    for sub in range(4):
                        s_idx = tt * 4 + sub
                        for half in range(2):
                            ps = C.psum()
                            S.op("pe", lambda e, ps=ps, half=half, b2_=b2_: e.matmul(
                                ps[:, :], lhsT=C.ones_b[0:1, :], rhs=b2_[0:1, half * 512:(half + 1) * 512], start=True, stop=False),
                                reads=[C.ones_b, b2_], writes=[ps])
                            for ft in range(8):
                                S.op("pe", lambda e, ft=ft, ps=ps, half=half, sub=sub, a=a, w2_=w2_: e.matmul(
                                    ps[:, :], lhsT=a[:, ft, sub * 128:(sub + 1) * 128], rhs=w2_[:, ft, half * 512:(half + 1) * 512],
                                    start=False, stop=(ft == 7)), reads=[a, w2_], writes=[ps])
                            S.op("dve", lambda e, ps=ps, half=half, s_idx=s_idx, ex=ex: e.scalar_tensor_tensor(
                                out=acc[:, s_idx, half * 512:(half + 1) * 512], in0=ps[:, :], scalar=gt[:, s_idx, ex:ex + 1],
                                in1=acc[:, s_idx, half * 512:(half + 1) * 512], op0=ALU.mult, op1=ALU.add),
                                reads=[ps, gt, acc], writes=[acc])
            for tt in range(ntt):
                tile_i = sti * ntt + tt
                for sub in range(4):
                    s_idx = tt * 4 + sub
                    ln_tile(C, (acc[:, s_idx, :], acc), (acc[:, s_idx, :], acc), g_bc, b_bc)
                    S_store_sub(C, acc, s_idx, hTs[tile_i % 2], tile_i, sub, out_ap)
        S.barrier()
        for t_ in [g_bc, b_bc, acc, hTt, gt] + w1t + w2 + b1r + b2 + hTs:
            S.release(t_)


def S_store_sub(C, acc, s_idx, hTs, tile_i, sub, out_ap):
    S = C.S
    tok0 = tile_i * 512 + sub * 128
    S.dma("sp", C.h_tok[tok0:tok0 + 128, :], acc[:, s_idx, :], reads=[acc], sem_tile=acc)
    if out_ap is not None:
        S.dma("sp", out_ap[tok0:tok0 + 128, :], acc[:, s_idx, :], reads=[acc], sem_tile=acc)
    transpose_to(C, acc[:, s_idx, :], acc, hTs, lambda g: hTs[:, g * 4:(g + 1) * 4, sub * 128:(sub + 1) * 128])
    if sub == 3:
        S.dma("sp", C.hT.rearrange("(kc p) t -> p kc t", p=128)[:, :, tile_i * 512:(tile_i + 1) * 512], hTs[:],
              reads=[hTs], sem_tile=hTs)


RW_EPS = 64e-5
RW_FP32R = False
RW_C = math.exp(-0.5)


def phase_rwkv(C, l):
    S, T, P = C.S, C.T, C.P
    MT = 256
    NCH = MT // 32
    with ExitStack() as st:
        def cols64(nm, key):
            return load_cols(C, st, nm, P[key][l].rearrange("(h n) -> h n", n=64), 4, m=64)
        w0c, a0c, kkc, kac, lgc, lbc = (cols64("rw_" + k, "rw_" + k) for k in ("w0", "a0", "k_k", "k_a", "ln_g", "ln_b"))
        rkc = load_cols(C, st, "rw_rk", P["rw_r_k"][l], 4, m=64)
        wup = S.sb("rw_wup", [64, 256], F32, st)
        aup = S.sb("rw_aup", [64, 256], F32, st)
        gup = S.sb("rw_gup", [128, 256], F32, st)
        S.dma("sp", wup[:], P["rw_w_up"][l], writes=[wup], sem_tile=wup)
        S.dma("sp", aup[:], P["rw_a_up"][l], writes=[aup], sem_tile=aup)
        S.dma("sp", gup[:], P["rw_g_up"][l], writes=[gup], sem_tile=gup)
        bd = S.sb("rw_bd", [128, 128], F32, st)
        S.op("pool", lambda e: e.memset(bd[:], 0.0), writes=[bd])
        for h in range(4):
            S.op("pool", lambda e, h=h: e.memset(bd[32 * h:32 * h + 32, 32 * h:32 * h + 32], 1.0), reads=[bd], writes=[bd])
        mA = S.sb("rw_mA", [128, 4, 128], F32, st)
        mB = S.sb("rw_mB", [128, 2, 128], F32, st)
        for j in range(4):
            cmp = ALU.is_gt if j < 2 else ALU.is_ge
            S.op("pool", lambda e, j=j, cmp=cmp: e.affine_select(out=mA[:, j, :], in_=bd[:], compare_op=cmp, fill=0.0, base=0,
                                                                 pattern=[[1, 128]], channel_multiplier=-1), reads=[bd], writes=[mA])
        for j in range(2):
            S.op("pool", lambda e, j=j: e.affine_select(out=mB[:, j, :], in_=bd[:], compare_op=ALU.is_gt, fill=0.0, base=0,
                                                        pattern=[[-1, 128]], channel_multiplier=1), reads=[bd], writes=[mB])
        cmask = S.sb("rw_cmask", [64, 4 * MT], F32, st)
        S.op("pool", lambda e: e.memset(cmask[:], 1.0), writes=[cmask])
        S.op("pool", lambda e: e.memset(cmask[:, :].rearrange("p (c l) -> p c l", l=32)[:, :, 0:1], 0.0), reads=[cmask], writes=[cmask])
        o64 = S.sb("rw_o64", [64, 64], F32, st)
        S.op("pool", lambda e: e.memset(o64[:], 1.0), writes=[o64])
        o64m = S.sb("rw_o64m", [64, 64], F32, st)
        S.op("pool", lambda e: e.memset(o64m[:], 1.0 / 64.0), writes=[o64m])
        Ebd = S.sb("rw_Ebd", [128, 256], F32, st)
        S.op("pool", lambda e: e.memset(Ebd[:], 0.0), writes=[Ebd])
        S0 = [S.sb("rw_S%d" % i, [64, 256], F32, st) for i in range(2)]
        S.op("pool", lambda e: e.memset(S0[0][:], 0.0), writes=[S0[0]])
        nS = 0
        nSl = [0]

        def kh(nm):
            return S.sb(nm, [64, 4, MT], F32, st)
        r_t, k_t, v_t, a_t, g_t, ka_t, b_t, lw_t, G_t, eG_t, bon_t, Y_t, x1, x2 = (
            kh("rw_" + n) for n in ("r", "k", "v", "a", "g", "ka", "b", "lw", "G", "eG", "bon", "Y", "x1", "x2"))
        rC, kC, bC, kaC, vC = (S.sb("rw_c" + n, [64, NCH, 128], F32, st) for n in ("r", "k", "b", "ka", "v"))
        xw_t = S.sb("rw_xw", [64, 2, MT], F32, st)
        xg_t = S.sb("rw_xg", [128, MT], F32, st)
        o_t = S.sb("rw_o", [64, 4, MT], BF16, st)
        GRP = 4
        NSET = 8
        RR = (lambda ap: ap.bitcast(mybir.dt.float32r)) if RW_FP32R else (lambda ap: ap)
        TT_ = [S.sb("rw_TT%d" % i, [128, 4, 64], F32, st) for i in range(NSET)]
        AA = [S.sb("rw_AA%d" % i, [128, 4, 128], F32, st) for i in range(NSET)]
        AB = [S.sb("rw_AB%d" % i, [128, 2, 128], F32, st) for i in range(NSET)]
        Vbds = [S.sb("rw_Vbd%d" % i, [128, 256], F32, st) for i in range(NSET)]
        for vb in Vbds:
            S.op("pool", lambda e: e.memset(vb[:], 0.0), writes=[vb])
        PP = [None, None]
        PPx = [S.sb("rw_PP%d" % i, [128, 2, 128], F32, st) for i in range(2 * NSET)]
        MM = [S.sb("rw_MM%d" % i, [128, 2, 128], F32, st) for i in range(NSET)]
        Kh = [S.sb("rw_Kh%d" % i, [64, 128], F32, st) for i in range(NSET)]
        Gh = [S.sb("rw_Gh%d" % i, [128, 128], F32, st) for i in range(NSET)]
        E2 = [S.sb("rw_E2%d" % i, [128, 64], F32, st) for i in range(NSET)]
        ET = [S.sb("rw_ET%d" % i, [128, 64], F32, st) for i in range(NSET)]
        tmpS = S.sb("rw_tmpS", [64, 256], F32, st)
        nchunk = 0
        zc = C.zT

        def perhead(fn):
            for h in range(4):
                fn(h)

        def fl(t):
            return t[:, :, :].rearrange("p h t -> p (h t)")

        for mi in range(T // MT):
            t0 = mi * MT
            for j, tl in enumerate((r_t, k_t, v_t)):
                S.dma("sp", tl[:], zc[OFF_C + j * 256:OFF_C + (j + 1) * 256, t0:t0 + MT].rearrange("(h n) t -> n h t", n=64),
                      writes=[tl], sem_tile=tl)
            S.dma("sp", xw_t[:], zc[OFF_C + 768:OFF_C + 896, t0:t0 + MT].rearrange("(a n) t -> n a t", n=64), writes=[xw_t], sem_tile=xw_t)
            S.dma("sp", xg_t[:], zc[OFF_C + 896:OFF_C + 1024, t0:t0 + MT], writes=[xg_t], sem_tile=xg_t)
            S.op("act", lambda e: e.activation(out=xw_t[:, 0, :], in_=xw_t[:, 0, :], func=AF.Tanh), reads=[xw_t], writes=[xw_t])
            S.op("act", lambda e: e.activation(out=xg_t[:], in_=xg_t[:], func=AF.Sigmoid), reads=[xg_t], writes=[xg_t])
            for h in range(4):
                ps = C.psum()
                S.op("pe", lambda e, h=h, ps=ps: e.matmul(ps[0:64, 0:MT], lhsT=wup[:, h * 64:(h + 1) * 64], rhs=xw_t[:, 0, :], start=True, stop=True),
                     reads=[wup, xw_t], writes=[ps])
                S.op("act", lambda e, h=h, ps=ps: e.activation(out=lw_t[:, h, :], in_=ps[0:64, 0:MT], func=AF.Sigmoid, bias=w0c[:, h:h + 1], scale=1.0),
                     reads=[ps, w0c], writes=[lw_t])
                ps = C.psum()
                S.op("pe", lambda e, h=h, ps=ps: e.matmul(ps[0:64, 0:MT], lhsT=aup[:, h * 64:(h + 1) * 64], rhs=xw_t[:, 1, :], start=True, stop=True),
                     reads=[aup, xw_t], writes=[ps])
                S.op("act", lambda e, h=h, ps=ps: e.activation(out=a_t[:, h, :], in_=ps[0:64, 0:MT], func=AF.Sigmoid, bias=a0c[:, h:h + 1], scale=1.0),
                     reads=[ps, a0c], writes=[a_t])
                ps = C.psum()
                S.op("pe", lambda e, h=h, ps=ps: e.matmul(ps[0:64, 0:MT], lhsT=gup[:, h * 64:(h + 1) * 64], rhs=xg_t[:, :], start=True, stop=True),
                     reads=[gup, xg_t], writes=[ps])
                S.op("dve", lambda e, h=h, ps=ps: e.tensor_copy(out=g_t[:, h, :], in_=ps[0:64, 0:MT]), reads=[ps], writes=[g_t])
            S.op("pool", lambda e: e.tensor_scalar(out=fl(lw_t), in0=fl(lw_t), scalar1=-RW_C, scalar2=None, op0=ALU.mult), reads=[lw_t], writes=[lw_t])
            for h in range(4):
                S.op("dve", lambda e, h=h: e.tensor_scalar(out=ka_t[:, h, :], in0=k_t[:, h, :], scalar1=kkc[:, h:h + 1], scalar2=None, op0=ALU.mult),
                     reads=[k_t, kkc], writes=[ka_t])
            S.op("pool", lambda e: e.tensor_tensor(out=fl(x1), in0=fl(ka_t), in1=fl(ka_t), op=ALU.mult), reads=[ka_t], writes=[x1])
            for h in range(4):
                ps = C.psum()
                S.op("pe", lambda e, h=h, ps=ps: e.matmul(ps[0:64, 0:MT], lhsT=o64[:, :], rhs=x1[:, h, :], start=True, stop=True), reads=[o64, x1], writes=[ps])
                S.op("act", lambda e, h=h, ps=ps: e.activation(out=x2[:, h, :], in_=ps[0:64, 0:MT], func=AF.Sqrt), reads=[ps], writes=[x2])
            S.op("dve", lambda e: e.tensor_scalar(out=fl(x2), in0=fl(x2), scalar1=1e-12, scalar2=None, op0=ALU.max), reads=[x2], writes=[x2])
            S.op("dve", lambda e: e.reciprocal(out=fl(x2), in_=fl(x2)), reads=[x2], writes=[x2])
            S.op("pool", lambda e: e.tensor_tensor(out=fl(ka_t), in0=fl(ka_t), in1=fl(x2), op=ALU.mult), reads=[ka_t, x2], writes=[ka_t])
            for h in range(4):
                S.op("dve", lambda e, h=h: e.tensor_scalar(out=x1[:, h, :], in0=a_t[:, h, :], scalar1=-1.0, scalar2=kac[:, h:h + 1], op0=ALU.add, op1=ALU.mult),
                     reads=[a_t, kac], writes=[x1])
            S.op("dve", lambda e: e.scalar_tensor_tensor(out=fl(k_t), in0=fl(x1), scalar=1.0, in1=fl(k_t), op0=ALU.add, op1=ALU.mult),
                 reads=[x1, k_t], writes=[k_t])
            S.op("pool", lambda e: e.tensor_tensor(out=fl(b_t), in0=fl(ka_t), in1=fl(a_t), op=ALU.mult), reads=[ka_t, a_t], writes=[b_t])
            S.op("pool", lambda e: e.tensor_tensor(out=fl(x1), in0=fl(r_t), in1=fl(k_t), op=ALU.mult), reads=[r_t, k_t], writes=[x1])
            for h in range(4):
                S.op("dve", lambda e, h=h: e.tensor_scalar(out=x1[:, h, :], in0=x1[:, h, :], scalar1=rkc[:, h:h + 1], scalar2=None, op0=ALU.mult),
                     reads=[x1, rkc], writes=[x1])
            for h in range(4):
                ps = C.psum()
                S.op("pe", lambda e, h=h, ps=ps: e.matmul(ps[0:64, 0:MT], lhsT=o64[:, :], rhs=x1[:, h, :], start=True, stop=True), reads=[o64, x1], writes=[ps])
                S.op("dve", lambda e, h=h, ps=ps: e.tensor_tensor(out=bon_t[:, h, :], in0=ps[0:64, 0:MT], in1=v_t[:, h, :], op=ALU.mult),
                     reads=[ps, v_t], writes=[bon_t])
            S.op("dve", lambda e: e.tensor_tensor_scan(out=fl(G_t), data0=cmask[:, :], data1=fl(lw_t), initial=0.0, op0=ALU.mult, op1=ALU.add),
                 reads=[cmask, lw_t], writes=[G_t])
            S.op("act", lambda e: e.activation(out=fl(eG_t), in_=fl(G_t), func=AF.Exp), reads=[G_t], writes=[eG_t])
            S.op("pool", lambda e: e.tensor_tensor(out=fl(r_t), in0=fl(r_t), in1=fl(eG_t), op=ALU.mult), reads=[r_t, eG_t], writes=[r_t])
            S.op("pool", lambda e: e.tensor_tensor(out=fl(x1), in0=fl(G_t), in1=fl(lw_t), op=ALU.subtract), reads=[G_t, lw_t], writes=[x1])
            S.op("act", lambda e: e.activation(out=fl(x1), in_=fl(x1), func=AF.Exp), reads=[x1], writes=[x1])
            S.op("pool", lambda e: e.tensor_tensor(out=fl(ka_t), in0=fl(ka_t), in1=fl(x1), op=ALU.mult), reads=[ka_t, x1], writes=[ka_t])
            S.op("act", lambda e: e.activation(out=fl(x2), in_=fl(G_t), func=AF.Exp, scale=-1.0), reads=[G_t], writes=[x2])
            S.op("pool", lambda e: e.tensor_tensor(out=fl(b_t), in0=fl(b_t), in1=fl(x2), op=ALU.mult), reads=[b_t, x2], writes=[b_t])
            S.op("pool", lambda e: e.tensor_tensor(out=fl(k_t), in0=fl(k_t), in1=fl(x2), op=ALU.mult), reads=[k_t, x2], writes=[k_t])
            for j, (src, dst) in enumerate(((r_t, rC), (k_t, kC), (b_t, bC), (ka_t, kaC), (v_t, vC))):
                en = "act" if j % 2 else "pool"
                S.op(en, copy_op("act" if en == "act" else "dve", dst[:, :, :].rearrange("p c (h t) -> p h c t", h=4),
                                 src[:, :, :].rearrange("p h (c t) -> p h c t", t=32)), reads=[src], writes=[dst])
            def stage_a(c, si):
                TT, A_, B_, M_, Kh_, Gh_, E2_, Vb_ = TT_[si], AA[si], AB[si], MM[si], Kh[si], Gh[si], E2[si], Vbds[si]
                PPs = (PPx[2 * si], PPx[2 * si + 1])
                rc, kc_, bc, kac_, vc = rC[:, c, :], kC[:, c, :], bC[:, c, :], kaC[:, c, :], vC[:, c, :]
                ps = C.psum()
                for j, (src, srct) in enumerate(((bc, bC), (kc_, kC), (kac_, kaC), (vc, vC))):
                    S.op("pe", lambda e: e.transpose(out=ps[:, j * 64:(j + 1) * 64], in_=src, identity=C.ident[0:64, 0:64]),
                         reads=[srct, C.ident], writes=[ps])
                S.op("act", lambda e: e.copy(out=TT[:], in_=ps[:, 0:256].rearrange("p (a b) -> p a b", a=4)), reads=[ps], writes=[TT])
                for h in range(4):
                    S.op("pool", lambda e: e.tensor_copy(out=Vb_[32 * h:32 * h + 32, 64 * h:64 * h + 64], in_=TT[32 * h:32 * h + 32, 3, :]),
                         reads=[TT], writes=[Vb_])
                yield
                psA = C.psum()
                for j, (lt, ltt, rt, rtt) in enumerate(((bc, bC, kac_, kaC), (kc_, kC, kac_, kaC), (bc, bC, rc, rC), (kc_, kC, rc, rC))):
                    S.op("pe", lambda e: e.matmul(psA[:, j * 128:(j + 1) * 128], lhsT=RR(lt), rhs=RR(rt), start=True, stop=True),
                         reads=[ltt, rtt], writes=[psA])
                S.op("dve", lambda e: e.tensor_tensor(out=A_[:], in0=psA[:, :].rearrange("p (a b) -> p a b", a=4), in1=mA[:], op=ALU.mult),
                     reads=[psA, mA], writes=[A_])
                psB = C.psum()
                for j, (lt, ltt, rt, rtt) in enumerate(((kac_, kaC, bc, bC), (kac_, kaC, kc_, kC))):
                    S.op("pe", lambda e: e.matmul(psB[:, j * 128:(j + 1) * 128], lhsT=RR(lt), rhs=RR(rt), start=True, stop=True),
                         reads=[ltt, rtt], writes=[psB])
                S.op("dve", lambda e: e.tensor_tensor(out=B_[:], in0=psB[:, 0:256].rearrange("p (a b) -> p a b", a=2), in1=mB[:], op=ALU.mult),
                     reads=[psB, mB], writes=[B_])
                S.op("pool", lambda e: e.tensor_tensor(out=M_[:, 0, :], in0=C.ident[:], in1=A_[:, 0, :], op=ALU.subtract), reads=[A_, C.ident], writes=[M_])
                S.op("pool", lambda e: e.tensor_tensor(out=M_[:, 1, :], in0=C.ident[:], in1=B_[:, 0, :], op=ALU.subtract), reads=[B_, C.ident, M_], writes=[M_])
                yield
                cur = (A_[:, 0, :], B_[:, 0, :], A_, B_)
                for it in range(4):
                    p_ap, pt_ap, p_t1, p_t2 = cur
                    psQ = C.psum()
                    S.op("pe", lambda e: e.matmul(psQ[:, 0:128], lhsT=RR(pt_ap), rhs=RR(p_ap), start=True, stop=True), reads=[p_t1, p_t2], writes=[psQ])
                    S.op("pe", lambda e: e.matmul(psQ[:, 128:256], lhsT=RR(p_ap), rhs=RR(pt_ap), start=True, stop=True), reads=[p_t1, p_t2], writes=[psQ])
                    Pn = PPs[it % 2]
                    S.op("act", lambda e: e.copy(out=Pn[:], in_=psQ[:, 0:256].rearrange("p (a b) -> p a b", a=2)), reads=[psQ], writes=[Pn])
                    yield
                    psU = C.psum()
                    S.op("pe", lambda e: e.matmul(psU[:, 0:128], lhsT=RR(M_[:, 1, :]), rhs=RR(Pn[:, 0, :]), start=True, stop=True), reads=[M_, Pn], writes=[psU])
                    if it < 3:
                        S.op("pe", lambda e: e.matmul(psU[:, 128:256], lhsT=RR(Pn[:, 0, :]), rhs=RR(M_[:, 1, :]), start=True, stop=True), reads=[M_, Pn], writes=[psU])
                        S.op("dve", lambda e: e.tensor_tensor(out=M_[:], in0=psU[:, 0:256].rearrange("p (a b) -> p a b", a=2), in1=M_[:], op=ALU.add),
                             reads=[psU, M_], writes=[M_])
                    else:
                        S.op("dve", lambda e: e.tensor_tensor(out=M_[:, 0, :], in0=psU[:, 0:128], in1=M_[:, 0, :], op=ALU.add), reads=[psU, M_], writes=[M_])
                    cur = (Pn[:, 0, :], Pn[:, 1, :], Pn, Pn)
                    yield
                psK = C.psum()
                S.op("pe", lambda e: e.matmul(psK[0:64, 0:128], lhsT=TT[:, 2, :], rhs=M_[:, 0, :], start=True, stop=True), reads=[TT, M_], writes=[psK])
                S.op("act", lambda e: e.copy(out=Kh_[:], in_=psK[0:64, 0:128]), reads=[psK], writes=[Kh_])
                psG = C.psum()
                S.op("pe", lambda e: e.matmul(psG[:, 0:128], lhsT=RR(B_[:, 1, :]), rhs=RR(M_[:, 0, :]), start=True, stop=True), reads=[B_, M_], writes=[psG])
                S.op("act", lambda e: e.copy(out=Gh_[:], in_=psG[:, 0:128]), reads=[psG], writes=[Gh_])
                yield
                psE2 = C.psum()
                S.op("pe", lambda e: e.matmul(psE2[:, 0:64], lhsT=Gh_[:, :], rhs=TT[:, 3, :], start=True, stop=True), reads=[Gh_, TT], writes=[psE2])
                S.op("act", lambda e: e.copy(out=E2_[:], in_=psE2[:, 0:64]), reads=[psE2], writes=[E2_])
                yield

            def run_rr(gens):
                alive = list(gens)
                while alive:
                    for g_ in list(alive):
                        try:
                            next(g_)
                        except StopIteration:
                            alive.remove(g_)

            def stage_b(c0, n0):
                for c in range(c0, c0 + GRP):
                    cs = slice(32 * c, 32 * c + 32)
                    si = (n0 + c - c0) % NSET
                    TT, A_, B_, M_, Kh_, Gh_, E2_, ET_, Vbd = TT_[si], AA[si], AB[si], MM[si], Kh[si], Gh[si], E2[si], ET[si], Vbds[si]
                    Sc = S0[nSl[0] % 2]
                    Sn = S0[(nSl[0] + 1) % 2]
                    nSl[0] += 1
                    psE = C.psum()
                    S.op("pe", lambda e, psE=psE, Kh_=Kh_, Sc=Sc: e.matmul(psE[:, 0:256], lhsT=Kh_[:, :], rhs=Sc[:, :], start=True, stop=True),
                         reads=[Kh_, Sc], writes=[psE])
                    for h in range(4):
                        S.op("dve", lambda e, h=h, psE=psE, ET_=ET_, E2_=E2_: e.scalar_tensor_tensor(
                            out=ET_[32 * h:32 * h + 32, :], in0=psE[32 * h:32 * h + 32, 64 * h:64 * h + 64], scalar=-1.0,
                            in1=E2_[32 * h:32 * h + 32, :], op0=ALU.mult, op1=ALU.subtract), reads=[psE, E2_, ET_], writes=[ET_])
                    for h in range(4):
                        S.op("pool", lambda e, h=h, ET_=ET_: e.tensor_copy(out=Ebd[32 * h:32 * h + 32, 64 * h:64 * h + 64], in_=ET_[32 * h:32 * h + 32, :]),
                             reads=[ET_], writes=[Ebd])
                    yield
                    psY = C.psum()
                    S.op("pe", lambda e, psY=psY, TT=TT, A_=A_: e.matmul(psY[0:64, 0:128], lhsT=TT[:, 3, :], rhs=A_[:, 3, :], start=True, stop=False),
                         reads=[TT, A_], writes=[psY])
                    S.op("pe", lambda e, psY=psY, ET_=ET_, A_=A_: e.matmul(psY[0:64, 0:128], lhsT=ET_[:, :], rhs=A_[:, 2, :], start=False, stop=False),
                         reads=[ET_, A_], writes=[psY])
                    for h in range(4):
                        S.op("pe", lambda e, h=h, psY=psY, Sc=Sc, cs=cs: e.matmul(psY[0:64, 32 * h:32 * h + 32], lhsT=Sc[:, 64 * h:64 * h + 64], rhs=rC[:, c, 32 * h:32 * h + 32],
                                                                             start=False, stop=(h == 3)), reads=[Sc, rC], writes=[psY])
                    S.op("act", lambda e, psY=psY, cs=cs: e.copy(out=Y_t[:, :, cs], in_=psY[0:64, 0:128].rearrange("p (h t) -> p h t", h=4)), reads=[psY], writes=[Y_t])
                    yield
                    psS = C.psum()
                    S.op("pe", lambda e, psS=psS, TT=TT: e.matmul(psS[0:64, 0:256], lhsT=TT[:, 0, :], rhs=Ebd[:, :], start=True, stop=False),
                         reads=[TT, Ebd], writes=[psS])
                    S.op("pe", lambda e, psS=psS, TT=TT: e.matmul(psS[0:64, 0:256], lhsT=TT[:, 1, :], rhs=Vbd[:, :], start=False, stop=True),
                         reads=[TT, Vbd], writes=[psS])
                    S.op("dve", lambda e, psS=psS, Sc=Sc: e.tensor_tensor(out=tmpS[:], in0=psS[0:64, 0:256], in1=Sc[:], op=ALU.add), reads=[psS, Sc], writes=[tmpS])
                    for h in range(4):
                        S.op("dve", lambda e, h=h, Sn=Sn, c=c: e.tensor_scalar(out=Sn[:, 64 * h:64 * h + 64], in0=tmpS[:, 64 * h:64 * h + 64],
                                                                            scalar1=eG_t[:, h, 32 * c + 31:32 * c + 32], scalar2=None, op0=ALU.mult),
                             reads=[tmpS, eG_t, Sn], writes=[Sn])

                    yield

            ngrp = NCH // GRP
            run_rr([stage_a(q, (nchunk + q) % NSET) for q in range(GRP)])
            for gi_ in range(ngrp):
                gens = [stage_b(gi_ * GRP, nchunk + gi_ * GRP)]
                if gi_ + 1 < ngrp:
                    gens += [stage_a((gi_ + 1) * GRP + q, (nchunk + (gi_ + 1) * GRP + q) % NSET) for q in range(GRP)]
                run_rr(gens)
            nchunk += NCH
            for h in range(4):
                ps = C.psum()
                S.op("pe", lambda e, h=h, ps=ps: e.matmul(ps[0:64, 0:MT], lhsT=o64m[:, :], rhs=Y_t[:, h, :], start=True, stop=True), reads=[o64m, Y_t], writes=[ps])
                S.op("dve", lambda e, h=h, ps=ps: e.tensor_tensor(out=x1[:, h, :], in0=Y_t[:, h, :], in1=ps[0:64, 0:MT], op=ALU.subtract),
                     reads=[ps, Y_t], writes=[x1])
            S.op("pool", lambda e: e.tensor_tensor(out=fl(x2), in0=fl(x1), in1=fl(x1), op=ALU.mult), reads=[x1], writes=[x2])
            for h in range(4):
                ps = C.psum()
                S.op("pe", lambda e, h=h, ps=ps: e.matmul(ps[0:64, 0:MT], lhsT=o64m[:, :], rhs=x2[:, h, :], start=True, stop=True), reads=[o64m, x2], writes=[ps])
                S.op("act", lambda e, h=h, ps=ps: e.activation(out=G_t[:, h, :], in_=ps[0:64, 0:MT], func=AF.Sqrt, bias=C.rweps[0:64, 0:1], scale=1.0),
                     reads=[ps, C.rweps], writes=[G_t])
            S.op("dve", lambda e: e.reciprocal(out=fl(G_t), in_=fl(G_t)), reads=[G_t], writes=[G_t])
            S.op("pool", lambda e: e.tensor_tensor(out=fl(x1), in0=fl(x1), in1=fl(G_t), op=ALU.mult), reads=[x1, G_t], writes=[x1])
            for h in range(4):
                S.op("dve", lambda e, h=h: e.tensor_scalar(out=x1[:, h, :], in0=x1[:, h, :], scalar1=lgc[:, h:h + 1], scalar2=lbc[:, h:h + 1],
                                                           op0=ALU.mult, op1=ALU.add), reads=[x1, lgc, lbc], writes=[x1])
            S.op("pool", lambda e: e.tensor_tensor(out=fl(x1), in0=fl(x1), in1=fl(bon_t), op=ALU.add), reads=[x1, bon_t], writes=[x1])
            S.op("pool", lambda e: e.tensor_tensor(out=fl(o_t), in0=fl(x1), in1=fl(g_t), op=ALU.mult), reads=[x1, g_t], writes=[o_t])
            S.dma("sp", C.oT[512:768, t0:t0 + MT].rearrange("(h n) t -> n h t", n=64), o_t[:], reads=[o_t], sem_tile=o_t)
        S.barrier()
        for t_ in [wup, aup, gup, r_t, k_t, v_t, xw_t, xg_t, o_t]:
            S.release(t_)


TWO_PI = 2.0 * math.pi


def phase_s5(C, l):
    S, T, NT, P = C.S, C.T, C.NT, C.P
    with ExitStack() as st:
        def small(nm, n=8, dt=F32):
            return S.sb("s5_" + nm, [128, n], dt, st)
        are = load_cols(C, st, "s5_are", P["s5_a_re"][l].rearrange("(k a) p -> k (a p)", a=2), 8)
        aim = load_cols(C, st, "s5_aim", P["s5_a_im"][l].rearrange("(k a) p -> k (a p)", a=2), 8)
        ldt = load_bc(C, st, "s5_ldt", P["s5_log_dt"][l], 16)
        dcol = load_cols(C, st, "s5_d", P["s5_d"][l].rearrange("(a p) -> a p", p=128), 2)
        gbcol = load_cols(C, st, "s5_gb", P["s5_glu_b"][l].rearrange("(a p) -> a p", p=128), 2)
        gluw = S.sb("s5_gluw", [128, 2, 256], BF16, st)
        S.dma("pool", gluw[:], P["s5_glu_w"][l].rearrange("(a p) n -> p a n", p=128), writes=[gluw], sem_tile=gluw)
        negpi = small("negpi", 1)
        S.op("pool", lambda e: e.memset(negpi[:], 0.0), writes=[negpi])
        dt_, lre, th, mag, thr, sc, cc, abr, abi, den, cre, cim, ncre, t1, t2, Rre, Rim = (
            small(n) for n in ("dt", "lre", "th", "mag", "thr", "sc", "cc", "abr", "abi", "den", "cre", "cim", "ncre", "t1", "t2", "Rre", "Rim"))
        qi = S.sb("s5_qi", [128, 512], I32, st)
        qf = S.sb("s5_qf", [128, 512], F32, st)
        tb = S.sb("s5_tb", [128, 512], F32, st)
        jf = S.sb("s5_jf", [128, 512], F32, st)
        ang = S.sb("s5_ang", [128, 512], F32, st)

        def rr_sin(dst_ap, dst_t, src_ap, src_t, n, shift=0.0):
            a_, q_, f_, t_ = ang[:, 0:n], qi[:, 0:n], qf[:, 0:n], tb[:, 0:n]
            S.op("dve", lambda e: e.tensor_scalar(out=a_, in0=src_ap, scalar1=shift, scalar2=None, op0=ALU.add), reads=[src_t], writes=[ang])
            S.op("dve", lambda e: e.tensor_scalar(out=q_, in0=a_, scalar1=1.0 / TWO_PI, scalar2=None, op0=ALU.mult), reads=[ang], writes=[qi])
            S.op("dve", lambda e: e.tensor_copy(out=f_, in_=q_), reads=[qi], writes=[qf])
            S.op("dve", lambda e: e.scalar_tensor_tensor(out=a_, in0=f_, scalar=-TWO_PI, in1=a_, op0=ALU.mult, op1=ALU.add), reads=[qf, ang], writes=[ang])
            S.op("dve", lambda e: e.tensor_scalar(out=t_, in0=a_, scalar1=math.pi, scalar2=None, op0=ALU.is_gt), reads=[ang], writes=[tb])
            S.op("dve", lambda e: e.scalar_tensor_tensor(out=a_, in0=t_, scalar=-TWO_PI, in1=a_, op0=ALU.mult, op1=ALU.add), reads=[tb, ang], writes=[ang])
            S.op("dve", lambda e: e.tensor_scalar(out=t_, in0=a_, scalar1=-math.pi, scalar2=None, op0=ALU.is_lt), reads=[ang], writes=[tb])
            S.op("dve", lambda e: e.scalar_tensor_tensor(out=a_, in0=t_, scalar=TWO_PI, in1=a_, op0=ALU.mult, op1=ALU.add), reads=[tb, ang], writes=[ang])
            S.op("dve", lambda e: e.tensor_scalar(out=a_, in0=a_, scalar1=3.1415925, scalar2=-3.1415925, op0=ALU.min, op1=ALU.max), reads=[ang], writes=[ang])
            S.op("act", lambda e: e.activation(out=dst_ap, in_=a_, func=AF.Sin, bias=negpi[:, 0:1], scale=1.0), reads=[ang, negpi], writes=[dst_t])

        S.op("dve", lambda e: e.tensor_copy(out=dt_[0:64, :], in_=ldt[0:64, 0:16:2]), reads=[ldt], writes=[dt_])
        S.op("dve", lambda e: e.tensor_copy(out=dt_[64:128, :], in_=ldt[64:128, 1:16:2]), reads=[ldt, dt_], writes=[dt_])
        S.op("act", lambda e: e.activation(out=dt_[:], in_=dt_[:], func=AF.Exp), reads=[dt_], writes=[dt_])
        S.op("dve", lambda e: e.tensor_tensor(out=lre[:], in0=are[:], in1=dt_[:], op=ALU.mult), reads=[are, dt_], writes=[lre])
        S.op("dve", lambda e: e.tensor_tensor(out=th[:], in0=aim[:], in1=dt_[:], op=ALU.mult), reads=[aim, dt_], writes=[th])
        S.op("act", lambda e: e.activation(out=mag[:], in_=lre[:], func=AF.Exp), reads=[lre], writes=[mag])
        rr_sin(sc[:], sc, th[:], th, 8)
        rr_sin(cc[:], cc, th[:], th, 8, shift=math.pi / 2)
        rr_sin(t1[:], t1, th[:], th, 8)
        S.op("dve", lambda e: e.tensor_copy(out=thr[:], in_=ang[:, 0:8]), reads=[ang], writes=[thr])
        S.op("dve", lambda e: e.tensor_tensor(out=abr[:], in0=mag[:], in1=cc[:], op=ALU.mult), reads=[mag, cc], writes=[abr])
        S.op("dve", lambda e: e.tensor_tensor(out=abi[:], in0=mag[:], in1=sc[:], op=ALU.mult), reads=[mag, sc], writes=[abi])
        S.op("dve", lambda e: e.tensor_tensor(out=den[:], in0=are[:], in1=are[:], op=ALU.mult), reads=[are], writes=[den])
        S.op("dve", lambda e: e.tensor_tensor(out=t1[:], in0=aim[:], in1=aim[:], op=ALU.mult), reads=[aim], writes=[t1])
        S.op("dve", lambda e: e.tensor_tensor(out=den[:], in0=den[:], in1=t1[:], op=ALU.add), reads=[den, t1], writes=[den])
        S.op("dve", lambda e: e.reciprocal(out=den[:], in_=den[:]), reads=[den], writes=[den])
        S.op("dve", lambda e: e.tensor_scalar(out=t1[:], in0=abr[:], scalar1=-1.0, scalar2=None, op0=ALU.add), reads=[abr], writes=[t1])
        S.op("dve", lambda e: e.tensor_tensor(out=cre[:], in0=t1[:], in1=are[:], op=ALU.mult), reads=[t1, are], writes=[cre])
        S.op("dve", lambda e: e.tensor_tensor(out=t2[:], in0=abi[:], in1=aim[:], op=ALU.mult), reads=[abi, aim], writes=[t2])
        S.op("dve", lambda e: e.tensor_tensor(out=cre[:], in0=cre[:], in1=t2[:], op=ALU.add), reads=[cre, t2], writes=[cre])
        S.op("dve", lambda e: e.tensor_tensor(out=cre[:], in0=cre[:], in1=den[:], op=ALU.mult), reads=[cre, den], writes=[cre])
        S.op("dve", lambda e: e.tensor_tensor(out=cim[:], in0=abi[:], in1=are[:], op=ALU.mult), reads=[abi, are], writes=[cim])
        S.op("dve", lambda e: e.tensor_tensor(out=t2[:], in0=t1[:], in1=aim[:], op=ALU.mult), reads=[t1, aim], writes=[t2])
        S.op("dve", lambda e: e.tensor_tensor(out=cim[:], in0=cim[:], in1=t2[:], op=ALU.subtract), reads=[cim, t2], writes=[cim])
        S.op("dve", lambda e: e.tensor_tensor(out=cim[:], in0=cim[:], in1=den[:], op=ALU.mult), reads=[cim, den], writes=[cim])
        S.op("dve", lambda e: e.tensor_scalar(out=ncre[:], in0=cre[:], scalar1=-1.0, scalar2=None, op0=ALU.mult), reads=[cre], writes=[ncre])
        S.op("dve", lambda e: e.tensor_scalar(out=t2[:], in0=thr[:], scalar1=512.0, scalar2=None, op0=ALU.mult), reads=[thr], writes=[t2])
        rr_sin(Rim[:], Rim, t2[:], t2, 8)
        rr_sin(Rre[:], Rre, t2[:], t2, 8, shift=math.pi / 2)
        S.op("pool", lambda e: e.iota(qi[:], pattern=[[1, 512]], base=0, channel_multiplier=0), writes=[qi])
        S.op("dve", lambda e: e.tensor_copy(out=jf[:], in_=qi[:]), reads=[qi], writes=[jf])
        cosT = S.sb("s5_cosT", [128, 8, 512], F32, st)
        sinT = S.sb("s5_sinT", [128, 8, 512], F32, st)
        TiR = S.sb("s5_TiR", [128, 8, 512], F32, st)
        TiI = S.sb("s5_TiI", [128, 8, 512], F32, st)
        magT = S.sb("s5_magT", [128, 8, 512], F32, st)
        a2 = S.sb("s5_a2", [128, 512], F32, st)
        for k in range(8):
            S.op("pool", lambda e, k=k: e.tensor_scalar(out=a2[:], in0=jf[:], scalar1=thr[:, k:k + 1], scalar2=None, op0=ALU.mult), reads=[jf, thr], writes=[a2])
            rr_sin(sinT[:, k, :], sinT, a2[:], a2, 512)
            rr_sin(cosT[:, k, :], cosT, a2[:], a2, 512, shift=math.pi / 2)
            S.op("pool", lambda e, k=k: e.tensor_scalar(out=magT[:, k, :], in0=jf[:], scalar1=0.0, scalar2=mag[:, k:k + 1], op0=ALU.mult, op1=ALU.add),
                 reads=[jf, mag], writes=[magT])
            S.op("dve", lambda e, k=k: e.tensor_scalar(out=TiR[:, k, :], in0=cosT[:, k, :], scalar1=cre[:, k:k + 1], scalar2=None, op0=ALU.mult), reads=[cosT, cre], writes=[TiR])
            S.op("dve", lambda e, k=k: e.scalar_tensor_tensor(out=TiR[:, k, :], in0=sinT[:, k, :], scalar=cim[:, k:k + 1], in1=TiR[:, k, :], op0=ALU.mult, op1=ALU.add),
                 reads=[sinT, cim, TiR], writes=[TiR])
            S.op("dve", lambda e, k=k: e.tensor_scalar(out=TiI[:, k, :], in0=cosT[:, k, :], scalar1=cim[:, k:k + 1], scalar2=None, op0=ALU.mult), reads=[cosT, cim], writes=[TiI])
            S.op("dve", lambda e, k=k: e.scalar_tensor_tensor(out=TiI[:, k, :], in0=sinT[:, k, :], scalar=ncre[:, k:k + 1], in1=TiI[:, k, :], op0=ALU.mult, op1=ALU.add),
                 reads=[sinT, ncre, TiI], writes=[TiI])
        BT = [S.sb("s5_BT%d" % i, [16, 16, 64], BF16, st) for i in range(2)]
        CT = [S.sb("s5_CT%d" % i, [128, 8, 128], BF16, st) for i in range(2)]
        with ExitStack() as st2:
            braw = S.sb("s5_braw", [64, 16, 16], F32, st2)
            craw = S.sb("s5_craw", [16, 16, 64], F32, st2)
            for ri, (bk, ck) in enumerate((("s5_b_re", "s5_c_re"), ("s5_b_im", "s5_c_im"))):
                S.dma("sp", braw[:], P[bk][l].rearrange("g p c -> p g c"), writes=[braw], sem_tile=braw)
                for half in range(2):
                    ps = C.psum()
                    for g8 in range(8):
                        g = half * 8 + g8
                        S.op("pe", lambda e, g=g, g8=g8, ps=ps: e.transpose(out=ps[0:16, g8 * 64:(g8 + 1) * 64], in_=braw[:, g, :], identity=C.ident[0:64, 0:64]),
                             reads=[braw, C.ident], writes=[ps])
                    S.op("act", lambda e, half=half, ps=ps, ri=ri: e.copy(out=BT[ri][:, half * 8:(half + 1) * 8, :], in_=ps[0:16, :].rearrange("p (g q) -> p g q", g=8)),
                         reads=[ps], writes=[BT[ri]])
                S.op("pool", lambda e, ri=ri: e.memset(CT[ri][:], 0.0), writes=[CT[ri]])
                S.dma("sp", craw[:], P[ck][l].rearrange("g c p -> c g p"), writes=[craw], sem_tile=craw)
                for k in range(8):
                    ps = C.psum()
                    S.op("pe", lambda e, k=k, ps=ps: e.transpose(out=ps[:, 0:16], in_=craw[:, 2 * k:2 * k + 2, :].rearrange("c a p -> c (a p)"), identity=C.ident[0:16, 0:16]),
                         reads=[craw, C.ident], writes=[ps])
                    c0 = (k % 4) * 32
                    sgn = 1.0 if ri == 0 else -1.0
                    S.op("act", lambda e, k=k, ps=ps, ri=ri, c0=c0, sgn=sgn: e.activation(out=CT[ri][0:64, k, c0:c0 + 16], in_=ps[0:64, 0:16], func=AF.Copy, scale=sgn),
                         reads=[ps], writes=[CT[ri]])
                    S.op("act", lambda e, k=k, ps=ps, ri=ri, c0=c0, sgn=sgn: e.activation(out=CT[ri][64:128, k, c0 + 16:c0 + 32], in_=ps[64:128, 0:16], func=AF.Copy, scale=sgn),
                         reads=[ps], writes=[CT[ri]])
            S.barrier()
            S.release(braw)
            S.release(craw)
        u16b = [S.sb("s5_u16b%d" % i, [16, 16, 512], BF16, st) for i in range(1)]
        ufm = [S.sb("s5_ufm%d" % i, [128, 2, 512], F32, st) for i in range(2)]
        pr = [S.sb("s5_pr%d" % i, [128, 4, 512], F32, st) for i in range(1)] * 2
        win = [S.sb("s5_win%d" % i, [128, 2, 512], F32, st) for i in range(2)]
        ww = [S.sb("s5_w%d" % i, [128, 2, 512], F32, st) for i in range(2)]
        xr = [S.sb("s5_xr%d" % i, [128, 4, 512], F32, st) for i in range(1)] * 2
        xx = [S.sb("s5_x%d" % i, [128, 2, 512], BF16, st) for i in range(8)] * 2
        wl = S.sb("s5_wl", [128, 2, 8], F32, st)
        cy = [S.sb("s5_cy%d" % i, [128, 2], F32, st) for i in range(2)]
        ct1 = [S.sb("s5_ct%d" % i, [128, 2], F32, st) for i in range(2)]
        yv = [S.sb("s5_yv%d" % i, [128, 512], F32, st) for i in range(2)]
        yg = [S.sb("s5_yg%d" % i, [128, 2, 512], BF16, st) for i in range(2)]
        sgt = [S.sb("s5_sg%d" % i, [128, 512], BF16, st) for i in range(2)]
        ost = [S.sb("s5_o%d" % i, [128, 2, 512], BF16, st) for i in range(2)]
        n = 0
        for i in range(NT):
            t0 = i * 512
            ub_, uf_ = u16b[0], ufm[i % 2]
            S.dma("pool", ub_[:], C.zT[OFF_D:OFF_D + 256, t0:t0 + 512].rearrange("(g c) t -> c g t", c=16), writes=[ub_], sem_tile=ub_)
            S.dma("sp", uf_[:], C.zT[OFF_D:OFF_D + 256, t0:t0 + 512].rearrange("(a p) t -> p a t", p=128), writes=[uf_], sem_tile=uf_)
            for k in range(8):
                pr_, win_, w_, xr_, cy_, ct_ = pr[n % 2], win[n % 2], ww[n % 2], xr[n % 2], cy[n % 2], ct1[n % 2]
                x_ = xx[(i % 2) * 8 + k]
                n += 1
                psr = C.psum()
                psi = C.psum()
                for gl in range(2):
                    g = 2 * k + gl
                    S.op("pe", lambda e, g=g, gl=gl, psr=psr, ub_=ub_: e.matmul(psr[64 * gl:64 * gl + 64, :], lhsT=BT[0][:, g, :], rhs=ub_[:, g, :], start=True, stop=True),
                         reads=[BT[0], ub_], writes=[psr])
                    S.op("pe", lambda e, g=g, gl=gl, psi=psi, ub_=ub_: e.matmul(psi[64 * gl:64 * gl + 64, :], lhsT=BT[1][:, g, :], rhs=ub_[:, g, :], start=True, stop=True),
                         reads=[BT[1], ub_], writes=[psi])
                S.op("dve", lambda e, k=k, psr=psr, pr_=pr_: e.tensor_tensor(out=pr_[:, 0, :], in0=psr[:, :], in1=TiR[:, k, :], op=ALU.mult), reads=[psr, TiR], writes=[pr_])
                S.op("dve", lambda e, k=k, psi=psi, pr_=pr_: e.tensor_tensor(out=pr_[:, 1, :], in0=psi[:, :], in1=TiI[:, k, :], op=ALU.mult), reads=[psi, TiI, pr_], writes=[pr_])
                S.op("dve", lambda e, k=k, psi=psi, pr_=pr_: e.tensor_tensor(out=pr_[:, 2, :], in0=psi[:, :], in1=TiR[:, k, :], op=ALU.mult), reads=[psi, TiR, pr_], writes=[pr_])
                S.op("dve", lambda e, k=k, psr=psr, pr_=pr_: e.tensor_tensor(out=pr_[:, 3, :], in0=psr[:, :], in1=TiI[:, k, :], op=ALU.mult), reads=[psr, TiI, pr_], writes=[pr_])
                S.op("pool", lambda e, pr_=pr_, win_=win_: e.tensor_tensor(out=win_[:, 0, :], in0=pr_[:, 0, :], in1=pr_[:, 1, :], op=ALU.subtract), reads=[pr_], writes=[win_])
                S.op("pool", lambda e, pr_=pr_, win_=win_: e.tensor_tensor(out=win_[:, 1, :], in0=pr_[:, 2, :], in1=pr_[:, 3, :], op=ALU.add), reads=[pr_, win_], writes=[win_])
                if i == 0:
                    S.op("dve", lambda e, cy_=cy_: e.memset(cy_[:], 0.0), writes=[cy_])
                else:
                    S.op("dve", lambda e, k=k, ct_=ct_: e.tensor_scalar(out=ct_[:, 0:1], in0=wl[:, 1, k:k + 1], scalar1=Rim[:, k:k + 1], scalar2=None, op0=ALU.mult),
                         reads=[wl, Rim], writes=[ct_])
                    S.op("dve", lambda e, k=k, ct_=ct_: e.tensor_scalar(out=ct_[:, 1:2], in0=wl[:, 0, k:k + 1], scalar1=Rim[:, k:k + 1], scalar2=None, op0=ALU.mult),
                         reads=[wl, Rim, ct_], writes=[ct_])
                    S.op("dve", lambda e, k=k, ct_=ct_, cy_=cy_: e.scalar_tensor_tensor(out=cy_[:, 0:1], in0=wl[:, 0, k:k + 1], scalar=Rre[:, k:k + 1], in1=ct_[:, 0:1],
                                                                                     op0=ALU.mult, op1=ALU.subtract), reads=[wl, Rre, ct_], writes=[cy_])
                    S.op("dve", lambda e, k=k, ct_=ct_, cy_=cy_: e.scalar_tensor_tensor(out=cy_[:, 1:2], in0=wl[:, 1, k:k + 1], scalar=Rre[:, k:k + 1], in1=ct_[:, 1:2],
                                                                                     op0=ALU.mult, op1=ALU.add), reads=[wl, Rre, ct_, cy_], writes=[cy_])
                for c2 in range(2):
                    S.op("dve", lambda e, k=k, c2=c2, w_=w_, win_=win_, cy_=cy_: e.tensor_tensor_scan(
                        out=w_[:, c2, :], data0=magT[:, k, :], data1=win_[:, c2, :], initial=cy_[:, c2:c2 + 1], op0=ALU.mult, op1=ALU.add),
                        reads=[magT, win_, cy_, w_], writes=[w_])
                S.op("dve", lambda e, k=k, w_=w_: e.tensor_copy(out=wl[:, :, k:k + 1], in_=w_[:, :, 511:512]), reads=[w_, wl], writes=[wl])
                S.op("pool", lambda e, k=k, w_=w_, xr_=xr_: e.tensor_tensor(out=xr_[:, 0, :], in0=w_[:, 0, :], in1=cosT[:, k, :], op=ALU.mult), reads=[w_, cosT], writes=[xr_])
                S.op("pool", lambda e, k=k, w_=w_, xr_=xr_: e.tensor_tensor(out=xr_[:, 1, :], in0=w_[:, 1, :], in1=sinT[:, k, :], op=ALU.mult), reads=[w_, sinT, xr_], writes=[xr_])
                S.op("pool", lambda e, k=k, w_=w_, xr_=xr_: e.tensor_tensor(out=xr_[:, 2, :], in0=w_[:, 0, :], in1=sinT[:, k, :], op=ALU.mult), reads=[w_, sinT, xr_], writes=[xr_])
                S.op("pool", lambda e, k=k, w_=w_, xr_=xr_: e.tensor_tensor(out=xr_[:, 3, :], in0=w_[:, 1, :], in1=cosT[:, k, :], op=ALU.mult), reads=[w_, cosT, xr_], writes=[xr_])
                S.op("pool", lambda e, xr_=xr_, x_=x_: e.tensor_tensor(out=x_[:, 0, :], in0=xr_[:, 0, :], in1=xr_[:, 1, :], op=ALU.subtract), reads=[xr_], writes=[x_])
                S.op("pool", lambda e, xr_=xr_, x_=x_: e.tensor_tensor(out=x_[:, 1, :], in0=xr_[:, 2, :], in1=xr_[:, 3, :], op=ALU.add), reads=[xr_, x_], writes=[x_])
            yg_ = yg[i % 2]
            o_ = ost[i % 2]
            for ct in range(2):
                yv_ = yv[ct]
                psy = C.psum()
                for kk in range(4):
                    k = ct * 4 + kk
                    x_ = xx[(i % 2) * 8 + k]
                    S.op("pe", lambda e, k=k, kk=kk, psy=psy, x_=x_: e.matmul(psy[:, :], lhsT=CT[0][:, k, :], rhs=x_[:, 0, :], start=(kk == 0), stop=False),
                         reads=[CT[0], x_], writes=[psy])
                    S.op("pe", lambda e, k=k, kk=kk, psy=psy, x_=x_: e.matmul(psy[:, :], lhsT=CT[1][:, k, :], rhs=x_[:, 1, :], start=False, stop=(kk == 3)),
                         reads=[CT[1], x_], writes=[psy])
                S.op("dve", lambda e, ct=ct, psy=psy, yv_=yv_, uf_=uf_: e.scalar_tensor_tensor(out=yv_[:], in0=uf_[:, ct, :], scalar=dcol[:, ct:ct + 1], in1=psy[:, :],
                                                                                      op0=ALU.mult, op1=ALU.add), reads=[uf_, dcol, psy], writes=[yv_])
                S.op("act", lambda e, ct=ct, yv_=yv_, yg_=yg_: e.activation(out=yg_[:, ct, :], in_=yv_[:], func=AF.Gelu), reads=[yv_], writes=[yg_])
            for co in range(2):
                sg_ = sgt[co]
                psz = C.psum()
                for ct in range(2):
                    S.op("pe", lambda e, ct=ct, co=co, psz=psz, yg_=yg_: e.matmul(psz[:, :], lhsT=gluw[:, ct, co * 128:(co + 1) * 128], rhs=yg_[:, ct, :],
                                                                          start=(ct == 0), stop=(ct == 1)), reads=[gluw, yg_], writes=[psz])
                S.op("act", lambda e, co=co, psz=psz, sg_=sg_: e.activation(out=sg_[:], in_=psz[:, :], func=AF.Sigmoid, bias=gbcol[:, co:co + 1], scale=1.0),
                     reads=[psz, gbcol], writes=[sg_])
                S.op("pool", lambda e, co=co, sg_=sg_, yg_=yg_, o_=o_: e.tensor_tensor(out=o_[:, co, :], in0=yg_[:, co, :], in1=sg_[:], op=ALU.mult),
                     reads=[sg_, yg_], writes=[o_])
            S.dma("sp", C.oT[768:1024, t0:t0 + 512].rearrange("(a p) t -> p a t", p=128), o_[:], reads=[o_], sem_tile=o_)
        S.barrier()
        for t_ in [gluw, ldt] + u16b + ufm + ost:
            S.release(t_)


_NC_CACHE = {}


def kernel(**inputs):
    x = np.ascontiguousarray(np.asarray(inputs["x"], dtype=np.float32))
    mem = np.ascontiguousarray(np.asarray(inputs["mem"], dtype=np.float32))
    B, T, _ = x.shape
    if T not in _NC_CACHE:
        _NC_CACHE[T] = build(T, nlayers=DEPTH, dbg=False)
    nc = _NC_CACHE[T]
    params = {name: np.ascontiguousarray(np.asarray(inputs[name], dtype=np.float32)) for name, _ in PARAMS}
    n_cores = 8
    in_maps = []
    for c in range(n_cores):
        b = c % B
        m = {"x": x[b], "mem": mem[b]}
        m.update(params)
        in_maps.append(m)
    res = run_bass_kernel_spmd(nc, in_maps, core_ids=list(range(n_cores)))
    out = np.stack([np.asarray(res.results[b]["out"], dtype=np.float32) for b in range(B)], axis=0)
    return out


def phase_moe_sparse(C, l, out_ap):
    S, T, NT, P = C.S, C.T, C.NT, C.P
    NS = T // 128
    BLK = 512
    NB = C.MOE_NB
    w1flat = P["ex_w1"].rearrange("l e k n -> (l e k) n")
    w2flat = P["ex_w2"].rearrange("l e k n -> (l e k) n")
    U32 = mybir.dt.uint32
    with ExitStack() as st:
        g_bc = load_bc(C, st, "ms_g", P["ln3_g"][l], D)
        b_bc = load_bc(C, st, "ms_b", P["ln3_b"][l], D)
        e_all = S.sb("ms_e", [128, NS, 4], F32, st)
        r_all = S.sb("ms_r", [128, NS, 4], F32, st)
        w_all = S.sb("ms_w", [128, NS, 4], F32, st)
        d_all = S.sb("ms_d", [128, NS, 4], F32, st)
        d_int = S.sb("ms_di", [128, NS, 4], I32, st)
        blk = S.sb("ms_blk", [128, NB], F32, st)
        blk2 = S.sb("ms_blk2", [128, NB], F32, st)
        skp = S.sb("ms_skp", [128, NB], F32, st)
        oh_all = S.sb("ms_oh", [128, NB], F32, st)
        widx_f = S.sb("ms_wf", [128, NB, 8], F32, st)
        widx_i = S.sb("ms_wi", [128, NB, 8], I32, st)
        base = [S.sb("ms_base%d" % i, [128, NE], F32, st) for i in range(2)]
        iota_e = S.sb("ms_iotae", [128, NE], F32, st)
        iota_p = S.sb("ms_iotap", [128, 1], F32, st)
        iw = S.sb("ms_iw", [128, 8], F32, st)
        itmp = S.sb("ms_itmp", [128, NE], I32, st)
        Ls = S.sb("ms_Ls", [128, 128], F32, st)
        ones32 = S.sb("ms_ones32", [128, NE], F32, st)
        padf = S.sb("ms_padf", [128, NE], F32, st)
        pend = S.sb("ms_pend", [128, NE], F32, st)
        pstart = S.sb("ms_pstart", [128, NE], F32, st)
        b1all = S.sb("ms_b1", [32, 2048], BF16, st)
        b2all = S.sb("ms_b2", [32, 1024], BF16, st)
        S.dma("pool", b1all[:], P["ex_b1"][l], writes=[b1all], sem_tile=b1all)
        S.dma("pool", b2all[:], P["ex_b2"][l], writes=[b2all], sem_tile=b2all)
        S.op("pool", lambda e: e.iota(itmp[:], pattern=[[1, NE]], base=0, channel_multiplier=0), writes=[itmp])
        S.op("dve", lambda e: e.tensor_copy(out=iota_e[:], in_=itmp[:]), reads=[itmp], writes=[iota_e])
        S.op("pool", lambda e: e.iota(itmp[:, 0:1], pattern=[[1, 1]], base=0, channel_multiplier=1), reads=[iota_e], writes=[itmp])
        S.op("dve", lambda e: e.tensor_copy(out=iota_p[:], in_=itmp[:, 0:1]), reads=[itmp], writes=[iota_p])
        S.op("pool", lambda e: e.iota(itmp[:, 0:8], pattern=[[128, 8]], base=0, channel_multiplier=1), reads=[iota_p], writes=[itmp])
        S.op("dve", lambda e: e.tensor_copy(out=iw[:], in_=itmp[:, 0:8]), reads=[itmp], writes=[iw])
        S.op("pool", lambda e: e.memset(ones32[:], 1.0), writes=[ones32])
        S.op("pool", lambda e: e.memset(base[0][:], 0.0), writes=[base[0]])
        S.op("pool", lambda e: e.affine_select(out=Ls[:], in_=C.ones_f[:], compare_op=ALU.is_gt, fill=0.0, base=0,
                                               pattern=[[1, 128]], channel_multiplier=-1), reads=[C.ones_f], writes=[Ls])
        with ExitStack() as st2:
            rw = S.sb("rt_w", [128, 8, NE], F32, st2)
            S.dma("sp", rw[:], P["router_w"][l].rearrange("(kc p) n -> p kc n", p=128), writes=[rw], sem_tile=rw)
            rb = load_bc(C, st2, "rt_b", P["router_b"][l], NE)
            ht = [S.sb("rt_h%d" % i, [128, D], F32, st2) for i in range(2)]
            hTf = [S.sb("rt_hT%d" % i, [128, 8, 128], F32, st2) for i in range(2)]
            lg = [S.sb("rt_lg%d" % i, [128, NE], F32, st2) for i in range(2)]
            t8 = [S.sb("rt_t8%d" % i, [128, 8], F32, st2) for i in range(2)]
            mk = [S.sb("rt_mk%d" % i, [128, NE], F32, st2) for i in range(2)]
            rg = [S.sb("rt_rg%d" % i, [128, NE], F32, st2) for i in range(2)]
            ew = [S.sb("rt_ew%d" % i, [128, 4], F32, st2) for i in range(2)]
            sm = [S.sb("rt_sm%d" % i, [128, 2], F32, st2) for i in range(2)]
            tq = [S.sb("rt_tq%d" % i, [128, NE], F32, st2) for i in range(4)]
            ntq = 0
            for n in range(NS):
                h, hf, lg_, t8_, mk_, rg_, ew_, sm_ = ht[n % 2], hTf[n % 2], lg[n % 2], t8[n % 2], mk[n % 2], rg[n % 2], ew[n % 2], sm[n % 2]
                bc_, bn_ = base[n % 2], base[(n + 1) % 2]
                S.dma("sp", h[:], C.h_tok[n * 128:(n + 1) * 128, :], writes=[h], sem_tile=h)
                transpose_to(C, h[:], h, hf, lambda g, hf=hf: hf[:, g * 4:(g + 1) * 4, :])
                ps = C.psum()
                for kc in range(8):
                    S.op("pe", lambda e, kc=kc, ps=ps, hf=hf: e.matmul(ps[:, 0:NE], lhsT=hf[:, kc, :], rhs=rw[:, kc, :],
                                                                        start=(kc == 0), stop=(kc == 7)), reads=[hf, rw], writes=[ps])
                S.op("dve", lambda e: e.tensor_tensor(out=lg_[:], in0=ps[:, 0:NE], in1=rb[:], op=ALU.add), reads=[ps, rb], writes=[lg_])
                S.op("dve", lambda e: e.max(out=t8_[:], in_=lg_[:]), reads=[lg_], writes=[t8_])
                S.op("dve", lambda e: e.tensor_scalar(out=mk_[:], in0=lg_[:], scalar1=t8_[:, 3:4], scalar2=None, op0=ALU.is_ge), reads=[lg_, t8_], writes=[mk_])
                S.op("dve", lambda e: e.tensor_scalar(out=sm_[:, 0:1], in0=t8_[:, 0:1], scalar1=-1.0, scalar2=None, op0=ALU.mult), reads=[t8_], writes=[sm_])
                S.op("act", lambda e: e.activation(out=ew_[:], in_=t8_[:, 0:4], func=AF.Exp, bias=sm_[:, 0:1], scale=1.0), reads=[t8_, sm_], writes=[ew_])
                S.op("dve", lambda e: e.reduce_sum(out=sm_[:, 1:2], in_=ew_[:], axis=AX.X), reads=[ew_, sm_], writes=[sm_])
                S.op("dve", lambda e: e.reciprocal(out=sm_[:, 1:2], in_=sm_[:, 1:2]), reads=[sm_], writes=[sm_])
                S.op("dve", lambda e: e.tensor_scalar(out=w_all[:, n, :], in0=ew_[:], scalar1=sm_[:, 1:2], scalar2=None, op0=ALU.mult), reads=[ew_, sm_], writes=[w_all])
                ps2 = C.psum()
                S.op("pe", lambda e: e.matmul(ps2[:, 0:NE], lhsT=Ls[:, :], rhs=mk_[:, :], start=True, stop=True), reads=[Ls, mk_], writes=[ps2])
                S.op("pe", lambda e: e.matmul(ps2[:, NE:2 * NE], lhsT=C.ones_f[:, :], rhs=mk_[:, :], start=True, stop=True), reads=[C.ones_f, mk_], writes=[ps2])
                S.op("dve", lambda e: e.tensor_tensor(out=rg_[:], in0=ps2[:, 0:NE], in1=bc_[:], op=ALU.add), reads=[ps2, bc_], writes=[rg_])
                S.op("dve", lambda e: e.tensor_tensor(out=bn_[:], in0=ps2[:, NE:2 * NE], in1=bc_[:], op=ALU.add), reads=[ps2, bc_], writes=[bn_])
                for j in range(4):
                    ta, tb_ = tq[ntq % 4], tq[(ntq + 1) % 4]
                    ntq += 2
                    S.op("dve", lambda e: e.scalar_tensor_tensor(out=ta[:], in0=lg_[:], scalar=t8_[:, j:j + 1], in1=iota_e[:], op0=ALU.is_equal, op1=ALU.mult),
                         reads=[lg_, t8_, iota_e], writes=[ta])
                    S.op("dve", lambda e: e.reduce_sum(out=e_all[:, n, j:j + 1], in_=ta[:], axis=AX.X), reads=[ta], writes=[e_all])
                    S.op("dve", lambda e: e.scalar_tensor_tensor(out=tb_[:], in0=lg_[:], scalar=t8_[:, j:j + 1], in1=rg_[:], op0=ALU.is_equal, op1=ALU.mult),
                         reads=[lg_, t8_, rg_], writes=[tb_])
                    S.op("dve", lambda e: e.reduce_sum(out=r_all[:, n, j:j + 1], in_=tb_[:], axis=AX.X), reads=[tb_], writes=[r_all])
            cnt = base[NS % 2]
            S.op("dve", lambda e: e.tensor_scalar(out=padf[:], in0=cnt[:], scalar1=float(BLK - 1), scalar2=None, op0=ALU.add), reads=[cnt], writes=[padf])
            S.op("dve", lambda e: e.tensor_copy(out=itmp[:], in_=padf[:]), reads=[padf], writes=[itmp])
            S.op("dve", lambda e: e.tensor_scalar(out=itmp[:], in0=itmp[:], scalar1=9, scalar2=9, op0=ALU.arith_shift_right, op1=ALU.logical_shift_left),
                 reads=[itmp], writes=[itmp])
            S.op("dve", lambda e: e.tensor_copy(out=padf[:], in_=itmp[:]), reads=[itmp], writes=[padf])
            S.op("dve", lambda e: e.tensor_tensor_scan(out=pend[:], data0=ones32[:], data1=padf[:], initial=0.0, op0=ALU.mult, op1=ALU.add),
                 reads=[ones32, padf], writes=[pend])
            S.op("dve", lambda e: e.tensor_tensor(out=pstart[:], in0=pend[:], in1=padf[:], op=ALU.subtract), reads=[pend, padf], writes=[pstart])
            for n in range(NS):
                for j in range(4):
                    ta = tq[ntq % 4]
                    ntq += 1
                    S.op("dve", lambda e: e.scalar_tensor_tensor(out=ta[:], in0=iota_e[:], scalar=e_all[:, n, j:j + 1], in1=pstart[:], op0=ALU.is_equal, op1=ALU.mult),
                         reads=[iota_e, e_all, pstart], writes=[ta])
                    S.op("dve", lambda e: e.reduce_sum(out=d_all[:, n, j:j + 1], in_=ta[:], axis=AX.X), reads=[ta], writes=[d_all])
            fl3 = lambda t: t[:, :, :].rearrange("p a b -> p (a b)")
            S.op("dve", lambda e: e.tensor_tensor(out=fl3(d_all), in0=fl3(d_all), in1=fl3(r_all), op=ALU.add), reads=[d_all, r_all], writes=[d_all])
            S.op("dve", lambda e: e.tensor_copy(out=fl3(d_int), in_=fl3(d_all)), reads=[d_all], writes=[d_int])
            for b in range(NB):
                ta = tq[ntq % 4]
                ntq += 1
                S.op("dve", lambda e: e.tensor_scalar(out=ta[:], in0=pend[:], scalar1=float(b * BLK), scalar2=None, op0=ALU.is_le), reads=[pend], writes=[ta])
                S.op("dve", lambda e: e.reduce_sum(out=blk[:, b:b + 1], in_=ta[:], axis=AX.X), reads=[ta], writes=[blk])
            S.op("dve", lambda e: e.tensor_scalar(out=blk[:], in0=blk[:], scalar1=float(NE - 1), scalar2=None, op0=ALU.min), reads=[blk], writes=[blk])
            S.op("dve", lambda e: e.tensor_scalar(out=oh_all[:], in0=blk[:], scalar1=iota_p[:, 0:1], scalar2=None, op0=ALU.is_equal), reads=[blk, iota_p], writes=[oh_all])
            S.op("dve", lambda e: e.tensor_scalar(out=blk2[:], in0=blk[:], scalar1=1024.0, scalar2=float(l * NE * 1024), op0=ALU.mult, op1=ALU.add),
                 reads=[blk], writes=[blk2])
            S.op("dve", lambda e: e.memset(skp[:], 0.0), writes=[skp])
            S.op("dve", lambda e: e.tensor_tensor(out=skp[:, 2:NB], in0=blk[:, 2:NB], in1=blk[:, 0:NB - 2], op=ALU.is_equal), reads=[blk, skp], writes=[skp])
            S.op("dve", lambda e: e.scalar_tensor_tensor(out=blk2[:], in0=skp[:], scalar=float(1 << 22), in1=blk2[:], op0=ALU.mult, op1=ALU.add),
                 reads=[skp, blk2], writes=[blk2])
            for b in range(NB):
                S.op("dve", lambda e: e.tensor_scalar(out=widx_f[:, b, :], in0=iw[:], scalar1=blk2[:, b:b + 1], scalar2=None, op0=ALU.add),
                     reads=[iw, blk2], writes=[widx_f])
            S.op("dve", lambda e: e.tensor_copy(out=fl3(widx_i), in_=fl3(widx_f)), reads=[widx_f], writes=[widx_i])
            for n in range(NS):
                h = ht[n % 2]
                S.dma("sp", h[:], C.h_tok[n * 128:(n + 1) * 128, :], writes=[h], sem_tile=h)
                for j in range(4):
                    S.dma_fn("pool", lambda e: e.indirect_dma_start(
                        out=C.xs_d, out_offset=bass.IndirectOffsetOnAxis(d_int[:, n, j:j + 1].bitcast(U32), 0), in_=h[:], in_offset=None),
                        reads=[h, d_int], sem_tile=h)
            S.barrier()
            for t_ in [rw, rb] + ht:
                S.release(t_)
        with ExitStack() as st2:
            w1t = [S.sb("mo_w1%d" % i, [128, 8, 2048], BF16, st2) for i in range(2)]
            w2t = [S.sb("mo_w2%d" % i, [128, 8, 1024], BF16, st2) for i in range(2)]
            xsb = S.sb("mo_xs", [128, 4, 1024], F32, st2)
            ysb = S.sb("mo_ys", [128, 4, 1024], F32, st2)
            xT = [S.sb("mo_xT%d" % i, [128, 8, 512], BF16, st2) for i in range(2)]
            actT = [S.sb("mo_act%d" % i, [128, 8, 512], BF16, st2) for i in range(2)]
            ohb = [S.sb("mo_ohb%d" % i, [32, 512], BF16, st2) for i in range(2)]
            ones_r = S.sb("mo_onesr", [32, 512], F32, st2)
            S.op("pool", lambda e: e.memset(ones_r[:], 1.0), writes=[ones_r])
            gq = [S.sb("mo_gq%d" % i, [128, 512], F32, st2) for i in range(2)]
            sg = [S.sb("mo_sg%d" % i, [128, 512], F32, st2) for i in range(2)]
            uq = [S.sb("mo_uq%d" % i, [128, 512], F32, st2) for i in range(2)]
            nq = 0
            for b in range(NB):
                w1_, w2_, x_, a, oh_ = w1t[b % 2], w2t[b % 2], xT[b % 2], actT[b % 2], ohb[b % 2]
                for kc in range(8):
                    S.dma_fn("pool", lambda e: e.indirect_dma_start(
                        out=w1_[:, kc, :], out_offset=None, in_=w1flat, in_offset=bass.IndirectOffsetOnAxis(widx_i[:, b, kc:kc + 1].bitcast(U32), 0),
                        bounds_check=RegConst(2 * NE * 1024 - 1), oob_is_err=False),
                        reads=[widx_i], writes=[w1_], sem_tile=w1_)
                for kc in range(8):
                    S.dma_fn("pool", lambda e: e.indirect_dma_start(
                        out=w2_[:, kc, :], out_offset=None, in_=w2flat, in_offset=bass.IndirectOffsetOnAxis(widx_i[:, b, kc:kc + 1].bitcast(U32), 0),
                        bounds_check=RegConst(2 * NE * 1024 - 1), oob_is_err=False),
                        reads=[widx_i], writes=[w2_], sem_tile=w2_)
                S.dma("sp", xsb[:], C.xs_d[b * BLK:(b + 1) * BLK, :].rearrange("(s p) c -> p s c", p=128), writes=[xsb], sem_tile=xsb)
                for s4 in range(4):
                    transpose_to(C, xsb[:, s4, :], xsb, x_, lambda g, s4=s4, x_=x_: x_[:, g * 4:(g + 1) * 4, s4 * 128:(s4 + 1) * 128])
                S.op("dve", lambda e: e.tensor_scalar(out=oh_[:], in0=ones_r[:], scalar1=oh_all[0:32, b:b + 1], scalar2=None, op0=ALU.mult),
                     reads=[ones_r, oh_all], writes=[oh_])
                for ft in range(8):
                    g_, s_, u_ = gq[nq % 2], sg[nq % 2], uq[nq % 2]
                    nq += 1
                    psg = C.psum()
                    S.op("pe", lambda e: e.matmul(psg[:, :], lhsT=b1all[0:32, ft * 256:(ft + 1) * 256:2], rhs=oh_[:, :], start=True, stop=False),
                         reads=[b1all, oh_], writes=[psg])
                    for kc in range(8):
                        S.op("pe", lambda e: e.matmul(psg[:, :], lhsT=w1_[:, kc, ft * 256:(ft + 1) * 256:2], rhs=x_[:, kc, :], start=False, stop=(kc == 7)),
                             reads=[w1_, x_], writes=[psg])
                    psu = C.psum()
                    S.op("pe", lambda e: e.matmul(psu[:, :], lhsT=b1all[0:32, ft * 256 + 1:(ft + 1) * 256:2], rhs=oh_[:, :], start=True, stop=False),
                         reads=[b1all, oh_], writes=[psu])
                    for kc in range(8):
                        S.op("pe", lambda e: e.matmul(psu[:, :], lhsT=w1_[:, kc, ft * 256 + 1:(ft + 1) * 256:2], rhs=x_[:, kc, :], start=False, stop=(kc == 7)),
                             reads=[w1_, x_], writes=[psu])
                    S.op("dve", lambda e: e.tensor_scalar(out=g_[:], in0=psg[:, :], scalar1=7.0, scalar2=None, op0=ALU.min), reads=[psg], writes=[g_])
                    S.op("act", lambda e: e.activation(out=s_[:], in_=g_[:], func=AF.Sigmoid, scale=1.702), reads=[g_], writes=[s_])
                    S.op("dve", lambda e: e.tensor_scalar(out=u_[:], in0=psu[:, :], scalar1=7.0, scalar2=-7.0, op0=ALU.min, op1=ALU.max), reads=[psu], writes=[u_])
                    S.op("dve", lambda e: e.tensor_tensor(out=s_[:], in0=g_[:], in1=s_[:], op=ALU.mult), reads=[g_, s_], writes=[s_])
                    S.op("dve", lambda e: e.scalar_tensor_tensor(out=a[:, ft, :], in0=u_[:], scalar=1.0, in1=s_[:], op0=ALU.add, op1=ALU.mult),
                         reads=[s_, u_], writes=[a])
                for s4 in range(4):
                    for half in range(2):
                        ps = C.psum()
                        S.op("pe", lambda e: e.matmul(ps[:, :], lhsT=oh_[:, 0:128], rhs=b2all[0:32, half * 512:(half + 1) * 512], start=True, stop=False),
                             reads=[oh_, b2all], writes=[ps])
                        for ft in range(8):
                            S.op("pe", lambda e: e.matmul(ps[:, :], lhsT=a[:, ft, s4 * 128:(s4 + 1) * 128], rhs=w2_[:, ft, half * 512:(half + 1) * 512],
                                                          start=False, stop=(ft == 7)), reads=[a, w2_], writes=[ps])
                        en = evac_eng(C)
                        S.op(en, copy_op(en, ysb[:, s4, half * 512:(half + 1) * 512], ps[:, :]), reads=[ps], writes=[ysb])
                S.dma("sp", C.ys_d[b * BLK:(b + 1) * BLK, :].rearrange("(s p) c -> p s c", p=128), ysb[:], reads=[ysb], sem_tile=ysb)
            S.barrier()
            for t_ in w1t + w2t + [xsb, ysb]:
                S.release(t_)
        with ExitStack() as st2:
            acc = [S.sb("mc_acc%d" % i, [128, D], F32, st2) for i in range(2)]
            gj = [S.sb("mc_g%d" % i, [128, D], F32, st2) for i in range(4)]
            hTs = [S.sb("mc_hT%d" % i, [128, 8, 512], BF16, st2) for i in range(2)]
            ng = 0
            for n in range(NS):
                a_ = acc[n % 2]
                S.dma("sp", a_[:], C.h_tok[n * 128:(n + 1) * 128, :], writes=[a_], sem_tile=a_)
                S.op("act", lambda e: e.activation(out=a_[:], in_=a_[:], func=AF.Copy, scale=DN_ALPHA), reads=[a_], writes=[a_])
                for j in range(4):
                    g_ = gj[ng % 4]
                    ng += 1
                    S.dma_fn("pool", lambda e: e.indirect_dma_start(
                        out=g_[:], out_offset=None, in_=C.ys_d, in_offset=bass.IndirectOffsetOnAxis(d_int[:, n, j:j + 1].bitcast(U32), 0)),
                        reads=[d_int], writes=[g_], sem_tile=g_)
                    S.op("dve", lambda e: e.scalar_tensor_tensor(out=a_[:], in0=g_[:], scalar=w_all[:, n, j:j + 1], in1=a_[:], op0=ALU.mult, op1=ALU.add),
                         reads=[g_, w_all, a_], writes=[a_])
                ln_tile(C, (a_[:], a_), (a_[:], a_), g_bc, b_bc)
                store_h(C, st2, a_, hTs[(n // 4) % 2], n // 4, n % 4, out_ap)
            S.barrier()
            for t_ in acc + gj + hTs:
                S.release(t_)
        for t_ in [g_bc, b_bc, b1all, b2all]:
            S.release(t_)
```

```python
import math
from contextlib import ExitStack
import numpy as np
import concourse.bass as bass
import concourse.mybir as mybir
from concourse.bass_utils import run_bass_kernel_spmd

F32 = mybir.dt.float32
BF16 = mybir.dt.bfloat16
I32 = mybir.dt.int32
AF = mybir.ActivationFunctionType
ALU = mybir.AluOpType
AX = mybir.AxisListType

ENGS = ("pe", "dve", "act", "pool", "sp")
STORE_Q = "act"


class Tl:
    __slots__ = ("name", "t", "w", "r", "dkey")

    def __init__(self, name, t=None):
        self.name = name
        self.t = t
        self.w = None
        self.r = {}
        self.dkey = None

    def __getitem__(self, idx):
        return self.t[idx]


class _Proxy:
    def __init__(self):
        self.call = None

    def __getattr__(self, name):
        def rec(*a, **k):
            assert self.call is None
            self.call = (name, a, k)
        return rec


class RegConst:
    cache = {}

    def __init__(self, v):
        self.v = v

    def get(self, e):
        key = (id(e), self.v)
        if key not in RegConst.cache:
            RegConst.cache[key] = e.to_reg(self.v)
        return RegConst.cache[key]


def _record(fn):
    p = _Proxy()
    fn(p)
    name, a, k = p.call

    def run(e):
        k2 = {kk: (vv.get(e) if isinstance(vv, RegConst) else vv) for kk, vv in k.items()}
        return getattr(e, name)(*a, **k2)
    return run


class Sched:
    def __init__(self, nc, es):
        self.nc = nc
        self.es = es
        self.q = {e: [] for e in ENGS}
        self.cnt = {e: 0 for e in ENGS}
        self.seen = {e: {} for e in ENGS}
        self.sem = {}
        for e in ENGS:
            self.sem[e] = es.enter_context(nc.semaphore("c_" + e))
        self.dtot = {}
        self.ndsem = 0
        self.free_dsems = []

    def sb(self, name, shape, dt, st=None):
        self.uid = getattr(self, "uid", 0) + 1
        name = "t%d_%s" % (self.uid, name)
        t = (st or self.es).enter_context(self.nc.sbuf_tensor(name, list(shape), dt))
        return Tl(name, t)

    def ps(self, name, shape, dt=F32, st=None):
        name = "pp_" + name
        t = (st or self.es).enter_context(self.nc.psum_tensor(name, list(shape), dt))
        return Tl(name, t)

    def res(self, name):
        return Tl(name, None)

    def _dsem(self, tl):
        if tl.dkey is None:
            if self.free_dsems:
                tl.dkey = self.free_dsems.pop()
            else:
                k = "d%d" % self.ndsem
                self.ndsem += 1
                self.sem[k] = self.es.enter_context(self.nc.semaphore(k))
                self.dtot[k] = 0
                tl.dkey = k
        return tl.dkey

    def release(self, tl):
        if tl.dkey is not None:
            self.free_dsems.append(tl.dkey)
            tl.dkey = None

    def _waits(self, eng, reads, writes):
        waits = {}
        seen = self.seen[eng]

        def need(ev):
            if ev is None:
                return
            k, v = ev
            if k in self.dtot:
                v = self.dtot[k]
            elif k == eng and eng in ("pe", "sp"):
                return
            if seen.get(k, 0) < v and waits.get(k, 0) < v:
                waits[k] = v

        for t in reads:
            need(t.w)
        for t in writes:
            need(t.w)
            for k, v in t.r.items():
                need((k, v))
        for k, v in waits.items():
            seen[k] = v
        return list(waits.items())

    def _mark(self, ev, reads, writes):
        k, v = ev
        for t in reads:
            if t.r.get(k, 0) < v:
                t.r[k] = v
        for t in writes:
            t.w = ev
            t.r = {}

    def op(self, eng, fn, reads=(), writes=()):
        fn = _record(fn)
        waits = self._waits(eng, reads, writes)
        self.cnt[eng] += 1
        ev = (eng, self.cnt[eng])
        self._mark(ev, reads, writes)
        self.q[eng].append((waits, fn, (eng, 1)))

    def dma(self, q, out, in_, reads=(), writes=(), sem_tile=None, **kw):
        if q == "sp" and not writes:
            q = STORE_Q
        waits = self._waits(q, reads, writes)
        k = self._dsem(sem_tile)
        self.dtot[k] += 16
        ev = (k, self.dtot[k])
        self._mark(ev, reads, writes)
        self.q[q].append((waits, lambda e, out=out, in_=in_, kw=kw: e.dma_start(out=out, in_=in_, **kw), (k, 16)))

    def dma_fn(self, q, fn, reads=(), writes=(), sem_tile=None):
        waits = self._waits(q, reads, writes)
        k = self._dsem(sem_tile)
        self.dtot[k] += 16
        ev = (k, self.dtot[k])
        self._mark(ev, reads, writes)
        self.q[q].append((waits, _record(fn), (k, 16)))

    def barrier(self):
        waits = []
        seen = self.seen["sp"]
        for e in ENGS:
            if e != "sp" and seen.get(e, 0) < self.cnt[e]:
                waits.append((e, self.cnt[e]))
                seen[e] = self.cnt[e]
        for k, v in self.dtot.items():
            if seen.get(k, 0) < v:
                waits.append((k, v))
                seen[k] = v
        self.cnt["sp"] += 1
        ev = ("sp", self.cnt["sp"])
        self.q["sp"].append((waits, lambda e: e.nop(), ("sp", 1)))
        for e in ENGS:
            if e != "sp":
                self.q[e].append(([ev], None, None))
                self.seen[e]["sp"] = ev[1]
                for k, v in self.dtot.items():
                    self.seen[e][k] = v
                for e2 in ENGS:
                    self.seen[e][e2] = max(self.seen[e].get(e2, 0), self.cnt[e2])

    def emit(self):
        nc = self.nc
        self.barrier()
        with nc.Block() as block:
            def run(engname):
                def body(eng):
                    for waits, fn, inc in self.q[engname]:
                        for k, v in waits:
                            eng.wait_ge(self.sem[k], v)
                        if fn is not None:
                            ins = fn(eng)
                            ins.then_inc(self.sem[inc[0]], inc[1])
                return body
            block.tensor(run("pe"))
            block.vector(run("dve"))
            block.scalar(run("act"))
            block.gpsimd(run("pool"))
            block.sync(run("sp"))


D = 1024
NMEM = 256
OFF_A, OFF_B, OFF_C, OFF_D, OFF_G = 0, 768, 1280, 2304, 2560
N_IN = 6656
NE = 32
LN_EPS = 1e-5
DEPTH = 2
DN_ALPHA = (2 * DEPTH) ** 0.25

PARAMS = [
    ("ln_in_g", (D,)), ("ln_in_b", (D,)), ("w_in", (2, D, N_IN)), ("conv_w", (2, 3, 256)),
    ("sg_norm_g", (2, 256)), ("sg_norm_b", (2, 256)), ("sg_w", (2, 4, 128, 128)), ("sg_b", (2, 4, 128)),
    ("rw_mu", (2, 1024)), ("rw_w0", (2, 256)), ("rw_w_up", (2, 64, 256)), ("rw_a0", (2, 256)),
    ("rw_a_up", (2, 64, 256)), ("rw_g_up", (2, 128, 256)), ("rw_k_k", (2, 256)), ("rw_k_a", (2, 256)),
    ("rw_r_k", (2, 4, 64)), ("rw_ln_g", (2, 256)), ("rw_ln_b", (2, 256)),
    ("s5_a_re", (2, 16, 64)), ("s5_a_im", (2, 16, 64)), ("s5_b_re", (2, 16, 64, 16)), ("s5_b_im", (2, 16, 64, 16)),
    ("s5_c_re", (2, 16, 16, 64)), ("s5_c_im", (2, 16, 16, 64)), ("s5_d", (2, 256)), ("s5_log_dt", (2, 16)),
    ("s5_glu_w", (2, 256, 256)), ("s5_glu_b", (2, 256)), ("br_proj", (2, 4, 256, D)), ("gate_b", (2, 4, D)),
    ("w_out", (2, D, D)), ("ln1_g", (2, D)), ("ln1_b", (2, D)), ("xa_wq", (2, D, D)), ("xa_wk", (2, D, D)),
    ("xa_wv", (2, D, D)), ("xa_wo", (2, D, D)), ("ln2_g", (2, D)), ("ln2_b", (2, D)),
    ("router_w", (2, D, NE)), ("router_b", (2, NE)), ("ex_w1", (2, NE, D, 2 * D)), ("ex_b1", (2, NE, 2 * D)),
    ("ex_w2", (2, NE, D, D)), ("ex_b2", (2, NE, D)), ("ln3_g", (2, D)), ("ln3_b", (2, D)),
]


def col(ap1d):
    return ap1d.rearrange("(p o) -> p o", o=1)


class Ctx:
    pass


def build(T, nlayers=2, dbg=False, phases=None):
    assert T % 512 == 0
    NT = T // 512
    nc = bass.Bass("TRN2", target_bir_lowering=False)
    RegConst.cache = {}
    P = {}
    x_in = nc.dram_tensor("x", [T, D], F32, kind="ExternalInput").ap()
    mem_in = nc.dram_tensor("mem", [NMEM, D], F32, kind="ExternalInput").ap()
    for name, shp in PARAMS:
        P[name] = nc.dram_tensor(name, list(shp), F32, kind="ExternalInput").ap()
    out = nc.dram_tensor("out", [T, D], F32, kind="ExternalOutput").ap()
    skind = "ExternalOutput" if dbg else "Internal"

    def scr(name, shape, dt):
        return nc.dram_tensor(name, list(shape), dt, kind=skind).ap()

    h_tok = scr("h_tok", [T, D], F32)
    hT = scr("hT", [D, T], BF16)
    zT = scr("zT", [OFF_G, T], F32)
    zbv = scr("zbv", [T, 256], F32)
    gT = scr("gT", [4 * D, T], BF16)
    oT = scr("oT", [4 * 256, T], BF16)
    gate_d = scr("gate_d", [T, NE], F32)
    MOE_BLK = 512
    MOE_NB = (T * 4) // MOE_BLK + NE
    xs_d = scr("xs_d", [MOE_NB * MOE_BLK, D], F32)
    ys_d = scr("ys_d", [MOE_NB * MOE_BLK, D], F32)

    with ExitStack() as es:
        S = Sched(nc, es)
        C = Ctx()
        C.nc, C.S, C.P, C.T, C.NT = nc, S, P, T, NT
        C.ident = S.sb("ident", [128, 128], F32)
        S.op("pool", lambda e: e.memset(C.ident[:], 0.0), writes=[C.ident])
        S.op("pool", lambda e: e.affine_select(out=C.ident[:], in_=C.ident[:], compare_op=ALU.not_equal, fill=1.0,
                                               base=0, pattern=[[-1, 128]], channel_multiplier=1),
             reads=[C.ident], writes=[C.ident])
        C.ones_b = S.sb("ones_b", [128, 128], BF16)
        S.op("pool", lambda e: e.memset(C.ones_b[:], 1.0), writes=[C.ones_b])
        C.ones_f = S.sb("ones_f", [128, 128], F32)
        S.op("pool", lambda e: e.memset(C.ones_f[:], 1.0), writes=[C.ones_f])
        C.rweps = S.sb("rweps", [128, 1], F32)
        S.op("pool", lambda e: e.memset(C.rweps[:], 64e-5), writes=[C.rweps])
        C.psl = [S.ps("ps%d" % i, [128, 512], F32) for i in range(8)]
        C.psi = 0

        def psum():
            t = C.psl[C.psi % 8]
            C.psi += 1
            return t
        C.psum = psum
        C.lnst = [S.sb("lnst%d" % i, [128, 2, 6], F32) for i in range(2)]
        C.lnmv = [S.sb("lnmv%d" % i, [128, 2], F32) for i in range(2)]
        C.lnrs = [S.sb("lnrs%d" % i, [128, 1], F32) for i in range(2)]
        C.lni = 0
        C.evi = 0
        C.h_tok, C.hT, C.zT, C.zbv, C.gT, C.oT, C.gate_d = h_tok, hT, zT, zbv, gT, oT, gate_d
        C.x_in, C.mem_in, C.out = x_in, mem_in, out
        C.xs_d, C.ys_d, C.MOE_NB = xs_d, ys_d, MOE_NB

        ph = phases
        for l in range(nlayers):
            last = (l == nlayers - 1)
            if l == 0:
                phase_ln_in(C)
                S.barrier()
            if ph is None or "inproj" in ph:
                phase_inproj(C, l)
                S.barrier()
            if ph is None or "conv" in ph:
                phase_conv(C, l)
                S.barrier()
            if ph is None or "sgu" in ph:
                phase_sgu(C, l)
                S.barrier()
            if ph is None or "rwkv" in ph:
                phase_rwkv(C, l)
                S.barrier()
            if ph is None or "s5" in ph:
                phase_s5(C, l)
                S.barrier()
            if ph is None or "merge" in ph:
                phase_merge(C, l)
                S.barrier()
            if ph is None or "attn" in ph:
                phase_attn(C, l)
                S.barrier()
            if ph is None or "moe" in ph:
                phase_moe_sparse(C, l, C.out if last else None)
                S.barrier()
        S.emit()
    return nc


def evac_eng(C):
    C.evi += 1
    return "act" if C.evi % 2 else "dve"


def copy_op(eng_name, out, in_):
    if eng_name == "act":
        return lambda e: e.copy(out=out, in_=in_)
    return lambda e: e.tensor_copy(out=out, in_=in_)


def load_bc(C, st, name, ap1d, n, q="sp"):
    S = C.S
    t = S.sb(name, [128, n], F32, st)
    S.dma(q, t[:], ap1d.partition_broadcast(128), writes=[t], sem_tile=t)
    return t


def ln_tile(C, src, dst, g_bc, b_bc, eps=LN_EPS):
    S = C.S
    i = C.lni % 2
    C.lni += 1
    st, mv, rs = C.lnst[i], C.lnmv[i], C.lnrs[i]
    sa, da = src[0], dst[0]
    srct, dstt = src[1], dst[1]
    S.op("dve", lambda e: e.bn_stats(out=st[:, 0, :], in_=sa[:, 0:512]), reads=[srct], writes=[st])
    S.op("dve", lambda e: e.bn_stats(out=st[:, 1, :], in_=sa[:, 512:1024]), reads=[srct, st], writes=[st])
    S.op("dve", lambda e: e.bn_aggr(out=mv[:], in_=st[:].rearrange("p a b -> p (a b)")), reads=[st], writes=[mv])
    S.op("act", lambda e: e.activation(out=rs[:], in_=mv[:, 1:2], func=AF.Sqrt, bias=C.eps_col[:, 0:1], scale=1.0),
         reads=[mv, C.eps_col], writes=[rs])
    S.op("dve", lambda e: e.reciprocal(out=rs[:], in_=rs[:]), reads=[rs], writes=[rs])
    S.op("dve", lambda e: e.scalar_tensor_tensor(out=da, in0=sa, scalar=mv[:, 0:1], in1=g_bc[:], op0=ALU.subtract, op1=ALU.mult),
         reads=[srct, mv, g_bc], writes=[dstt])
    S.op("dve", lambda e: e.scalar_tensor_tensor(out=da, in0=da, scalar=rs[:, 0:1], in1=b_bc[:], op0=ALU.mult, op1=ALU.add),
         reads=[dstt, rs, b_bc], writes=[dstt])


def transpose_to(C, src_ap, src_t, dst_t, dst_fn, nblk=8):
    S = C.S
    for g in range(nblk // 4):
        ps = C.psum()
        for j in range(4):
            kc = g * 4 + j
            S.op("pe", lambda e, kc=kc, j=j, ps=ps: e.transpose(out=ps[:, j * 128:(j + 1) * 128],
                                                                 in_=src_ap[:, kc * 128:(kc + 1) * 128],
                                                                 identity=C.ident[:]),
                 reads=[src_t, C.ident], writes=[ps])
        en = evac_eng(C)
        S.op(en, copy_op(en, dst_fn(g), ps[:, :].rearrange("p (a b) -> p a b", a=4)), reads=[ps], writes=[dst_t])


def store_h(C, st, hts, hTs, tile_i, sub, out_ap=None):
    S = C.S
    tok0 = tile_i * 512 + sub * 128
    S.dma("sp", C.h_tok[tok0:tok0 + 128, :], hts[:], reads=[hts], sem_tile=hts)
    if out_ap is not None:
        S.dma("sp", out_ap[tok0:tok0 + 128, :], hts[:], reads=[hts], sem_tile=hts)
    transpose_to(C, hts[:], hts, hTs, lambda g: hTs[:, g * 4:(g + 1) * 4, sub * 128:(sub + 1) * 128])
    if sub == 3:
        S.dma("sp", C.hT.rearrange("(kc p) t -> p kc t", p=128)[:, :, tile_i * 512:(tile_i + 1) * 512], hTs[:],
              reads=[hTs], sem_tile=hTs)


def load_w_bf16(C, t, w_ap, q="pool"):
    C.S.dma(q, t[:], w_ap.rearrange("(kc p) n -> p kc n", p=128), writes=[t], sem_tile=t)


def phase_ln_in(C):
    S, T, NT = C.S, C.T, C.NT
    with ExitStack() as st:
        C.eps_col = S.sb("eps_col", [128, 1], F32)
        g_bc = load_bc(C, st, "lnin_g", C.P["ln_in_g"], D)
        b_bc = load_bc(C, st, "lnin_b", C.P["ln_in_b"], D)
        xt = [S.sb("lnin_x%d" % i, [128, D], F32, st) for i in range(2)]
        hTs = [S.sb("lnin_hT%d" % i, [128, 8, 512], BF16, st) for i in range(2)]
        S.op("pool", lambda e: e.memset(C.eps_col[:], LN_EPS), writes=[C.eps_col])
        n = 0
        for i in range(NT):
            for sub in range(4):
                x = xt[n % 2]
                n += 1
                tok0 = i * 512 + sub * 128
                S.dma("sp", x[:], C.x_in[tok0:tok0 + 128, :], writes=[x], sem_tile=x)
                ln_tile(C, (x[:], x), (x[:], x), g_bc, b_bc)
                store_h(C, st, x, hTs[i % 2], i, sub)
        S.barrier()
        for t in [g_bc, b_bc] + xt + hTs:
            S.release(t)


def load_cols(C, st, name, ap_rows, n, q="sp", m=128):
    S = C.S
    tmp = S.sb(name + "_r", [n, m], F32, st)
    res = S.sb(name, [m, n], F32, st)
    S.dma(q, tmp[:], ap_rows, writes=[tmp], sem_tile=tmp)
    ps = C.psum()
    S.op("pe", lambda e: e.transpose(out=ps[0:m, 0:n], in_=tmp[:, :], identity=C.ident[0:n, 0:n]),
         reads=[tmp, C.ident], writes=[ps])
    S.op("dve", lambda e: e.tensor_copy(out=res[:], in_=ps[0:m, 0:n]), reads=[ps], writes=[res])
    C.S.release_later = getattr(C.S, "release_later", [])
    return res


def phase_inproj(C, l):
    S, T, NT, P = C.S, C.T, C.NT, C.P
    w_in = P["w_in"][l]
    with ExitStack() as st:
        wbuf = [S.sb("ip_w%d" % i, [128, 8, 1024], BF16, st) for i in range(2)]
        wb2 = S.sb("ip_wb", [128, 8, 1024], BF16, st)
        hTh = [S.sb("ip_h%d" % i, [128, 8, 513], BF16, st) for i in range(2)]
        zst = [S.sb("ip_z%d" % i, [128, 4, 512], F32, st) for i in range(2)]
        gst = [S.sb("ip_g%d" % i, [128, 4, 512], BF16, st) for i in range(2)]
        gb = load_cols(C, st, "ip_gb", P["gate_b"][l].rearrange("i (j p) -> (i j) p", p=128), 32)
        groups = [("F", 0, 1024, 0), ("V", 1024, 256, 0), ("C", OFF_C, 1024, OFF_C), ("F", OFF_D, 256, OFF_D)]
        for i in range(4):
            groups.append(("G", OFF_G + i * 1024, 1024, i * 1024))
        nld = 0
        nst = 0
        for gi, (kind, c0, ncol, r0) in enumerate(groups):
            w = wbuf[gi % 2]
            if kind != "C":
                S.dma("pool", w[:, :, 0:ncol], w_in[:, c0:c0 + ncol].rearrange("(kc p) n -> p kc n", p=128),
                      writes=[w], sem_tile=w)
            else:
                with ExitStack() as st2:
                    wraw = S.sb("ip_wraw", [128, 8, 1024], F32, st2)
                    mu = load_bc(C, st2, "ip_mu", P["rw_mu"][l], 1024)
                    tmp = [S.sb("ip_tmp%d" % i, [128, 1024], F32, st2) for i in range(2)]
                    S.dma("sp", wraw[:], w_in[:, c0:c0 + ncol].rearrange("(kc p) n -> p kc n", p=128),
                          writes=[wraw], sem_tile=wraw)
                    for kc in range(8):
                        tk = tmp[kc % 2]
                        S.op("dve", lambda e, kc=kc, tk=tk: e.tensor_tensor(out=tk[:], in0=wraw[:, kc, :], in1=mu[:], op=ALU.mult),
                             reads=[wraw, mu], writes=[tk])
                        S.op("act", lambda e, kc=kc, tk=tk: e.copy(out=wb2[:, kc, :], in_=tk[:]), reads=[tk], writes=[wb2])
                        S.op("pool", lambda e, kc=kc, tk=tk: e.tensor_tensor(out=w[:, kc, :], in0=wraw[:, kc, :], in1=tk[:], op=ALU.subtract),
                             reads=[wraw, tk], writes=[w])
                    S.barrier()
                    for t_ in [wraw, mu] + tmp:
                        S.release(t_)
            for i in range(NT):
                hh = hTh[nld % 2]
                prev = hTh[(nld + 1) % 2]
                nld += 1
                t0 = i * 512
                S.dma("sp", hh[:, :, 1:513], C.hT.rearrange("(kc p) t -> p kc t", p=128)[:, :, t0:t0 + 512],
                      writes=[hh], sem_tile=hh)
                if kind == "C":
                    if i == 0:
                        S.op("pool", lambda e, hh=hh: e.memset(hh[:, :, 0:1], 0.0), writes=[hh])
                    else:
                        S.op("pool", lambda e, hh=hh, prev=prev: e.tensor_copy(out=hh[:, :, 0:1], in_=prev[:, :, 512:513]),
                             reads=[prev], writes=[hh])
                if kind == "V":
                    zs = zst[nst % 2]
                    nst += 1
                    for sub in range(4):
                        ps = C.psum()
                        for kc in range(8):
                            S.op("pe", lambda e, kc=kc, sub=sub, ps=ps, hh=hh: e.matmul(
                                ps[:, 0:256], lhsT=hh[:, kc, 1 + sub * 128:1 + (sub + 1) * 128], rhs=w[:, kc, 0:256],
                                start=(kc == 0), stop=(kc == 7)), reads=[hh, w], writes=[ps])
                        en = evac_eng(C)
                        S.op(en, copy_op(en, zs[:, sub, 0:256], ps[:, 0:256]), reads=[ps], writes=[zs])
                    S.dma("sp", C.zbv[t0:t0 + 512, :].rearrange("(s p) c -> p s c", p=128), zs[:, :, 0:256],
                          reads=[zs], sem_tile=zs)
                    continue
                nct = ncol // 128
                for cg in range(0, nct, 4):
                    ncg = min(4, nct - cg)
                    if kind == "G":
                        zs = gst[nst % 2]
                    else:
                        zs = zst[nst % 2]
                    nst += 1
                    for j in range(ncg):
                        ct = cg + j
                        ps = C.psum()
                        if kind == "C":
                            for kc in range(8):
                                S.op("pe", lambda e, kc=kc, ct=ct, ps=ps, hh=hh: e.matmul(
                                    ps[:, :], lhsT=w[:, kc, ct * 128:(ct + 1) * 128], rhs=hh[:, kc, 1:513],
                                    start=(kc == 0), stop=False), reads=[hh, w], writes=[ps])
                            for kc in range(8):
                                S.op("pe", lambda e, kc=kc, ct=ct, ps=ps, hh=hh: e.matmul(
                                    ps[:, :], lhsT=wb2[:, kc, ct * 128:(ct + 1) * 128], rhs=hh[:, kc, 0:512],
                                    start=False, stop=(kc == 7)), reads=[hh, wb2], writes=[ps])
                        else:
                            for kc in range(8):
                                S.op("pe", lambda e, kc=kc, ct=ct, ps=ps, hh=hh: e.matmul(
                                    ps[:, :], lhsT=w[:, kc, ct * 128:(ct + 1) * 128], rhs=hh[:, kc, 1:513],
                                    start=(kc == 0), stop=(kc == 7)), reads=[hh, w], writes=[ps])
                        if kind == "G":
                            gcol = (r0 // 128) + ct
                            S.op("act", lambda e, j=j, ps=ps, zs=zs, gcol=gcol: e.activation(
                                out=zs[:, j, :], in_=ps[:, :], func=AF.Sigmoid, bias=gb[:, gcol:gcol + 1], scale=1.0),
                                reads=[ps, gb], writes=[zs])
                        else:
                            en = evac_eng(C)
                            S.op(en, copy_op(en, zs[:, j, :], ps[:, :]), reads=[ps], writes=[zs])
                    dst = C.gT if kind == "G" else C.zT
                    rr = r0 + cg * 128
                    S.dma("sp", dst[rr:rr + ncg * 128, t0:t0 + 512].rearrange("(j p) t -> p j t", p=128), zs[:, 0:ncg, :],
                          reads=[zs], sem_tile=zs)
        S.barrier()
        for t_ in wbuf + [wb2] + hTh + zst + gst:
            S.release(t_)


def phase_conv(C, l):
    S, T, NT, P = C.S, C.T, C.NT, C.P
    with ExitStack() as st:
        cw = load_cols(C, st, "cv_w", P["conv_w"][l].rearrange("k (f p) -> (k f) p", p=128), 6)
        za = [S.sb("cv_za%d" % i, [128, 6, 512], F32, st) for i in range(2)]
        ch = [S.sb("cv_ch%d" % i, [128, 2, 514], F32, st) for i in range(2)]
        yt = [S.sb("cv_y%d" % i, [128, 512], F32, st) for i in range(2)]
        ost = [S.sb("cv_o%d" % i, [128, 2, 512], BF16, st) for i in range(2)]
        ny = 0
        for i in range(NT):
            t0 = i * 512
            z = za[i % 2]
            c = ch[i % 2]
            cp = ch[(i + 1) % 2]
            o = ost[i % 2]
            S.dma("sp", z[:], C.zT[0:768, t0:t0 + 512].rearrange("(j p) t -> p j t", p=128), writes=[z], sem_tile=z)
            if i == 0:
                S.op("pool", lambda e, c=c: e.memset(c[:, :, 0:2], 0.0), writes=[c])
            else:
                S.op("pool", lambda e, c=c, cp=cp: e.tensor_copy(out=c[:, :, 0:2], in_=cp[:, :, 512:514]), reads=[cp], writes=[c])
            S.op("pool", lambda e, c=c, z=z: e.tensor_tensor(out=c[:, :, 2:514], in0=z[:, 2:4, :], in1=z[:, 4:6, :], op=ALU.mult),
                 reads=[z, c], writes=[c])
            for f in range(2):
                y = yt[ny % 2]
                ny += 1
                S.op("dve", lambda e, f=f, y=y, c=c: e.tensor_scalar(out=y[:], in0=c[:, f, 2:514], scalar1=cw[:, 4 + f:5 + f],
                                                                      scalar2=None, op0=ALU.mult), reads=[c, cw], writes=[y])
                S.op("dve", lambda e, f=f, y=y, c=c: e.scalar_tensor_tensor(out=y[:], in0=c[:, f, 1:513], scalar=cw[:, 2 + f:3 + f],
                                                                             in1=y[:], op0=ALU.mult, op1=ALU.add), reads=[c, cw, y], writes=[y])
                S.op("dve", lambda e, f=f, y=y, c=c: e.scalar_tensor_tensor(out=y[:], in0=c[:, f, 0:512], scalar=cw[:, f:f + 1],
                                                                             in1=y[:], op0=ALU.mult, op1=ALU.add), reads=[c, cw, y], writes=[y])
                S.op("pool", lambda e, f=f, y=y, z=z, o=o: e.tensor_tensor(out=o[:, f, :], in0=y[:], in1=z[:, f, :], op=ALU.mult),
                     reads=[y, z], writes=[o])
            S.dma("sp", C.oT[0:256, t0:t0 + 512].rearrange("(f p) t -> p f t", p=128), o[:], reads=[o], sem_tile=o)
        S.barrier()
        for t_ in za + ch + yt + ost:
            S.release(t_)


def phase_sgu(C, l):
    S, T, NT, P = C.S, C.T, C.NT, C.P
    with ExitStack() as st:
        g_bc = load_bc(C, st, "sg_g", P["sg_norm_g"][l], 256)
        b_bc = load_bc(C, st, "sg_b", P["sg_norm_b"][l], 256)
        sb_bc = load_bc(C, st, "sg_sb", P["sg_b"][l].rearrange("g i -> (g i)"), 512)
        wraw = S.sb("sg_wraw", [128, 4, 128], F32, st)
        wsT = S.sb("sg_wsT", [128, 4, 128], F32, st)
        S.dma("sp", wraw[:], P["sg_w"][l].rearrange("g i j -> i g j"), writes=[wraw], sem_tile=wraw)
        ps = C.psum()
        for g in range(4):
            S.op("pe", lambda e, g=g: e.transpose(out=ps[:, g * 128:(g + 1) * 128], in_=wraw[:, g, :], identity=C.ident[:]),
                 reads=[wraw, C.ident], writes=[ps])
        S.op("dve", lambda e: e.tensor_copy(out=wsT[:], in_=ps[:, :].rearrange("p (g i) -> p g i", g=4)), reads=[ps], writes=[wsT])
        S.op("dve", lambda e: e.memset(wsT[64:128, :, 0:64], 0.0), reads=[wsT], writes=[wsT])
        vt = [S.sb("sg_v%d" % i, [128, 4, 256], F32, st) for i in range(2)]
        ut = [S.sb("sg_u%d" % i, [64, 4, 512], F32, st) for i in range(2)]
        ot = [S.sb("sg_o%d" % i, [64, 4, 512], BF16, st) for i in range(2)]
        svt = [S.sb("sg_sv%d" % i, [64, 512], F32, st) for i in range(2)]
        stt = [S.sb("sg_st%d" % i, [128, 6], F32, st) for i in range(2)]
        mvt = [S.sb("sg_mv%d" % i, [128, 2], F32, st) for i in range(2)]
        rst = [S.sb("sg_rs%d" % i, [128, 1], F32, st) for i in range(2)]
        n = 0
        for i in range(NT):
            t0 = i * 512
            v, u, o = vt[i % 2], ut[i % 2], ot[i % 2]
            S.dma("sp", v[:], C.zbv[t0:t0 + 512, :].rearrange("(s p) c -> p s c", p=128), writes=[v], sem_tile=v)
            S.dma("sp", u[:], C.zT[768:1024, t0:t0 + 512].rearrange("(g c) t -> c g t", c=64), writes=[u], sem_tile=u)
            for s in range(4):
                sx, mv, rs, sv = stt[n % 2], mvt[n % 2], rst[n % 2], svt[n % 2]
                n += 1
                S.op("dve", lambda e, s=s, sx=sx, v=v: e.bn_stats(out=sx[:], in_=v[:, s, :]), reads=[v], writes=[sx])
                S.op("dve", lambda e, sx=sx, mv=mv: e.bn_aggr(out=mv[:], in_=sx[:]), reads=[sx], writes=[mv])
                S.op("act", lambda e, mv=mv, rs=rs: e.activation(out=rs[:], in_=mv[:, 1:2], func=AF.Sqrt, bias=C.eps_col[:, 0:1], scale=1.0),
                     reads=[mv, C.eps_col], writes=[rs])
                S.op("dve", lambda e, rs=rs: e.reciprocal(out=rs[:], in_=rs[:]), reads=[rs], writes=[rs])
                S.op("dve", lambda e, s=s, v=v, mv=mv, rs=rs: e.tensor_scalar(out=v[:, s, :], in0=v[:, s, :], scalar1=mv[:, 0:1],
                                                                                scalar2=rs[:, 0:1], op0=ALU.subtract, op1=ALU.mult),
                     reads=[v, mv, rs], writes=[v])
                S.op("pool", lambda e, s=s, v=v: e.tensor_tensor(out=v[:, s, :], in0=v[:, s, :], in1=g_bc[:], op=ALU.mult),
                     reads=[v, g_bc], writes=[v])
                S.op("pool", lambda e, s=s, v=v: e.tensor_tensor(out=v[:, s, :], in0=v[:, s, :], in1=b_bc[:], op=ALU.add),
                     reads=[v, b_bc], writes=[v])
                ps = C.psum()
                for g in range(4):
                    S.op("pe", lambda e, g=g, s=s, v=v, ps=ps: e.matmul(ps[0:64, g * 128:(g + 1) * 128], lhsT=v[:, s, g * 64:(g + 1) * 64],
                                                                         rhs=wsT[:, g, :], start=True, stop=True),
                         reads=[v, wsT], writes=[ps])
                S.op("dve", lambda e, ps=ps, sv=sv: e.tensor_tensor(out=sv[:], in0=ps[0:64, :], in1=sb_bc[0:64, :], op=ALU.add),
                     reads=[ps, sb_bc], writes=[sv])
                S.op("pool", lambda e, s=s, sv=sv, u=u, o=o: e.tensor_tensor(out=o[:, :, s * 128:(s + 1) * 128],
                                                                              in0=sv[:, :].rearrange("c (g i) -> c g i", g=4),
                                                                              in1=u[:, :, s * 128:(s + 1) * 128], op=ALU.mult),
                     reads=[sv, u], writes=[o])
            S.dma("sp", C.oT[256:512, t0:t0 + 512].rearrange("(g c) t -> c g t", c=64), o[:], reads=[o], sem_tile=o)
        S.barrier()
        for t_ in [g_bc, b_bc, sb_bc, wraw] + vt + ut + ot:
            S.release(t_)


def phase_zero_branch(C, r0):
    S, T, NT = C.S, C.T, C.NT
    with ExitStack() as st:
        z = S.sb("zb_z", [128, 2, 512], BF16, st)
        S.op("pool", lambda e: e.memset(z[:], 0.0), writes=[z])
        for i in range(NT):
            S.dma("sp", C.oT[r0:r0 + 256, i * 512:(i + 1) * 512].rearrange("(f p) t -> p f t", p=128), z[:], reads=[z], sem_tile=z)
        S.barrier()
        S.release(z)


def proj_res_ln(C, st, inT, W, g_bc, b_bc, tile_i, hres, hTs, out_ap=None):
    S = C.S
    for sub in range(4):
        hr = hres[C.hri % 2]
        C.hri += 1
        tok0 = tile_i * 512 + sub * 128
        S.dma("sp", hr[:], C.h_tok[tok0:tok0 + 128, :], writes=[hr], sem_tile=hr)
        for half in range(2):
            ps = C.psum()
            for kc in range(8):
                S.op("pe", lambda e, kc=kc, ps=ps, half=half, sub=sub: e.matmul(
                    ps[:, :], lhsT=inT[:, kc, sub * 128:(sub + 1) * 128], rhs=W[:, kc, half * 512:(half + 1) * 512],
                    start=(kc == 0), stop=(kc == 7)), reads=[inT, W], writes=[ps])
            S.op("dve", lambda e, ps=ps, half=half, hr=hr: e.scalar_tensor_tensor(
                out=hr[:, half * 512:(half + 1) * 512], in0=hr[:, half * 512:(half + 1) * 512], scalar=DN_ALPHA, in1=ps[:, :],
                op0=ALU.mult, op1=ALU.add), reads=[hr, ps], writes=[hr])
        ln_tile(C, (hr[:], hr), (hr[:], hr), g_bc, b_bc)
        store_h(C, st, hr, hTs, tile_i, sub, out_ap)


def phase_merge(C, l):
    S, T, NT, P = C.S, C.T, C.NT, C.P
    with ExitStack() as st:
        brp = S.sb("mg_brp", [128, 8, 1024], BF16, st)
        S.dma("pool", brp[:], P["br_proj"][l].rearrange("i (kc p) n -> p (i kc) n", p=128), writes=[brp], sem_tile=brp)
        wout = S.sb("mg_wout", [128, 8, 1024], BF16, st)
        load_w_bf16(C, wout, P["w_out"][l])
        g_bc = load_bc(C, st, "mg_g", P["ln1_g"][l], D)
        b_bc = load_bc(C, st, "mg_b", P["ln1_b"][l], D)
        oTt = [S.sb("mg_o%d" % i, [128, 8, 512], BF16, st) for i in range(2)]
        gTt = [S.sb("mg_g%d" % i, [128, 4, 512], BF16, st) for i in range(2)]
        mT = [S.sb("mg_m%d" % i, [128, 8, 512], BF16, st) for i in range(2)]
        tm = [S.sb("mg_t%d" % i, [128, 4, 512], F32, st) for i in range(2)]
        hres = [S.sb("mg_hr%d" % i, [128, D], F32, st) for i in range(2)]
        hTs = [S.sb("mg_hT%d" % i, [128, 8, 512], BF16, st) for i in range(2)]
        C.hri = 0
        ng = 0
        for i in range(NT):
            t0 = i * 512
            o = oTt[i % 2]
            m = mT[i % 2]
            S.dma("sp", o[:], C.oT[:, t0:t0 + 512].rearrange("(j p) t -> p j t", p=128), writes=[o], sem_tile=o)
            for ct in range(8):
                g = gTt[ng % 2]
                t4 = tm[ng % 2]
                ng += 1
                S.dma("sp", g[:], C.gT.rearrange("(i ct p) t -> p i ct t", p=128, ct=8)[:, :, ct, t0:t0 + 512], writes=[g], sem_tile=g)
                for b in range(4):
                    ps = C.psum()
                    for kc in range(2):
                        S.op("pe", lambda e, b=b, kc=kc, ct=ct, ps=ps, o=o: e.matmul(
                            ps[:, :], lhsT=brp[:, b * 2 + kc, ct * 128:(ct + 1) * 128], rhs=o[:, b * 2 + kc, :],
                            start=(kc == 0), stop=(kc == 1)), reads=[brp, o], writes=[ps])
                    S.op("dve", lambda e, b=b, ps=ps, g=g, t4=t4: e.tensor_tensor(out=t4[:, b, :], in0=ps[:, :], in1=g[:, b, :], op=ALU.mult),
                         reads=[ps, g], writes=[t4])
                S.op("pool", lambda e, t4=t4: e.tensor_tensor(out=t4[:, 0:2, :], in0=t4[:, 0:2, :], in1=t4[:, 2:4, :], op=ALU.add),
                     reads=[t4], writes=[t4])
                S.op("pool", lambda e, t4=t4, m=m, ct=ct: e.tensor_tensor(out=m[:, ct, :], in0=t4[:, 0, :], in1=t4[:, 1, :], op=ALU.add),
                     reads=[t4], writes=[m])
            proj_res_ln(C, st, m, wout, g_bc, b_bc, i, hres, hTs[i % 2])
        S.barrier()
        for t_ in [brp, wout, g_bc, b_bc] + oTt + gTt + mT + hres + hTs:
            S.release(t_)


def phase_attn(C, l):
    S, T, NT, P = C.S, C.T, C.NT, C.P
    with ExitStack() as st:
        wq = S.sb("at_wq", [128, 8, 1024], BF16, st)
        wo = S.sb("at_wo", [128, 8, 1024], BF16, st)
        load_w_bf16(C, wq, P["xa_wq"][l])
        load_w_bf16(C, wo, P["xa_wo"][l])
        g_bc = load_bc(C, st, "at_g", P["ln2_g"][l], D)
        b_bc = load_bc(C, st, "at_b", P["ln2_b"][l], D)
        kT = S.sb("at_kT", [128, 8, 256], BF16, st)
        vv = S.sb("at_v", [128, 2, 1024], BF16, st)
        with ExitStack() as st2:
            wk = S.sb("at_wk", [128, 8, 1024], BF16, st2)
            wv = S.sb("at_wv", [128, 8, 1024], BF16, st2)
            load_w_bf16(C, wk, P["xa_wk"][l])
            load_w_bf16(C, wv, P["xa_wv"][l])
            mt = S.sb("at_mem", [128, 2, 1024], F32, st2)
            memT = S.sb("at_memT", [128, 8, 256], BF16, st2)
            S.dma("sp", mt[:], C.mem_in.rearrange("(s p) c -> p s c", p=128), writes=[mt], sem_tile=mt)
            for s in range(2):
                transpose_to(C, mt[:, s, :], mt, memT, lambda g, s=s: memT[:, g * 4:(g + 1) * 4, s * 128:(s + 1) * 128])
            for ct in range(8):
                ps = C.psum()
                for kc in range(8):
                    S.op("pe", lambda e, kc=kc, ct=ct, ps=ps: e.matmul(ps[:, 0:256], lhsT=wk[:, kc, ct * 128:(ct + 1) * 128],
                                                                        rhs=memT[:, kc, :], start=(kc == 0), stop=(kc == 7)),
                         reads=[wk, memT], writes=[ps])
                en = evac_eng(C)
                S.op(en, copy_op(en, kT[:, ct, :], ps[:, 0:256]), reads=[ps], writes=[kT])
            for s in range(2):
                for half in range(2):
                    ps = C.psum()
                    for kc in range(8):
                        S.op("pe", lambda e, kc=kc, s=s, half=half, ps=ps: e.matmul(
                            ps[:, :], lhsT=memT[:, kc, s * 128:(s + 1) * 128], rhs=wv[:, kc, half * 512:(half + 1) * 512],
                            start=(kc == 0), stop=(kc == 7)), reads=[wv, memT], writes=[ps])
                    en = evac_eng(C)
                    S.op(en, copy_op(en, vv[:, s, half * 512:(half + 1) * 512], ps[:, :]), reads=[ps], writes=[vv])
            S.barrier()
            for t_ in [wk, wv, mt]:
                S.release(t_)
        hTt = [S.sb("at_h%d" % i, [128, 8, 512], BF16, st) for i in range(2)]
        qT = [S.sb("at_q%d" % i, [128, 8, 512], BF16, st) for i in range(2)]
        aT = [S.sb("at_a%d" % i, [128, 8, 512], BF16, st) for i in range(2)]
        pt = [S.sb("at_p%d" % i, [128, 256], F32, st) for i in range(4)]
        pT = [S.sb("at_pT%d" % i, [128, 2, 128], BF16, st) for i in range(4)]
        mx = [S.sb("at_mx%d" % i, [128, 1], F32, st) for i in range(4)]
        sm = [S.sb("at_sm%d" % i, [128, 1], F32, st) for i in range(4)]
        hres = [S.sb("at_hr%d" % i, [128, D], F32, st) for i in range(2)]
        hTs = [S.sb("at_hT%d" % i, [128, 8, 512], BF16, st) for i in range(2)]
        C.hri = 0
        n = 0
        for i in range(NT):
            t0 = i * 512
            hh, q, a = hTt[i % 2], qT[i % 2], aT[i % 2]
            S.dma("sp", hh[:], C.hT.rearrange("(kc p) t -> p kc t", p=128)[:, :, t0:t0 + 512], writes=[hh], sem_tile=hh)
            for ct in range(8):
                ps = C.psum()
                for kc in range(8):
                    S.op("pe", lambda e, kc=kc, ct=ct, ps=ps, hh=hh: e.matmul(ps[:, :], lhsT=wq[:, kc, ct * 128:(ct + 1) * 128],
                                                                               rhs=hh[:, kc, :], start=(kc == 0), stop=(kc == 7)),
                         reads=[wq, hh], writes=[ps])
                S.op("act", lambda e, ct=ct, ps=ps, q=q: e.activation(out=q[:, ct, :], in_=ps[:, :], func=AF.Copy, scale=1.0 / 16.0),
                     reads=[ps], writes=[q])
            for sub in range(4):
                pss = []
                for hd in range(4):
                    ps = C.psum()
                    pss.append(ps)
                    for j in range(2):
                        S.op("pe", lambda e: e.matmul(ps[:, 0:256], lhsT=q[:, 2 * hd + j, sub * 128:(sub + 1) * 128], rhs=kT[:, 2 * hd + j, :],
                                                      start=(j == 0), stop=(j == 1)), reads=[q, kT], writes=[ps])
                for hd in range(4):
                    ps, p_, mx_, sm_ = pss[hd], pt[hd], mx[hd], sm[hd]
                    S.op("dve", lambda e: e.reduce_max(out=mx_[:], in_=ps[:, 0:256], axis=AX.X, negate=True), reads=[ps], writes=[mx_])
                    S.op("act", lambda e: e.activation(out=p_[:], in_=ps[:, 0:256], func=AF.Exp, bias=mx_[:, 0:1], scale=1.0, accum_out=sm_[:]),
                         reads=[ps, mx_], writes=[p_, sm_])
                    S.op("dve", lambda e: e.reciprocal(out=sm_[:], in_=sm_[:]), reads=[sm_], writes=[sm_])
                    S.op("dve", lambda e: e.tensor_scalar(out=p_[:], in0=p_[:], scalar1=sm_[:, 0:1], scalar2=None, op0=ALU.mult), reads=[p_, sm_], writes=[p_])
                for hd in range(4):
                    p_, pT_ = pt[hd], pT[hd]
                    ps2 = C.psum()
                    for j in range(2):
                        S.op("pe", lambda e: e.transpose(out=ps2[:, j * 128:(j + 1) * 128], in_=p_[:, j * 128:(j + 1) * 128], identity=C.ident[:]),
                             reads=[p_, C.ident], writes=[ps2])
                    S.op("act", lambda e: e.copy(out=pT_[:], in_=ps2[:, 0:256].rearrange("p (a b) -> p a b", a=2)), reads=[ps2], writes=[pT_])
                for hd in range(4):
                    pT_ = pT[hd]
                    ps3 = C.psum()
                    for j in range(2):
                        ct = 2 * hd + j
                        for mt_ in range(2):
                            S.op("pe", lambda e: e.matmul(ps3[:, j * 128:(j + 1) * 128], lhsT=vv[:, mt_, ct * 128:(ct + 1) * 128], rhs=pT_[:, mt_, :],
                                                          start=(mt_ == 0), stop=(mt_ == 1)), reads=[vv, pT_], writes=[ps3])
                    S.op("dve", lambda e: e.tensor_copy(out=a[:, 2 * hd:2 * hd + 2, sub * 128:(sub + 1) * 128],
                                                        in_=ps3[:, 0:256].rearrange("p (a b) -> p a b", a=2)), reads=[ps3], writes=[a])
            proj_res_ln(C, st, a, wo, g_bc, b_bc, i, hres, hTs[i % 2])
        S.barrier()
        for t_ in [wq, wo, g_bc, b_bc] + hTt + hres + hTs:
            S.release(t_)


def phase_router(C, l):
    S, T, NT, P = C.S, C.T, C.NT, C.P
    with ExitStack() as st:
        rw = S.sb("rt_w", [128, 8, NE], F32, st)
        S.dma("sp", rw[:], P["router_w"][l].rearrange("(kc p) n -> p kc n", p=128), writes=[rw], sem_tile=rw)
        rb = load_bc(C, st, "rt_b", P["router_b"][l], NE)
        ht = [S.sb("rt_h%d" % i, [128, D], F32, st) for i in range(2)]
        hTf = [S.sb("rt_hT%d" % i, [128, 8, 128], F32, st) for i in range(2)]
        lg = [S.sb("rt_lg%d" % i, [128, NE], F32, st) for i in range(2)]
        t8 = [S.sb("rt_t8%d" % i, [128, 8], F32, st) for i in range(2)]
        ex = [S.sb("rt_ex%d" % i, [128, NE], F32, st) for i in range(2)]
        mk = [S.sb("rt_mk%d" % i, [128, NE], F32, st) for i in range(2)]
        sm = [S.sb("rt_sm%d" % i, [128, 1], F32, st) for i in range(2)]
        for n in range(T // 128):
            h, hf, lg_, t8_, ex_, mk_, sm_ = ht[n % 2], hTf[n % 2], lg[n % 2], t8[n % 2], ex[n % 2], mk[n % 2], sm[n % 2]
            S.dma("sp", h[:], C.h_tok[n * 128:(n + 1) * 128, :], writes=[h], sem_tile=h)
            transpose_to(C, h[:], h, hf, lambda g, hf=hf: hf[:, g * 4:(g + 1) * 4, :])
            ps = C.psum()
            for kc in range(8):
                S.op("pe", lambda e, kc=kc, ps=ps, hf=hf: e.matmul(ps[:, 0:NE], lhsT=hf[:, kc, :], rhs=rw[:, kc, :],
                                                                    start=(kc == 0), stop=(kc == 7)), reads=[hf, rw], writes=[ps])
            S.op("dve", lambda e, ps=ps, lg_=lg_: e.tensor_tensor(out=lg_[:], in0=ps[:, 0:NE], in1=rb[:], op=ALU.add),
                 reads=[ps, rb], writes=[lg_])
            S.op("dve", lambda e, lg_=lg_, t8_=t8_: e.max(out=t8_[:], in_=lg_[:]), reads=[lg_], writes=[t8_])
            S.op("dve", lambda e, lg_=lg_, t8_=t8_, mk_=mk_: e.tensor_scalar(out=mk_[:], in0=lg_[:], scalar1=t8_[:, 3:4], scalar2=None, op0=ALU.is_ge),
                 reads=[lg_, t8_], writes=[mk_])
            S.op("dve", lambda e, lg_=lg_, t8_=t8_, ex_=ex_: e.tensor_scalar(out=ex_[:], in0=lg_[:], scalar1=t8_[:, 0:1], scalar2=None, op0=ALU.subtract),
                 reads=[lg_, t8_], writes=[ex_])
            S.op("act", lambda e, ex_=ex_: e.activation(out=ex_[:], in_=ex_[:], func=AF.Exp), reads=[ex_], writes=[ex_])
            S.op("dve", lambda e, ex_=ex_, mk_=mk_: e.tensor_tensor(out=ex_[:], in0=ex_[:], in1=mk_[:], op=ALU.mult), reads=[ex_, mk_], writes=[ex_])
            S.op("dve", lambda e, ex_=ex_, sm_=sm_: e.reduce_sum(out=sm_[:], in_=ex_[:], axis=AX.X), reads=[ex_], writes=[sm_])
            S.op("dve", lambda e, sm_=sm_: e.reciprocal(out=sm_[:], in_=sm_[:]), reads=[sm_], writes=[sm_])
            S.op("dve", lambda e, ex_=ex_, sm_=sm_: e.tensor_scalar(out=ex_[:], in0=ex_[:], scalar1=sm_[:, 0:1], scalar2=None, op0=ALU.mult),
                 reads=[ex_, sm_], writes=[ex_])
            S.dma("sp", C.gate_d[n * 128:(n + 1) * 128, :], ex_[:], reads=[ex_], sem_tile=ex_)
        S.barrier()
        for t_ in [rw, rb] + ht + ex:
            S.release(t_)


def phase_moe(C, l, out_ap):
    S, T, NT, P = C.S, C.T, C.NT, C.P
    ST = 1024 if T >= 1024 else 512
    nsub = ST // 128
    ntt = ST // 512
    with ExitStack() as st:
        g_bc = load_bc(C, st, "mo_g", P["ln3_g"][l], D)
        b_bc = load_bc(C, st, "mo_b", P["ln3_b"][l], D)
        w1t = [S.sb("mo_w1%d" % i, [128, 8, 2048], BF16, st) for i in range(2)]
        w2 = [S.sb("mo_w2%d" % i, [128, 8, 1024], BF16, st) for i in range(2)]
        b1r = [S.sb("mo_b1r%d" % i, [1, 2048], BF16, st) for i in range(2)]
        ones_row = S.sb("mo_ones", [1, 512], BF16, st)
        S.op("pool", lambda e: e.memset(ones_row[:], 1.0), writes=[ones_row])
        b2 = [S.sb("mo_b2%d" % i, [1, 1024], BF16, st) for i in range(2)]
        acc = S.sb("mo_acc", [128, nsub, 1024], F32, st)
        hTt = S.sb("mo_hT", [128, 8, ST], BF16, st)
        gt = S.sb("mo_gate", [128, nsub, NE], F32, st)
        actT = [S.sb("mo_act%d" % i, [128, 8, 512], BF16, st) for i in range(2)]
        gq = [S.sb("mo_gq%d" % i, [128, 512], F32, st) for i in range(2)]
        sg = [S.sb("mo_sg%d" % i, [128, 512], F32, st) for i in range(2)]
        uq = [S.sb("mo_uq%d" % i, [128, 512], F32, st) for i in range(2)]
        hTs = [S.sb("mo_hTs%d" % i, [128, 8, 512], BF16, st) for i in range(1)] * 2
        w1 = P["ex_w1"][l]
        nw = 0
        na = 0
        nq = 0
        for sti in range(T // ST):
            tok0 = sti * ST
            S.dma("sp", acc[:], C.h_tok[tok0:tok0 + ST, :].rearrange("(s p) c -> p s c", p=128), writes=[acc], sem_tile=acc)
            S.dma("sp", hTt[:], C.hT.rearrange("(kc p) t -> p kc t", p=128)[:, :, tok0:tok0 + ST], writes=[hTt], sem_tile=hTt)
            S.dma("sp", gt[:], C.gate_d[tok0:tok0 + ST, :].rearrange("(s p) c -> p s c", p=128), writes=[gt], sem_tile=gt)
            S.op("pool", lambda e: e.tensor_scalar(out=acc[:], in0=acc[:], scalar1=DN_ALPHA, scalar2=None, op0=ALU.mult),
                 reads=[acc], writes=[acc])
            for ex in range(NE):
                w1_, w2_, b1r_, b2_ = w1t[nw % 2], w2[nw % 2], b1r[nw % 2], b2[nw % 2]
                nw += 1
                S.dma("pool", w1_[:], w1[ex].rearrange("(kc p) n -> p kc n", p=128), writes=[w1_], sem_tile=w1_)
                S.dma("pool", w2_[:], P["ex_w2"][l, ex].rearrange("(kc p) n -> p kc n", p=128), writes=[w2_], sem_tile=w2_)
                S.dma("pool", b2_[:], P["ex_b2"][l, ex:ex + 1, :], writes=[b2_], sem_tile=b2_)
                S.dma("pool", b1r_[:], P["ex_b1"][l, ex:ex + 1, :], writes=[b1r_], sem_tile=b1r_)
                for tt in range(ntt):
                    a = actT[na % 2]
                    na += 1
                    for ft in range(8):
                        g_, s_, u_ = gq[nq % 2], sg[nq % 2], uq[nq % 2]
                        nq += 1
                        psg = C.psum()
                        S.op("pe", lambda e, ft=ft, psg=psg, b1r_=b1r_: e.matmul(
                            psg[:, :], lhsT=b1r_[0:1, ft * 256:(ft + 1) * 256:2], rhs=ones_row[0:1, :], start=True, stop=False),
                            reads=[b1r_, ones_row], writes=[psg])
                        for kc in range(8):
                            S.op("pe", lambda e, kc=kc, ft=ft, psg=psg, w1_=w1_, tt=tt: e.matmul(
                                psg[:, :], lhsT=w1_[:, kc, ft * 256:(ft + 1) * 256:2], rhs=hTt[:, kc, tt * 512:(tt + 1) * 512],
                                start=False, stop=(kc == 7)), reads=[w1_, hTt], writes=[psg])
                        psu = C.psum()
                        S.op("pe", lambda e, ft=ft, psu=psu, b1r_=b1r_: e.matmul(
                            psu[:, :], lhsT=b1r_[0:1, ft * 256 + 1:(ft + 1) * 256:2], rhs=ones_row[0:1, :], start=True, stop=False),
                            reads=[b1r_, ones_row], writes=[psu])
                        for kc in range(8):
                            S.op("pe", lambda e, kc=kc, ft=ft, psu=psu, w1_=w1_, tt=tt: e.matmul(
                                psu[:, :], lhsT=w1_[:, kc, ft * 256 + 1:(ft + 1) * 256:2], rhs=hTt[:, kc, tt * 512:(tt + 1) * 512],
                                start=False, stop=(kc == 7)), reads=[w1_, hTt], writes=[psu])
                        S.op("dve", lambda e, psg=psg, g_=g_: e.tensor_scalar(
                            out=g_[:], in0=psg[:, :], scalar1=7.0, scalar2=None, op0=ALU.min), reads=[psg], writes=[g_])
                        S.op("act", lambda e, g_=g_, s_=s_: e.activation(out=s_[:], in_=g_[:], func=AF.Sigmoid, scale=1.702),
                             reads=[g_], writes=[s_])
                        S.op("dve", lambda e, psu=psu, u_=u_: e.tensor_scalar(
                            out=u_[:], in0=psu[:, :], scalar1=7.0, scalar2=-7.0, op0=ALU.min, op1=ALU.max), reads=[psu], writes=[u_])
                        S.op("pool", lambda e, g_=g_, s_=s_: e.tensor_tensor(out=s_[:], in0=g_[:], in1=s_[:], op=ALU.mult),
                             reads=[g_, s_], writes=[s_])
                        S.op("dve", lambda e, ft=ft, s_=s_, u_=u_, a=a: e.scalar_tensor_tensor(out=a[:, ft, :], in0=u_[:], scalar=1.0, in1=s_[:],
                                                                                         op0=ALU.add, op1=ALU.mult), reads=[s_, u_], writes=[a])
                    for sub in range(4):
                        s_idx = tt * 4 + sub
                        for half in range(2):
                            ps = C.psum()
                            S.op("pe", lambda e, ps=ps, half=half, b2_=b2_: e.matmul(
                                ps[:, :], lhsT=C.ones_b[0:1, :], rhs=b2_[0:1, half * 512:(half + 1) * 512], start=True, stop=False),
                                reads=[C.ones_b, b2_], writes=[ps])
                            for ft in range(8):
                                S.op("pe", lambda e, ft=ft, ps=ps, half=half, sub=sub, a=a, w2_=w2_: e.matmul(
                                    ps[:, :], lhsT=a[:, ft, sub * 128:(sub + 1) * 128], rhs=w2_[:, ft, half * 512:(half + 1) * 512],
                                    start=False, stop=(ft == 7)), reads=[a, w2_], writes=[ps])
                            S.op("dve", lambda e, ps=ps, half=half, s_idx=s_idx, ex=ex: e.scalar_tensor_tensor(
                                out=acc[:, s_idx, half * 512:(half + 1) * 512], in0=ps[:, :], scalar=gt[:, s_idx, ex:ex + 1],
                                in1=acc[:, s_idx, half * 512:(half + 1) * 512], op0=ALU.mult, op1=ALU.add),
                                reads=[ps, gt, acc], writes=[acc])
            for tt in range(ntt):
                tile_i = sti * ntt + tt
                for sub in range(4):
                    s_idx = tt * 4 + sub
                    ln_tile(C, (acc[:, s_idx, :], acc), (acc[:, s_idx, :], acc), g_bc, b_bc)
                    S_store_sub(C, acc, s_idx, hTs[tile_i % 2], tile_i, sub, out_ap)
        S.barrier()
        for t_ in [g_bc, b_bc, acc, hTt, gt] + w1t + w2 + b1r + b2 + hTs:
            S.release(t_)


def S_store_sub(C, acc, s_idx, hTs, tile_i, sub, out_ap):
    S = C.S
    tok0 = tile_i * 512 + sub * 128
    S.dma("sp", C.h_tok[tok0:tok0 + 128, :], acc[:, s_idx, :], reads=[acc], sem_tile=acc)
    if out_ap is not None:
        S.dma("sp", out_ap[tok0:tok0 + 128, :], acc[:, s_idx, :], reads=[acc], sem_tile=acc)
    transpose_to(C, acc[:, s_idx, :], acc, hTs, lambda g: hTs[:, g * 4:(g + 1) * 4, sub * 128:(sub + 1) * 128])
    if sub == 3:
        S.dma("sp", C.hT.rearrange("(kc p) t -> p kc t", p=128)[:, :, tile_i * 512:(tile_i + 1) * 512], hTs[:],
              reads=[hTs], sem_tile=hTs)


RW_EPS = 64e-5
RW_FP32R = False
RW_C = math.exp(-0.5)


def phase_rwkv(C, l):
    S, T, P = C.S, C.T, C.P
    MT = 256
    NCH = MT // 32
    with ExitStack() as st:
        def cols64(nm, key):
            return load_cols(C, st, nm, P[key][l].rearrange("(h n) -> h n", n=64), 4, m=64)
        w0c, a0c, kkc, kac, lgc, lbc = (cols64("rw_" + k, "rw_" + k) for k in ("w0", "a0", "k_k", "k_a", "ln_g", "ln_b"))
        rkc = load_cols(C, st, "rw_rk", P["rw_r_k"][l], 4, m=64)
        wup = S.sb("rw_wup", [64, 256], F32, st)
        aup = S.sb("rw_aup", [64, 256], F32, st)
        gup = S.sb("rw_gup", [128, 256], F32, st)
        S.dma("sp", wup[:], P["rw_w_up"][l], writes=[wup], sem_tile=wup)
        S.dma("sp", aup[:], P["rw_a_up"][l], writes=[aup], sem_tile=aup)
        S.dma("sp", gup[:], P["rw_g_up"][l], writes=[gup], sem_tile=gup)
        bd = S.sb("rw_bd", [128, 128], F32, st)
        S.op("pool", lambda e: e.memset(bd[:], 0.0), writes=[bd])
        for h in range(4):
            S.op("pool", lambda e, h=h: e.memset(bd[32 * h:32 * h + 32, 32 * h:32 * h + 32], 1.0), reads=[bd], writes=[bd])
        mA = S.sb("rw_mA", [128, 4, 128], F32, st)
        mB = S.sb("rw_mB", [128, 2, 128], F32, st)
        for j in range(4):
            cmp = ALU.is_gt if j < 2 else ALU.is_ge
            S.op("pool", lambda e, j=j, cmp=cmp: e.affine_select(out=mA[:, j, :], in_=bd[:], compare_op=cmp, fill=0.0, base=0,
                                                                 pattern=[[1, 128]], channel_multiplier=-1), reads=[bd], writes=[mA])
        for j in range(2):
            S.op("pool", lambda e, j=j: e.affine_select(out=mB[:, j, :], in_=bd[:], compare_op=ALU.is_gt, fill=0.0, base=0,
                                                        pattern=[[-1, 128]], channel_multiplier=1), reads=[bd], writes=[mB])
        cmask = S.sb("rw_cmask", [64, 4 * MT], F32, st)
        S.op("pool", lambda e: e.memset(cmask[:], 1.0), writes=[cmask])
        S.op("pool", lambda e: e.memset(cmask[:, :].rearrange("p (c l) -> p c l", l=32)[:, :, 0:1], 0.0), reads=[cmask], writes=[cmask])
        o64 = S.sb("rw_o64", [64, 64], F32, st)
        S.op("pool", lambda e: e.memset(o64[:], 1.0), writes=[o64])
        o64m = S.sb("rw_o64m", [64, 64], F32, st)
        S.op("pool", lambda e: e.memset(o64m[:], 1.0 / 64.0), writes=[o64m])
        Ebd = S.sb("rw_Ebd", [128, 256], F32, st)
        S.op("pool", lambda e: e.memset(Ebd[:], 0.0), writes=[Ebd])
        S0 = [S.sb("rw_S%d" % i, [64, 256], F32, st) for i in range(2)]
        S.op("pool", lambda e: e.memset(S0[0][:], 0.0), writes=[S0[0]])
        nS = 0
        nSl = [0]

        def kh(nm):
            return S.sb(nm, [64, 4, MT], F32, st)
        r_t, k_t, v_t, a_t, g_t, ka_t, b_t, lw_t, G_t, eG_t, bon_t, Y_t, x1, x2 = (
            kh("rw_" + n) for n in ("r", "k", "v", "a", "g", "ka", "b", "lw", "G", "eG", "bon", "Y", "x1", "x2"))
        rC, kC, bC, kaC, vC = (S.sb("rw_c" + n, [64, NCH, 128], F32, st) for n in ("r", "k", "b", "ka", "v"))
        xw_t = S.sb("rw_xw", [64, 2, MT], F32, st)
        xg_t = S.sb("rw_xg", [128, MT], F32, st)
        o_t = S.sb("rw_o", [64, 4, MT], BF16, st)
        GRP = 4
        NSET = 8
        RR = (lambda ap: ap.bitcast(mybir.dt.float32r)) if RW_FP32R else (lambda ap: ap)
        TT_ = [S.sb("rw_TT%d" % i, [128, 4, 64], F32, st) for i in range(NSET)]
        AA = [S.sb("rw_AA%d" % i, [128, 4, 128], F32, st) for i in range(NSET)]
        AB = [S.sb("rw_AB%d" % i, [128, 2, 128], F32, st) for i in range(NSET)]
        Vbds = [S.sb("rw_Vbd%d" % i, [128, 256], F32, st) for i in range(NSET)]
        for vb in Vbds:
            S.op("pool", lambda e: e.memset(vb[:], 0.0), writes=[vb])
        PP = [None, None]
        PPx = [S.sb("rw_PP%d" % i, [128, 2, 128], F32, st) for i in range(2 * NSET)]
        MM = [S.sb("rw_MM%d" % i, [128, 2, 128], F32, st) for i in range(NSET)]
        Kh = [S.sb("rw_Kh%d" % i, [64, 128], F32, st) for i in range(NSET)]
        Gh = [S.sb("rw_Gh%d" % i, [128, 128], F32, st) for i in range(NSET)]
        E2 = [S.sb("rw_E2%d" % i, [128, 64], F32, st) for i in range(NSET)]
        ET = [S.sb("rw_ET%d" % i, [128, 64], F32, st) for i in range(NSET)]
        tmpS = S.sb("rw_tmpS", [64, 256], F32, st)
        nchunk = 0
        zc = C.zT

        def perhead(fn):
            for h in range(4):
                fn(h)

        def fl(t):
            return t[:, :, :].rearrange("p h t -> p (h t)")

        for mi in range(T // MT):
            t0 = mi * MT
            for j, tl in enumerate((r_t, k_t, v_t)):
                S.dma("sp", tl[:], zc[OFF_C + j * 256:OFF_C + (j + 1) * 256, t0:t0 + MT].rearrange("(h n) t -> n h t", n=64),
                      writes=[tl], sem_tile=tl)
            S.dma("sp", xw_t[:], zc[OFF_C + 768:OFF_C + 896, t0:t0 + MT].rearrange("(a n) t -> n a t", n=64), writes=[xw_t], sem_tile=xw_t)
            S.dma("sp", xg_t[:], zc[OFF_C + 896:OFF_C + 1024, t0:t0 + MT], writes=[xg_t], sem_tile=xg_t)
            S.op("act", lambda e: e.activation(out=xw_t[:, 0, :], in_=xw_t[:, 0, :], func=AF.Tanh), reads=[xw_t], writes=[xw_t])
            S.op("act", lambda e: e.activation(out=xg_t[:], in_=xg_t[:], func=AF.Sigmoid), reads=[xg_t], writes=[xg_t])
            for h in range(4):
                ps = C.psum()
                S.op("pe", lambda e, h=h, ps=ps: e.matmul(ps[0:64, 0:MT], lhsT=wup[:, h * 64:(h + 1) * 64], rhs=xw_t[:, 0, :], start=True, stop=True),
                     reads=[wup, xw_t], writes=[ps])
                S.op("act", lambda e, h=h, ps=ps: e.activation(out=lw_t[:, h, :], in_=ps[0:64, 0:MT], func=AF.Sigmoid, bias=w0c[:, h:h + 1], scale=1.0),
                     reads=[ps, w0c], writes=[lw_t])
                ps = C.psum()
                S.op("pe", lambda e, h=h, ps=ps: e.matmul(ps[0:64, 0:MT], lhsT=aup[:, h * 64:(h + 1) * 64], rhs=xw_t[:, 1, :], start=True, stop=True),
                     reads=[aup, xw_t], writes=[ps])
                S.op("act", lambda e, h=h, ps=ps: e.activation(out=a_t[:, h, :], in_=ps[0:64, 0:MT], func=AF.Sigmoid, bias=a0c[:, h:h + 1], scale=1.0),
                     reads=[ps, a0c], writes=[a_t])
                ps = C.psum()
                S.op("pe", lambda e, h=h, ps=ps: e.matmul(ps[0:64, 0:MT], lhsT=gup[:, h * 64:(h + 1) * 64], rhs=xg_t[:, :], start=True, stop=True),
                     reads=[gup, xg_t], writes=[ps])
                S.op("dve", lambda e, h=h, ps=ps: e.tensor_copy(out=g_t[:, h, :], in_=ps[0:64, 0:MT]), reads=[ps], writes=[g_t])
            S.op("pool", lambda e: e.tensor_scalar(out=fl(lw_t), in0=fl(lw_t), scalar1=-RW_C, scalar2=None, op0=ALU.mult), reads=[lw_t], writes=[lw_t])
            for h in range(4):
                S.op("dve", lambda e, h=h: e.tensor_scalar(out=ka_t[:, h, :], in0=k_t[:, h, :], scalar1=kkc[:, h:h + 1], scalar2=None, op0=ALU.mult),
                     reads=[k_t, kkc], writes=[ka_t])
            S.op("pool", lambda e: e.tensor_tensor(out=fl(x1), in0=fl(ka_t), in1=fl(ka_t), op=ALU.mult), reads=[ka_t], writes=[x1])
            for h in range(4):
                ps = C.psum()
                S.op("pe", lambda e, h=h, ps=ps: e.matmul(ps[0:64, 0:MT], lhsT=o64[:, :], rhs=x1[:, h, :], start=True, stop=True), reads=[o64, x1], writes=[ps])
                S.op("act", lambda e, h=h, ps=ps: e.activation(out=x2[:, h, :], in_=ps[0:64, 0:MT], func=AF.Sqrt), reads=[ps], writes=[x2])
            S.op("dve", lambda e: e.tensor_scalar(out=fl(x2), in0=fl(x2), scalar1=1e-12, scalar2=None, op0=ALU.max), reads=[x2], writes=[x2])
            S.op("dve", lambda e: e.reciprocal(out=fl(x2), in_=fl(x2)), reads=[x2], writes=[x2])
            S.op("pool", lambda e: e.tensor_tensor(out=fl(ka_t), in0=fl(ka_t), in1=fl(x2), op=ALU.mult), reads=[ka_t, x2], writes=[ka_t])
            for h in range(4):
                S.op("dve", lambda e, h=h: e.tensor_scalar(out=x1[:, h, :], in0=a_t[:, h, :], scalar1=-1.0, scalar2=kac[:, h:h + 1], op0=ALU.add, op1=ALU.mult),
                     reads=[a_t, kac], writes=[x1])
            S.op("dve", lambda e: e.scalar_tensor_tensor(out=fl(k_t), in0=fl(x1), scalar=1.0, in1=fl(k_t), op0=ALU.add, op1=ALU.mult),
                 reads=[x1, k_t], writes=[k_t])
            S.op("pool", lambda e: e.tensor_tensor(out=fl(b_t), in0=fl(ka_t), in1=fl(a_t), op=ALU.mult), reads=[ka_t, a_t], writes=[b_t])
            S.op("pool", lambda e: e.tensor_tensor(out=fl(x1), in0=fl(r_t), in1=fl(k_t), op=ALU.mult), reads=[r_t, k_t], writes=[x1])
            for h in range(4):
                S.op("dve", lambda e, h=h: e.tensor_scalar(out=x1[:, h, :], in0=x1[:, h, :], scalar1=rkc[:, h:h + 1], scalar2=None, op0=ALU.mult),
                     reads=[x1, rkc], writes=[x1])
            for h in range(4):
                ps = C.psum()
                S.op("pe", lambda e, h=h, ps=ps: e.matmul(ps[0:64, 0:MT], lhsT=o64[:, :], rhs=x1[:, h, :], start=True, stop=True), reads=[o64, x1], writes=[ps])
                S.op("dve", lambda e, h=h, ps=ps: e.tensor_tensor(out=bon_t[:, h, :], in0=ps[0:64, 0:MT], in1=v_t[:, h, :], op=ALU.mult),
                     reads=[ps, v_t], writes=[bon_t])
            S.op("dve", lambda e: e.tensor_tensor_scan(out=fl(G_t), data0=cmask[:, :], data1=fl(lw_t), initial=0.0, op0=ALU.mult, op1=ALU.add),
                 reads=[cmask, lw_t], writes=[G_t])
            S.op("act", lambda e: e.activation(out=fl(eG_t), in_=fl(G_t), func=AF.Exp), reads=[G_t], writes=[eG_t])
            S.op("pool", lambda e: e.tensor_tensor(out=fl(r_t), in0=fl(r_t), in1=fl(eG_t), op=ALU.mult), reads=[r_t, eG_t], writes=[r_t])
            S.op("pool", lambda e: e.tensor_tensor(out=fl(x1), in0=fl(G_t), in1=fl(lw_t), op=ALU.subtract), reads=[G_t, lw_t], writes=[x1])
            S.op("act", lambda e: e.activation(out=fl(x1), in_=fl(x1), func=AF.Exp), reads=[x1], writes=[x1])
            S.op("pool", lambda e: e.tensor_tensor(out=fl(ka_t), in0=fl(ka_t), in1=fl(x1), op=ALU.mult), reads=[ka_t, x1], writes=[ka_t])
            S.op("act", lambda e: e.activation(out=fl(x2), in_=fl(G_t), func=AF.Exp, scale=-1.0), reads=[G_t], writes=[x2])
            S.op("pool", lambda e: e.tensor_tensor(out=fl(b_t), in0=fl(b_t), in1=fl(x2), op=ALU.mult), reads=[b_t, x2], writes=[b_t])
            S.op("pool", lambda e: e.tensor_tensor(out=fl(k_t), in0=fl(k_t), in1=fl(x2), op=ALU.mult), reads=[k_t, x2], writes=[k_t])
            for j, (src, dst) in enumerate(((r_t, rC), (k_t, kC), (b_t, bC), (ka_t, kaC), (v_t, vC))):
                en = "act" if j % 2 else "pool"
                S.op(en, copy_op("act" if en == "act" else "dve", dst[:, :, :].rearrange("p c (h t) -> p h c t", h=4),
                                 src[:, :, :].rearrange("p h (c t) -> p h c t", t=32)), reads=[src], writes=[dst])
            def stage_a(c, si):
                TT, A_, B_, M_, Kh_, Gh_, E2_, Vb_ = TT_[si], AA[si], AB[si], MM[si], Kh[si], Gh[si], E2[si], Vbds[si]
                PPs = (PPx[2 * si], PPx[2 * si + 1])
                rc, kc_, bc, kac_, vc = rC[:, c, :], kC[:, c, :], bC[:, c, :], kaC[:, c, :], vC[:, c, :]
                ps = C.psum()
                for j, (src, srct) in enumerate(((bc, bC), (kc_, kC), (kac_, kaC), (vc, vC))):
                    S.op("pe", lambda e: e.transpose(out=ps[:, j * 64:(j + 1) * 64], in_=src, identity=C.ident[0:64, 0:64]),
                         reads=[srct, C.ident], writes=[ps])
                S.op("act", lambda e: e.copy(out=TT[:], in_=ps[:, 0:256].rearrange("p (a b) -> p a b", a=4)), reads=[ps], writes=[TT])
                for h in range(4):
                    S.op("pool", lambda e: e.tensor_copy(out=Vb_[32 * h:32 * h + 32, 64 * h:64 * h + 64], in_=TT[32 * h:32 * h + 32, 3, :]),
                         reads=[TT], writes=[Vb_])
                yield
                psA = C.psum()
                for j, (lt, ltt, rt, rtt) in enumerate(((bc, bC, kac_, kaC), (kc_, kC, kac_, kaC), (bc, bC, rc, rC), (kc_, kC, rc, rC))):
                    S.op("pe", lambda e: e.matmul(psA[:, j * 128:(j + 1) * 128], lhsT=RR(lt), rhs=RR(rt), start=True, stop=True),
                         reads=[ltt, rtt], writes=[psA])
                S.op("dve", lambda e: e.tensor_tensor(out=A_[:], in0=psA[:, :].rearrange("p (a b) -> p a b", a=4), in1=mA[:], op=ALU.mult),
                     reads=[psA, mA], writes=[A_])
                psB = C.psum()
                for j, (lt, ltt, rt, rtt) in enumerate(((kac_, kaC, bc, bC), (kac_, kaC, kc_, kC))):
                    S.op("pe", lambda e: e.matmul(psB[:, j * 128:(j + 1) * 128], lhsT=RR(lt), rhs=RR(rt), start=True, stop=True),
                         reads=[ltt, rtt], writes=[psB])
                S.op("dve", lambda e: e.tensor_tensor(out=B_[:], in0=psB[:, 0:256].rearrange("p (a b) -> p a b", a=2), in1=mB[:], op=ALU.mult),
                     reads=[psB, mB], writes=[B_])
                S.op("pool", lambda e: e.tensor_tensor(out=M_[:, 0, :], in0=C.ident[:], in1=A_[:, 0, :], op=ALU.subtract), reads=[A_, C.ident], writes=[M_])
                S.op("pool", lambda e: e.tensor_tensor(out=M_[:, 1, :], in0=C.ident[:], in1=B_[:, 0, :], op=ALU.subtract), reads=[B_, C.ident, M_], writes=[M_])
                yield
                cur = (A_[:, 0, :], B_[:, 0, :], A_, B_)
                for it in range(4):
                    p_ap, pt_ap, p_t1, p_t2 = cur
                    psQ = C.psum()
                    S.op("pe", lambda e: e.matmul(psQ[:, 0:128], lhsT=RR(pt_ap), rhs=RR(p_ap), start=True, stop=True), reads=[p_t1, p_t2], writes=[psQ])
                    S.op("pe", lambda e: e.matmul(psQ[:, 128:256], lhsT=RR(p_ap), rhs=RR(pt_ap), start=True, stop=True), reads=[p_t1, p_t2], writes=[psQ])
                    Pn = PPs[it % 2]
                    S.op("act", lambda e: e.copy(out=Pn[:], in_=psQ[:, 0:256].rearrange("p (a b) -> p a b", a=2)), reads=[psQ], writes=[Pn])
                    yield
                    psU = C.psum()
                    S.op("pe", lambda e: e.matmul(psU[:, 0:128], lhsT=RR(M_[:, 1, :]), rhs=RR(Pn[:, 0, :]), start=True, stop=True), reads=[M_, Pn], writes=[psU])
                    if it < 3:
                        S.op("pe", lambda e: e.matmul(psU[:, 128:256], lhsT=RR(Pn[:, 0, :]), rhs=RR(M_[:, 1, :]), start=True, stop=True), reads=[M_, Pn], writes=[psU])
                        S.op("dve", lambda e: e.tensor_tensor(out=M_[:], in0=psU[:, 0:256].rearrange("p (a b) -> p a b", a=2), in1=M_[:], op=ALU.add),
                             reads=[psU, M_], writes=[M_])
                    else:
                        S.op("dve", lambda e: e.tensor_tensor(out=M_[:, 0, :], in0=psU[:, 0:128], in1=M_[:, 0, :], op=ALU.add), reads=[psU, M_], writes=[M_])
                    cur = (Pn[:, 0, :], Pn[:, 1, :], Pn, Pn)
                    yield
                psK = C.psum()
                S.op("pe", lambda e: e.matmul(psK[0:64, 0:128], lhsT=TT[:, 2, :], rhs=M_[:, 0, :], start=True, stop=True), reads=[TT, M_], writes=[psK])
                S.op("act", lambda e: e.copy(out=Kh_[:], in_=psK[0:64, 0:128]), reads=[psK], writes=[Kh_])
                psG = C.psum()
                S.op("pe", lambda e: e.matmul(psG[:, 0:128], lhsT=RR(B_[:, 1, :]), rhs=RR(M_[:, 0, :]), start=True, stop=True), reads=[B_, M_], writes=[psG])
                S.op("act", lambda e: e.copy(out=Gh_[:], in_=psG[:, 0:128]), reads=[psG], writes=[Gh_])
                yield
                psE2 = C.psum()
                S.op("pe", lambda e: e.matmul(psE2[:, 0:64], lhsT=Gh_[:, :], rhs=TT[:, 3, :], start=True, stop=True), reads=[Gh_, TT], writes=[psE2])
                S.op("act", lambda e: e.copy(out=E2_[:], in_=psE2[:, 0:64]), reads=[psE2], writes=[E2_])
                yield

            def run_rr(gens):
                alive = list(gens)
                while alive:
                    for g_ in list(alive):
                        try:
                            next(g_)
                        except StopIteration:
                            alive.remove(g_)

            def stage_b(c0, n0):
                for c in range(c0, c0 + GRP):
                    cs = slice(32 * c, 32 * c + 32)
                    si = (n0 + c - c0) % NSET
                    TT, A_, B_, M_, Kh_, Gh_, E2_, ET_, Vbd = TT_[si], AA[si], AB[si], MM[si], Kh[si], Gh[si], E2[si], ET[si], Vbds[si]
                    Sc = S0[nSl[0] % 2]
                    Sn = S0[(nSl[0] + 1) % 2]
                    nSl[0] += 1
                    psE = C.psum()
                    S.op("pe", lambda e, psE=psE, Kh_=Kh_, Sc=Sc: e.matmul(psE[:, 0:256], lhsT=Kh_[:, :], rhs=Sc[:, :], start=True, stop=True),
                         reads=[Kh_, Sc], writes=[psE])
                    for h in range(4):
                        S.op("dve", lambda e, h=h, psE=psE, ET_=ET_, E2_=E2_: e.scalar_tensor_tensor(
                            out=ET_[32 * h:32 * h + 32, :], in0=psE[32 * h:32 * h + 32, 64 * h:64 * h + 64], scalar=-1.0,
                            in1=E2_[32 * h:32 * h + 32, :], op0=ALU.mult, op1=ALU.subtract), reads=[psE, E2_, ET_], writes=[ET_])
                    for h in range(4):
                        S.op("pool", lambda e, h=h, ET_=ET_: e.tensor_copy(out=Ebd[32 * h:32 * h + 32, 64 * h:64 * h + 64], in_=ET_[32 * h:32 * h + 32, :]),
                             reads=[ET_], writes=[Ebd])
                    yield
                    psY = C.psum()
                    S.op("pe", lambda e, psY=psY, TT=TT, A_=A_: e.matmul(psY[0:64, 0:128], lhsT=TT[:, 3, :], rhs=A_[:, 3, :], start=True, stop=False),
                         reads=[TT, A_], writes=[psY])
                    S.op("pe", lambda e, psY=psY, ET_=ET_, A_=A_: e.matmul(psY[0:64, 0:128], lhsT=ET_[:, :], rhs=A_[:, 2, :], start=False, stop=False),
                         reads=[ET_, A_], writes=[psY])
                    for h in range(4):
                        S.op("pe", lambda e, h=h, psY=psY, Sc=Sc, cs=cs: e.matmul(psY[0:64, 32 * h:32 * h + 32], lhsT=Sc[:, 64 * h:64 * h + 64], rhs=rC[:, c, 32 * h:32 * h + 32],
                                                                             start=False, stop=(h == 3)), reads=[Sc, rC], writes=[psY])
                    S.op("act", lambda e, psY=psY, cs=cs: e.copy(out=Y_t[:, :, cs], in_=psY[0:64, 0:128].rearrange("p (h t) -> p h t", h=4)), reads=[psY], writes=[Y_t])
                    yield
                    psS = C.psum()
                    S.op("pe", lambda e, psS=psS, TT=TT: e.matmul(psS[0:64, 0:256], lhsT=TT[:, 0, :], rhs=Ebd[:, :], start=True, stop=False),
                         reads=[TT, Ebd], writes=[psS])
                    S.op("pe", lambda e, psS=psS, TT=TT: e.matmul(psS[0:64, 0:256], lhsT=TT[:, 1, :], rhs=Vbd[:, :], start=False, stop=True),
                         reads=[TT, Vbd], writes=[psS])
                    S.op("dve", lambda e, psS=psS, Sc=Sc: e.tensor_tensor(out=tmpS[:], in0=psS[0:64, 0:256], in1=Sc[:], op=ALU.add), reads=[psS, Sc], writes=[tmpS])
                    for h in range(4):
                        S.op("dve", lambda e, h=h, Sn=Sn, c=c: e.tensor_scalar(out=Sn[:, 64 * h:64 * h + 64], in0=tmpS[:, 64 * h:64 * h + 64],
                                                                            scalar1=eG_t[:, h, 32 * c + 31:32 * c + 32], scalar2=None, op0=ALU.mult),
                             reads=[tmpS, eG_t, Sn], writes=[Sn])

                    yield

            ngrp = NCH // GRP
            run_rr([stage_a(q, (nchunk + q) % NSET) for q in range(GRP)])
            for gi_ in range(ngrp):
                gens = [stage_b(gi_ * GRP, nchunk + gi_ * GRP)]
                if gi_ + 1 < ngrp:
                    gens += [stage_a((gi_ + 1) * GRP + q, (nchunk + (gi_ + 1) * GRP + q) % NSET) for q in range(GRP)]
                run_rr(gens)
            nchunk += NCH
            for h in range(4):
                ps = C.psum()
                S.op("pe", lambda e, h=h, ps=ps: e.matmul(ps[0:64, 0:MT], lhsT=o64m[:, :], rhs=Y_t[:, h, :], start=True, stop=True), reads=[o64m, Y_t], writes=[ps])
                S.op("dve", lambda e, h=h, ps=ps: e.tensor_tensor(out=x1[:, h, :], in0=Y_t[:, h, :], in1=ps[0:64, 0:MT], op=ALU.subtract),
                     reads=[ps, Y_t], writes=[x1])
            S.op("pool", lambda e: e.tensor_tensor(out=fl(x2), in0=fl(x1), in1=fl(x1), op=ALU.mult), reads=[x1], writes=[x2])
            for h in range(4):
                ps = C.psum()
                S.op("pe", lambda e, h=h, ps=ps: e.matmul(ps[0:64, 0:MT], lhsT=o64m[:, :], rhs=x2[:, h, :], start=True, stop=True), reads=[o64m, x2], writes=[ps])
                S.op("act", lambda e, h=h, ps=ps: e.activation(out=G_t[:, h, :], in_=ps[0:64, 0:MT], func=AF.Sqrt, bias=C.rweps[0:64, 0:1], scale=1.0),
                     reads=[ps, C.rweps], writes=[G_t])
            S.op("dve", lambda e: e.reciprocal(out=fl(G_t), in_=fl(G_t)), reads=[G_t], writes=[G_t])
            S.op("pool", lambda e: e.tensor_tensor(out=fl(x1), in0=fl(x1), in1=fl(G_t), op=ALU.mult), reads=[x1, G_t], writes=[x1])
            for h in range(4):
                S.op("dve", lambda e, h=h: e.tensor_scalar(out=x1[:, h, :], in0=x1[:, h, :], scalar1=lgc[:, h:h + 1], scalar2=lbc[:, h:h + 1],
                                                           op0=ALU.mult, op1=ALU.add), reads=[x1, lgc, lbc], writes=[x1])
            S.op("pool", lambda e: e.tensor_tensor(out=fl(x1), in0=fl(x1), in1=fl(bon_t), op=ALU.add), reads=[x1, bon_t], writes=[x1])
            S.op("pool", lambda e: e.tensor_tensor(out=fl(o_t), in0=fl(x1), in1=fl(g_t), op=ALU.mult), reads=[x1, g_t], writes=[o_t])
            S.dma("sp", C.oT[512:768, t0:t0 + MT].rearrange("(h n) t -> n h t", n=64), o_t[:], reads=[o_t], sem_tile=o_t)
        S.barrier()
        for t_ in [wup, aup, gup, r_t, k_t, v_t, xw_t, xg_t, o_t]:
            S.release(t_)


TWO_PI = 2.0 * math.pi


def phase_s5(C, l):
    S, T, NT, P = C.S, C.T, C.NT, C.P
    with ExitStack() as st:
        def small(nm, n=8, dt=F32):
            return S.sb("s5_" + nm, [128, n], dt, st)
        are = load_cols(C, st, "s5_are", P["s5_a_re"][l].rearrange("(k a) p -> k (a p)", a=2), 8)
        aim = load_cols(C, st, "s5_aim", P["s5_a_im"][l].rearrange("(k a) p -> k (a p)", a=2), 8)
        ldt = load_bc(C, st, "s5_ldt", P["s5_log_dt"][l], 16)
        dcol = load_cols(C, st, "s5_d", P["s5_d"][l].rearrange("(a p) -> a p", p=128), 2)
        gbcol = load_cols(C, st, "s5_gb", P["s5_glu_b"][l].rearrange("(a p) -> a p", p=128), 2)
        gluw = S.sb("s5_gluw", [128, 2, 256], BF16, st)
        S.dma("pool", gluw[:], P["s5_glu_w"][l].rearrange("(a p) n -> p a n", p=128), writes=[gluw], sem_tile=gluw)
        negpi = small("negpi", 1)
        S.op("pool", lambda e: e.memset(negpi[:], 0.0), writes=[negpi])
        dt_, lre, th, mag, thr, sc, cc, abr, abi, den, cre, cim, ncre, t1, t2, Rre, Rim = (
            small(n) for n in ("dt", "lre", "th", "mag", "thr", "sc", "cc", "abr", "abi", "den", "cre", "cim", "ncre", "t1", "t2", "Rre", "Rim"))
        qi = S.sb("s5_qi", [128, 512], I32, st)
        qf = S.sb("s5_qf", [128, 512], F32, st)
        tb = S.sb("s5_tb", [128, 512], F32, st)
        jf = S.sb("s5_jf", [128, 512], F32, st)
        ang = S.sb("s5_ang", [128, 512], F32, st)

        def rr_sin(dst_ap, dst_t, src_ap, src_t, n, shift=0.0):
            a_, q_, f_, t_ = ang[:, 0:n], qi[:, 0:n], qf[:, 0:n], tb[:, 0:n]
            S.op("dve", lambda e: e.tensor_scalar(out=a_, in0=src_ap, scalar1=shift, scalar2=None, op0=ALU.add), reads=[src_t], writes=[ang])
            S.op("dve", lambda e: e.tensor_scalar(out=q_, in0=a_, scalar1=1.0 / TWO_PI, scalar2=None, op0=ALU.mult), reads=[ang], writes=[qi])
            S.op("dve", lambda e: e.tensor_copy(out=f_, in_=q_), reads=[qi], writes=[qf])
            S.op("dve", lambda e: e.scalar_tensor_tensor(out=a_, in0=f_, scalar=-TWO_PI, in1=a_, op0=ALU.mult, op1=ALU.add), reads=[qf, ang], writes=[ang])
            S.op("dve", lambda e: e.tensor_scalar(out=t_, in0=a_, scalar1=math.pi, scalar2=None, op0=ALU.is_gt), reads=[ang], writes=[tb])
            S.op("dve", lambda e: e.scalar_tensor_tensor(out=a_, in0=t_, scalar=-TWO_PI, in1=a_, op0=ALU.mult, op1=ALU.add), reads=[tb, ang], writes=[ang])
            S.op("dve", lambda e: e.tensor_scalar(out=t_, in0=a_, scalar1=-math.pi, scalar2=None, op0=ALU.is_lt), reads=[ang], writes=[tb])
            S.op("dve", lambda e: e.scalar_tensor_tensor(out=a_, in0=t_, scalar=TWO_PI, in1=a_, op0=ALU.mult, op1=ALU.add), reads=[tb, ang], writes=[ang])
            S.op("dve", lambda e: e.tensor_scalar(out=a_, in0=a_, scalar1=3.1415925, scalar2=-3.1415925, op0=ALU.min, op1=ALU.max), reads=[ang], writes=[ang])
            S.op("act", lambda e: e.activation(out=dst_ap, in_=a_, func=AF.Sin, bias=negpi[:, 0:1], scale=1.0), reads=[ang, negpi], writes=[dst_t])

        S.op("dve", lambda e: e.tensor_copy(out=dt_[0:64, :], in_=ldt[0:64, 0:16:2]), reads=[ldt], writes=[dt_])
        S.op("dve", lambda e: e.tensor_copy(out=dt_[64:128, :], in_=ldt[64:128, 1:16:2]), reads=[ldt, dt_], writes=[dt_])
        S.op("act", lambda e: e.activation(out=dt_[:], in_=dt_[:], func=AF.Exp), reads=[dt_], writes=[dt_])
        S.op("dve", lambda e: e.tensor_tensor(out=lre[:], in0=are[:], in1=dt_[:], op=ALU.mult), reads=[are, dt_], writes=[lre])
        S.op("dve", lambda e: e.tensor_tensor(out=th[:], in0=aim[:], in1=dt_[:], op=ALU.mult), reads=[aim, dt_], writes=[th])
        S.op("act", lambda e: e.activation(out=mag[:], in_=lre[:], func=AF.Exp), reads=[lre], writes=[mag])
        rr_sin(sc[:], sc, th[:], th, 8)
        rr_sin(cc[:], cc, th[:], th, 8, shift=math.pi / 2)
        rr_sin(t1[:], t1, th[:], th, 8)
        S.op("dve", lambda e: e.tensor_copy(out=thr[:], in_=ang[:, 0:8]), reads=[ang], writes=[thr])
        S.op("dve", lambda e: e.tensor_tensor(out=abr[:], in0=mag[:], in1=cc[:], op=ALU.mult), reads=[mag, cc], writes=[abr])
        S.op("dve", lambda e: e.tensor_tensor(out=abi[:], in0=mag[:], in1=sc[:], op=ALU.mult), reads=[mag, sc], writes=[abi])
        S.op("dve", lambda e: e.tensor_tensor(out=den[:], in0=are[:], in1=are[:], op=ALU.mult), reads=[are], writes=[den])
        S.op("dve", lambda e: e.tensor_tensor(out=t1[:], in0=aim[:], in1=aim[:], op=ALU.mult), reads=[aim], writes=[t1])
        S.op("dve", lambda e: e.tensor_tensor(out=den[:], in0=den[:], in1=t1[:], op=ALU.add), reads=[den, t1], writes=[den])
        S.op("dve", lambda e: e.reciprocal(out=den[:], in_=den[:]), reads=[den], writes=[den])
        S.op("dve", lambda e: e.tensor_scalar(out=t1[:], in0=abr[:], scalar1=-1.0, scalar2=None, op0=ALU.add), reads=[abr], writes=[t1])
        S.op("dve", lambda e: e.tensor_tensor(out=cre[:], in0=t1[:], in1=are[:], op=ALU.mult), reads=[t1, are], writes=[cre])
        S.op("dve", lambda e: e.tensor_tensor(out=t2[:], in0=abi[:], in1=aim[:], op=ALU.mult), reads=[abi, aim], writes=[t2])
        S.op("dve", lambda e: e.tensor_tensor(out=cre[:], in0=cre[:], in1=t2[:], op=ALU.add), reads=[cre, t2], writes=[cre])
        S.op("dve", lambda e: e.tensor_tensor(out=cre[:], in0=cre[:], in1=den[:], op=ALU.mult), reads=[cre, den], writes=[cre])
        S.op("dve", lambda e: e.tensor_tensor(out=cim[:], in0=abi[:], in1=are[:], op=ALU.mult), reads=[abi, are], writes=[cim])
        S.op("dve", lambda e: e.tensor_tensor(out=t2[:], in0=t1[:], in1=aim[:], op=ALU.mult), reads=[t1, aim], writes=[t2])
        S.op("dve", lambda e: e.tensor_tensor(out=cim[:], in0=cim[:], in1=t2[:], op=ALU.subtract), reads=[cim, t2], writes=[cim])
        S.op("dve", lambda e: e.tensor_tensor(out=cim[:], in0=cim[:], in1=den[:], op=ALU.mult), reads=[cim, den], writes=[cim])
        S.op("dve", lambda e: e.tensor_scalar(out=ncre[:], in0=cre[:], scalar1=-1.0, scalar2=None, op0=ALU.mult), reads=[cre], writes=[ncre])
        S.op("dve", lambda e: e.tensor_scalar(out=t2[:], in0=thr[:], scalar1=512.0, scalar2=None, op0=ALU.mult), reads=[thr], writes=[t2])
        rr_sin(Rim[:], Rim, t2[:], t2, 8)
        rr_sin(Rre[:], Rre, t2[:], t2, 8, shift=math.pi / 2)
        S.op("pool", lambda e: e.iota(qi[:], pattern=[[1, 512]], base=0, channel_multiplier=0), writes=[qi])
        S.op("dve", lambda e: e.tensor_copy(out=jf[:], in_=qi[:]), reads=[qi], writes=[jf])
        cosT = S.sb("s5_cosT", [128, 8, 512], F32, st)
        sinT = S.sb("s5_sinT", [128, 8, 512], F32, st)
        TiR = S.sb("s5_TiR", [128, 8, 512], F32, st)
        TiI = S.sb("s5_TiI", [128, 8, 512], F32, st)
        magT = S.sb("s5_magT", [128, 8, 512], F32, st)
        a2 = S.sb("s5_a2", [128, 512], F32, st)
        for k in range(8):
            S.op("pool", lambda e, k=k: e.tensor_scalar(out=a2[:], in0=jf[:], scalar1=thr[:, k:k + 1], scalar2=None, op0=ALU.mult), reads=[jf, thr], writes=[a2])
            rr_sin(sinT[:, k, :], sinT, a2[:], a2, 512)
            rr_sin(cosT[:, k, :], cosT, a2[:], a2, 512, shift=math.pi / 2)
            S.op("pool", lambda e, k=k: e.tensor_scalar(out=magT[:, k, :], in0=jf[:], scalar1=0.0, scalar2=mag[:, k:k + 1], op0=ALU.mult, op1=ALU.add),
                 reads=[jf, mag], writes=[magT])
            S.op("dve", lambda e, k=k: e.tensor_scalar(out=TiR[:, k, :], in0=cosT[:, k, :], scalar1=cre[:, k:k + 1], scalar2=None, op0=ALU.mult), reads=[cosT, cre], writes=[TiR])
            S.op("dve", lambda e, k=k: e.scalar_tensor_tensor(out=TiR[:, k, :], in0=sinT[:, k, :], scalar=cim[:, k:k + 1], in1=TiR[:, k, :], op0=ALU.mult, op1=ALU.add),
                 reads=[sinT, cim, TiR], writes=[TiR])
            S.op("dve", lambda e, k=k: e.tensor_scalar(out=TiI[:, k, :], in0=cosT[:, k, :], scalar1=cim[:, k:k + 1], scalar2=None, op0=ALU.mult), reads=[cosT, cim], writes=[TiI])
            S.op("dve", lambda e, k=k: e.scalar_tensor_tensor(out=TiI[:, k, :], in0=sinT[:, k, :], scalar=ncre[:, k:k + 1], in1=TiI[:, k, :], op0=ALU.mult, op1=ALU.add),
                 reads=[sinT, ncre, TiI], writes=[TiI])
        BT = [S.sb("s5_BT%d" % i, [16, 16, 64], BF16, st) for i in range(2)]
        CT = [S.sb("s5_CT%d" % i, [128, 8, 128], BF16, st) for i in range(2)]
        with ExitStack() as st2:
            braw = S.sb("s5_braw", [64, 16, 16], F32, st2)
            craw = S.sb("s5_craw", [16, 16, 64], F32, st2)
            for ri, (bk, ck) in enumerate((("s5_b_re", "s5_c_re"), ("s5_b_im", "s5_c_im"))):
                S.dma("sp", braw[:], P[bk][l].rearrange("g p c -> p g c"), writes=[braw], sem_tile=braw)
                for half in range(2):
                    ps = C.psum()
                    for g8 in range(8):
                        g = half * 8 + g8
                        S.op("pe", lambda e, g=g, g8=g8, ps=ps: e.transpose(out=ps[0:16, g8 * 64:(g8 + 1) * 64], in_=braw[:, g, :], identity=C.ident[0:64, 0:64]),
                             reads=[braw, C.ident], writes=[ps])
                    S.op("act", lambda e, half=half, ps=ps, ri=ri: e.copy(out=BT[ri][:, half * 8:(half + 1) * 8, :], in_=ps[0:16, :].rearrange("p (g q) -> p g q", g=8)),
                         reads=[ps], writes=[BT[ri]])
                S.op("pool", lambda e, ri=ri: e.memset(CT[ri][:], 0.0), writes=[CT[ri]])
                S.dma("sp", craw[:], P[ck][l].rearrange("g c p -> c g p"), writes=[craw], sem_tile=craw)
                for k in range(8):
                    ps = C.psum()
                    S.op("pe", lambda e, k=k, ps=ps: e.transpose(out=ps[:, 0:16], in_=craw[:, 2 * k:2 * k + 2, :].rearrange("c a p -> c (a p)"), identity=C.ident[0:16, 0:16]),
                         reads=[craw, C.ident], writes=[ps])
                    c0 = (k % 4) * 32
                    sgn = 1.0 if ri == 0 else -1.0
                    S.op("act", lambda e, k=k, ps=ps, ri=ri, c0=c0, sgn=sgn: e.activation(out=CT[ri][0:64, k, c0:c0 + 16], in_=ps[0:64, 0:16], func=AF.Copy, scale=sgn),
                         reads=[ps], writes=[CT[ri]])
                    S.op("act", lambda e, k=k, ps=ps, ri=ri, c0=c0, sgn=sgn: e.activation(out=CT[ri][64:128, k, c0 + 16:c0 + 32], in_=ps[64:128, 0:16], func=AF.Copy, scale=sgn),
                         reads=[ps], writes=[CT[ri]])
            S.barrier()
            S.release(braw)
            S.release(craw)
        u16b = [S.sb("s5_u16b%d" % i, [16, 16, 512], BF16, st) for i in range(1)]
        ufm = [S.sb("s5_ufm%d" % i, [128, 2, 512], F32, st) for i in range(2)]
        pr = [S.sb("s5_pr%d" % i, [128, 4, 512], F32, st) for i in range(1)] * 2
        win = [S.sb("s5_win%d" % i, [128, 2, 512], F32, st) for i in range(2)]
        ww = [S.sb("s5_w%d" % i, [128, 2, 512], F32, st) for i in range(2)]
        xr = [S.sb("s5_xr%d" % i, [128, 4, 512], F32, st) for i in range(1)] * 2
        xx = [S.sb("s5_x%d" % i, [128, 2, 512], BF16, st) for i in range(8)] * 2
        wl = S.sb("s5_wl", [128, 2, 8], F32, st)
        cy = [S.sb("s5_cy%d" % i, [128, 2], F32, st) for i in range(2)]
        ct1 = [S.sb("s5_ct%d" % i, [128, 2], F32, st) for i in range(2)]
        yv = [S.sb("s5_yv%d" % i, [128, 512], F32, st) for i in range(2)]
        yg = [S.sb("s5_yg%d" % i, [128, 2, 512], BF16, st) for i in range(2)]
        sgt = [S.sb("s5_sg%d" % i, [128, 512], BF16, st) for i in range(2)]
        ost = [S.sb("s5_o%d" % i, [128, 2, 512], BF16, st) for i in range(2)]
        n = 0
        for i in range(NT):
            t0 = i * 512
            ub_, uf_ = u16b[0], ufm[i % 2]
            S.dma("pool", ub_[:], C.zT[OFF_D:OFF_D + 256, t0:t0 + 512].rearrange("(g c) t -> c g t", c=16), writes=[ub_], sem_tile=ub_)
            S.dma("sp", uf_[:], C.zT[OFF_D:OFF_D + 256, t0:t0 + 512].rearrange("(a p) t -> p a t", p=128), writes=[uf_], sem_tile=uf_)
            for k in range(8):
                pr_, win_, w_, xr_, cy_, ct_ = pr[n % 2], win[n % 2], ww[n % 2], xr[n % 2], cy[n % 2], ct1[n % 2]
                x_ = xx[(i % 2) * 8 + k]
                n += 1
                psr = C.psum()
                psi = C.psum()
                for gl in range(2):
                    g = 2 * k + gl
                    S.op("pe", lambda e, g=g, gl=gl, psr=psr, ub_=ub_: e.matmul(psr[64 * gl:64 * gl + 64, :], lhsT=BT[0][:, g, :], rhs=ub_[:, g, :], start=True, stop=True),
                         reads=[BT[0], ub_], writes=[psr])
                    S.op("pe", lambda e, g=g, gl=gl, psi=psi, ub_=ub_: e.matmul(psi[64 * gl:64 * gl + 64, :], lhsT=BT[1][:, g, :], rhs=ub_[:, g, :], start=True, stop=True),
                         reads=[BT[1], ub_], writes=[psi])
                S.op("dve", lambda e, k=k, psr=psr, pr_=pr_: e.tensor_tensor(out=pr_[:, 0, :], in0=psr[:, :], in1=TiR[:, k, :], op=ALU.mult), reads=[psr, TiR], writes=[pr_])
                S.op("dve", lambda e, k=k, psi=psi, pr_=pr_: e.tensor_tensor(out=pr_[:, 1, :], in0=psi[:, :], in1=TiI[:, k, :], op=ALU.mult), reads=[psi, TiI, pr_], writes=[pr_])
                S.op("dve", lambda e, k=k, psi=psi, pr_=pr_: e.tensor_tensor(out=pr_[:, 2, :], in0=psi[:, :], in1=TiR[:, k, :], op=ALU.mult), reads=[psi, TiR, pr_], writes=[pr_])
                S.op("dve", lambda e, k=k, psr=psr, pr_=pr_: e.tensor_tensor(out=pr_[:, 3, :], in0=psr[:, :], in1=TiI[:, k, :], op=ALU.mult), reads=[psr, TiI, pr_], writes=[pr_])
                S.op("pool", lambda e, pr_=pr_, win_=win_: e.tensor_tensor(out=win_[:, 0, :], in0=pr_[:, 0, :], in1=pr_[:, 1, :], op=ALU.subtract), reads=[pr_], writes=[win_])
                S.op("pool", lambda e, pr_=pr_, win_=win_: e.tensor_tensor(out=win_[:, 1, :], in0=pr_[:, 2, :], in1=pr_[:, 3, :], op=ALU.add), reads=[pr_, win_], writes=[win_])
                if i == 0:
                    S.op("dve", lambda e, cy_=cy_: e.memset(cy_[:], 0.0), writes=[cy_])
                else:
                    S.op("dve", lambda e, k=k, ct_=ct_: e.tensor_scalar(out=ct_[:, 0:1], in0=wl[:, 1, k:k + 1], scalar1=Rim[:, k:k + 1], scalar2=None, op0=ALU.mult),
                         reads=[wl, Rim], writes=[ct_])
                    S.op("dve", lambda e, k=k, ct_=ct_: e.tensor_scalar(out=ct_[:, 1:2], in0=wl[:, 0, k:k + 1], scalar1=Rim[:, k:k + 1], scalar2=None, op0=ALU.mult),
                         reads=[wl, Rim, ct_], writes=[ct_])
                    S.op("dve", lambda e, k=k, ct_=ct_, cy_=cy_: e.scalar_tensor_tensor(out=cy_[:, 0:1], in0=wl[:, 0, k:k + 1], scalar=Rre[:, k:k + 1], in1=ct_[:, 0:1],
                                                                                     op0=ALU.mult, op1=ALU.subtract), reads=[wl, Rre, ct_], writes=[cy_])
                    S.op("dve", lambda e, k=k, ct_=ct_, cy_=cy_: e.scalar_tensor_tensor(out=cy_[:, 1:2], in0=wl[:, 1, k:k + 1], scalar=Rre[:, k:k + 1], in1=ct_[:, 1:2],
                                                                                     op0=ALU.mult, op1=ALU.add), reads=[wl, Rre, ct_, cy_], writes=[cy_])
                for c2 in range(2):
                    S.op("dve", lambda e, k=k, c2=c2, w_=w_, win_=win_, cy_=cy_: e.tensor_tensor_scan(
                        out=w_[:, c2, :], data0=magT[:, k, :], data1=win_[:, c2, :], initial=cy_[:, c2:c2 + 1], op0=ALU.mult, op1=ALU.add),
                        reads=[magT, win_, cy_, w_], writes=[w_])
                S.op("dve", lambda e, k=k, w_=w_: e.tensor_copy(out=wl[:, :, k:k + 1], in_=w_[:, :, 511:512]), reads=[w_, wl], writes=[wl])
                S.op("pool", lambda e, k=k, w_=w_, xr_=xr_: e.tensor_tensor(out=xr_[:, 0, :], in0=w_[:, 0, :], in1=cosT[:, k, :], op=ALU.mult), reads=[w_, cosT], writes=[xr_])
                S.op("pool", lambda e, k=k, w_=w_, xr_=xr_: e.tensor_tensor(out=xr_[:, 1, :], in0=w_[:, 1, :], in1=sinT[:, k, :], op=ALU.mult), reads=[w_, sinT, xr_], writes=[xr_])
                S.op("pool", lambda e, k=k, w_=w_, xr_=xr_: e.tensor_tensor(out=xr_[:, 2, :], in0=w_[:, 0, :], in1=sinT[:, k, :], op=ALU.mult), reads=[w_, sinT, xr_], writes=[xr_])
                S.op("pool", lambda e, k=k, w_=w_, xr_=xr_: e.tensor_tensor(out=xr_[:, 3, :], in0=w_[:, 1, :], in1=cosT[:, k, :], op=ALU.mult), reads=[w_, cosT, xr_], writes=[xr_])
                S.op("pool", lambda e, xr_=xr_, x_=x_: e.tensor_tensor(out=x_[:, 0, :], in0=xr_[:, 0, :], in1=xr_[:, 1, :], op=ALU.subtract), reads=[xr_], writes=[x_])
                S.op("pool", lambda e, xr_=xr_, x_=x_: e.tensor_tensor(out=x_[:, 1, :], in0=xr_[:, 2, :], in1=xr_[:, 3, :], op=ALU.add), reads=[xr_, x_], writes=[x_])
            yg_ = yg[i % 2]
            o_ = ost[i % 2]
            for ct in range(2):
                yv_ = yv[ct]
                psy = C.psum()
                for kk in range(4):
                    k = ct * 4 + kk
                    x_ = xx[(i % 2) * 8 + k]
                    S.op("pe", lambda e, k=k, kk=kk, psy=psy, x_=x_: e.matmul(psy[:, :], lhsT=CT[0][:, k, :], rhs=x_[:, 0, :], start=(kk == 0), stop=False),
                         reads=[CT[0], x_], writes=[psy])
                    S.op("pe", lambda e, k=k, kk=kk, psy=psy, x_=x_: e.matmul(psy[:, :], lhsT=CT[1][:, k, :], rhs=x_[:, 1, :], start=False, stop=(kk == 3)),
                         reads=[CT[1], x_], writes=[psy])
                S.op("dve", lambda e, ct=ct, psy=psy, yv_=yv_, uf_=uf_: e.scalar_tensor_tensor(out=yv_[:], in0=uf_[:, ct, :], scalar=dcol[:, ct:ct + 1], in1=psy[:, :],
                                                                                      op0=ALU.mult, op1=ALU.add), reads=[uf_, dcol, psy], writes=[yv_])
                S.op("act", lambda e, ct=ct, yv_=yv_, yg_=yg_: e.activation(out=yg_[:, ct, :], in_=yv_[:], func=AF.Gelu), reads=[yv_], writes=[yg_])
            for co in range(2):
                sg_ = sgt[co]
                psz = C.psum()
                for ct in range(2):
                    S.op("pe", lambda e, ct=ct, co=co, psz=psz, yg_=yg_: e.matmul(psz[:, :], lhsT=gluw[:, ct, co * 128:(co + 1) * 128], rhs=yg_[:, ct, :],
                                                                          start=(ct == 0), stop=(ct == 1)), reads=[gluw, yg_], writes=[psz])
                S.op("act", lambda e, co=co, psz=psz, sg_=sg_: e.activation(out=sg_[:], in_=psz[:, :], func=AF.Sigmoid, bias=gbcol[:, co:co + 1], scale=1.0),
                     reads=[psz, gbcol], writes=[sg_])
                S.op("pool", lambda e, co=co, sg_=sg_, yg_=yg_, o_=o_: e.tensor_tensor(out=o_[:, co, :], in0=yg_[:, co, :], in1=sg_[:], op=ALU.mult),
                     reads=[sg_, yg_], writes=[o_])
            S.dma("sp", C.oT[768:1024, t0:t0 + 512].rearrange("(a p) t -> p a t", p=128), o_[:], reads=[o_], sem_tile=o_)
        S.barrier()
        for t_ in [gluw, ldt] + u16b + ufm + ost:
            S.release(t_)


_NC_CACHE = {}


def kernel(**inputs):
    x = np.ascontiguousarray(np.asarray(inputs["x"], dtype=np.float32))
    mem = np.ascontiguousarray(np.asarray(inputs["mem"], dtype=np.float32))
    B, T, _ = x.shape
    if T not in _NC_CACHE:
        _NC_CACHE[T] = build(T, nlayers=DEPTH, dbg=False)
    nc = _NC_CACHE[T]
    params = {name: np.ascontiguousarray(np.asarray(inputs[name], dtype=np.float32)) for name, _ in PARAMS}
    n_cores = 8
    in_maps = []
    for c in range(n_cores):
        b = c % B
        m = {"x": x[b], "mem": mem[b]}
        m.update(params)
        in_maps.append(m)
    res = run_bass_kernel_spmd(nc, in_maps, core_ids=list(range(n_cores)))
    out = np.stack([np.asarray(res.results[b]["out"], dtype=np.float32) for b in range(B)], axis=0)
    return out


def phase_moe_sparse(C, l, out_ap):
    S, T, NT, P = C.S, C.T, C.NT, C.P
    NS = T // 128
    BLK = 512
    NB = C.MOE_NB
    w1flat = P["ex_w1"].rearrange("l e k n -> (l e k) n")
    w2flat = P["ex_w2"].rearrange("l e k n -> (l e k) n")
    U32 = mybir.dt.uint32
    with ExitStack() as st:
        g_bc = load_bc(C, st, "ms_g", P["ln3_g"][l], D)
        b_bc = load_bc(C, st, "ms_b", P["ln3_b"][l], D)
        e_all = S.sb("ms_e", [128, NS, 4], F32, st)
        r_all = S.sb("ms_r", [128, NS, 4], F32, st)
        w_all = S.sb("ms_w", [128, NS, 4], F32, st)
        d_all = S.sb("ms_d", [128, NS, 4], F32, st)
        d_int = S.sb("ms_di", [128, NS, 4], I32, st)
        blk = S.sb("ms_blk", [128, NB], F32, st)
        blk2 = S.sb("ms_blk2", [128, NB], F32, st)
        skp = S.sb("ms_skp", [128, NB], F32, st)
        oh_all = S.sb("ms_oh", [128, NB], F32, st)
        widx_f = S.sb("ms_wf", [128, NB, 8], F32, st)
        widx_i = S.sb("ms_wi", [128, NB, 8], I32, st)
        base = [S.sb("ms_base%d" % i, [128, NE], F32, st) for i in range(2)]
        iota_e = S.sb("ms_iotae", [128, NE], F32, st)
        iota_p = S.sb("ms_iotap", [128, 1], F32, st)
        iw = S.sb("ms_iw", [128, 8], F32, st)
        itmp = S.sb("ms_itmp", [128, NE], I32, st)
        Ls = S.sb("ms_Ls", [128, 128], F32, st)
        ones32 = S.sb("ms_ones32", [128, NE], F32, st)
        padf = S.sb("ms_padf", [128, NE], F32, st)
        pend = S.sb("ms_pend", [128, NE], F32, st)
        pstart = S.sb("ms_pstart", [128, NE], F32, st)
        b1all = S.sb("ms_b1", [32, 2048], BF16, st)
        b2all = S.sb("ms_b2", [32, 1024], BF16, st)
        S.dma("pool", b1all[:], P["ex_b1"][l], writes=[b1all], sem_tile=b1all)
        S.dma("pool", b2all[:], P["ex_b2"][l], writes=[b2all], sem_tile=b2all)
        S.op("pool", lambda e: e.iota(itmp[:], pattern=[[1, NE]], base=0, channel_multiplier=0), writes=[itmp])
        S.op("dve", lambda e: e.tensor_copy(out=iota_e[:], in_=itmp[:]), reads=[itmp], writes=[iota_e])
        S.op("pool", lambda e: e.iota(itmp[:, 0:1], pattern=[[1, 1]], base=0, channel_multiplier=1), reads=[iota_e], writes=[itmp])
        S.op("dve", lambda e: e.tensor_copy(out=iota_p[:], in_=itmp[:, 0:1]), reads=[itmp], writes=[iota_p])
        S.op("pool", lambda e: e.iota(itmp[:, 0:8], pattern=[[128, 8]], base=0, channel_multiplier=1), reads=[iota_p], writes=[itmp])
        S.op("dve", lambda e: e.tensor_copy(out=iw[:], in_=itmp[:, 0:8]), reads=[itmp], writes=[iw])
        S.op("pool", lambda e: e.memset(ones32[:], 1.0), writes=[ones32])
        S.op("pool", lambda e: e.memset(base[0][:], 0.0), writes=[base[0]])
        S.op("pool", lambda e: e.affine_select(out=Ls[:], in_=C.ones_f[:], compare_op=ALU.is_gt, fill=0.0, base=0,
                                               pattern=[[1, 128]], channel_multiplier=-1), reads=[C.ones_f], writes=[Ls])
        with ExitStack() as st2:
            rw = S.sb("rt_w", [128, 8, NE], F32, st2)
            S.dma("sp", rw[:], P["router_w"][l].rearrange("(kc p) n -> p kc n", p=128), writes=[rw], sem_tile=rw)
            rb = load_bc(C, st2, "rt_b", P["router_b"][l], NE)
            ht = [S.sb("rt_h%d" % i, [128, D], F32, st2) for i in range(2)]
            hTf = [S.sb("rt_hT%d" % i, [128, 8, 128], F32, st2) for i in range(2)]
            lg = [S.sb("rt_lg%d" % i, [128, NE], F32, st2) for i in range(2)]
            t8 = [S.sb("rt_t8%d" % i, [128, 8], F32, st2) for i in range(2)]
            mk = [S.sb("rt_mk%d" % i, [128, NE], F32, st2) for i in range(2)]
            rg = [S.sb("rt_rg%d" % i, [128, NE], F32, st2) for i in range(2)]
            ew = [S.sb("rt_ew%d" % i, [128, 4], F32, st2) for i in range(2)]
            sm = [S.sb("rt_sm%d" % i, [128, 2], F32, st2) for i in range(2)]
            tq = [S.sb("rt_tq%d" % i, [128, NE], F32, st2) for i in range(4)]
            ntq = 0
            for n in range(NS):
                h, hf, lg_, t8_, mk_, rg_, ew_, sm_ = ht[n % 2], hTf[n % 2], lg[n % 2], t8[n % 2], mk[n % 2], rg[n % 2], ew[n % 2], sm[n % 2]
                bc_, bn_ = base[n % 2], base[(n + 1) % 2]
                S.dma("sp", h[:], C.h_tok[n * 128:(n + 1) * 128, :], writes=[h], sem_tile=h)
                transpose_to(C, h[:], h, hf, lambda g, hf=hf: hf[:, g * 4:(g + 1) * 4, :])
                ps = C.psum()
                for kc in range(8):
                    S.op("pe", lambda e, kc=kc, ps=ps, hf=hf: e.matmul(ps[:, 0:NE], lhsT=hf[:, kc, :], rhs=rw[:, kc, :],
                                                                        start=(kc == 0), stop=(kc == 7)), reads=[hf, rw], writes=[ps])
                S.op("dve", lambda e: e.tensor_tensor(out=lg_[:], in0=ps[:, 0:NE], in1=rb[:], op=ALU.add), reads=[ps, rb], writes=[lg_])
                S.op("dve", lambda e: e.max(out=t8_[:], in_=lg_[:]), reads=[lg_], writes=[t8_])
                S.op("dve", lambda e: e.tensor_scalar(out=mk_[:], in0=lg_[:], scalar1=t8_[:, 3:4], scalar2=None, op0=ALU.is_ge), reads=[lg_, t8_], writes=[mk_])
                S.op("dve", lambda e: e.tensor_scalar(out=sm_[:, 0:1], in0=t8_[:, 0:1], scalar1=-1.0, scalar2=None, op0=ALU.mult), reads=[t8_], writes=[sm_])
                S.op("act", lambda e: e.activation(out=ew_[:], in_=t8_[:, 0:4], func=AF.Exp, bias=sm_[:, 0:1], scale=1.0), reads=[t8_, sm_], writes=[ew_])
                S.op("dve", lambda e: e.reduce_sum(out=sm_[:, 1:2], in_=ew_[:], axis=AX.X), reads=[ew_, sm_], writes=[sm_])
                S.op("dve", lambda e: e.reciprocal(out=sm_[:, 1:2], in_=sm_[:, 1:2]), reads=[sm_], writes=[sm_])
                S.op("dve", lambda e: e.tensor_scalar(out=w_all[:, n, :], in0=ew_[:], scalar1=sm_[:, 1:2], scalar2=None, op0=ALU.mult), reads=[ew_, sm_], writes=[w_all])
                ps2 = C.psum()
                S.op("pe", lambda e: e.matmul(ps2[:, 0:NE], lhsT=Ls[:, :], rhs=mk_[:, :], start=True, stop=True), reads=[Ls, mk_], writes=[ps2])
                S.op("pe", lambda e: e.matmul(ps2[:, NE:2 * NE], lhsT=C.ones_f[:, :], rhs=mk_[:, :], start=True, stop=True), reads=[C.ones_f, mk_], writes=[ps2])
                S.op("dve", lambda e: e.tensor_tensor(out=rg_[:], in0=ps2[:, 0:NE], in1=bc_[:], op=ALU.add), reads=[ps2, bc_], writes=[rg_])
                S.op("dve", lambda e: e.tensor_tensor(out=bn_[:], in0=ps2[:, NE:2 * NE], in1=bc_[:], op=ALU.add), reads=[ps2, bc_], writes=[bn_])
                for j in range(4):
                    ta, tb_ = tq[ntq % 4], tq[(ntq + 1) % 4]
                    ntq += 2
                    S.op("dve", lambda e: e.scalar_tensor_tensor(out=ta[:], in0=lg_[:], scalar=t8_[:, j:j + 1], in1=iota_e[:], op0=ALU.is_equal, op1=ALU.mult),
                         reads=[lg_, t8_, iota_e], writes=[ta])
                    S.op("dve", lambda e: e.reduce_sum(out=e_all[:, n, j:j + 1], in_=ta[:], axis=AX.X), reads=[ta], writes=[e_all])
                    S.op("dve", lambda e: e.scalar_tensor_tensor(out=tb_[:], in0=lg_[:], scalar=t8_[:, j:j + 1], in1=rg_[:], op0=ALU.is_equal, op1=ALU.mult),
                         reads=[lg_, t8_, rg_], writes=[tb_])
                    S.op("dve", lambda e: e.reduce_sum(out=r_all[:, n, j:j + 1], in_=tb_[:], axis=AX.X), reads=[tb_], writes=[r_all])
            cnt = base[NS % 2]
            S.op("dve", lambda e: e.tensor_scalar(out=padf[:], in0=cnt[:], scalar1=float(BLK - 1), scalar2=None, op0=ALU.add), reads=[cnt], writes=[padf])
            S.op("dve", lambda e: e.tensor_copy(out=itmp[:], in_=padf[:]), reads=[padf], writes=[itmp])
            S.op("dve", lambda e: e.tensor_scalar(out=itmp[:], in0=itmp[:], scalar1=9, scalar2=9, op0=ALU.arith_shift_right, op1=ALU.logical_shift_left),
                 reads=[itmp], writes=[itmp])
            S.op("dve", lambda e: e.tensor_copy(out=padf[:], in_=itmp[:]), reads=[itmp], writes=[padf])
            S.op("dve", lambda e: e.tensor_tensor_scan(out=pend[:], data0=ones32[:], data1=padf[:], initial=0.0, op0=ALU.mult, op1=ALU.add),
                 reads=[ones32, padf], writes=[pend])
            S.op("dve", lambda e: e.tensor_tensor(out=pstart[:], in0=pend[:], in1=padf[:], op=ALU.subtract), reads=[pend, padf], writes=[pstart])
            for n in range(NS):
                for j in range(4):
                    ta = tq[ntq % 4]
                    ntq += 1
                    S.op("dve", lambda e: e.scalar_tensor_tensor(out=ta[:], in0=iota_e[:], scalar=e_all[:, n, j:j + 1], in1=pstart[:], op0=ALU.is_equal, op1=ALU.mult),
                         reads=[iota_e, e_all, pstart], writes=[ta])
                    S.op("dve", lambda e: e.reduce_sum(out=d_all[:, n, j:j + 1], in_=ta[:], axis=AX.X), reads=[ta], writes=[d_all])
            fl3 = lambda t: t[:, :, :].rearrange("p a b -> p (a b)")
            S.op("dve", lambda e: e.tensor_tensor(out=fl3(d_all), in0=fl3(d_all), in1=fl3(r_all), op=ALU.add), reads=[d_all, r_all], writes=[d_all])
            S.op("dve", lambda e: e.tensor_copy(out=fl3(d_int), in_=fl3(d_all)), reads=[d_all], writes=[d_int])
            for b in range(NB):
                ta = tq[ntq % 4]
                ntq += 1
                S.op("dve", lambda e: e.tensor_scalar(out=ta[:], in0=pend[:], scalar1=float(b * BLK), scalar2=None, op0=ALU.is_le), reads=[pend], writes=[ta])
                S.op("dve", lambda e: e.reduce_sum(out=blk[:, b:b + 1], in_=ta[:], axis=AX.X), reads=[ta], writes=[blk])
            S.op("dve", lambda e: e.tensor_scalar(out=blk[:], in0=blk[:], scalar1=float(NE - 1), scalar2=None, op0=ALU.min), reads=[blk], writes=[blk])
            S.op("dve", lambda e: e.tensor_scalar(out=oh_all[:], in0=blk[:], scalar1=iota_p[:, 0:1], scalar2=None, op0=ALU.is_equal), reads=[blk, iota_p], writes=[oh_all])
            S.op("dve", lambda e: e.tensor_scalar(out=blk2[:], in0=blk[:], scalar1=1024.0, scalar2=float(l * NE * 1024), op0=ALU.mult, op1=ALU.add),
                 reads=[blk], writes=[blk2])
            S.op("dve", lambda e: e.memset(skp[:], 0.0), writes=[skp])
            S.op("dve", lambda e: e.tensor_tensor(out=skp[:, 2:NB], in0=blk[:, 2:NB], in1=blk[:, 0:NB - 2], op=ALU.is_equal), reads=[blk, skp], writes=[skp])
            S.op("dve", lambda e: e.scalar_tensor_tensor(out=blk2[:], in0=skp[:], scalar=float(1 << 22), in1=blk2[:], op0=ALU.mult, op1=ALU.add),
                 reads=[skp, blk2], writes=[blk2])
            for b in range(NB):
                S.op("dve", lambda e: e.tensor_scalar(out=widx_f[:, b, :], in0=iw[:], scalar1=blk2[:, b:b + 1], scalar2=None, op0=ALU.add),
                     reads=[iw, blk2], writes=[widx_f])
            S.op("dve", lambda e: e.tensor_copy(out=fl3(widx_i), in_=fl3(widx_f)), reads=[widx_f], writes=[widx_i])
            for n in range(NS):
                h = ht[n % 2]
                S.dma("sp", h[:], C.h_tok[n * 128:(n + 1) * 128, :], writes=[h], sem_tile=h)
                for j in range(4):
                    S.dma_fn("pool", lambda e: e.indirect_dma_start(
                        out=C.xs_d, out_offset=bass.IndirectOffsetOnAxis(d_int[:, n, j:j + 1].bitcast(U32), 0), in_=h[:], in_offset=None),
                        reads=[h, d_int], sem_tile=h)
            S.barrier()
            for t_ in [rw, rb] + ht:
                S.release(t_)
        with ExitStack() as st2:
            w1t = [S.sb("mo_w1%d" % i, [128, 8, 2048], BF16, st2) for i in range(2)]
            w2t = [S.sb("mo_w2%d" % i, [128, 8, 1024], BF16, st2) for i in range(2)]
            xsb = S.sb("mo_xs", [128, 4, 1024], F32, st2)
            ysb = S.sb("mo_ys", [128, 4, 1024], F32, st2)
            xT = [S.sb("mo_xT%d" % i, [128, 8, 512], BF16, st2) for i in range(2)]
            actT = [S.sb("mo_act%d" % i, [128, 8, 512], BF16, st2) for i in range(2)]
            ohb = [S.sb("mo_ohb%d" % i, [32, 512], BF16, st2) for i in range(2)]
            ones_r = S.sb("mo_onesr", [32, 512], F32, st2)
            S.op("pool", lambda e: e.memset(ones_r[:], 1.0), writes=[ones_r])
            gq = [S.sb("mo_gq%d" % i, [128, 512], F32, st2) for i in range(2)]
            sg = [S.sb("mo_sg%d" % i, [128, 512], F32, st2) for i in range(2)]
            uq = [S.sb("mo_uq%d" % i, [128, 512], F32, st2) for i in range(2)]
            nq = 0
            for b in range(NB):
                w1_, w2_, x_, a, oh_ = w1t[b % 2], w2t[b % 2], xT[b % 2], actT[b % 2], ohb[b % 2]
                for kc in range(8):
                    S.dma_fn("pool", lambda e: e.indirect_dma_start(
                        out=w1_[:, kc, :], out_offset=None, in_=w1flat, in_offset=bass.IndirectOffsetOnAxis(widx_i[:, b, kc:kc + 1].bitcast(U32), 0),
                        bounds_check=RegConst(2 * NE * 1024 - 1), oob_is_err=False),
                        reads=[widx_i], writes=[w1_], sem_tile=w1_)
                for kc in range(8):
                    S.dma_fn("pool", lambda e: e.indirect_dma_start(
                        out=w2_[:, kc, :], out_offset=None, in_=w2flat, in_offset=bass.IndirectOffsetOnAxis(widx_i[:, b, kc:kc + 1].bitcast(U32), 0),
                        bounds_check=RegConst(2 * NE * 1024 - 1), oob_is_err=False),
                        reads=[widx_i], writes=[w2_], sem_tile=w2_)
                S.dma("sp", xsb[:], C.xs_d[b * BLK:(b + 1) * BLK, :].rearrange("(s p) c -> p s c", p=128), writes=[xsb], sem_tile=xsb)
                for s4 in range(4):
                    transpose_to(C, xsb[:, s4, :], xsb, x_, lambda g, s4=s4, x_=x_: x_[:, g * 4:(g + 1) * 4, s4 * 128:(s4 + 1) * 128])
                S.op("dve", lambda e: e.tensor_scalar(out=oh_[:], in0=ones_r[:], scalar1=oh_all[0:32, b:b + 1], scalar2=None, op0=ALU.mult),
                     reads=[ones_r, oh_all], writes=[oh_])
                for ft in range(8):
                    g_, s_, u_ = gq[nq % 2], sg[nq % 2], uq[nq % 2]
                    nq += 1
                    psg = C.psum()
                    S.op("pe", lambda e: e.matmul(psg[:, :], lhsT=b1all[0:32, ft * 256:(ft + 1) * 256:2], rhs=oh_[:, :], start=True, stop=False),
                         reads=[b1all, oh_], writes=[psg])
                    for kc in range(8):
                        S.op("pe", lambda e: e.matmul(psg[:, :], lhsT=w1_[:, kc, ft * 256:(ft + 1) * 256:2], rhs=x_[:, kc, :], start=False, stop=(kc == 7)),
                             reads=[w1_, x_], writes=[psg])
                    psu = C.psum()
                    S.op("pe", lambda e: e.matmul(psu[:, :], lhsT=b1all[0:32, ft * 256 + 1:(ft + 1) * 256:2], rhs=oh_[:, :], start=True, stop=False),
                         reads=[b1all, oh_], writes=[psu])
                    for kc in range(8):
                        S.op("pe", lambda e: e.matmul(psu[:, :], lhsT=w1_[:, kc, ft * 256 + 1:(ft + 1) * 256:2], rhs=x_[:, kc, :], start=False, stop=(kc == 7)),
                             reads=[w1_, x_], writes=[psu])
                    S.op("dve", lambda e: e.tensor_scalar(out=g_[:], in0=psg[:, :], scalar1=7.0, scalar2=None, op0=ALU.min), reads=[psg], writes=[g_])
                    S.op("act", lambda e: e.activation(out=s_[:], in_=g_[:], func=AF.Sigmoid, scale=1.702), reads=[g_], writes=[s_])
                    S.op("dve", lambda e: e.tensor_scalar(out=u_[:], in0=psu[:, :], scalar1=7.0, scalar2=-7.0, op0=ALU.min, op1=ALU.max), reads=[psu], writes=[u_])
                    S.op("dve", lambda e: e.tensor_tensor(out=s_[:], in0=g_[:], in1=s_[:], op=ALU.mult), reads=[g_, s_], writes=[s_])
                    S.op("dve", lambda e: e.scalar_tensor_tensor(out=a[:, ft, :], in0=u_[:], scalar=1.0, in1=s_[:], op0=ALU.add, op1=ALU.mult),
                         reads=[s_, u_], writes=[a])
                for s4 in range(4):
                    for half in range(2):
                        ps = C.psum()
                        S.op("pe", lambda e: e.matmul(ps[:, :], lhsT=oh_[:, 0:128], rhs=b2all[0:32, half * 512:(half + 1) * 512], start=True, stop=False),
                             reads=[oh_, b2all], writes=[ps])
                        for ft in range(8):
                            S.op("pe", lambda e: e.matmul(ps[:, :], lhsT=a[:, ft, s4 * 128:(s4 + 1) * 128], rhs=w2_[:, ft, half * 512:(half + 1) * 512],
                                                          start=False, stop=(ft == 7)), reads=[a, w2_], writes=[ps])
                        en = evac_eng(C)
                        S.op(en, copy_op(en, ysb[:, s4, half * 512:(half + 1) * 512], ps[:, :]), reads=[ps], writes=[ysb])
                S.dma("sp", C.ys_d[b * BLK:(b + 1) * BLK, :].rearrange("(s p) c -> p s c", p=128), ysb[:], reads=[ysb], sem_tile=ysb)
            S.barrier()
            for t_ in w1t + w2t + [xsb, ysb]:
                S.release(t_)
        with ExitStack() as st2:
            acc = [S.sb("mc_acc%d" % i, [128, D], F32, st2) for i in range(2)]
            gj = [S.sb("mc_g%d" % i, [128, D], F32, st2) for i in range(4)]
            hTs = [S.sb("mc_hT%d" % i, [128, 8, 512], BF16, st2) for i in range(2)]
            ng = 0
            for n in range(NS):
                a_ = acc[n % 2]
                S.dma("sp", a_[:], C.h_tok[n * 128:(n + 1) * 128, :], writes=[a_], sem_tile=a_)
                S.op("act", lambda e: e.activation(out=a_[:], in_=a_[:], func=AF.Copy, scale=DN_ALPHA), reads=[a_], writes=[a_])
                for j in range(4):
                    g_ = gj[ng % 4]
                    ng += 1
                    S.dma_fn("pool", lambda e: e.indirect_dma_start(
                        out=g_[:], out_offset=None, in_=C.ys_d, in_offset=bass.IndirectOffsetOnAxis(d_int[:, n, j:j + 1].bitcast(U32), 0)),
                        reads=[d_int], writes=[g_], sem_tile=g_)
                    S.op("dve", lambda e: e.scalar_tensor_tensor(out=a_[:], in0=g_[:], scalar=w_all[:, n, j:j + 1], in1=a_[:], op0=ALU.mult, op1=ALU.add),
                         reads=[g_, w_all, a_], writes=[a_])
                ln_tile(C, (a_[:], a_), (a_[:], a_), g_bc, b_bc)
                store_h(C, st2, a_, hTs[(n // 4) % 2], n // 4, n % 4, out_ap)
            S.barrier()
            for t_ in acc + gj + hTs:
                S.release(t_)
        for t_ in [g_bc, b_bc, b1all, b2all]:
            S.release(t_)
```

```python
import math
from contextlib import ExitStack
import numpy as np
import concourse.bass as bass
import concourse.mybir as mybir
from concourse.bass_utils import run_bass_kernel_spmd

F32 = mybir.dt.float32
BF16 = mybir.dt.bfloat16
I32 = mybir.dt.int32
AF = mybir.ActivationFunctionType
ALU = mybir.AluOpType
AX = mybir.AxisListType

ENGS = ("pe", "dve", "act", "pool", "sp")
STORE_Q = "act"


class Tl:
    __slots__ = ("name", "t", "w", "r", "dkey")

    def __init__(self, name, t=None):
        self.name = name
        self.t = t
        self.w = None
        self.r = {}
        self.dkey = None

    def __getitem__(self, idx):
        return self.t[idx]


class _Proxy:
    def __init__(self):
        self.call = None

    def __getattr__(self, name):
        def rec(*a, **k):
            assert self.call is None
            self.call = (name, a, k)
        return rec


class RegConst:
    cache = {}

    def __init__(self, v):
        self.v = v

    def get(self, e):
        key = (id(e), self.v)
        if key not in RegConst.cache:
            RegConst.cache[key] = e.to_reg(self.v)
        return RegConst.cache[key]


def _record(fn):
    p = _Proxy()
    fn(p)
    name, a, k = p.call

    def run(e):
        k2 = {kk: (vv.get(e) if isinstance(vv, RegConst) else vv) for kk, vv in k.items()}
        return getattr(e, name)(*a, **k2)
    return run


class Sched:
    def __init__(self, nc, es):
        self.nc = nc
        self.es = es
        self.q = {e: [] for e in ENGS}
        self.cnt = {e: 0 for e in ENGS}
        self.seen = {e: {} for e in ENGS}
        self.sem = {}
        for e in ENGS:
            self.sem[e] = es.enter_context(nc.semaphore("c_" + e))
        self.dtot = {}
        self.ndsem = 0
        self.free_dsems = []

    def sb(self, name, shape, dt, st=None):
        self.uid = getattr(self, "uid", 0) + 1
        name = "t%d_%s" % (self.uid, name)
        t = (st or self.es).enter_context(self.nc.sbuf_tensor(name, list(shape), dt))
        return Tl(name, t)

    def ps(self, name, shape, dt=F32, st=None):
        name = "pp_" + name
        t = (st or self.es).enter_context(self.nc.psum_tensor(name, list(shape), dt))
        return Tl(name, t)

    def res(self, name):
        return Tl(name, None)

    def _dsem(self, tl):
        if tl.dkey is None:
            if self.free_dsems:
                tl.dkey = self.free_dsems.pop()
            else:
                k = "d%d" % self.ndsem
                self.ndsem += 1
                self.sem[k] = self.es.enter_context(self.nc.semaphore(k))
                self.dtot[k] = 0
                tl.dkey = k
        return tl.dkey

    def release(self, tl):
        if tl.dkey is not None:
            self.free_dsems.append(tl.dkey)
            tl.dkey = None

    def _waits(self, eng, reads, writes):
        waits = {}
        seen = self.seen[eng]

        def need(ev):
            if ev is None:
                return
            k, v = ev
            if k in self.dtot:
                v = self.dtot[k]
            elif k == eng and eng in ("pe", "sp"):
                return
            if seen.get(k, 0) < v and waits.get(k, 0) < v:
                waits[k] = v

        for t in reads:
            need(t.w)
        for t in writes:
            need(t.w)
            for k, v in t.r.items():
                need((k, v))
        for k, v in waits.items():
            seen[k] = v
        return list(waits.items())

    def _mark(self, ev, reads, writes):
        k, v = ev
        for t in reads:
            if t.r.get(k, 0) < v:
                t.r[k] = v
        for t in writes:
            t.w = ev
            t.r = {}

    def op(self, eng, fn, reads=(), writes=()):
        fn = _record(fn)
        waits = self._waits(eng, reads, writes)
        self.cnt[eng] += 1
        ev = (eng, self.cnt[eng])
        self._mark(ev, reads, writes)
        self.q[eng].append((waits, fn, (eng, 1)))

    def dma(self, q, out, in_, reads=(), writes=(), sem_tile=None, **kw):
        if q == "sp" and not writes:
            q = STORE_Q
        waits = self._waits(q, reads, writes)
        k = self._dsem(sem_tile)
        self.dtot[k] += 16
        ev = (k, self.dtot[k])
        self._mark(ev, reads, writes)
        self.q[q].append((waits, lambda e, out=out, in_=in_, kw=kw: e.dma_start(out=out, in_=in_, **kw), (k, 16)))

    def dma_fn(self, q, fn, reads=(), writes=(), sem_tile=None):
        waits = self._waits(q, reads, writes)
        k = self._dsem(sem_tile)
        self.dtot[k] += 16
        ev = (k, self.dtot[k])
        self._mark(ev, reads, writes)
        self.q[q].append((waits, _record(fn), (k, 16)))

    def barrier(self):
        waits = []
        seen = self.seen["sp"]
        for e in ENGS:
            if e != "sp" and seen.get(e, 0) < self.cnt[e]:
                waits.append((e, self.cnt[e]))
                seen[e] = self.cnt[e]
        for k, v in self.dtot.items():
            if seen.get(k, 0) < v:
                waits.append((k, v))
                seen[k] = v
        self.cnt["sp"] += 1
        ev = ("sp", self.cnt["sp"])
        self.q["sp"].append((waits, lambda e: e.nop(), ("sp", 1)))
        for e in ENGS:
            if e != "sp":
                self.q[e].append(([ev], None, None))
                self.seen[e]["sp"] = ev[1]
                for k, v in self.dtot.items():
                    self.seen[e][k] = v
                for e2 in ENGS:
                    self.seen[e][e2] = max(self.seen[e].get(e2, 0), self.cnt[e2])

    def emit(self):
        nc = self.nc
        self.barrier()
        with nc.Block() as block:
            def run(engname):
                def body(eng):
                    for waits, fn, inc in self.q[engname]:
                        for k, v in waits:
                            eng.wait_ge(self.sem[k], v)
                        if fn is not None:
                            ins = fn(eng)
                            ins.then_inc(self.sem[inc[0]], inc[1])
                return body
            block.tensor(run("pe"))
            block.vector(run("dve"))
            block.scalar(run("act"))
            block.gpsimd(run("pool"))
            block.sync(run("sp"))


D = 1024
NMEM = 256
OFF_A, OFF_B, OFF_C, OFF_D, OFF_G = 0, 768, 1280, 2304, 2560
N_IN = 6656
NE = 32
LN_EPS = 1e-5
DEPTH = 2
DN_ALPHA = (2 * DEPTH) ** 0.25

PARAMS = [
    ("ln_in_g", (D,)), ("ln_in_b", (D,)), ("w_in", (2, D, N_IN)), ("conv_w", (2, 3, 256)),
    ("sg_norm_g", (2, 256)), ("sg_norm_b", (2, 256)), ("sg_w", (2, 4, 128, 128)), ("sg_b", (2, 4, 128)),
    ("rw_mu", (2, 1024)), ("rw_w0", (2, 256)), ("rw_w_up", (2, 64, 256)), ("rw_a0", (2, 256)),
    ("rw_a_up", (2, 64, 256)), ("rw_g_up", (2, 128, 256)), ("rw_k_k", (2, 256)), ("rw_k_a", (2, 256)),
    ("rw_r_k", (2, 4, 64)), ("rw_ln_g", (2, 256)), ("rw_ln_b", (2, 256)),
    ("s5_a_re", (2, 16, 64)), ("s5_a_im", (2, 16, 64)), ("s5_b_re", (2, 16, 64, 16)), ("s5_b_im", (2, 16, 64, 16)),
    ("s5_c_re", (2, 16, 16, 64)), ("s5_c_im", (2, 16, 16, 64)), ("s5_d", (2, 256)), ("s5_log_dt", (2, 16)),
    ("s5_glu_w", (2, 256, 256)), ("s5_glu_b", (2, 256)), ("br_proj", (2, 4, 256, D)), ("gate_b", (2, 4, D)),
    ("w_out", (2, D, D)), ("ln1_g", (2, D)), ("ln1_b", (2, D)), ("xa_wq", (2, D, D)), ("xa_wk", (2, D, D)),
    ("xa_wv", (2, D, D)), ("xa_wo", (2, D, D)), ("ln2_g", (2, D)), ("ln2_b", (2, D)),
    ("router_w", (2, D, NE)), ("router_b", (2, NE)), ("ex_w1", (2, NE, D, 2 * D)), ("ex_b1", (2, NE, 2 * D)),
    ("ex_w2", (2, NE, D, D)), ("ex_b2", (2, NE, D)), ("ln3_g", (2, D)), ("ln3_b", (2, D)),
]


def col(ap1d):
    return ap1d.rearrange("(p o) -> p o", o=1)


class Ctx:
    pass


def build(T, nlayers=2, dbg=False, phases=None):
    assert T % 512 == 0
    NT = T // 512
    nc = bass.Bass("TRN2", target_bir_lowering=False)
    RegConst.cache = {}
    P = {}
    x_in = nc.dram_tensor("x", [T, D], F32, kind="ExternalInput").ap()
    mem_in = nc.dram_tensor("mem", [NMEM, D], F32, kind="ExternalInput").ap()
    for name, shp in PARAMS:
        P[name] = nc.dram_tensor(name, list(shp), F32, kind="ExternalInput").ap()
    out = nc.dram_tensor("out", [T, D], F32, kind="ExternalOutput").ap()
    skind = "ExternalOutput" if dbg else "Internal"

    def scr(name, shape, dt):
        return nc.dram_tensor(name, list(shape), dt, kind=skind).ap()

    h_tok = scr("h_tok", [T, D], F32)
    hT = scr("hT", [D, T], BF16)
    zT = scr("zT", [OFF_G, T], F32)
    zbv = scr("zbv", [T, 256], F32)
    gT = scr("gT", [4 * D, T], BF16)
    oT = scr("oT", [4 * 256, T], BF16)
    gate_d = scr("gate_d", [T, NE], F32)
    MOE_BLK = 512
    MOE_NB = (T * 4) // MOE_BLK + NE
    xs_d = scr("xs_d", [MOE_NB * MOE_BLK, D], F32)
    ys_d = scr("ys_d", [MOE_NB * MOE_BLK, D], F32)

    with ExitStack() as es:
        S = Sched(nc, es)
        C = Ctx()
        C.nc, C.S, C.P, C.T, C.NT = nc, S, P, T, NT
        C.ident = S.sb("ident", [128, 128], F32)
        S.op("pool", lambda e: e.memset(C.ident[:], 0.0), writes=[C.ident])
        S.op("pool", lambda e: e.affine_select(out=C.ident[:], in_=C.ident[:], compare_op=ALU.not_equal, fill=1.0,
                                               base=0, pattern=[[-1, 128]], channel_multiplier=1),
             reads=[C.ident], writes=[C.ident])
        C.ones_b = S.sb("ones_b", [128, 128], BF16)
        S.op("pool", lambda e: e.memset(C.ones_b[:], 1.0), writes=[C.ones_b])
        C.ones_f = S.sb("ones_f", [128, 128], F32)
        S.op("pool", lambda e: e.memset(C.ones_f[:], 1.0), writes=[C.ones_f])
        C.rweps = S.sb("rweps", [128, 1], F32)
        S.op("pool", lambda e: e.memset(C.rweps[:], 64e-5), writes=[C.rweps])
        C.psl = [S.ps("ps%d" % i, [128, 512], F32) for i in range(8)]
        C.psi = 0

        def psum():
            t = C.psl[C.psi % 8]
            C.psi += 1
            return t
        C.psum = psum
        C.lnst = [S.sb("lnst%d" % i, [128, 2, 6], F32) for i in range(2)]
        C.lnmv = [S.sb("lnmv%d" % i, [128, 2], F32) for i in range(2)]
        C.lnrs = [S.sb("lnrs%d" % i, [128, 1], F32) for i in range(2)]
        C.lni = 0
        C.evi = 0
        C.h_tok, C.hT, C.zT, C.zbv, C.gT, C.oT, C.gate_d = h_tok, hT, zT, zbv, gT, oT, gate_d
        C.x_in, C.mem_in, C.out = x_in, mem_in, out
        C.xs_d, C.ys_d, C.MOE_NB = xs_d, ys_d, MOE_NB

        ph = phases
        for l in range(nlayers):
            last = (l == nlayers - 1)
            if l == 0:
                phase_ln_in(C)
                S.barrier()
            if ph is None or "inproj" in ph:
                phase_inproj(C, l)
                S.barrier()
            if ph is None or "conv" in ph:
                phase_conv(C, l)
                S.barrier()
            if ph is None or "sgu" in ph:
                phase_sgu(C, l)
                S.barrier()
            if ph is None or "rwkv" in ph:
                phase_rwkv(C, l)
                S.barrier()
            if ph is None or "s5" in ph:
                phase_s5(C, l)
                S.barrier()
            if ph is None or "merge" in ph:
                phase_merge(C, l)
                S.barrier()
            if ph is None or "attn" in ph:
                phase_attn(C, l)
                S.barrier()
            if ph is None or "moe" in ph:
                phase_moe_sparse(C, l, C.out if last else None)
                S.barrier()
        S.emit()
    return nc


def evac_eng(C):
    C.evi += 1
    return "act" if C.evi % 2 else "dve"


def copy_op(eng_name, out, in_):
    if eng_name == "act":
        return lambda e: e.copy(out=out, in_=in_)
    return lambda e: e.tensor_copy(out=out, in_=in_)


def load_bc(C, st, name, ap1d, n, q="sp"):
    S = C.S
    t = S.sb(name, [128, n], F32, st)
    S.dma(q, t[:], ap1d.partition_broadcast(128), writes=[t], sem_tile=t)
    return t


def ln_tile(C, src, dst, g_bc, b_bc, eps=LN_EPS):
    S = C.S
    i = C.lni % 2
    C.lni += 1
    st, mv, rs = C.lnst[i], C.lnmv[i], C.lnrs[i]
    sa, da = src[0], dst[0]
    srct, dstt = src[1], dst[1]
    S.op("dve", lambda e: e.bn_stats(out=st[:, 0, :], in_=sa[:, 0:512]), reads=[srct], writes=[st])
    S.op("dve", lambda e: e.bn_stats(out=st[:, 1, :], in_=sa[:, 512:1024]), reads=[srct, st], writes=[st])
    S.op("dve", lambda e: e.bn_aggr(out=mv[:], in_=st[:].rearrange("p a b -> p (a b)")), reads=[st], writes=[mv])
    S.op("act", lambda e: e.activation(out=rs[:], in_=mv[:, 1:2], func=AF.Sqrt, bias=C.eps_col[:, 0:1], scale=1.0),
         reads=[mv, C.eps_col], writes=[rs])
    S.op("dve", lambda e: e.reciprocal(out=rs[:], in_=rs[:]), reads=[rs], writes=[rs])
    S.op("dve", lambda e: e.scalar_tensor_tensor(out=da, in0=sa, scalar=mv[:, 0:1], in1=g_bc[:], op0=ALU.subtract, op1=ALU.mult),
         reads=[srct, mv, g_bc], writes=[dstt])
    S.op("dve", lambda e: e.scalar_tensor_tensor(out=da, in0=da, scalar=rs[:, 0:1], in1=b_bc[:], op0=ALU.mult, op1=ALU.add),
         reads=[dstt, rs, b_bc], writes=[dstt])


def transpose_to(C, src_ap, src_t, dst_t, dst_fn, nblk=8):
    S = C.S
    for g in range(nblk // 4):
        ps = C.psum()
        for j in range(4):
            kc = g * 4 + j
            S.op("pe", lambda e, kc=kc, j=j, ps=ps: e.transpose(out=ps[:, j * 128:(j + 1) * 128],
                                                                 in_=src_ap[:, kc * 128:(kc + 1) * 128],
                                                                 identity=C.ident[:]),
                 reads=[src_t, C.ident], writes=[ps])
        en = evac_eng(C)
        S.op(en, copy_op(en, dst_fn(g), ps[:, :].rearrange("p (a b) -> p a b", a=4)), reads=[ps], writes=[dst_t])


def store_h(C, st, hts, hTs, tile_i, sub, out_ap=None):
    S = C.S
    tok0 = tile_i * 512 + sub * 128
    S.dma("sp", C.h_tok[tok0:tok0 + 128, :], hts[:], reads=[hts], sem_tile=hts)
    if out_ap is not None:
        S.dma("sp", out_ap[tok0:tok0 + 128, :], hts[:], reads=[hts], sem_tile=hts)
    transpose_to(C, hts[:], hts, hTs, lambda g: hTs[:, g * 4:(g + 1) * 4, sub * 128:(sub + 1) * 128])
    if sub == 3:
        S.dma("sp", C.hT.rearrange("(kc p) t -> p kc t", p=128)[:, :, tile_i * 512:(tile_i + 1) * 512], hTs[:],
              reads=[hTs], sem_tile=hTs)


def load_w_bf16(C, t, w_ap, q="pool"):
    C.S.dma(q, t[:], w_ap.rearrange("(kc p) n -> p kc n", p=128), writes=[t], sem_tile=t)


def phase_ln_in(C):
    S, T, NT = C.S, C.T, C.NT
    with ExitStack() as st:
        C.eps_col = S.sb("eps_col", [128, 1], F32)
        g_bc = load_bc(C, st, "lnin_g", C.P["ln_in_g"], D)
        b_bc = load_bc(C, st, "lnin_b", C.P["ln_in_b"], D)
        xt = [S.sb("lnin_x%d" % i, [128, D], F32, st) for i in range(2)]
        hTs = [S.sb("lnin_hT%d" % i, [128, 8, 512], BF16, st) for i in range(2)]
        S.op("pool", lambda e: e.memset(C.eps_col[:], LN_EPS), writes=[C.eps_col])
        n = 0
        for i in range(NT):
            for sub in range(4):
                x = xt[n % 2]
                n += 1
                tok0 = i * 512 + sub * 128
                S.dma("sp", x[:], C.x_in[tok0:tok0 + 128, :], writes=[x], sem_tile=x)
                ln_tile(C, (x[:], x), (x[:], x), g_bc, b_bc)
                store_h(C, st, x, hTs[i % 2], i, sub)
        S.barrier()
        for t in [g_bc, b_bc] + xt + hTs:
            S.release(t)


def load_cols(C, st, name, ap_rows, n, q="sp", m=128):
    S = C.S
    tmp = S.sb(name + "_r", [n, m], F32, st)
    res = S.sb(name, [m, n], F32, st)
    S.dma(q, tmp[:], ap_rows, writes=[tmp], sem_tile=tmp)
    ps = C.psum()
    S.op("pe", lambda e: e.transpose(out=ps[0:m, 0:n], in_=tmp[:, :], identity=C.ident[0:n, 0:n]),
         reads=[tmp, C.ident], writes=[ps])
    S.op("dve", lambda e: e.tensor_copy(out=res[:], in_=ps[0:m, 0:n]), reads=[ps], writes=[res])
    C.S.release_later = getattr(C.S, "release_later", [])
    return res


def phase_inproj(C, l):
    S, T, NT, P = C.S, C.T, C.NT, C.P
    w_in = P["w_in"][l]
    with ExitStack() as st:
        wbuf = [S.sb("ip_w%d" % i, [128, 8, 1024], BF16, st) for i in range(2)]
        wb2 = S.sb("ip_wb", [128, 8, 1024], BF16, st)
        hTh = [S.sb("ip_h%d" % i, [128, 8, 513], BF16, st) for i in range(2)]
        zst = [S.sb("ip_z%d" % i, [128, 4, 512], F32, st) for i in range(2)]
        gst = [S.sb("ip_g%d" % i, [128, 4, 512], BF16, st) for i in range(2)]
        gb = load_cols(C, st, "ip_gb", P["gate_b"][l].rearrange("i (j p) -> (i j) p", p=128), 32)
        groups = [("F", 0, 1024, 0), ("V", 1024, 256, 0), ("C", OFF_C, 1024, OFF_C), ("F", OFF_D, 256, OFF_D)]
        for i in range(4):
            groups.append(("G", OFF_G + i * 1024, 1024, i * 1024))
        nld = 0
        nst = 0
        for gi, (kind, c0, ncol, r0) in enumerate(groups):
            w = wbuf[gi % 2]
            if kind != "C":
                S.dma("pool", w[:, :, 0:ncol], w_in[:, c0:c0 + ncol].rearrange("(kc p) n -> p kc n", p=128),
                      writes=[w], sem_tile=w)
            else:
                with ExitStack() as st2:
                    wraw = S.sb("ip_wraw", [128, 8, 1024], F32, st2)
                    mu = load_bc(C, st2, "ip_mu", P["rw_mu"][l], 1024)
                    tmp = [S.sb("ip_tmp%d" % i, [128, 1024], F32, st2) for i in range(2)]
                    S.dma("sp", wraw[:], w_in[:, c0:c0 + ncol].rearrange("(kc p) n -> p kc n", p=128),
                          writes=[wraw], sem_tile=wraw)
                    for kc in range(8):
                        tk = tmp[kc % 2]
                        S.op("dve", lambda e, kc=kc, tk=tk: e.tensor_tensor(out=tk[:], in0=wraw[:, kc, :], in1=mu[:], op=ALU.mult),
                             reads=[wraw, mu], writes=[tk])
                        S.op("act", lambda e, kc=kc, tk=tk: e.copy(out=wb2[:, kc, :], in_=tk[:]), reads=[tk], writes=[wb2])
                        S.op("pool", lambda e, kc=kc, tk=tk: e.tensor_tensor(out=w[:, kc, :], in0=wraw[:, kc, :], in1=tk[:], op=ALU.subtract),
                             reads=[wraw, tk], writes=[w])
                    S.barrier()
                    for t_ in [wraw, mu] + tmp:
                        S.release(t_)
            for i in range(NT):
                hh = hTh[nld % 2]
                prev = hTh[(nld + 1) % 2]
                nld += 1
                t0 = i * 512
                S.dma("sp", hh[:, :, 1:513], C.hT.rearrange("(kc p) t -> p kc t", p=128)[:, :, t0:t0 + 512],
                      writes=[hh], sem_tile=hh)
                if kind == "C":
                    if i == 0:
                        S.op("pool", lambda e, hh=hh: e.memset(hh[:, :, 0:1], 0.0), writes=[hh])
                    else:
                        S.op("pool", lambda e, hh=hh, prev=prev: e.tensor_copy(out=hh[:, :, 0:1], in_=prev[:, :, 512:513]),
                             reads=[prev], writes=[hh])
                if kind == "V":
                    zs = zst[nst % 2]
                    nst += 1
                    for sub in range(4):
                        ps = C.psum()
                        for kc in range(8):
                            S.op("pe", lambda e, kc=kc, sub=sub, ps=ps, hh=hh: e.matmul(
                                ps[:, 0:256], lhsT=hh[:, kc, 1 + sub * 128:1 + (sub + 1) * 128], rhs=w[:, kc, 0:256],
                                start=(kc == 0), stop=(kc == 7)), reads=[hh, w], writes=[ps])
                        en = evac_eng(C)
                        S.op(en, copy_op(en, zs[:, sub, 0:256], ps[:, 0:256]), reads=[ps], writes=[zs])
                    S.dma("sp", C.zbv[t0:t0 + 512, :].rearrange("(s p) c -> p s c", p=128), zs[:, :, 0:256],
                          reads=[zs], sem_tile=zs)
                    continue
                nct = ncol // 128
                for cg in range(0, nct, 4):
                    ncg = min(4, nct - cg)
                    if kind == "G":
                        zs = gst[nst % 2]
                    else:
                        zs = zst[nst % 2]
                    nst += 1
                    for j in range(ncg):
                        ct = cg + j
                        ps = C.psum()
                        if kind == "C":
                            for kc in range(8):
                                S.op("pe", lambda e, kc=kc, ct=ct, ps=ps, hh=hh: e.matmul(
                                    ps[:, :], lhsT=w[:, kc, ct * 128:(ct + 1) * 128], rhs=hh[:, kc, 1:513],
                                    start=(kc == 0), stop=False), reads=[hh, w], writes=[ps])
                            for kc in range(8):
                                S.op("pe", lambda e, kc=kc, ct=ct, ps=ps, hh=hh: e.matmul(
                                    ps[:, :], lhsT=wb2[:, kc, ct * 128:(ct + 1) * 128], rhs=hh[:, kc, 0:512],
                                    start=False, stop=(kc == 7)), reads=[hh, wb2], writes=[ps])
                        else:
                            for kc in range(8):
                                S.op("pe", lambda e, kc=kc, ct=ct, ps=ps, hh=hh: e.matmul(
                                    ps[:, :], lhsT=w[:, kc, ct * 128:(ct + 1) * 128], rhs=hh[:, kc, 1:513],
                                    start=(kc == 0), stop=(kc == 7)), reads=[hh, w], writes=[ps])
                        if kind == "G":
                            gcol = (r0 // 128) + ct
                            S.op("act", lambda e, j=j, ps=ps, zs=zs, gcol=gcol: e.activation(
                                out=zs[:, j, :], in_=ps[:, :], func=AF.Sigmoid, bias=gb[:, gcol:gcol + 1], scale=1.0),
                                reads=[ps, gb], writes=[zs])
                        else:
                            en = evac_eng(C)
                            S.op(en, copy_op(en, zs[:, j, :], ps[:, :]), reads=[ps], writes=[zs])
                    dst = C.gT if kind == "G" else C.zT
                    rr = r0 + cg * 128
                    S.dma("sp", dst[rr:rr + ncg * 128, t0:t0 + 512].rearrange("(j p) t -> p j t", p=128), zs[:, 0:ncg, :],
                          reads=[zs], sem_tile=zs)
        S.barrier()
        for t_ in wbuf + [wb2] + hTh + zst + gst:
            S.release(t_)


def phase_conv(C, l):
    S, T, NT, P = C.S, C.T, C.NT, C.P
    with ExitStack() as st:
        cw = load_cols(C, st, "cv_w", P["conv_w"][l].rearrange("k (f p) -> (k f) p", p=128), 6)
        za = [S.sb("cv_za%d" % i, [128, 6, 512], F32, st) for i in range(2)]
        ch = [S.sb("cv_ch%d" % i, [128, 2, 514], F32, st) for i in range(2)]
        yt = [S.sb("cv_y%d" % i, [128, 512], F32, st) for i in range(2)]
        ost = [S.sb("cv_o%d" % i, [128, 2, 512], BF16, st) for i in range(2)]
        ny = 0
        for i in range(NT):
            t0 = i * 512
            z = za[i % 2]
            c = ch[i % 2]
            cp = ch[(i + 1) % 2]
            o = ost[i % 2]
            S.dma("sp", z[:], C.zT[0:768, t0:t0 + 512].rearrange("(j p) t -> p j t", p=128), writes=[z], sem_tile=z)
            if i == 0:
                S.op("pool", lambda e, c=c: e.memset(c[:, :, 0:2], 0.0), writes=[c])
            else:
                S.op("pool", lambda e, c=c, cp=cp: e.tensor_copy(out=c[:, :, 0:2], in_=cp[:, :, 512:514]), reads=[cp], writes=[c])
            S.op("pool", lambda e, c=c, z=z: e.tensor_tensor(out=c[:, :, 2:514], in0=z[:, 2:4, :], in1=z[:, 4:6, :], op=ALU.mult),
                 reads=[z, c], writes=[c])
            for f in range(2):
                y = yt[ny % 2]
                ny += 1
                S.op("dve", lambda e, f=f, y=y, c=c: e.tensor_scalar(out=y[:], in0=c[:, f, 2:514], scalar1=cw[:, 4 + f:5 + f],
                                                                      scalar2=None, op0=ALU.mult), reads=[c, cw], writes=[y])
                S.op("dve", lambda e, f=f, y=y, c=c: e.scalar_tensor_tensor(out=y[:], in0=c[:, f, 1:513], scalar=cw[:, 2 + f:3 + f],
                                                                             in1=y[:], op0=ALU.mult, op1=ALU.add), reads=[c, cw, y], writes=[y])
                S.op("dve", lambda e, f=f, y=y, c=c: e.scalar_tensor_tensor(out=y[:], in0=c[:, f, 0:512], scalar=cw[:, f:f + 1],
                                                                             in1=y[:], op0=ALU.mult, op1=ALU.add), reads=[c, cw, y], writes=[y])
                S.op("pool", lambda e, f=f, y=y, z=z, o=o: e.tensor_tensor(out=o[:, f, :], in0=y[:], in1=z[:, f, :], op=ALU.mult),
                     reads=[y, z], writes=[o])
            S.dma("sp", C.oT[0:256, t0:t0 + 512].rearrange("(f p) t -> p f t", p=128), o[:], reads=[o], sem_tile=o)
        S.barrier()
        for t_ in za + ch + yt + ost:
            S.release(t_)


def phase_sgu(C, l):
    S, T, NT, P = C.S, C.T, C.NT, C.P
    with ExitStack() as st:
        g_bc = load_bc(C, st, "sg_g", P["sg_norm_g"][l], 256)
        b_bc = load_bc(C, st, "sg_b", P["sg_norm_b"][l], 256)
        sb_bc = load_bc(C, st, "sg_sb", P["sg_b"][l].rearrange("g i -> (g i)"), 512)
        wraw = S.sb("sg_wraw", [128, 4, 128], F32, st)
        wsT = S.sb("sg_wsT", [128, 4, 128], F32, st)
        S.dma("sp", wraw[:], P["sg_w"][l].rearrange("g i j -> i g j"), writes=[wraw], sem_tile=wraw)
        ps = C.psum()
        for g in range(4):
            S.op("pe", lambda e, g=g: e.transpose(out=ps[:, g * 128:(g + 1) * 128], in_=wraw[:, g, :], identity=C.ident[:]),
                 reads=[wraw, C.ident], writes=[ps])
        S.op("dve", lambda e: e.tensor_copy(out=wsT[:], in_=ps[:, :].rearrange("p (g i) -> p g i", g=4)), reads=[ps], writes=[wsT])
        S.op("dve", lambda e: e.memset(wsT[64:128, :, 0:64], 0.0), reads=[wsT], writes=[wsT])
        vt = [S.sb("sg_v%d" % i, [128, 4, 256], F32, st) for i in range(2)]
        ut = [S.sb("sg_u%d" % i, [64, 4, 512], F32, st) for i in range(2)]
        ot = [S.sb("sg_o%d" % i, [64, 4, 512], BF16, st) for i in range(2)]
        svt = [S.sb("sg_sv%d" % i, [64, 512], F32, st) for i in range(2)]
        stt = [S.sb("sg_st%d" % i, [128, 6], F32, st) for i in range(2)]
        mvt = [S.sb("sg_mv%d" % i, [128, 2], F32, st) for i in range(2)]
        rst = [S.sb("sg_rs%d" % i, [128, 1], F32, st) for i in range(2)]
        n = 0
        for i in range(NT):
            t0 = i * 512
            v, u, o = vt[i % 2], ut[i % 2], ot[i % 2]
            S.dma("sp", v[:], C.zbv[t0:t0 + 512, :].rearrange("(s p) c -> p s c", p=128), writes=[v], sem_tile=v)
            S.dma("sp", u[:], C.zT[768:1024, t0:t0 + 512].rearrange("(g c) t -> c g t", c=64), writes=[u], sem_tile=u)
            for s in range(4):
                sx, mv, rs, sv = stt[n % 2], mvt[n % 2], rst[n % 2], svt[n % 2]
                n += 1
                S.op("dve", lambda e, s=s, sx=sx, v=v: e.bn_stats(out=sx[:], in_=v[:, s, :]), reads=[v], writes=[sx])
                S.op("dve", lambda e, sx=sx, mv=mv: e.bn_aggr(out=mv[:], in_=sx[:]), reads=[sx], writes=[mv])
                S.op("act", lambda e, mv=mv, rs=rs: e.activation(out=rs[:], in_=mv[:, 1:2], func=AF.Sqrt, bias=C.eps_col[:, 0:1], scale=1.0),
                     reads=[mv, C.eps_col], writes=[rs])
                S.op("dve", lambda e, rs=rs: e.reciprocal(out=rs[:], in_=rs[:]), reads=[rs], writes=[rs])
                S.op("dve", lambda e, s=s, v=v, mv=mv, rs=rs: e.tensor_scalar(out=v[:, s, :], in0=v[:, s, :], scalar1=mv[:, 0:1],
                                                                                scalar2=rs[:, 0:1], op0=ALU.subtract, op1=ALU.mult),
                     reads=[v, mv, rs], writes=[v])
                S.op("pool", lambda e, s=s, v=v: e.tensor_tensor(out=v[:, s, :], in0=v[:, s, :], in1=g_bc[:], op=ALU.mult),
                     reads=[v, g_bc], writes=[v])
                S.op("pool", lambda e, s=s, v=v: e.tensor_tensor(out=v[:, s, :], in0=v[:, s, :], in1=b_bc[:], op=ALU.add),
                     reads=[v, b_bc], writes=[v])
                ps = C.psum()
                for g in range(4):
                    S.op("pe", lambda e, g=g, s=s, v=v, ps=ps: e.matmul(ps[0:64, g * 128:(g + 1) * 128], lhsT=v[:, s, g * 64:(g + 1) * 64],
                                                                         rhs=wsT[:, g, :], start=True, stop=True),
                         reads=[v, wsT], writes=[ps])
                S.op("dve", lambda e, ps=ps, sv=sv: e.tensor_tensor(out=sv[:], in0=ps[0:64, :], in1=sb_bc[0:64, :], op=ALU.add),
                     reads=[ps, sb_bc], writes=[sv])
                S.op("pool", lambda e, s=s, sv=sv, u=u, o=o: e.tensor_tensor(out=o[:, :, s * 128:(s + 1) * 128],
                                                                              in0=sv[:, :].rearrange("c (g i) -> c g i", g=4),
                                                                              in1=u[:, :, s * 128:(s + 1) * 128], op=ALU.mult),
                     reads=[sv, u], writes=[o])
            S.dma("sp", C.oT[256:512, t0:t0 + 512].rearrange("(g c) t -> c g t", c=64), o[:], reads=[o], sem_tile=o)
        S.barrier()
        for t_ in [g_bc, b_bc, sb_bc, wraw] + vt + ut + ot:
            S.release(t_)


def phase_zero_branch(C, r0):
    S, T, NT = C.S, C.T, C.NT
    with ExitStack() as st:
        z = S.sb("zb_z", [128, 2, 512], BF16, st)
        S.op("pool", lambda e: e.memset(z[:], 0.0), writes=[z])
        for i in range(NT):
            S.dma("sp", C.oT[r0:r0 + 256, i * 512:(i + 1) * 512].rearrange("(f p) t -> p f t", p=128), z[:], reads=[z], sem_tile=z)
        S.barrier()
        S.release(z)


def proj_res_ln(C, st, inT, W, g_bc, b_bc, tile_i, hres, hTs, out_ap=None):
    S = C.S
    for sub in range(4):
        hr = hres[C.hri % 2]
        C.hri += 1
        tok0 = tile_i * 512 + sub * 128
        S.dma("sp", hr[:], C.h_tok[tok0:tok0 + 128, :], writes=[hr], sem_tile=hr)
        for half in range(2):
            ps = C.psum()
            for kc in range(8):
                S.op("pe", lambda e, kc=kc, ps=ps, half=half, sub=sub: e.matmul(
                    ps[:, :], lhsT=inT[:, kc, sub * 128:(sub + 1) * 128], rhs=W[:, kc, half * 512:(half + 1) * 512],
                    start=(kc == 0), stop=(kc == 7)), reads=[inT, W], writes=[ps])
            S.op("dve", lambda e, ps=ps, half=half, hr=hr: e.scalar_tensor_tensor(
                out=hr[:, half * 512:(half + 1) * 512], in0=hr[:, half * 512:(half + 1) * 512], scalar=DN_ALPHA, in1=ps[:, :],
                op0=ALU.mult, op1=ALU.add), reads=[hr, ps], writes=[hr])
        ln_tile(C, (hr[:], hr), (hr[:], hr), g_bc, b_bc)
        store_h(C, st, hr, hTs, tile_i, sub, out_ap)


def phase_merge(C, l):
    S, T, NT, P = C.S, C.T, C.NT, C.P
    with ExitStack() as st:
        brp = S.sb("mg_brp", [128, 8, 1024], BF16, st)
        S.dma("pool", brp[:], P["br_proj"][l].rearrange("i (kc p) n -> p (i kc) n", p=128), writes=[brp], sem_tile=brp)
        wout = S.sb("mg_wout", [128, 8, 1024], BF16, st)
        load_w_bf16(C, wout, P["w_out"][l])
        g_bc = load_bc(C, st, "mg_g", P["ln1_g"][l], D)
        b_bc = load_bc(C, st, "mg_b", P["ln1_b"][l], D)
        oTt = [S.sb("mg_o%d" % i, [128, 8, 512], BF16, st) for i in range(2)]
        gTt = [S.sb("mg_g%d" % i, [128, 4, 512], BF16, st) for i in range(2)]
        mT = [S.sb("mg_m%d" % i, [128, 8, 512], BF16, st) for i in range(2)]
        tm = [S.sb("mg_t%d" % i, [128, 4, 512], F32, st) for i in range(2)]
        hres = [S.sb("mg_hr%d" % i, [128, D], F32, st) for i in range(2)]
        hTs = [S.sb("mg_hT%d" % i, [128, 8, 512], BF16, st) for i in range(2)]
        C.hri = 0
        ng = 0
        for i in range(NT):
            t0 = i * 512
            o = oTt[i % 2]
            m = mT[i % 2]
            S.dma("sp", o[:], C.oT[:, t0:t0 + 512].rearrange("(j p) t -> p j t", p=128), writes=[o], sem_tile=o)
            for ct in range(8):
                g = gTt[ng % 2]
                t4 = tm[ng % 2]
                ng += 1
                S.dma("sp", g[:], C.gT.rearrange("(i ct p) t -> p i ct t", p=128, ct=8)[:, :, ct, t0:t0 + 512], writes=[g], sem_tile=g)
                for b in range(4):
                    ps = C.psum()
                    for kc in range(2):
                        S.op("pe", lambda e, b=b, kc=kc, ct=ct, ps=ps, o=o: e.matmul(
                            ps[:, :], lhsT=brp[:, b * 2 + kc, ct * 128:(ct + 1) * 128], rhs=o[:, b * 2 + kc, :],
                            start=(kc == 0), stop=(kc == 1)), reads=[brp, o], writes=[ps])
                    S.op("dve", lambda e, b=b, ps=ps, g=g, t4=t4: e.tensor_tensor(out=t4[:, b, :], in0=ps[:, :], in1=g[:, b, :], op=ALU.mult),
                         reads=[ps, g], writes=[t4])
                S.op("dve", lambda e, t4=t4: e.tensor_tensor(out=t4[:, 0:2, :], in0=t4[:, 0:2, :], in1=t4[:, 2:4, :], op=ALU.add),
                     reads=[t4], writes=[t4])
                S.op("dve", lambda e, t4=t4, m=m, ct=ct: e.tensor_tensor(out=m[:, ct, :], in0=t4[:, 0, :], in1=t4[:, 1, :], op=ALU.add),
                     reads=[t4], writes=[m])
            proj_res_ln(C, st, m, wout, g_bc, b_bc, i, hres, hTs[i % 2])
        S.barrier()
        for t_ in [brp, wout, g_bc, b_bc] + oTt + gTt + mT + hres + hTs:
            S.release(t_)


def phase_attn(C, l):
    S, T, NT, P = C.S, C.T, C.NT, C.P
    with ExitStack() as st:
        wq = S.sb("at_wq", [128, 8, 1024], BF16, st)
        wo = S.sb("at_wo", [128, 8, 1024], BF16, st)
        load_w_bf16(C, wq, P["xa_wq"][l])
        load_w_bf16(C, wo, P["xa_wo"][l])
        g_bc = load_bc(C, st, "at_g", P["ln2_g"][l], D)
        b_bc = load_bc(C, st, "at_b", P["ln2_b"][l], D)
        kT = S.sb("at_kT", [128, 8, 256], BF16, st)
        vv = S.sb("at_v", [128, 2, 1024], BF16, st)
        with ExitStack() as st2:
            wk = S.sb("at_wk", [128, 8, 1024], BF16, st2)
            wv = S.sb("at_wv", [128, 8, 1024], BF16, st2)
            load_w_bf16(C, wk, P["xa_wk"][l])
            load_w_bf16(C, wv, P["xa_wv"][l])
            mt = S.sb("at_mem", [128, 2, 1024], F32, st2)
            memT = S.sb("at_memT", [128, 8, 256], BF16, st2)
            S.dma("sp", mt[:], C.mem_in.rearrange("(s p) c -> p s c", p=128), writes=[mt], sem_tile=mt)
            for s in range(2):
                transpose_to(C, mt[:, s, :], mt, memT, lambda g, s=s: memT[:, g * 4:(g + 1) * 4, s * 128:(s + 1) * 128])
            for ct in range(8):
                ps = C.psum()
                for kc in range(8):
                    S.op("pe", lambda e, kc=kc, ct=ct, ps=ps: e.matmul(ps[:, 0:256], lhsT=wk[:, kc, ct * 128:(ct + 1) * 128],
                                                                        rhs=memT[:, kc, :], start=(kc == 0), stop=(kc == 7)),
                         reads=[wk, memT], writes=[ps])
                en = evac_eng(C)
                S.op(en, copy_op(en, kT[:, ct, :], ps[:, 0:256]), reads=[ps], writes=[kT])
            for s in range(2):
                for half in range(2):
                    ps = C.psum()
                    for kc in range(8):
                        S.op("pe", lambda e, kc=kc, s=s, half=half, ps=ps: e.matmul(
                            ps[:, :], lhsT=memT[:, kc, s * 128:(s + 1) * 128], rhs=wv[:, kc, half * 512:(half + 1) * 512],
                            start=(kc == 0), stop=(kc == 7)), reads=[wv, memT], writes=[ps])
                    en = evac_eng(C)
                    S.op(en, copy_op(en, vv[:, s, half * 512:(half + 1) * 512], ps[:, :]), reads=[ps], writes=[vv])
            S.barrier()
            for t_ in [wk, wv, mt]:
                S.release(t_)
        hTt = [S.sb("at_h%d" % i, [128, 8, 512], BF16, st) for i in range(2)]
        qT = [S.sb("at_q%d" % i, [128, 8, 512], BF16, st) for i in range(2)]
        aT = [S.sb("at_a%d" % i, [128, 8, 512], BF16, st) for i in range(2)]
        pt = [S.sb("at_p%d" % i, [128, 256], F32, st) for i in range(4)]
        pT = [S.sb("at_pT%d" % i, [128, 2, 128], BF16, st) for i in range(4)]
        mx = [S.sb("at_mx%d" % i, [128, 1], F32, st) for i in range(4)]
        sm = [S.sb("at_sm%d" % i, [128, 1], F32, st) for i in range(4)]
        hres = [S.sb("at_hr%d" % i, [128, D], F32, st) for i in range(2)]
        hTs = [S.sb("at_hT%d" % i, [128, 8, 512], BF16, st) for i in range(2)]
        C.hri = 0
        n = 0
        for i in range(NT):
            t0 = i * 512
            hh, q, a = hTt[i % 2], qT[i % 2], aT[i % 2]
            S.dma("sp", hh[:], C.hT.rearrange("(kc p) t -> p kc t", p=128)[:, :, t0:t0 + 512], writes=[hh], sem_tile=hh)
            for ct in range(8):
                ps = C.psum()
                for kc in range(8):
                    S.op("pe", lambda e, kc=kc, ct=ct, ps=ps, hh=hh: e.matmul(ps[:, :], lhsT=wq[:, kc, ct * 128:(ct + 1) * 128],
                                                                               rhs=hh[:, kc, :], start=(kc == 0), stop=(kc == 7)),
                         reads=[wq, hh], writes=[ps])
                S.op("act", lambda e, ct=ct, ps=ps, q=q: e.activation(out=q[:, ct, :], in_=ps[:, :], func=AF.Copy, scale=1.0 / 16.0),
                     reads=[ps], writes=[q])
            for sub in range(4):
                pss = []
                for hd in range(4):
                    ps = C.psum()
                    pss.append(ps)
                    for j in range(2):
                        S.op("pe", lambda e: e.matmul(ps[:, 0:256], lhsT=q[:, 2 * hd + j, sub * 128:(sub + 1) * 128], rhs=kT[:, 2 * hd + j, :],
                                                      start=(j == 0), stop=(j == 1)), reads=[q, kT], writes=[ps])
                for hd in range(4):
                    ps, p_, mx_, sm_ = pss[hd], pt[hd], mx[hd], sm[hd]
                    S.op("dve", lambda e: e.reduce_max(out=mx_[:], in_=ps[:, 0:256], axis=AX.X, negate=True), reads=[ps], writes=[mx_])
                    S.op("act", lambda e: e.activation(out=p_[:], in_=ps[:, 0:256], func=AF.Exp, bias=mx_[:, 0:1], scale=1.0, accum_out=sm_[:]),
                         reads=[ps, mx_], writes=[p_, sm_])
                    S.op("dve", lambda e: e.reciprocal(out=sm_[:], in_=sm_[:]), reads=[sm_], writes=[sm_])
                    S.op("dve", lambda e: e.tensor_scalar(out=p_[:], in0=p_[:], scalar1=sm_[:, 0:1], scalar2=None, op0=ALU.mult), reads=[p_, sm_], writes=[p_])
                for hd in range(4):
                    p_, pT_ = pt[hd], pT[hd]
                    ps2 = C.psum()
                    for j in range(2):
                        S.op("pe", lambda e: e.transpose(out=ps2[:, j * 128:(j + 1) * 128], in_=p_[:, j * 128:(j + 1) * 128], identity=C.ident[:]),
                             reads=[p_, C.ident], writes=[ps2])
                    S.op("act", lambda e: e.copy(out=pT_[:], in_=ps2[:, 0:256].rearrange("p (a b) -> p a b", a=2)), reads=[ps2], writes=[pT_])
                for hd in range(4):
                    pT_ = pT[hd]
                    ps3 = C.psum()
                    for j in range(2):
                        ct = 2 * hd + j
                        for mt_ in range(2):
                            S.op("pe", lambda e: e.matmul(ps3[:, j * 128:(j + 1) * 128], lhsT=vv[:, mt_, ct * 128:(ct + 1) * 128], rhs=pT_[:, mt_, :],
                                                          start=(mt_ == 0), stop=(mt_ == 1)), reads=[vv, pT_], writes=[ps3])
                    S.op("dve", lambda e: e.tensor_copy(out=a[:, 2 * hd:2 * hd + 2, sub * 128:(sub + 1) * 128],
                                                        in_=ps3[:, 0:256].rearrange("p (a b) -> p a b", a=2)), reads=[ps3], writes=[a])
            proj_res_ln(C, st, a, wo, g_bc, b_bc, i, hres, hTs[i % 2])
        S.barrier()
        for t_ in [wq, wo, g_bc, b_bc] + hTt + hres + hTs:
            S.release(t_)


def phase_router(C, l):
    S, T, NT, P = C.S, C.T, C.NT, C.P
    with ExitStack() as st:
        rw = S.sb("rt_w", [128, 8, NE], F32, st)
        S.dma("sp", rw[:], P["router_w"][l].rearrange("(kc p) n -> p kc n", p=128), writes=[rw], sem_tile=rw)
        rb = load_bc(C, st, "rt_b", P["router_b"][l], NE)
        ht = [S.sb("rt_h%d" % i, [128, D], F32, st) for i in range(2)]
        hTf = [S.sb("rt_hT%d" % i, [128, 8, 128], F32, st) for i in range(2)]
        lg = [S.sb("rt_lg%d" % i, [128, NE], F32, st) for i in range(2)]
        t8 = [S.sb("rt_t8%d" % i, [128, 8], F32, st) for i in range(2)]
        ex = [S.sb("rt_ex%d" % i, [128, NE], F32, st) for i in range(2)]
        mk = [S.sb("rt_mk%d" % i, [128, NE], F32, st) for i in range(2)]
        sm = [S.sb("rt_sm%d" % i, [128, 1], F32, st) for i in range(2)]
        for n in range(T // 128):
            h, hf, lg_, t8_, ex_, mk_, sm_ = ht[n % 2], hTf[n % 2], lg[n % 2], t8[n % 2], ex[n % 2], mk[n % 2], sm[n % 2]
            S.dma("sp", h[:], C.h_tok[n * 128:(n + 1) * 128, :], writes=[h], sem_tile=h)
            transpose_to(C, h[:], h, hf, lambda g, hf=hf: hf[:, g * 4:(g + 1) * 4, :])
            ps = C.psum()
            for kc in range(8):
                S.op("pe", lambda e, kc=kc, ps=ps, hf=hf: e.matmul(ps[:, 0:NE], lhsT=hf[:, kc, :], rhs=rw[:, kc, :],
                                                                    start=(kc == 0), stop=(kc == 7)), reads=[hf, rw], writes=[ps])
            S.op("dve", lambda e, ps=ps, lg_=lg_: e.tensor_tensor(out=lg_[:], in0=ps[:, 0:NE], in1=rb[:], op=ALU.add),
                 reads=[ps, rb], writes=[lg_])
            S.op("dve", lambda e, lg_=lg_, t8_=t8_: e.max(out=t8_[:], in_=lg_[:]), reads=[lg_], writes=[t8_])
            S.op("dve", lambda e, lg_=lg_, t8_=t8_, mk_=mk_: e.tensor_scalar(out=mk_[:], in0=lg_[:], scalar1=t8_[:, 3:4], scalar2=None, op0=ALU.is_ge),
                 reads=[lg_, t8_], writes=[mk_])
            S.op("dve", lambda e, lg_=lg_, t8_=t8_, ex_=ex_: e.tensor_scalar(out=ex_[:], in0=lg_[:], scalar1=t8_[:, 0:1], scalar2=None, op0=ALU.subtract),
                 reads=[lg_, t8_], writes=[ex_])
            S.op("act", lambda e, ex_=ex_: e.activation(out=ex_[:], in_=ex_[:], func=AF.Exp), reads=[ex_], writes=[ex_])
            S.op("dve", lambda e, ex_=ex_, mk_=mk_: e.tensor_tensor(out=ex_[:], in0=ex_[:], in1=mk_[:], op=ALU.mult), reads=[ex_, mk_], writes=[ex_])
            S.op("dve", lambda e, ex_=ex_, sm_=sm_: e.reduce_sum(out=sm_[:], in_=ex_[:], axis=AX.X), reads=[ex_], writes=[sm_])
            S.op("dve", lambda e, sm_=sm_: e.reciprocal(out=sm_[:], in_=sm_[:]), reads=[sm_], writes=[sm_])
            S.op("dve", lambda e, ex_=ex_, sm_=sm_: e.tensor_scalar(out=ex_[:], in0=ex_[:], scalar1=sm_[:, 0:1], scalar2=None, op0=ALU.mult),
                 reads=[ex_, sm_], writes=[ex_])
            S.dma("sp", C.gate_d[n * 128:(n + 1) * 128, :], ex_[:], reads=[ex_], sem_tile=ex_)
        S.barrier()
        for t_ in [rw, rb] + ht + ex:
            S.release(t_)


def phase_moe(C, l, out_ap):
    S, T, NT, P = C.S, C.T, C.NT, C.P
    ST = 1024 if T >= 1024 else 512
    nsub = ST // 128
    ntt = ST // 512
    with ExitStack() as st:
        g_bc = load_bc(C, st, "mo_g", P["ln3_g"][l], D)
        b_bc = load_bc(C, st, "mo_b", P["ln3_b"][l], D)
        w1t = [S.sb("mo_w1%d" % i, [128, 8, 2048], BF16, st) for i in range(2)]
        w2 = [S.sb("mo_w2%d" % i, [128, 8, 1024], BF16, st) for i in range(2)]
        b1r = [S.sb("mo_b1r%d" % i, [1, 2048], BF16, st) for i in range(2)]
        ones_row = S.sb("mo_ones", [1, 512], BF16, st)
        S.op("pool", lambda e: e.memset(ones_row[:], 1.0), writes=[ones_row])
        b2 = [S.sb("mo_b2%d" % i, [1, 1024], BF16, st) for i in range(2)]
        acc = S.sb("mo_acc", [128, nsub, 1024], F32, st)
        hTt = S.sb("mo_hT", [128, 8, ST], BF16, st)
        gt = S.sb("mo_gate", [128, nsub, NE], F32, st)
        actT = [S.sb("mo_act%d" % i, [128, 8, 512], BF16, st) for i in range(2)]
        gq = [S.sb("mo_gq%d" % i, [128, 512], F32, st) for i in range(2)]
        sg = [S.sb("mo_sg%d" % i, [128, 512], F32, st) for i in range(2)]
        uq = [S.sb("mo_uq%d" % i, [128, 512], F32, st) for i in range(2)]
        hTs = [S.sb("mo_hTs%d" % i, [128, 8, 512], BF16, st) for i in range(1)] * 2
        w1 = P["ex_w1"][l]
        nw = 0
        na = 0
        nq = 0
        for sti in range(T // ST):
            tok0 = sti * ST
            S.dma("sp", acc[:], C.h_tok[tok0:tok0 + ST, :].rearrange("(s p) c -> p s c", p=128), writes=[acc], sem_tile=acc)
            S.dma("sp", hTt[:], C.hT.rearrange("(kc p) t -> p kc t", p=128)[:, :, tok0:tok0 + ST], writes=[hTt], sem_tile=hTt)
            S.dma("sp", gt[:], C.gate_d[tok0:tok0 + ST, :].rearrange("(s p) c -> p s c", p=128), writes=[gt], sem_tile=gt)
            S.op("pool", lambda e: e.tensor_scalar(out=acc[:], in0=acc[:], scalar1=DN_ALPHA, scalar2=None, op0=ALU.mult),
                 reads=[acc], writes=[acc])
            for ex in range(NE):
                w1_, w2_, b1r_, b2_ = w1t[nw % 2], w2[nw % 2], b1r[nw % 2], b2[nw % 2]
                nw += 1
                S.dma("pool", w1_[:], w1[ex].rearrange("(kc p) n -> p kc n", p=128), writes=[w1_], sem_tile=w1_)
                S.dma("pool", w2_[:], P["ex_w2"][l, ex].rearrange("(kc p) n -> p kc n", p=128), writes=[w2_], sem_tile=w2_)
                S.dma("pool", b2_[:], P["ex_b2"][l, ex:ex + 1, :], writes=[b2_], sem_tile=b2_)
                S.dma("pool", b1r_[:], P["ex_b1"][l, ex:ex + 1, :], writes=[b1r_], sem_tile=b1r_)
                for tt in range(ntt):
                    a = actT[na % 2]
                    na += 1
                    for ft in range(8):
                        g_, s_, u_ = gq[nq % 2], sg[nq % 2], uq[nq % 2]
                        nq += 1
                        psg = C.psum()
                        S.op("pe", lambda e, ft=ft, psg=psg, b1r_=b1r_: e.matmul(
                            psg[:, :], lhsT=b1r_[0:1, ft * 256:(ft + 1) * 256:2], rhs=ones_row[0:1, :], start=True, stop=False),
                            reads=[b1r_, ones_row], writes=[psg])
                        for kc in range(8):
                            S.op("pe", lambda e, kc=kc, ft=ft, psg=psg, w1_=w1_, tt=tt: e.matmul(
                                psg[:, :], lhsT=w1_[:, kc, ft * 256:(ft + 1) * 256:2], rhs=hTt[:, kc, tt * 512:(tt + 1) * 512],
                                start=False, stop=(kc == 7)), reads=[w1_, hTt], writes=[psg])
                        psu = C.psum()
                        S.op("pe", lambda e, ft=ft, psu=psu, b1r_=b1r_: e.matmul(
                            psu[:, :], lhsT=b1r_[0:1, ft * 256 + 1:(ft + 1) * 256:2], rhs=ones_row[0:1, :], start=True, stop=False),
                            reads=[b1r_, ones_row], writes=[psu])
                        for kc in range(8):
                            S.op("pe", lambda e, kc=kc, ft=ft, psu=psu, w1_=w1_, tt=tt: e.matmul(
                                psu[:, :], lhsT=w1_[:, kc, ft * 256 + 1:(ft + 1) * 256:2], rhs=hTt[:, kc, tt * 512:(tt + 1) * 512],
                                start=False, stop=(kc == 7)), reads=[w1_, hTt], writes=[psu])
                        S.op("dve", lambda e, psg=psg, g_=g_: e.tensor_scalar(
                            out=g_[:], in0=psg[:, :], scalar1=7.0, scalar2=None, op0=ALU.min), reads=[psg], writes=[g_])
                        S.op("act", lambda e, g_=g_, s_=s_: e.activation(out=s_[:], in_=g_[:], func=AF.Sigmoid, scale=1.702),
                             reads=[g_], writes=[s_])
                        S.op("dve", lambda e, psu=psu, u_=u_: e.tensor_scalar(
                            out=u_[:], in0=psu[:, :], scalar1=7.0, scalar2=-7.0, op0=ALU.min, op1=ALU.max), reads=[psu], writes=[u_])
                        S.op("pool", lambda e, g_=g_, s_=s_: e.tensor_tensor(out=s_[:], in0=g_[:], in1=s_[:], op=ALU.mult),
                             reads=[g_, s_], writes=[s_])
                        S.op("dve", lambda e, ft=ft, s_=s_, u_=u_, a=a: e.scalar_tensor_tensor(out=a[:, ft, :], in0=u_[:], scalar=1.0, in1=s_[:],
                                                                                         op0=ALU.add, op1=ALU.mult), reads=[s_, u_], writes=[a])
                    for sub in range(4):
                        s_idx = tt * 4 + sub
                        for half in range(2):
                            ps = C.psum()
                            S.op("pe", lambda e, ps=ps, half=half, b2_=b2_: e.matmul(
                                ps[:, :], lhsT=C.ones_b[0:1, :], rhs=b2_[0:1, half * 512:(half + 1) * 512], start=True, stop=False),
                                reads=[C.ones_b, b2_], writes=[ps])
                            for ft in range(8):
                                S.op("pe", lambda e, ft=ft, ps=ps, half=half, sub=sub, a=a, w2_=w2_: e.matmul(
                                    ps[:, :], lhsT=a[:, ft, sub * 128:(sub + 1) * 128], rhs=w2_[:, ft, half * 512:(half + 1) * 512],
                                    start=False, stop=(ft == 7)), reads=[a, w2_], writes=[ps])
                            S.op("dve", lambda e, ps=ps, half=half, s_idx=s_idx, ex=ex: e.scalar_tensor_tensor(
                                out=acc[:, s_idx, half * 512:(half + 1) * 512], in0=ps[:, :], scalar=gt[:, s_idx, ex:ex + 1],
                                in1=acc[:, s_idx, half * 512:(half + 1) * 512], op0=ALU.mult, op1=ALU.add),
                                reads=[ps, gt, acc], writes=[acc])
            for tt in range(ntt):
                tile_i = sti * ntt + tt
                for sub in range(4):
                    s_idx = tt * 4 + sub
                    ln_tile(C, (acc[:, s_idx, :], acc), (acc[:, s_idx, :], acc), g_bc, b_bc)
                    S_store_sub(C, acc, s_idx, hTs[tile_i % 2], tile_i, sub, out_ap)
        S.barrier()
        for t_ in [g_bc, b_bc, acc, hTt, gt] + w1t + w2 + b1r + b2 + hTs:
            S.release(t_)


def S_store_sub(C, acc, s_idx, hTs, tile_i, sub, out_ap):
    S = C.S
    tok0 = tile_i * 512 + sub * 128
    S.dma("sp", C.h_tok[tok0:tok0 + 128, :], acc[:, s_idx, :], reads=[acc], sem_tile=acc)
    if out_ap is not None:
        S.dma("sp", out_ap[tok0:tok0 + 128, :], acc[:, s_idx, :], reads=[acc], sem_tile=acc)
    transpose_to(C, acc[:, s_idx, :], acc, hTs, lambda g: hTs[:, g * 4:(g + 1) * 4, sub * 128:(sub + 1) * 128])
    if sub == 3:
        S.dma("sp", C.hT.rearrange("(kc p) t -> p kc t", p=128)[:, :, tile_i * 512:(tile_i + 1) * 512], hTs[:],
              reads=[hTs], sem_tile=hTs)


RW_EPS = 64e-5
RW_FP32R = False
RW_C = math.exp(-0.5)


def phase_rwkv(C, l):
    S, T, P = C.S, C.T, C.P
    MT = 256
    NCH = MT // 32
    with ExitStack() as st:
        def cols64(nm, key):
            return load_cols(C, st, nm, P[key][l].rearrange("(h n) -> h n", n=64), 4, m=64)
        w0c, a0c, kkc, kac, lgc, lbc = (cols64("rw_" + k, "rw_" + k) for k in ("w0", "a0", "k_k", "k_a", "ln_g", "ln_b"))
        rkc = load_cols(C, st, "rw_rk", P["rw_r_k"][l], 4, m=64)
        wup = S.sb("rw_wup", [64, 256], F32, st)
        aup = S.sb("rw_aup", [64, 256], F32, st)
        gup = S.sb("rw_gup", [128, 256], F32, st)
        S.dma("sp", wup[:], P["rw_w_up"][l], writes=[wup], sem_tile=wup)
        S.dma("sp", aup[:], P["rw_a_up"][l], writes=[aup], sem_tile=aup)
        S.dma("sp", gup[:], P["rw_g_up"][l], writes=[gup], sem_tile=gup)
        bd = S.sb("rw_bd", [128, 128], F32, st)
        S.op("pool", lambda e: e.memset(bd[:], 0.0), writes=[bd])
        for h in range(4):
            S.op("pool", lambda e, h=h: e.memset(bd[32 * h:32 * h + 32, 32 * h:32 * h + 32], 1.0), reads=[bd], writes=[bd])
        mA = S.sb("rw_mA", [128, 4, 128], F32, st)
        mB = S.sb("rw_mB", [128, 2, 128], F32, st)
        for j in range(4):
            cmp = ALU.is_gt if j < 2 else ALU.is_ge
            S.op("pool", lambda e, j=j, cmp=cmp: e.affine_select(out=mA[:, j, :], in_=bd[:], compare_op=cmp, fill=0.0, base=0,
                                                                 pattern=[[1, 128]], channel_multiplier=-1), reads=[bd], writes=[mA])
        for j in range(2):
            S.op("pool", lambda e, j=j: e.affine_select(out=mB[:, j, :], in_=bd[:], compare_op=ALU.is_gt, fill=0.0, base=0,
                                                        pattern=[[-1, 128]], channel_multiplier=1), reads=[bd], writes=[mB])
        cmask = S.sb("rw_cmask", [64, 4 * MT], F32, st)
        S.op("pool", lambda e: e.memset(cmask[:], 1.0), writes=[cmask])
        S.op("pool", lambda e: e.memset(cmask[:, :].rearrange("p (c l) -> p c l", l=32)[:, :, 0:1], 0.0), reads=[cmask], writes=[cmask])
        o64 = S.sb("rw_o64", [64, 64], F32, st)
        S.op("pool", lambda e: e.memset(o64[:], 1.0), writes=[o64])
        o64m = S.sb("rw_o64m", [64, 64], F32, st)
        S.op("pool", lambda e: e.memset(o64m[:], 1.0 / 64.0), writes=[o64m])
        Ebd = S.sb("rw_Ebd", [128, 256], F32, st)
        S.op("pool", lambda e: e.memset(Ebd[:], 0.0), writes=[Ebd])
        S0 = [S.sb("rw_S%d" % i, [64, 256], F32, st) for i in range(2)]
        S.op("pool", lambda e: e.memset(S0[0][:], 0.0), writes=[S0[0]])
        nS = 0
        nSl = [0]

        def kh(nm):
            return S.sb(nm, [64, 4, MT], F32, st)
        r_t, k_t, v_t, a_t, g_t, ka_t, b_t, lw_t, G_t, eG_t, bon_t, Y_t, x1, x2 = (
            kh("rw_" + n) for n in ("r", "k", "v", "a", "g", "ka", "b", "lw", "G", "eG", "bon", "Y", "x1", "x2"))
        rC, kC, bC, kaC, vC = (S.sb("rw_c" + n, [64, NCH, 128], F32, st) for n in ("r", "k", "b", "ka", "v"))
        xw_t = S.sb("rw_xw", [64, 2, MT], F32, st)
        xg_t = S.sb("rw_xg", [128, MT], F32, st)
        o_t = S.sb("rw_o", [64, 4, MT], BF16, st)
        GRP = 4
        NSET = 8
        RR = (lambda ap: ap.bitcast(mybir.dt.float32r)) if RW_FP32R else (lambda ap: ap)
        TT_ = [S.sb("rw_TT%d" % i, [128, 4, 64], F32, st) for i in range(NSET)]
        AA = [S.sb("rw_AA%d" % i, [128, 4, 128], F32, st) for i in range(NSET)]
        AB = [S.sb("rw_AB%d" % i, [128, 2, 128], F32, st) for i in range(NSET)]
        Vbds = [S.sb("rw_Vbd%d" % i, [128, 256], F32, st) for i in range(NSET)]
        for vb in Vbds:
            S.op("pool", lambda e: e.memset(vb[:], 0.0), writes=[vb])
        PP = [None, None]
        PPx = [S.sb("rw_PP%d" % i, [128, 2, 128], F32, st) for i in range(2 * NSET)]
        MM = [S.sb("rw_MM%d" % i, [128, 2, 128], F32, st) for i in range(NSET)]
        Kh = [S.sb("rw_Kh%d" % i, [64, 128], F32, st) for i in range(NSET)]
        Gh = [S.sb("rw_Gh%d" % i, [128, 128], F32, st) for i in range(NSET)]
        E2 = [S.sb("rw_E2%d" % i, [128, 64], F32, st) for i in range(NSET)]
        ET = [S.sb("rw_ET%d" % i, [128, 64], F32, st) for i in range(NSET)]
        tmpS = S.sb("rw_tmpS", [64, 256], F32, st)
        nchunk = 0
        zc = C.zT

        def perhead(fn):
            for h in range(4):
                fn(h)

        def fl(t):
            return t[:, :, :].rearrange("p h t -> p (h t)")

        for mi in range(T // MT):
            t0 = mi * MT
            for j, tl in enumerate((r_t, k_t, v_t)):
                S.dma("sp", tl[:], zc[OFF_C + j * 256:OFF_C + (j + 1) * 256, t0:t0 + MT].rearrange("(h n) t -> n h t", n=64),
                      writes=[tl], sem_tile=tl)
            S.dma("sp", xw_t[:], zc[OFF_C + 768:OFF_C + 896, t0:t0 + MT].rearrange("(a n) t -> n a t", n=64), writes=[xw_t], sem_tile=xw_t)
            S.dma("sp", xg_t[:], zc[OFF_C + 896:OFF_C + 1024, t0:t0 + MT], writes=[xg_t], sem_tile=xg_t)
            S.op("act", lambda e: e.activation(out=xw_t[:, 0, :], in_=xw_t[:, 0, :], func=AF.Tanh), reads=[xw_t], writes=[xw_t])
            S.op("act", lambda e: e.activation(out=xg_t[:], in_=xg_t[:], func=AF.Sigmoid), reads=[xg_t], writes=[xg_t])
            for h in range(4):
                ps = C.psum()
                S.op("pe", lambda e, h=h, ps=ps: e.matmul(ps[0:64, 0:MT], lhsT=wup[:, h * 64:(h + 1) * 64], rhs=xw_t[:, 0, :], start=True, stop=True),
                     reads=[wup, xw_t], writes=[ps])
                S.op("act", lambda e, h=h, ps=ps: e.activation(out=lw_t[:, h, :], in_=ps[0:64, 0:MT], func=AF.Sigmoid, bias=w0c[:, h:h + 1], scale=1.0),
                     reads=[ps, w0c], writes=[lw_t])
                ps = C.psum()
                S.op("pe", lambda e, h=h, ps=ps: e.matmul(ps[0:64, 0:MT], lhsT=aup[:, h * 64:(h + 1) * 64], rhs=xw_t[:, 1, :], start=True, stop=True),
                     reads=[aup, xw_t], writes=[ps])
                S.op("act", lambda e, h=h, ps=ps: e.activation(out=a_t[:, h, :], in_=ps[0:64, 0:MT], func=AF.Sigmoid, bias=a0c[:, h:h + 1], scale=1.0),
                     reads=[ps, a0c], writes=[a_t])
                ps = C.psum()
                S.op("pe", lambda e, h=h, ps=ps: e.matmul(ps[0:64, 0:MT], lhsT=gup[:, h * 64:(h + 1) * 64], rhs=xg_t[:, :], start=True, stop=True),
                     reads=[gup, xg_t], writes=[ps])
                S.op("dve", lambda e, h=h, ps=ps: e.tensor_copy(out=g_t[:, h, :], in_=ps[0:64, 0:MT]), reads=[ps], writes=[g_t])
            S.op("pool", lambda e: e.tensor_scalar(out=fl(lw_t), in0=fl(lw_t), scalar1=-RW_C, scalar2=None, op0=ALU.mult), reads=[lw_t], writes=[lw_t])
            for h in range(4):
                S.op("dve", lambda e, h=h: e.tensor_scalar(out=ka_t[:, h, :], in0=k_t[:, h, :], scalar1=kkc[:, h:h + 1], scalar2=None, op0=ALU.mult),
                     reads=[k_t, kkc], writes=[ka_t])
            S.op("pool", lambda e: e.tensor_tensor(out=fl(x1), in0=fl(ka_t), in1=fl(ka_t), op=ALU.mult), reads=[ka_t], writes=[x1])
            for h in range(4):
                ps = C.psum()
                S.op("pe", lambda e, h=h, ps=ps: e.matmul(ps[0:64, 0:MT], lhsT=o64[:, :], rhs=x1[:, h, :], start=True, stop=True), reads=[o64, x1], writes=[ps])
                S.op("act", lambda e, h=h, ps=ps: e.activation(out=x2[:, h, :], in_=ps[0:64, 0:MT], func=AF.Sqrt), reads=[ps], writes=[x2])
            S.op("dve", lambda e: e.tensor_scalar(out=fl(x2), in0=fl(x2), scalar1=1e-12, scalar2=None, op0=ALU.max), reads=[x2], writes=[x2])
            S.op("dve", lambda e: e.reciprocal(out=fl(x2), in_=fl(x2)), reads=[x2], writes=[x2])
            S.op("pool", lambda e: e.tensor_tensor(out=fl(ka_t), in0=fl(ka_t), in1=fl(x2), op=ALU.mult), reads=[ka_t, x2], writes=[ka_t])
            for h in range(4):
                S.op("dve", lambda e, h=h: e.tensor_scalar(out=x1[:, h, :], in0=a_t[:, h, :], scalar1=-1.0, scalar2=kac[:, h:h + 1], op0=ALU.add, op1=ALU.mult),
                     reads=[a_t, kac], writes=[x1])
            S.op("dve", lambda e: e.scalar_tensor_tensor(out=fl(k_t), in0=fl(x1), scalar=1.0, in1=fl(k_t), op0=ALU.add, op1=ALU.mult),
                 reads=[x1, k_t], writes=[k_t])
            S.op("pool", lambda e: e.tensor_tensor(out=fl(b_t), in0=fl(ka_t), in1=fl(a_t), op=ALU.mult), reads=[ka_t, a_t], writes=[b_t])
            S.op("pool", lambda e: e.tensor_tensor(out=fl(x1), in0=fl(r_t), in1=fl(k_t), op=ALU.mult), reads=[r_t, k_t], writes=[x1])
            for h in range(4):
                S.op("dve", lambda e, h=h: e.tensor_scalar(out=x1[:, h, :], in0=x1[:, h, :], scalar1=rkc[:, h:h + 1], scalar2=None, op0=ALU.mult),
                     reads=[x1, rkc], writes=[x1])
            for h in range(4):
                ps = C.psum()
                S.op("pe", lambda e, h=h, ps=ps: e.matmul(ps[0:64, 0:MT], lhsT=o64[:, :], rhs=x1[:, h, :], start=True, stop=True), reads=[o64, x1], writes=[ps])
                S.op("dve", lambda e, h=h, ps=ps: e.tensor_tensor(out=bon_t[:, h, :], in0=ps[0:64, 0:MT], in1=v_t[:, h, :], op=ALU.mult),
                     reads=[ps, v_t], writes=[bon_t])
            S.op("dve", lambda e: e.tensor_tensor_scan(out=fl(G_t), data0=cmask[:, :], data1=fl(lw_t), initial=0.0, op0=ALU.mult, op1=ALU.add),
                 reads=[cmask, lw_t], writes=[G_t])
            S.op("act", lambda e: e.activation(out=fl(eG_t), in_=fl(G_t), func=AF.Exp), reads=[G_t], writes=[eG_t])
            S.op("pool", lambda e: e.tensor_tensor(out=fl(r_t), in0=fl(r_t), in1=fl(eG_t), op=ALU.mult), reads=[r_t, eG_t], writes=[r_t])
            S.op("pool", lambda e: e.tensor_tensor(out=fl(x1), in0=fl(G_t), in1=fl(lw_t), op=ALU.subtract), reads=[G_t, lw_t], writes=[x1])
            S.op("act", lambda e: e.activation(out=fl(x1), in_=fl(x1), func=AF.Exp), reads=[x1], writes=[x1])
            S.op("pool", lambda e: e.tensor_tensor(out=fl(ka_t), in0=fl(ka_t), in1=fl(x1), op=ALU.mult), reads=[ka_t, x1], writes=[ka_t])
            S.op("act", lambda e: e.activation(out=fl(x2), in_=fl(G_t), func=AF.Exp, scale=-1.0), reads=[G_t], writes=[x2])
            S.op("pool", lambda e: e.tensor_tensor(out=fl(b_t), in0=fl(b_t), in1=fl(x2), op=ALU.mult), reads=[b_t, x2], writes=[b_t])
            S.op("pool", lambda e: e.tensor_tensor(out=fl(k_t), in0=fl(k_t), in1=fl(x2), op=ALU.mult), reads=[k_t, x2], writes=[k_t])
            for j, (src, dst) in enumerate(((r_t, rC), (k_t, kC), (b_t, bC), (ka_t, kaC), (v_t, vC))):
                en = "act" if j % 2 else "pool"
                S.op(en, copy_op("act" if en == "act" else "dve", dst[:, :, :].rearrange("p c (h t) -> p h c t", h=4),
                                 src[:, :, :].rearrange("p h (c t) -> p h c t", t=32)), reads=[src], writes=[dst])
            def stage_a(c, si):
                TT, A_, B_, M_, Kh_, Gh_, E2_, Vb_ = TT_[si], AA[si], AB[si], MM[si], Kh[si], Gh[si], E2[si], Vbds[si]
                PPs = (PPx[2 * si], PPx[2 * si + 1])
                rc, kc_, bc, kac_, vc = rC[:, c, :], kC[:, c, :], bC[:, c, :], kaC[:, c, :], vC[:, c, :]
                ps = C.psum()
                for j, (src, srct) in enumerate(((bc, bC), (kc_, kC), (kac_, kaC), (vc, vC))):
                    S.op("pe", lambda e: e.transpose(out=ps[:, j * 64:(j + 1) * 64], in_=src, identity=C.ident[0:64, 0:64]),
                         reads=[srct, C.ident], writes=[ps])
                S.op("act", lambda e: e.copy(out=TT[:], in_=ps[:, 0:256].rearrange("p (a b) -> p a b", a=4)), reads=[ps], writes=[TT])
                for h in range(4):
                    S.op("pool", lambda e: e.tensor_copy(out=Vb_[32 * h:32 * h + 32, 64 * h:64 * h + 64], in_=TT[32 * h:32 * h + 32, 3, :]),
                         reads=[TT], writes=[Vb_])
                yield
                psA = C.psum()
                for j, (lt, ltt, rt, rtt) in enumerate(((bc, bC, kac_, kaC), (kc_, kC, kac_, kaC), (bc, bC, rc, rC), (kc_, kC, rc, rC))):
                    S.op("pe", lambda e: e.matmul(psA[:, j * 128:(j + 1) * 128], lhsT=RR(lt), rhs=RR(rt), start=True, stop=True),
                         reads=[ltt, rtt], writes=[psA])
                S.op("dve", lambda e: e.tensor_tensor(out=A_[:], in0=psA[:, :].rearrange("p (a b) -> p a b", a=4), in1=mA[:], op=ALU.mult),
                     reads=[psA, mA], writes=[A_])
                psB = C.psum()
                for j, (lt, ltt, rt, rtt) in enumerate(((kac_, kaC, bc, bC), (kac_, kaC, kc_, kC))):
                    S.op("pe", lambda e: e.matmul(psB[:, j * 128:(j + 1) * 128], lhsT=RR(lt), rhs=RR(rt), start=True, stop=True),
                         reads=[ltt, rtt], writes=[psB])
                S.op("dve", lambda e: e.tensor_tensor(out=B_[:], in0=psB[:, 0:256].rearrange("p (a b) -> p a b", a=2), in1=mB[:], op=ALU.mult),
                     reads=[psB, mB], writes=[B_])
                S.op("pool", lambda e: e.tensor_tensor(out=M_[:, 0, :], in0=C.ident[:], in1=A_[:, 0, :], op=ALU.subtract), reads=[A_, C.ident], writes=[M_])
                S.op("pool", lambda e: e.tensor_tensor(out=M_[:, 1, :], in0=C.ident[:], in1=B_[:, 0, :], op=ALU.subtract), reads=[B_, C.ident, M_], writes=[M_])
                yield
                cur = (A_[:, 0, :], B_[:, 0, :], A_, B_)
                for it in range(4):
                    p_ap, pt_ap, p_t1, p_t2 = cur
                    psQ = C.psum()
                    S.op("pe", lambda e: e.matmul(psQ[:, 0:128], lhsT=RR(pt_ap), rhs=RR(p_ap), start=True, stop=True), reads=[p_t1, p_t2], writes=[psQ])
                    S.op("pe", lambda e: e.matmul(psQ[:, 128:256], lhsT=RR(p_ap), rhs=RR(pt_ap), start=True, stop=True), reads=[p_t1, p_t2], writes=[psQ])
                    Pn = PPs[it % 2]
                    S.op("act", lambda e: e.copy(out=Pn[:], in_=psQ[:, 0:256].rearrange("p (a b) -> p a b", a=2)), reads=[psQ], writes=[Pn])
                    yield
                    psU = C.psum()
                    S.op("pe", lambda e: e.matmul(psU[:, 0:128], lhsT=RR(M_[:, 1, :]), rhs=RR(Pn[:, 0, :]), start=True, stop=True), reads=[M_, Pn], writes=[psU])
                    if it < 3:
                        S.op("pe", lambda e: e.matmul(psU[:, 128:256], lhsT=RR(Pn[:, 0, :]), rhs=RR(M_[:, 1, :]), start=True, stop=True), reads=[M_, Pn], writes=[psU])
                        S.op("dve", lambda e: e.tensor_tensor(out=M_[:], in0=psU[:, 0:256].rearrange("p (a b) -> p a b", a=2), in1=M_[:], op=ALU.add),
                             reads=[psU, M_], writes=[M_])
                    else:
                        S.op("dve", lambda e: e.tensor_tensor(out=M_[:, 0, :], in0=psU[:, 0:128], in1=M_[:, 0, :], op=ALU.add), reads=[psU, M_], writes=[M_])
                    cur = (Pn[:, 0, :], Pn[:, 1, :], Pn, Pn)
                    yield
                psK = C.psum()
                S.op("pe", lambda e: e.matmul(psK[0:64, 0:128], lhsT=TT[:, 2, :], rhs=M_[:, 0, :], start=True, stop=True), reads=[TT, M_], writes=[psK])
                S.op("act", lambda e: e.copy(out=Kh_[:], in_=psK[0:64, 0:128]), reads=[psK], writes=[Kh_])
                psG = C.psum()
                S.op("pe", lambda e: e.matmul(psG[:, 0:128], lhsT=RR(B_[:, 1, :]), rhs=RR(M_[:, 0, :]), start=True, stop=True), reads=[B_, M_], writes=[psG])
                S.op("act", lambda e: e.copy(out=Gh_[:], in_=psG[:, 0:128]), reads=[psG], writes=[Gh_])
                yield
                psE2 = C.psum()
                S.op("pe", lambda e: e.matmul(psE2[:, 0:64], lhsT=Gh_[:, :], rhs=TT[:, 3, :], start=True, stop=True), reads=[Gh_, TT], writes=[psE2])
                S.op("act", lambda e: e.copy(out=E2_[:], in_=psE2[:, 0:64]), reads=[psE2], writes=[E2_])
                yield

            def run_rr(gens):
                alive = list(gens)
                while alive:
                    for g_ in list(alive):
                        try:
                            next(g_)
                        except StopIteration:
                            alive.remove(g_)

            def stage_b(c0, n0):
                for c in range(c0, c0 + GRP):
                    cs = slice(32 * c, 32 * c + 32)
                    si = (n0 + c - c0) % NSET
                    TT, A_, B_, M_, Kh_, Gh_, E2_, ET_, Vbd = TT_[si], AA[si], AB[si], MM[si], Kh[si], Gh[si], E2[si], ET[si], Vbds[si]
                    Sc = S0[nSl[0] % 2]
                    Sn = S0[(nSl[0] + 1) % 2]
                    nSl[0] += 1
                    psE = C.psum()
                    S.op("pe", lambda e, psE=psE, Kh_=Kh_, Sc=Sc: e.matmul(psE[:, 0:256], lhsT=Kh_[:, :], rhs=Sc[:, :], start=True, stop=True),
                         reads=[Kh_, Sc], writes=[psE])
                    for h in range(4):
                        S.op("dve", lambda e, h=h, psE=psE, ET_=ET_, E2_=E2_: e.scalar_tensor_tensor(
                            out=ET_[32 * h:32 * h + 32, :], in0=psE[32 * h:32 * h + 32, 64 * h:64 * h + 64], scalar=-1.0,
                            in1=E2_[32 * h:32 * h + 32, :], op0=ALU.mult, op1=ALU.subtract), reads=[psE, E2_, ET_], writes=[ET_])
                    for h in range(4):
                        S.op("pool", lambda e, h=h, ET_=ET_: e.tensor_copy(out=Ebd[32 * h:32 * h + 32, 64 * h:64 * h + 64], in_=ET_[32 * h:32 * h + 32, :]),
                             reads=[ET_], writes=[Ebd])
                    yield
                    psY = C.psum()
                    S.op("pe", lambda e, psY=psY, TT=TT, A_=A_: e.matmul(psY[0:64, 0:128], lhsT=TT[:, 3, :], rhs=A_[:, 3, :], start=True, stop=False),
                         reads=[TT, A_], writes=[psY])
                    S.op("pe", lambda e, psY=psY, ET_=ET_, A_=A_: e.matmul(psY[0:64, 0:128], lhsT=ET_[:, :], rhs=A_[:, 2, :], start=False, stop=False),
                         reads=[ET_, A_], writes=[psY])
                    for h in range(4):
                        S.op("pe", lambda e, h=h, psY=psY, Sc=Sc, cs=cs: e.matmul(psY[0:64, 32 * h:32 * h + 32], lhsT=Sc[:, 64 * h:64 * h + 64], rhs=rC[:, c, 32 * h:32 * h + 32],
                                                                             start=False, stop=(h == 3)), reads=[Sc, rC], writes=[psY])
                    S.op("act", lambda e, psY=psY, cs=cs: e.copy(out=Y_t[:, :, cs], in_=psY[0:64, 0:128].rearrange("p (h t) -> p h t", h=4)), reads=[psY], writes=[Y_t])
                    yield
                    psS = C.psum()
                    S.op("pe", lambda e, psS=psS, TT=TT: e.matmul(psS[0:64, 0:256], lhsT=TT[:, 0, :], rhs=Ebd[:, :], start=True, stop=False),
                         reads=[TT, Ebd], writes=[psS])
                    S.op("pe", lambda e, psS=psS, TT=TT: e.matmul(psS[0:64, 0:256], lhsT=TT[:, 1, :], rhs=Vbd[:, :], start=False, stop=True),
                         reads=[TT, Vbd], writes=[psS])
                    S.op("dve", lambda e, psS=psS, Sc=Sc: e.tensor_tensor(out=tmpS[:], in0=psS[0:64, 0:256], in1=Sc[:], op=ALU.add), reads=[psS, Sc], writes=[tmpS])
                    for h in range(4):
                        S.op("dve", lambda e, h=h, Sn=Sn, c=c: e.tensor_scalar(out=Sn[:, 64 * h:64 * h + 64], in0=tmpS[:, 64 * h:64 * h + 64],
                                                                            scalar1=eG_t[:, h, 32 * c + 31:32 * c + 32], scalar2=None, op0=ALU.mult),
                             reads=[tmpS, eG_t, Sn], writes=[Sn])

                    yield

            ngrp = NCH // GRP
            run_rr([stage_a(q, (nchunk + q) % NSET) for q in range(GRP)])
            for gi_ in range(ngrp):
                gens = [stage_b(gi_ * GRP, nchunk + gi_ * GRP)]
                if gi_ + 1 < ngrp:
                    gens += [stage_a((gi_ + 1) * GRP + q, (nchunk + (gi_ + 1) * GRP + q) % NSET) for q in range(GRP)]
                run_rr(gens)
            nchunk += NCH
            for h in range(4):
                ps = C.psum()
                S.op("pe", lambda e, h=h, ps=ps: e.matmul(ps[0:64, 0:MT], lhsT=o64m[:, :], rhs=Y_t[:, h, :], start=True, stop=True), reads=[o64m, Y_t], writes=[ps])
                S.op("dve", lambda e, h=h, ps=ps: e.tensor_tensor(out=x1[:, h, :], in0=Y_t[:, h, :], in1=ps[0:64, 0:MT], op=ALU.subtract),
                     reads=[ps, Y_t], writes=[x1])
            S.op("pool", lambda e: e.tensor_tensor(out=fl(x2), in0=fl(x1), in1=fl(x1), op=ALU.mult), reads=[x1], writes=[x2])
            for h in range(4):
                ps = C.psum()
                S.op("pe", lambda e, h=h, ps=ps: e.matmul(ps[0:64, 0:MT], lhsT=o64m[:, :], rhs=x2[:, h, :], start=True, stop=True), reads=[o64m, x2], writes=[ps])
                S.op("act", lambda e, h=h, ps=ps: e.activation(out=G_t[:, h, :], in_=ps[0:64, 0:MT], func=AF.Sqrt, bias=C.rweps[0:64, 0:1], scale=1.0),
                     reads=[ps, C.rweps], writes=[G_t])
            S.op("dve", lambda e: e.reciprocal(out=fl(G_t), in_=fl(G_t)), reads=[G_t], writes=[G_t])
            S.op("pool", lambda e: e.tensor_tensor(out=fl(x1), in0=fl(x1), in1=fl(G_t), op=ALU.mult), reads=[x1, G_t], writes=[x1])
            for h in range(4):
                S.op("dve", lambda e, h=h: e.tensor_scalar(out=x1[:, h, :], in0=x1[:, h, :], scalar1=lgc[:, h:h + 1], scalar2=lbc[:, h:h + 1],
                                                           op0=ALU.mult, op1=ALU.add), reads=[x1, lgc, lbc], writes=[x1])
            S.op("pool", lambda e: e.tensor_tensor(out=fl(x1), in0=fl(x1), in1=fl(bon_t), op=ALU.add), reads=[x1, bon_t], writes=[x1])
            S.op("pool", lambda e: e.tensor_tensor(out=fl(o_t), in0=fl(x1), in1=fl(g_t), op=ALU.mult), reads=[x1, g_t], writes=[o_t])
            S.dma("sp", C.oT[512:768, t0:t0 + MT].rearrange("(h n) t -> n h t", n=64), o_t[:], reads=[o_t], sem_tile=o_t)
        S.barrier()
        for t_ in [wup, aup, gup, r_t, k_t, v_t, xw_t, xg_t, o_t]:
            S.release(t_)


TWO_PI = 2.0 * math.pi


def phase_s5(C, l):
    S, T, NT, P = C.S, C.T, C.NT, C.P
    with ExitStack() as st:
        def small(nm, n=8, dt=F32):
            return S.sb("s5_" + nm, [128, n], dt, st)
        are = load_cols(C, st, "s5_are", P["s5_a_re"][l].rearrange("(k a) p -> k (a p)", a=2), 8)
        aim = load_cols(C, st, "s5_aim", P["s5_a_im"][l].rearrange("(k a) p -> k (a p)", a=2), 8)
        ldt = load_bc(C, st, "s5_ldt", P["s5_log_dt"][l], 16)
        dcol = load_cols(C, st, "s5_d", P["s5_d"][l].rearrange("(a p) -> a p", p=128), 2)
        gbcol = load_cols(C, st, "s5_gb", P["s5_glu_b"][l].rearrange("(a p) -> a p", p=128), 2)
        gluw = S.sb("s5_gluw", [128, 2, 256], BF16, st)
        S.dma("pool", gluw[:], P["s5_glu_w"][l].rearrange("(a p) n -> p a n", p=128), writes=[gluw], sem_tile=gluw)
        negpi = small("negpi", 1)
        S.op("pool", lambda e: e.memset(negpi[:], 0.0), writes=[negpi])
        dt_, lre, th, mag, thr, sc, cc, abr, abi, den, cre, cim, ncre, t1, t2, Rre, Rim = (
            small(n) for n in ("dt", "lre", "th", "mag", "thr", "sc", "cc", "abr", "abi", "den", "cre", "cim", "ncre", "t1", "t2", "Rre", "Rim"))
        qi = S.sb("s5_qi", [128, 512], I32, st)
        qf = S.sb("s5_qf", [128, 512], F32, st)
        tb = S.sb("s5_tb", [128, 512], F32, st)
        jf = S.sb("s5_jf", [128, 512], F32, st)
        ang = S.sb("s5_ang", [128, 512], F32, st)

        def rr_sin(dst_ap, dst_t, src_ap, src_t, n, shift=0.0):
            a_, q_, f_, t_ = ang[:, 0:n], qi[:, 0:n], qf[:, 0:n], tb[:, 0:n]
            S.op("dve", lambda e: e.tensor_scalar(out=a_, in0=src_ap, scalar1=shift, scalar2=None, op0=ALU.add), reads=[src_t], writes=[ang])
            S.op("dve", lambda e: e.tensor_scalar(out=q_, in0=a_, scalar1=1.0 / TWO_PI, scalar2=None, op0=ALU.mult), reads=[ang], writes=[qi])
            S.op("dve", lambda e: e.tensor_copy(out=f_, in_=q_), reads=[qi], writes=[qf])
            S.op("dve", lambda e: e.scalar_tensor_tensor(out=a_, in0=f_, scalar=-TWO_PI, in1=a_, op0=ALU.mult, op1=ALU.add), reads=[qf, ang], writes=[ang])
            S.op("dve", lambda e: e.tensor_scalar(out=t_, in0=a_, scalar1=math.pi, scalar2=None, op0=ALU.is_gt), reads=[ang], writes=[tb])
            S.op("dve", lambda e: e.scalar_tensor_tensor(out=a_, in0=t_, scalar=-TWO_PI, in1=a_, op0=ALU.mult, op1=ALU.add), reads=[tb, ang], writes=[ang])
            S.op("dve", lambda e: e.tensor_scalar(out=t_, in0=a_, scalar1=-math.pi, scalar2=None, op0=ALU.is_lt), reads=[ang], writes=[tb])
            S.op("dve", lambda e: e.scalar_tensor_tensor(out=a_, in0=t_, scalar=TWO_PI, in1=a_, op0=ALU.mult, op1=ALU.add), reads=[tb, ang], writes=[ang])
            S.op("dve", lambda e: e.tensor_scalar(out=a_, in0=a_, scalar1=3.1415925, scalar2=-3.1415925, op0=ALU.min, op1=ALU.max), reads=[ang], writes=[ang])
            S.op("act", lambda e: e.activation(out=dst_ap, in_=a_, func=AF.Sin, bias=negpi[:, 0:1], scale=1.0), reads=[ang, negpi], writes=[dst_t])

        S.op("dve", lambda e: e.tensor_copy(out=dt_[0:64, :], in_=ldt[0:64, 0:16:2]), reads=[ldt], writes=[dt_])
        S.op("dve", lambda e: e.tensor_copy(out=dt_[64:128, :], in_=ldt[64:128, 1:16:2]), reads=[ldt, dt_], writes=[dt_])
        S.op("act", lambda e: e.activation(out=dt_[:], in_=dt_[:], func=AF.Exp), reads=[dt_], writes=[dt_])
        S.op("dve", lambda e: e.tensor_tensor(out=lre[:], in0=are[:], in1=dt_[:], op=ALU.mult), reads=[are, dt_], writes=[lre])
        S.op("dve", lambda e: e.tensor_tensor(out=th[:], in0=aim[:], in1=dt_[:], op=ALU.mult), reads=[aim, dt_], writes=[th])
        S.op("act", lambda e: e.activation(out=mag[:], in_=lre[:], func=AF.Exp), reads=[lre], writes=[mag])
        rr_sin(sc[:], sc, th[:], th, 8)
        rr_sin(cc[:], cc, th[:], th, 8, shift=math.pi / 2)
        rr_sin(t1[:], t1, th[:], th, 8)
        S.op("dve", lambda e: e.tensor_copy(out=thr[:], in_=ang[:, 0:8]), reads=[ang], writes=[thr])
        S.op("dve", lambda e: e.tensor_tensor(out=abr[:], in0=mag[:], in1=cc[:], op=ALU.mult), reads=[mag, cc], writes=[abr])
        S.op("dve", lambda e: e.tensor_tensor(out=abi[:], in0=mag[:], in1=sc[:], op=ALU.mult), reads=[mag, sc], writes=[abi])
        S.op("dve", lambda e: e.tensor_tensor(out=den[:], in0=are[:], in1=are[:], op=ALU.mult), reads=[are], writes=[den])
        S.op("dve", lambda e: e.tensor_tensor(out=t1[:], in0=aim[:], in1=aim[:], op=ALU.mult), reads=[aim], writes=[t1])
        S.op("dve", lambda e: e.tensor_tensor(out=den[:], in0=den[:], in1=t1[:], op=ALU.add), reads=[den, t1], writes=[den])
        S.op("dve", lambda e: e.reciprocal(out=den[:], in_=den[:]), reads=[den], writes=[den])
        S.op("dve", lambda e: e.tensor_scalar(out=t1[:], in0=abr[:], scalar1=-1.0, scalar2=None, op0=ALU.add), reads=[abr], writes=[t1])
        S.op("dve", lambda e: e.tensor_tensor(out=cre[:], in0=t1[:], in1=are[:], op=ALU.mult), reads=[t1, are], writes=[cre])
        S.op("dve", lambda e: e.tensor_tensor(out=t2[:], in0=abi[:], in1=aim[:], op=ALU.mult), reads=[abi, aim], writes=[t2])
        S.op("dve", lambda e: e.tensor_tensor(out=cre[:], in0=cre[:], in1=t2[:], op=ALU.add), reads=[cre, t2], writes=[cre])
        S.op("dve", lambda e: e.tensor_tensor(out=cre[:], in0=cre[:], in1=den[:], op=ALU.mult), reads=[cre, den], writes=[cre])
        S.op("dve", lambda e: e.tensor_tensor(out=cim[:], in0=abi[:], in1=are[:], op=ALU.mult), reads=[abi, are], writes=[cim])
        S.op("dve", lambda e: e.tensor_tensor(out=t2[:], in0=t1[:], in1=aim[:], op=ALU.mult), reads=[t1, aim], writes=[t2])
        S.op("dve", lambda e: e.tensor_tensor(out=cim[:], in0=cim[:], in1=t2[:], op=ALU.subtract), reads=[cim, t2], writes=[cim])
        S.op("dve", lambda e: e.tensor_tensor(out=cim[:], in0=cim[:], in1=den[:], op=ALU.mult), reads=[cim, den], writes=[cim])
        S.op("dve", lambda e: e.tensor_scalar(out=ncre[:], in0=cre[:], scalar1=-1.0, scalar2=None, op0=ALU.mult), reads=[cre], writes=[ncre])
        S.op("dve", lambda e: e.tensor_scalar(out=t2[:], in0=thr[:], scalar1=512.0, scalar2=None, op0=ALU.mult), reads=[thr], writes=[t2])
        rr_sin(Rim[:], Rim, t2[:], t2, 8)
        rr_sin(Rre[:], Rre, t2[:], t2, 8, shift=math.pi / 2)
        S.op("pool", lambda e: e.iota(qi[:], pattern=[[1, 512]], base=0, channel_multiplier=0), writes=[qi])
        S.op("dve", lambda e: e.tensor_copy(out=jf[:], in_=qi[:]), reads=[qi], writes=[jf])
        cosT = S.sb("s5_cosT", [128, 8, 512], F32, st)
        sinT = S.sb("s5_sinT", [128, 8, 512], F32, st)
        TiR = S.sb("s5_TiR", [128, 8, 512], F32, st)
        TiI = S.sb("s5_TiI", [128, 8, 512], F32, st)
        magT = S.sb("s5_magT", [128, 8, 512], F32, st)
        a2 = S.sb("s5_a2", [128, 512], F32, st)
        for k in range(8):
            S.op("pool", lambda e, k=k: e.tensor_scalar(out=a2[:], in0=jf[:], scalar1=thr[:, k:k + 1], scalar2=None, op0=ALU.mult), reads=[jf, thr], writes=[a2])
            rr_sin(sinT[:, k, :], sinT, a2[:], a2, 512)
            rr_sin(cosT[:, k, :], cosT, a2[:], a2, 512, shift=math.pi / 2)
            S.op("pool", lambda e, k=k: e.tensor_scalar(out=magT[:, k, :], in0=jf[:], scalar1=0.0, scalar2=mag[:, k:k + 1], op0=ALU.mult, op1=ALU.add),
                 reads=[jf, mag], writes=[magT])
            S.op("dve", lambda e, k=k: e.tensor_scalar(out=TiR[:, k, :], in0=cosT[:, k, :], scalar1=cre[:, k:k + 1], scalar2=None, op0=ALU.mult), reads=[cosT, cre], writes=[TiR])
            S.op("dve", lambda e, k=k: e.scalar_tensor_tensor(out=TiR[:, k, :], in0=sinT[:, k, :], scalar=cim[:, k:k + 1], in1=TiR[:, k, :], op0=ALU.mult, op1=ALU.add),
                 reads=[sinT, cim, TiR], writes=[TiR])
            S.op("dve", lambda e, k=k: e.tensor_scalar(out=TiI[:, k, :], in0=cosT[:, k, :], scalar1=cim[:, k:k + 1], scalar2=None, op0=ALU.mult), reads=[cosT, cim], writes=[TiI])
            S.op("dve", lambda e, k=k: e.scalar_tensor_tensor(out=TiI[:, k, :], in0=sinT[:, k, :], scalar=ncre[:, k:k + 1], in1=TiI[:, k, :], op0=ALU.mult, op1=ALU.add),
                 reads=[sinT, ncre, TiI], writes=[TiI])
        BT = [S.sb("s5_BT%d" % i, [16, 16, 64], BF16, st) for i in range(2)]
        CT = [S.sb("s5_CT%d" % i, [128, 8, 128], BF16, st) for i in range(2)]
        with ExitStack() as st2:
            braw = S.sb("s5_braw", [64, 16, 16], F32, st2)
            craw = S.sb("s5_craw", [16, 16, 64], F32, st2)
            for ri, (bk, ck) in enumerate((("s5_b_re", "s5_c_re"), ("s5_b_im", "s5_c_im"))):
                S.dma("sp", braw[:], P[bk][l].rearrange("g p c -> p g c"), writes=[braw], sem_tile=braw)
                for half in range(2):
                    ps = C.psum()
                    for g8 in range(8):
                        g = half * 8 + g8
                        S.op("pe", lambda e, g=g, g8=g8, ps=ps: e.transpose(out=ps[0:16, g8 * 64:(g8 + 1) * 64], in_=braw[:, g, :], identity=C.ident[0:64, 0:64]),
                             reads=[braw, C.ident], writes=[ps])
                    S.op("act", lambda e, half=half, ps=ps, ri=ri: e.copy(out=BT[ri][:, half * 8:(half + 1) * 8, :], in_=ps[0:16, :].rearrange("p (g q) -> p g q", g=8)),
                         reads=[ps], writes=[BT[ri]])
                S.op("pool", lambda e, ri=ri: e.memset(CT[ri][:], 0.0), writes=[CT[ri]])
                S.dma("sp", craw[:], P[ck][l].rearrange("g c p -> c g p"), writes=[craw], sem_tile=craw)
                for k in range(8):
                    ps = C.psum()
                    S.op("pe", lambda e, k=k, ps=ps: e.transpose(out=ps[:, 0:16], in_=craw[:, 2 * k:2 * k + 2, :].rearrange("c a p -> c (a p)"), identity=C.ident[0:16, 0:16]),
                         reads=[craw, C.ident], writes=[ps])
                    c0 = (k % 4) * 32
                    sgn = 1.0 if ri == 0 else -1.0
                    S.op("act", lambda e, k=k, ps=ps, ri=ri, c0=c0, sgn=sgn: e.activation(out=CT[ri][0:64, k, c0:c0 + 16], in_=ps[0:64, 0:16], func=AF.Copy, scale=sgn),
                         reads=[ps], writes=[CT[ri]])
                    S.op("act", lambda e, k=k, ps=ps, ri=ri, c0=c0, sgn=sgn: e.activation(out=CT[ri][64:128, k, c0 + 16:c0 + 32], in_=ps[64:128, 0:16], func=AF.Copy, scale=sgn),
                         reads=[ps], writes=[CT[ri]])
            S.barrier()
            S.release(braw)
            S.release(craw)
        u16b = [S.sb("s5_u16b%d" % i, [16, 16, 512], BF16, st) for i in range(1)]
        ufm = [S.sb("s5_ufm%d" % i, [128, 2, 512], F32, st) for i in range(2)]
        pr = [S.sb("s5_pr%d" % i, [128, 4, 512], F32, st) for i in range(1)] * 2
        win = [S.sb("s5_win%d" % i, [128, 2, 512], F32, st) for i in range(2)]
        ww = [S.sb("s5_w%d" % i, [128, 2, 512], F32, st) for i in range(2)]
        xr = [S.sb("s5_xr%d" % i, [128, 4, 512], F32, st) for i in range(1)] * 2
        xx = [S.sb("s5_x%d" % i, [128, 2, 512], BF16, st) for i in range(8)] * 2
        wl = S.sb("s5_wl", [128, 2, 8], F32, st)
        cy = [S.sb("s5_cy%d" % i, [128, 2], F32, st) for i in range(2)]
        ct1 = [S.sb("s5_ct%d" % i, [128, 2], F32, st) for i in range(2)]
        yv = [S.sb("s5_yv%d" % i, [128, 512], F32, st) for i in range(2)]
        yg = [S.sb("s5_yg%d" % i, [128, 2, 512], BF16, st) for i in range(2)]
        sgt = [S.sb("s5_sg%d" % i, [128, 512], BF16, st) for i in range(2)]
        ost = [S.sb("s5_o%d" % i, [128, 2, 512], BF16, st) for i in range(2)]
        n = 0
        for i in range(NT):
            t0 = i * 512
            ub_, uf_ = u16b[0], ufm[i % 2]
            S.dma("pool", ub_[:], C.zT[OFF_D:OFF_D + 256, t0:t0 + 512].rearrange("(g c) t -> c g t", c=16), writes=[ub_], sem_tile=ub_)
            S.dma("sp", uf_[:], C.zT[OFF_D:OFF_D + 256, t0:t0 + 512].rearrange("(a p) t -> p a t", p=128), writes=[uf_], sem_tile=uf_)
            for k in range(8):
                pr_, win_, w_, xr_, cy_, ct_ = pr[n % 2], win[n % 2], ww[n % 2], xr[n % 2], cy[n % 2], ct1[n % 2]
                x_ = xx[(i % 2) * 8 + k]
                n += 1
                psr = C.psum()
                psi = C.psum()
                for gl in range(2):
                    g = 2 * k + gl
                    S.op("pe", lambda e, g=g, gl=gl, psr=psr, ub_=ub_: e.matmul(psr[64 * gl:64 * gl + 64, :], lhsT=BT[0][:, g, :], rhs=ub_[:, g, :], start=True, stop=True),
                         reads=[BT[0], ub_], writes=[psr])
                    S.op("pe", lambda e, g=g, gl=gl, psi=psi, ub_=ub_: e.matmul(psi[64 * gl:64 * gl + 64, :], lhsT=BT[1][:, g, :], rhs=ub_[:, g, :], start=True, stop=True),
                         reads=[BT[1], ub_], writes=[psi])
                S.op("dve", lambda e, k=k, psr=psr, pr_=pr_: e.tensor_tensor(out=pr_[:, 0, :], in0=psr[:, :], in1=TiR[:, k, :], op=ALU.mult), reads=[psr, TiR], writes=[pr_])
                S.op("dve", lambda e, k=k, psi=psi, pr_=pr_: e.tensor_tensor(out=pr_[:, 1, :], in0=psi[:, :], in1=TiI[:, k, :], op=ALU.mult), reads=[psi, TiI, pr_], writes=[pr_])
                S.op("dve", lambda e, k=k, psi=psi, pr_=pr_: e.tensor_tensor(out=pr_[:, 2, :], in0=psi[:, :], in1=TiR[:, k, :], op=ALU.mult), reads=[psi, TiR, pr_], writes=[pr_])
                S.op("dve", lambda e, k=k, psr=psr, pr_=pr_: e.tensor_tensor(out=pr_[:, 3, :], in0=psr[:, :], in1=TiI[:, k, :], op=ALU.mult), reads=[psr, TiI, pr_], writes=[pr_])
                S.op("pool", lambda e, pr_=pr_, win_=win_: e.tensor_tensor(out=win_[:, 0, :], in0=pr_[:, 0, :], in1=pr_[:, 1, :], op=ALU.subtract), reads=[pr_], writes=[win_])
                S.op("pool", lambda e, pr_=pr_, win_=win_: e.tensor_tensor(out=win_[:, 1, :], in0=pr_[:, 2, :], in1=pr_[:, 3, :], op=ALU.add), reads=[pr_, win_], writes=[win_])
                if i == 0:
                    S.op("dve", lambda e, cy_=cy_: e.memset(cy_[:], 0.0), writes=[cy_])
                else:
                    S.op("dve", lambda e, k=k, ct_=ct_: e.tensor_scalar(out=ct_[:, 0:1], in0=wl[:, 1, k:k + 1], scalar1=Rim[:, k:k + 1], scalar2=None, op0=ALU.mult),
                         reads=[wl, Rim], writes=[ct_])
                    S.op("dve", lambda e, k=k, ct_=ct_: e.tensor_scalar(out=ct_[:, 1:2], in0=wl[:, 0, k:k + 1], scalar1=Rim[:, k:k + 1], scalar2=None, op0=ALU.mult),
                         reads=[wl, Rim, ct_], writes=[ct_])
                    S.op("dve", lambda e, k=k, ct_=ct_, cy_=cy_: e.scalar_tensor_tensor(out=cy_[:, 0:1], in0=wl[:, 0, k:k + 1], scalar=Rre[:, k:k + 1], in1=ct_[:, 0:1],
                                                                                     op0=ALU.mult, op1=ALU.subtract), reads=[wl, Rre, ct_], writes=[cy_])
                    S.op("dve", lambda e, k=k, ct_=ct_, cy_=cy_: e.scalar_tensor_tensor(out=cy_[:, 1:2], in0=wl[:, 1, k:k + 1], scalar=Rre[:, k:k + 1], in1=ct_[:, 1:2],
                                                                                     op0=ALU.mult, op1=ALU.add), reads=[wl, Rre, ct_, cy_], writes=[cy_])
                for c2 in range(2):
                    S.op("dve", lambda e, k=k, c2=c2, w_=w_, win_=win_, cy_=cy_: e.tensor_tensor_scan(
                        out=w_[:, c2, :], data0=magT[:, k, :], data1=win_[:, c2, :], initial=cy_[:, c2:c2 + 1], op0=ALU.mult, op1=ALU.add),
                        reads=[magT, win_, cy_, w_], writes=[w_])
                S.op("dve", lambda e, k=k, w_=w_: e.tensor_copy(out=wl[:, :, k:k + 1], in_=w_[:, :, 511:512]), reads=[w_, wl], writes=[wl])
                S.op("pool", lambda e, k=k, w_=w_, xr_=xr_: e.tensor_tensor(out=xr_[:, 0, :], in0=w_[:, 0, :], in1=cosT[:, k, :], op=ALU.mult), reads=[w_, cosT], writes=[xr_])
                S.op("pool", lambda e, k=k, w_=w_, xr_=xr_: e.tensor_tensor(out=xr_[:, 1, :], in0=w_[:, 1, :], in1=sinT[:, k, :], op=ALU.mult), reads=[w_, sinT, xr_], writes=[xr_])
                S.op("pool", lambda e, k=k, w_=w_, xr_=xr_: e.tensor_tensor(out=xr_[:, 2, :], in0=w_[:, 0, :], in1=sinT[:, k, :], op=ALU.mult), reads=[w_, sinT, xr_], writes=[xr_])
                S.op("pool", lambda e, k=k, w_=w_, xr_=xr_: e.tensor_tensor(out=xr_[:, 3, :], in0=w_[:, 1, :], in1=cosT[:, k, :], op=ALU.mult), reads=[w_, cosT, xr_], writes=[xr_])
                S.op("pool", lambda e, xr_=xr_, x_=x_: e.tensor_tensor(out=x_[:, 0, :], in0=xr_[:, 0, :], in1=xr_[:, 1, :], op=ALU.subtract), reads=[xr_], writes=[x_])
                S.op("pool", lambda e, xr_=xr_, x_=x_: e.tensor_tensor(out=x_[:, 1, :], in0=xr_[:, 2, :], in1=xr_[:, 3, :], op=ALU.add), reads=[xr_, x_], writes=[x_])
            yg_ = yg[i % 2]
            o_ = ost[i % 2]
            for ct in range(2):
                yv_ = yv[ct]
                psy = C.psum()
                for kk in range(4):
                    k = ct * 4 + kk
                    x_ = xx[(i % 2) * 8 + k]
                    S.op("pe", lambda e, k=k, kk=kk, psy=psy, x_=x_: e.matmul(psy[:, :], lhsT=CT[0][:, k, :], rhs=x_[:, 0, :], start=(kk == 0), stop=False),
                         reads=[CT[0], x_], writes=[psy])
                    S.op("pe", lambda e, k=k, kk=kk, psy=psy, x_=x_: e.matmul(psy[:, :], lhsT=CT[1][:, k, :], rhs=x_[:, 1, :], start=False, stop=(kk == 3)),
                         reads=[CT[1], x_], writes=[psy])
                S.op("dve", lambda e, ct=ct, psy=psy, yv_=yv_, uf_=uf_: e.scalar_tensor_tensor(out=yv_[:], in0=uf_[:, ct, :], scalar=dcol[:, ct:ct + 1], in1=psy[:, :],
                                                                                      op0=ALU.mult, op1=ALU.add), reads=[uf_, dcol, psy], writes=[yv_])
                S.op("act", lambda e, ct=ct, yv_=yv_, yg_=yg_: e.activation(out=yg_[:, ct, :], in_=yv_[:], func=AF.Gelu), reads=[yv_], writes=[yg_])
            for co in range(2):
                sg_ = sgt[co]
                psz = C.psum()
                for ct in range(2):
                    S.op("pe", lambda e, ct=ct, co=co, psz=psz, yg_=yg_: e.matmul(psz[:, :], lhsT=gluw[:, ct, co * 128:(co + 1) * 128], rhs=yg_[:, ct, :],
                                                                          start=(ct == 0), stop=(ct == 1)), reads=[gluw, yg_], writes=[psz])
                S.op("act", lambda e, co=co, psz=psz, sg_=sg_: e.activation(out=sg_[:], in_=psz[:, :], func=AF.Sigmoid, bias=gbcol[:, co:co + 1], scale=1.0),
                     reads=[psz, gbcol], writes=[sg_])
                S.op("pool", lambda e, co=co, sg_=sg_, yg_=yg_, o_=o_: e.tensor_tensor(out=o_[:, co, :], in0=yg_[:, co, :], in1=sg_[:], op=ALU.mult),
                     reads=[sg_, yg_], writes=[o_])
            S.dma("sp", C.oT[768:1024, t0:t0 + 512].rearrange("(a p) t -> p a t", p=128), o_[:], reads=[o_], sem_tile=o_)
        S.barrier()
        for t_ in [gluw, ldt] + u16b + ufm + ost:
            S.release(t_)


_NC_CACHE = {}


def kernel(**inputs):
    x = np.ascontiguousarray(np.asarray(inputs["x"], dtype=np.float32))
    mem = np.ascontiguousarray(np.asarray(inputs["mem"], dtype=np.float32))
    B, T, _ = x.shape
    if T not in _NC_CACHE:
        _NC_CACHE[T] = build(T, nlayers=DEPTH, dbg=False)
    nc = _NC_CACHE[T]
    params = {name: np.ascontiguousarray(np.asarray(inputs[name], dtype=np.float32)) for name, _ in PARAMS}
    n_cores = 8
    in_maps = []
    for c in range(n_cores):
        b = c % B
        m = {"x": x[b], "mem": mem[b]}
        m.update(params)
        in_maps.append(m)
    res = run_bass_kernel_spmd(nc, in_maps, core_ids=list(range(n_cores)))
    out = np.stack([np.asarray(res.results[b]["out"], dtype=np.float32) for b in range(B)], axis=0)
    return out


def phase_moe_sparse(C, l, out_ap):
    S, T, NT, P = C.S, C.T, C.NT, C.P
    NS = T // 128
    BLK = 512
    NB = C.MOE_NB
    w1flat = P["ex_w1"].rearrange("l e k n -> (l e k) n")
    w2flat = P["ex_w2"].rearrange("l e k n -> (l e k) n")
    U32 = mybir.dt.uint32
    with ExitStack() as st:
        g_bc = load_bc(C, st, "ms_g", P["ln3_g"][l], D)
        b_bc = load_bc(C, st, "ms_b", P["ln3_b"][l], D)
        e_all = S.sb("ms_e", [128, NS, 4], F32, st)
        r_all = S.sb("ms_r", [128, NS, 4], F32, st)
        w_all = S.sb("ms_w", [128, NS, 4], F32, st)
        d_all = S.sb("ms_d", [128, NS, 4], F32, st)
        d_int = S.sb("ms_di", [128, NS, 4], I32, st)
        blk = S.sb("ms_blk", [128, NB], F32, st)
        blk2 = S.sb("ms_blk2", [128, NB], F32, st)
        skp = S.sb("ms_skp", [128, NB], F32, st)
        oh_all = S.sb("ms_oh", [128, NB], F32, st)
        widx_f = S.sb("ms_wf", [128, NB, 8], F32, st)
        widx_i = S.sb("ms_wi", [128, NB, 8], I32, st)
        base = [S.sb("ms_base%d" % i, [128, NE], F32, st) for i in range(2)]
        iota_e = S.sb("ms_iotae", [128, NE], F32, st)
        iota_p = S.sb("ms_iotap", [128, 1], F32, st)
        iw = S.sb("ms_iw", [128, 8], F32, st)
        itmp = S.sb("ms_itmp", [128, NE], I32, st)
        Ls = S.sb("ms_Ls", [128, 128], F32, st)
        ones32 = S.sb("ms_ones32", [128, NE], F32, st)
        padf = S.sb("ms_padf", [128, NE], F32, st)
        pend = S.sb("ms_pend", [128, NE], F32, st)
        pstart = S.sb("ms_pstart", [128, NE], F32, st)
        b1all = S.sb("ms_b1", [32, 2048], BF16, st)
        b2all = S.sb("ms_b2", [32, 1024], BF16, st)
        S.dma("pool", b1all[:], P["ex_b1"][l], writes=[b1all], sem_tile=b1all)
        S.dma("pool", b2all[:], P["ex_b2"][l], writes=[b2all], sem_tile=b2all)
        S.op("pool", lambda e: e.iota(itmp[:], pattern=[[1, NE]], base=0, channel_multiplier=0), writes=[itmp])
        S.op("dve", lambda e: e.tensor_copy(out=iota_e[:], in_=itmp[:]), reads=[itmp], writes=[iota_e])
        S.op("pool", lambda e: e.iota(itmp[:, 0:1], pattern=[[1, 1]], base=0, channel_multiplier=1), reads=[iota_e], writes=[itmp])
        S.op("dve", lambda e: e.tensor_copy(out=iota_p[:], in_=itmp[:, 0:1]), reads=[itmp], writes=[iota_p])
        S.op("pool", lambda e: e.iota(itmp[:, 0:8], pattern=[[128, 8]], base=0, channel_multiplier=1), reads=[iota_p], writes=[itmp])
        S.op("dve", lambda e: e.tensor_copy(out=iw[:], in_=itmp[:, 0:8]), reads=[itmp], writes=[iw])
        S.op("pool", lambda e: e.memset(ones32[:], 1.0), writes=[ones32])
        S.op("pool", lambda e: e.memset(base[0][:], 0.0), writes=[base[0]])
        S.op("pool", lambda e: e.affine_select(out=Ls[:], in_=C.ones_f[:], compare_op=ALU.is_gt, fill=0.0, base=0,
                                               pattern=[[1, 128]], channel_multiplier=-1), reads=[C.ones_f], writes=[Ls])
        with ExitStack() as st2:
            rw = S.sb("rt_w", [128, 8, NE], F32, st2)
            S.dma("sp", rw[:], P["router_w"][l].rearrange("(kc p) n -> p kc n", p=128), writes=[rw], sem_tile=rw)
            rb = load_bc(C, st2, "rt_b", P["router_b"][l], NE)
            ht = [S.sb("rt_h%d" % i, [128, D], F32, st2) for i in range(2)]
            hTf = [S.sb("rt_hT%d" % i, [128, 8, 128], F32, st2) for i in range(2)]
            lg = [S.sb("rt_lg%d" % i, [128, NE], F32, st2) for i in range(2)]
            t8 = [S.sb("rt_t8%d" % i, [128, 8], F32, st2) for i in range(2)]
            mk = [S.sb("rt_mk%d" % i, [128, NE], F32, st2) for i in range(2)]
            rg = [S.sb("rt_rg%d" % i, [128, NE], F32, st2) for i in range(2)]
            ew = [S.sb("rt_ew%d" % i, [128, 4], F32, st2) for i in range(2)]
            sm = [S.sb("rt_sm%d" % i, [128, 2], F32, st2) for i in range(2)]
            tq = [S.sb("rt_tq%d" % i, [128, NE], F32, st2) for i in range(4)]
            ntq = 0
            for n in range(NS):
                h, hf, lg_, t8_, mk_, rg_, ew_, sm_ = ht[n % 2], hTf[n % 2], lg[n % 2], t8[n % 2], mk[n % 2], rg[n % 2], ew[n % 2], sm[n % 2]
                bc_, bn_ = base[n % 2], base[(n + 1) % 2]
                S.dma("sp", h[:], C.h_tok[n * 128:(n + 1) * 128, :], writes=[h], sem_tile=h)
                transpose_to(C, h[:], h, hf, lambda g, hf=hf: hf[:, g * 4:(g + 1) * 4, :])
                ps = C.psum()
                for kc in range(8):
                    S.op("pe", lambda e, kc=kc, ps=ps, hf=hf: e.matmul(ps[:, 0:NE], lhsT=hf[:, kc, :], rhs=rw[:, kc, :],
                                                                        start=(kc == 0), stop=(kc == 7)), reads=[hf, rw], writes=[ps])
                S.op("dve", lambda e: e.tensor_tensor(out=lg_[:], in0=ps[:, 0:NE], in1=rb[:], op=ALU.add), reads=[ps, rb], writes=[lg_])
                S.op("dve", lambda e: e.max(out=t8_[:], in_=lg_[:]), reads=[lg_], writes=[t8_])
                S.op("dve", lambda e: e.tensor_scalar(out=mk_[:], in0=lg_[:], scalar1=t8_[:, 3:4], scalar2=None, op0=ALU.is_ge), reads=[lg_, t8_], writes=[mk_])
                S.op("dve", lambda e: e.tensor_scalar(out=sm_[:, 0:1], in0=t8_[:, 0:1], scalar1=-1.0, scalar2=None, op0=ALU.mult), reads=[t8_], writes=[sm_])
                S.op("act", lambda e: e.activation(out=ew_[:], in_=t8_[:, 0:4], func=AF.Exp, bias=sm_[:, 0:1], scale=1.0), reads=[t8_, sm_], writes=[ew_])
                S.op("dve", lambda e: e.reduce_sum(out=sm_[:, 1:2], in_=ew_[:], axis=AX.X), reads=[ew_, sm_], writes=[sm_])
                S.op("dve", lambda e: e.reciprocal(out=sm_[:, 1:2], in_=sm_[:, 1:2]), reads=[sm_], writes=[sm_])
                S.op("dve", lambda e: e.tensor_scalar(out=w_all[:, n, :], in0=ew_[:], scalar1=sm_[:, 1:2], scalar2=None, op0=ALU.mult), reads=[ew_, sm_], writes=[w_all])
                ps2 = C.psum()
                S.op("pe", lambda e: e.matmul(ps2[:, 0:NE], lhsT=Ls[:, :], rhs=mk_[:, :], start=True, stop=True), reads=[Ls, mk_], writes=[ps2])
                S.op("pe", lambda e: e.matmul(ps2[:, NE:2 * NE], lhsT=C.ones_f[:, :], rhs=mk_[:, :], start=True, stop=True), reads=[C.ones_f, mk_], writes=[ps2])
                S.op("dve", lambda e: e.tensor_tensor(out=rg_[:], in0=ps2[:, 0:NE], in1=bc_[:], op=ALU.add), reads=[ps2, bc_], writes=[rg_])
                S.op("dve", lambda e: e.tensor_tensor(out=bn_[:], in0=ps2[:, NE:2 * NE], in1=bc_[:], op=ALU.add), reads=[ps2, bc_], writes=[bn_])
                for j in range(4):
                    ta, tb_ = tq[ntq % 4], tq[(ntq + 1) % 4]
                    ntq += 2
                    S.op("dve", lambda e: e.scalar_tensor_tensor(out=ta[:], in0=lg_[:], scalar=t8_[:, j:j + 1], in1=iota_e[:], op0=ALU.is_equal, op1=ALU.mult),
                         reads=[lg_, t8_, iota_e], writes=[ta])
                    S.op("dve", lambda e: e.reduce_sum(out=e_all[:, n, j:j + 1], in_=ta[:], axis=AX.X), reads=[ta], writes=[e_all])
                    S.op("dve", lambda e: e.scalar_tensor_tensor(out=tb_[:], in0=lg_[:], scalar=t8_[:, j:j + 1], in1=rg_[:], op0=ALU.is_equal, op1=ALU.mult),
                         reads=[lg_, t8_, rg_], writes=[tb_])
                    S.op("dve", lambda e: e.reduce_sum(out=r_all[:, n, j:j + 1], in_=tb_[:], axis=AX.X), reads=[tb_], writes=[r_all])
            cnt = base[NS % 2]
            S.op("dve", lambda e: e.tensor_scalar(out=padf[:], in0=cnt[:], scalar1=float(BLK - 1), scalar2=None, op0=ALU.add), reads=[cnt], writes=[padf])
            S.op("dve", lambda e: e.tensor_copy(out=itmp[:], in_=padf[:]), reads=[padf], writes=[itmp])
            S.op("dve", lambda e: e.tensor_scalar(out=itmp[:], in0=itmp[:], scalar1=9, scalar2=9, op0=ALU.arith_shift_right, op1=ALU.logical_shift_left),
                 reads=[itmp], writes=[itmp])
            S.op("dve", lambda e: e.tensor_copy(out=padf[:], in_=itmp[:]), reads=[itmp], writes=[padf])
            S.op("dve", lambda e: e.tensor_tensor_scan(out=pend[:], data0=ones32[:], data1=padf[:], initial=0.0, op0=ALU.mult, op1=ALU.add),
                 reads=[ones32, padf], writes=[pend])
            S.op("dve", lambda e: e.tensor_tensor(out=pstart[:], in0=pend[:], in1=padf[:], op=ALU.subtract), reads=[pend, padf], writes=[pstart])
            for n in range(NS):
                for j in range(4):
                    ta = tq[ntq % 4]
                    ntq += 1
                    S.op("dve", lambda e: e.scalar_tensor_tensor(out=ta[:], in0=iota_e[:], scalar=e_all[:, n, j:j + 1], in1=pstart[:], op0=ALU.is_equal, op1=ALU.mult),
                         reads=[iota_e, e_all, pstart], writes=[ta])
                    S.op("dve", lambda e: e.reduce_sum(out=d_all[:, n, j:j + 1], in_=ta[:], axis=AX.X), reads=[ta], writes=[d_all])
            fl3 = lambda t: t[:, :, :].rearrange("p a b -> p (a b)")
            S.op("dve", lambda e: e.tensor_tensor(out=fl3(d_all), in0=fl3(d_all), in1=fl3(r_all), op=ALU.add), reads=[d_all, r_all], writes=[d_all])
            S.op("dve", lambda e: e.tensor_copy(out=fl3(d_int), in_=fl3(d_all)), reads=[d_all], writes=[d_int])
            for b in range(NB):
                ta = tq[ntq % 4]
                ntq += 1
                S.op("dve", lambda e: e.tensor_scalar(out=ta[:], in0=pend[:], scalar1=float(b * BLK), scalar2=None, op0=ALU.is_le), reads=[pend], writes=[ta])
                S.op("dve", lambda e: e.reduce_sum(out=blk[:, b:b + 1], in_=ta[:], axis=AX.X), reads=[ta], writes=[blk])
            S.op("dve", lambda e: e.tensor_scalar(out=blk[:], in0=blk[:], scalar1=float(NE - 1), scalar2=None, op0=ALU.min), reads=[blk], writes=[blk])
            S.op("dve", lambda e: e.tensor_scalar(out=oh_all[:], in0=blk[:], scalar1=iota_p[:, 0:1], scalar2=None, op0=ALU.is_equal), reads=[blk, iota_p], writes=[oh_all])
            S.op("dve", lambda e: e.tensor_scalar(out=blk2[:], in0=blk[:], scalar1=1024.0, scalar2=float(l * NE * 1024), op0=ALU.mult, op1=ALU.add),
                 reads=[blk], writes=[blk2])
            S.op("dve", lambda e: e.memset(skp[:], 0.0), writes=[skp])
            S.op("dve", lambda e: e.tensor_tensor(out=skp[:, 2:NB], in0=blk[:, 2:NB], in1=blk[:, 0:NB - 2], op=ALU.is_equal), reads=[blk, skp], writes=[skp])
            S.op("dve", lambda e: e.scalar_tensor_tensor(out=blk2[:], in0=skp[:], scalar=float(1 << 22), in1=blk2[:], op0=ALU.mult, op1=ALU.add),
                 reads=[skp, blk2], writes=[blk2])
            for b in range(NB):
                S.op("dve", lambda e: e.tensor_scalar(out=widx_f[:, b, :], in0=iw[:], scalar1=blk2[:, b:b + 1], scalar2=None, op0=ALU.add),
                     reads=[iw, blk2], writes=[widx_f])
            S.op("dve", lambda e: e.tensor_copy(out=fl3(widx_i), in_=fl3(widx_f)), reads=[widx_f], writes=[widx_i])
            for n in range(NS):
                h = ht[n % 2]
                S.dma("sp", h[:], C.h_tok[n * 128:(n + 1) * 128, :], writes=[h], sem_tile=h)
                for j in range(4):
                    S.dma_fn("pool", lambda e: e.indirect_dma_start(
                        out=C.xs_d, out_offset=bass.IndirectOffsetOnAxis(d_int[:, n, j:j + 1].bitcast(U32), 0), in_=h[:], in_offset=None),
                        reads=[h, d_int], sem_tile=h)
            S.barrier()
            for t_ in [rw, rb] + ht:
                S.release(t_)
        with ExitStack() as st2:
            w1t = [S.sb("mo_w1%d" % i, [128, 8, 2048], BF16, st2) for i in range(2)]
            w2t = [S.sb("mo_w2%d" % i, [128, 8, 1024], BF16, st2) for i in range(2)]
            xsb = S.sb("mo_xs", [128, 4, 1024], F32, st2)
            ysb = S.sb("mo_ys", [128, 4, 1024], F32, st2)
            xT = [S.sb("mo_xT%d" % i, [128, 8, 512], BF16, st2) for i in range(2)]
            actT = [S.sb("mo_act%d" % i, [128, 8, 512], BF16, st2) for i in range(2)]
            ohb = [S.sb("mo_ohb%d" % i, [32, 512], BF16, st2) for i in range(2)]
            ones_r = S.sb("mo_onesr", [32, 512], F32, st2)
            S.op("pool", lambda e: e.memset(ones_r[:], 1.0), writes=[ones_r])
            gq = [S.sb("mo_gq%d" % i, [128, 512], F32, st2) for i in range(2)]
            sg = [S.sb("mo_sg%d" % i, [128, 512], F32, st2) for i in range(2)]
            uq = [S.sb("mo_uq%d" % i, [128, 512], F32, st2) for i in range(2)]
            nq = 0
            for b in range(NB):
                w1_, w2_, x_, a, oh_ = w1t[b % 2], w2t[b % 2], xT[b % 2], actT[b % 2], ohb[b % 2]
                for kc in range(8):
                    S.dma_fn("pool", lambda e: e.indirect_dma_start(
                        out=w1_[:, kc, :], out_offset=None, in_=w1flat, in_offset=bass.IndirectOffsetOnAxis(widx_i[:, b, kc:kc + 1].bitcast(U32), 0),
                        bounds_check=RegConst(2 * NE * 1024 - 1), oob_is_err=False),
                        reads=[widx_i], writes=[w1_], sem_tile=w1_)
                for kc in range(8):
                    S.dma_fn("pool", lambda e: e.indirect_dma_start(
                        out=w2_[:, kc, :], out_offset=None, in_=w2flat, in_offset=bass.IndirectOffsetOnAxis(widx_i[:, b, kc:kc + 1].bitcast(U32), 0),
                        bounds_check=RegConst(2 * NE * 1024 - 1), oob_is_err=False),
                        reads=[widx_i], writes=[w2_], sem_tile=w2_)
                S.dma("sp", xsb[:], C.xs_d[b * BLK:(b + 1) * BLK, :].rearrange("(s p) c -> p s c", p=128), writes=[xsb], sem_tile=xsb)
                for s4 in range(4):
                    transpose_to(C, xsb[:, s4, :], xsb, x_, lambda g, s4=s4, x_=x_: x_[:, g * 4:(g + 1) * 4, s4 * 128:(s4 + 1) * 128])
                S.op("dve", lambda e: e.tensor_scalar(out=oh_[:], in0=ones_r[:], scalar1=oh_all[0:32, b:b + 1], scalar2=None, op0=ALU.mult),
                     reads=[ones_r, oh_all], writes=[oh_])
                for ft in range(8):
                    g_, s_, u_ = gq[nq % 2], sg[nq % 2], uq[nq % 2]
                    nq += 1
                    psg = C.psum()
                    S.op("pe", lambda e: e.matmul(psg[:, :], lhsT=b1all[0:32, ft * 256:(ft + 1) * 256:2], rhs=oh_[:, :], start=True, stop=False),
                         reads=[b1all, oh_], writes=[psg])
                    for kc in range(8):
                        S.op("pe", lambda e: e.matmul(psg[:, :], lhsT=w1_[:, kc, ft * 256:(ft + 1) * 256:2], rhs=x_[:, kc, :], start=False, stop=(kc == 7)),
                             reads=[w1_, x_], writes=[psg])
                    psu = C.psum()
                    S.op("pe", lambda e: e.matmul(psu[:, :], lhsT=b1all[0:32, ft * 256 + 1:(ft + 1) * 256:2], rhs=oh_[:, :], start=True, stop=False),
                         reads=[b1all, oh_], writes=[psu])
                    for kc in range(8):
                        S.op("pe", lambda e: e.matmul(psu[:, :], lhsT=w1_[:, kc, ft * 256 + 1:(ft + 1) * 256:2], rhs=x_[:, kc, :], start=False, stop=(kc == 7)),
                             reads=[w1_, x_], writes=[psu])
                    S.op("dve", lambda e: e.tensor_scalar(out=g_[:], in0=psg[:, :], scalar1=7.0, scalar2=None, op0=ALU.min), reads=[psg], writes=[g_])
                    S.op("act", lambda e: e.activation(out=s_[:], in_=g_[:], func=AF.Sigmoid, scale=1.702), reads=[g_], writes=[s_])
                    S.op("dve", lambda e: e.tensor_scalar(out=u_[:], in0=psu[:, :], scalar1=7.0, scalar2=-7.0, op0=ALU.min, op1=ALU.max), reads=[psu], writes=[u_])
                    S.op("dve", lambda e: e.tensor_tensor(out=s_[:], in0=g_[:], in1=s_[:], op=ALU.mult), reads=[g_, s_], writes=[s_])
                    S.op("dve", lambda e: e.scalar_tensor_tensor(out=a[:, ft, :], in0=u_[:], scalar=1.0, in1=s_[:], op0=ALU.add, op1=ALU.mult),
                         reads=[s_, u_], writes=[a])
                for s4 in range(4):
                    for half in range(2):
                        ps = C.psum()
                        S.op("pe", lambda e: e.matmul(ps[:, :], lhsT=oh_[:, 0:128], rhs=b2all[0:32, half * 512:(half + 1) * 512], start=True, stop=False),
                             reads=[oh_, b2all], writes=[ps])
                        for ft in range(8):
                            S.op("pe", lambda e: e.matmul(ps[:, :], lhsT=a[:, ft, s4 * 128:(s4 + 1) * 128], rhs=w2_[:, ft, half * 512:(half + 1) * 512],
                                                          start=False, stop=(ft == 7)), reads=[a, w2_], writes=[ps])
                        en = evac_eng(C)
                        S.op(en, copy_op(en, ysb[:, s4, half * 512:(half + 1) * 512], ps[:, :]), reads=[ps], writes=[ysb])
                S.dma("sp", C.ys_d[b * BLK:(b + 1) * BLK, :].rearrange("(s p) c -> p s c", p=128), ysb[:], reads=[ysb], sem_tile=ysb)
            S.barrier()
            for t_ in w1t + w2t + [xsb, ysb]:
                S.release(t_)
        with ExitStack() as st2:
            acc = [S.sb("mc_acc%d" % i, [128, D], F32, st2) for i in range(2)]
            gj = [S.sb("mc_g%d" % i, [128, D], F32, st2) for i in range(4)]
            hTs = [S.sb("mc_hT%d" % i, [128, 8, 512], BF16, st2) for i in range(2)]
            ng = 0
            for n in range(NS):
                a_ = acc[n % 2]
                S.dma("sp", a_[:], C.h_tok[n * 128:(n + 1) * 128, :], writes=[a_], sem_tile=a_)
                S.op("act", lambda e: e.activation(out=a_[:], in_=a_[:], func=AF.Copy, scale=DN_ALPHA), reads=[a_], writes=[a_])
                for j in range(4):
                    g_ = gj[ng % 4]
                    ng += 1
                    S.dma_fn("pool", lambda e: e.indirect_dma_start(
                        out=g_[:], out_offset=None, in_=C.ys_d, in_offset=bass.IndirectOffsetOnAxis(d_int[:, n, j:j + 1].bitcast(U32), 0)),
                        reads=[d_int], writes=[g_], sem_tile=g_)
                    S.op("dve", lambda e: e.scalar_tensor_tensor(out=a_[:], in0=g_[:], scalar=w_all[:, n, j:j + 1], in1=a_[:], op0=ALU.mult, op1=ALU.add),
                         reads=[g_, w_all, a_], writes=[a_])
                ln_tile(C, (a_[:], a_), (a_[:], a_), g_bc, b_bc)
                store_h(C, st2, a_, hTs[(n // 4) % 2], n // 4, n % 4, out_ap)
            S.barrier()
            for t_ in acc + gj + hTs:
                S.release(t_)
        for t_ in [g_bc, b_bc, b1all, b2all]:
            S.release(t_)
```
